# Optimizing a Trainium2 kernel written in Bass

```python
import math
import jax, jax.numpy as jnp
from jax import lax
import numpy as np

D_MODEL = 1024
BATCH = 8
SEQ = 4096
DEPTH = 2

DA_HEADS = 4
DA_HEAD_DIM = 64
DA_WIDTH = DA_HEADS * 2 * DA_HEAD_DIM
CONV_CH = 512
CONV_WIDTH = 31
SB_HEADS = 8
SB_HEAD_DIM = 64
SB_WIDTH = SB_HEADS * SB_HEAD_DIM
N_BRANCH = 3
Q_BLOCK = 128
ROPE_THETA = 10000.0
MAX_POS_OFFSET = 1024
EPS = 1e-6
NEG_INF = -1e30
IN_SIZES = (DA_WIDTH, DA_WIDTH, DA_WIDTH, 2 * CONV_CH, SB_WIDTH, SB_WIDTH, SB_WIDTH, N_BRANCH * D_MODEL)
IN_COLS = sum(IN_SIZES)
IN_SPLITS = tuple(int(v) for v in np.cumsum(IN_SIZES)[:-1])
N_EXPERTS = 64
TOP_K = 8
N_GROUPS = 8
TOPK_GROUPS = 4
EXPERT_FF = 256
SHARED_FF = 256
ROUTED_SCALE = 2.5
EXPERT_BLOCK = 128

kernel_name = "hybrid_diffattn_conformer_stickbreak_moe"


def rms_norm(x, g):
    xf = x.astype(jnp.float32)
    y = xf * lax.rsqrt(jnp.mean(xf * xf, axis=-1, keepdims=True) + EPS)
    return (y * g.astype(jnp.float32)).astype(x.dtype)


def layer_norm(x, g, b):
    xf = x.astype(jnp.float32)
    mu = jnp.mean(xf, axis=-1, keepdims=True)
    var = jnp.mean(jnp.square(xf - mu), axis=-1, keepdims=True)
    y = (xf - mu) * lax.rsqrt(var + EPS) * g.astype(jnp.float32) + b.astype(jnp.float32)
    return y.astype(x.dtype)


def rope_tables(positions, dim):
    inv = ROPE_THETA ** (-jnp.arange(0, dim, 2, dtype=jnp.float32) / dim)
    ang = positions.astype(jnp.float32)[..., None] * inv
    return jnp.cos(ang)[:, :, None, :], jnp.sin(ang)[:, :, None, :]


def apply_rope(x, cos, sin):
    x1, x2 = jnp.split(x, 2, axis=-1)
    c = cos.astype(x.dtype)
    s = sin.astype(x.dtype)
    return jnp.concatenate([x1 * c - x2 * s, x2 * c + x1 * s], axis=-1)


def to_blocks(x):
    b, s = x.shape[:2]
    x = x.reshape((b, s // Q_BLOCK, Q_BLOCK) + x.shape[2:])
    return jnp.moveaxis(x, 1, 0)


def from_blocks(y):
    y = jnp.moveaxis(y, 0, 1)
    return y.reshape((y.shape[0], y.shape[1] * y.shape[2]) + y.shape[3:])


def diff_attention(q, k, v, lam, lambda_init, subln_g):
    S = k.shape[1]
    n_blocks = S // Q_BLOCK
    scale = DA_HEAD_DIM ** -0.5
    kpos = jnp.arange(S)

    def block(args):
        qb, start = args
        s = jnp.einsum('bqhmd,bkhmd->bhmqk', qb, k).astype(jnp.float32) * scale
        qpos = start + jnp.arange(Q_BLOCK)
        causal = kpos[None, :] <= qpos[:, None]
        p = jax.nn.softmax(jnp.where(causal, s, NEG_INF), axis=-1)
        a = p[:, :, 0] - lam * p[:, :, 1]
        return jnp.einsum('bhqk,bkhe->bqhe', a.astype(v.dtype), v)

    starts = jnp.arange(n_blocks, dtype=jnp.int32) * Q_BLOCK
    o = from_blocks(lax.map(block, (to_blocks(q), starts)))
    o = rms_norm(o, subln_g) * (1.0 - lambda_init)
    return o.reshape(o.shape[0], S, DA_WIDTH)


def stick_breaking(q, k, v):
    S = k.shape[1]
    n_blocks = S // Q_BLOCK
    scale = SB_HEAD_DIM ** -0.5
    kpos = jnp.arange(S)

    def block(args):
        qb, start = args
        z = jnp.einsum('bqhd,bkhd->bhqk', qb, k).astype(jnp.float32) * scale
        qpos = start + jnp.arange(Q_BLOCK)
        before = kpos[None, :] < qpos[:, None]
        log_beta = jax.nn.log_sigmoid(z)
        log_one_minus = jnp.where(before, jax.nn.log_sigmoid(-z), 0.0)
        key_axis = log_one_minus.ndim - 1
        suffix = lax.cumsum(log_one_minus, axis=key_axis, reverse=True) - log_one_minus
        a = jnp.where(before, jnp.exp(log_beta + suffix), 0.0)
        return jnp.einsum('bhqk,bkhd->bqhd', a.astype(v.dtype), v)

    starts = jnp.arange(n_blocks, dtype=jnp.int32) * Q_BLOCK
    o = from_blocks(lax.map(block, (to_blocks(q), starts)))
    return o.reshape(o.shape[0], S, SB_WIDTH)


def conformer_conv(u, w_dw, b_dw, ln_g, ln_b):
    a, g = jnp.split(u, 2, axis=-1)
    h = a * jax.nn.sigmoid(g)
    h = lax.conv_general_dilated(
        h, w_dw[:, None, :].astype(h.dtype), window_strides=(1,),
        padding=[(CONV_WIDTH - 1, 0)], dimension_numbers=('NWC', 'WIO', 'NWC'),
        feature_group_count=CONV_CH) + b_dw
    return jax.nn.silu(layer_norm(h, ln_g, ln_b))


def mixer(h, cos, sin, lambda_init, w_in, qn_g, kn_g, lam_q1, lam_k1, lam_q2, lam_k2, subln_g,
          w_proj_a, w_dw, b_dw, conv_ln_g, conv_ln_b, w_proj_b, b_proj_b, w_proj_c, w_out):
    B, S, _ = h.shape
    proj = h @ w_in
    qa, ka, va, ub, qc, kc, vc, gates = jnp.split(proj, IN_SPLITS, axis=-1)

    qa = apply_rope(rms_norm(qa.reshape(B, S, 2 * DA_HEADS, DA_HEAD_DIM), qn_g), cos, sin)
    ka = apply_rope(rms_norm(ka.reshape(B, S, 2 * DA_HEADS, DA_HEAD_DIM), kn_g), cos, sin)
    qa = qa.reshape(B, S, DA_HEADS, 2, DA_HEAD_DIM)
    ka = ka.reshape(B, S, DA_HEADS, 2, DA_HEAD_DIM)
    va = va.reshape(B, S, DA_HEADS, 2 * DA_HEAD_DIM)
    lam = (jnp.exp(jnp.sum(lam_q1.astype(jnp.float32) * lam_k1.astype(jnp.float32)))
           - jnp.exp(jnp.sum(lam_q2.astype(jnp.float32) * lam_k2.astype(jnp.float32)))
           + lambda_init)
    y_a = diff_attention(qa, ka, va, lam, lambda_init, subln_g) @ w_proj_a

    y_b = conformer_conv(ub, w_dw, b_dw, conv_ln_g, conv_ln_b) @ w_proj_b + b_proj_b

    y_c = stick_breaking(qc.reshape(B, S, SB_HEADS, SB_HEAD_DIM),
                         kc.reshape(B, S, SB_HEADS, SB_HEAD_DIM),
                         vc.reshape(B, S, SB_HEADS, SB_HEAD_DIM)) @ w_proj_c

    g = jax.nn.sigmoid(gates.reshape(B, S, N_BRANCH, D_MODEL))
    merged = g[:, :, 0] * y_a + g[:, :, 1] * y_b + g[:, :, 2] * y_c
    return merged @ w_out


def swiglu(x, w1, w3, w2):
    return (jax.nn.silu(x @ w1) * (x @ w3)) @ w2


def moe_ffn(u, w_router, b_router, w1, w3, w2, ws1, ws3, ws2):
    B, S, D = u.shape
    T = B * S
    xf = u.reshape(T, D)
    scores = jax.nn.sigmoid((xf @ w_router).astype(jnp.float32))
    biased = scores + b_router.astype(jnp.float32)
    grouped = biased.reshape(T, N_GROUPS, N_EXPERTS // N_GROUPS)
    group_score = jnp.sum(lax.top_k(grouped, 2)[0], axis=-1)
    _, top_groups = lax.top_k(group_score, TOPK_GROUPS)
    group_mask = jnp.any(top_groups[..., None] == jnp.arange(N_GROUPS), axis=-2)
    expert_mask = jnp.repeat(group_mask, N_EXPERTS // N_GROUPS, axis=-1)
    _, idx = lax.top_k(jnp.where(expert_mask, biased, NEG_INF), TOP_K)
    w = jnp.take_along_axis(scores, idx, axis=-1)
    w = w / (jnp.sum(w, axis=-1, keepdims=True) + 1e-20) * ROUTED_SCALE

    TK = T * TOP_K
    flat_e = idx.reshape(TK).astype(jnp.int32)
    flat_tok = jnp.arange(TK, dtype=jnp.int32) // TOP_K
    flat_w = w.reshape(TK)
    order = jnp.argsort(flat_e)
    se, st, sw = flat_e[order], flat_tok[order], flat_w[order]
    counts = jnp.bincount(flat_e, length=N_EXPERTS).astype(jnp.int32)
    starts = jnp.cumsum(counts) - counts
    padded = (counts + EXPERT_BLOCK - 1) // EXPERT_BLOCK * EXPERT_BLOCK
    pad_ends = jnp.cumsum(padded)
    pad_starts = pad_ends - padded
    dest = pad_starts[se] + (jnp.arange(TK, dtype=jnp.int32) - starts[se])
    n_blocks = (TK + N_EXPERTS * (EXPERT_BLOCK - 1) + EXPERT_BLOCK - 1) // EXPERT_BLOCK
    n_slots = n_blocks * EXPERT_BLOCK
    slot_tok = jnp.full((n_slots,), T, jnp.int32).at[dest].set(st)
    slot_w = jnp.zeros((n_slots,), jnp.float32).at[dest].set(sw)
    block_e = jnp.minimum(jnp.searchsorted(pad_ends, jnp.arange(n_blocks, dtype=jnp.int32) * EXPERT_BLOCK,
                                           side='right'), N_EXPERTS - 1).astype(jnp.int32)
    xpad = jnp.concatenate([xf, jnp.zeros((1, D), xf.dtype)], axis=0)

    def expert_block(args):
        toks, e, wts = args
        out = swiglu(xpad[toks], w1[e], w3[e], w2[e])
        return out * wts[:, None].astype(out.dtype)

    y = lax.map(expert_block, (slot_tok.reshape(n_blocks, EXPERT_BLOCK), block_e,
                               slot_w.reshape(n_blocks, EXPERT_BLOCK)))
    routed = jax.ops.segment_sum(y.reshape(n_slots, D), slot_tok, num_segments=T + 1)[:T]
    shared = swiglu(xf, ws1, ws3, ws2)
    return (routed + shared).reshape(B, S, D)


def setup_inputs(seed: int = 0) -> dict:
    key = jax.random.key(seed)
    ks = iter(jax.random.split(key, 40))
    L = DEPTH

    def nrm(shape, s):
        return jax.random.normal(next(ks), shape, jnp.float32) * s

    def gain(shape):
        return 1.0 + nrm(shape, 0.02)

    D = D_MODEL
    x = nrm((BATCH, SEQ, D), 1.0)
    c = nrm((BATCH, D), 1.0)
    positions = (jnp.arange(SEQ, dtype=jnp.int32)[None, :]
                 + jax.random.randint(next(ks), (BATCH, 1), 0, MAX_POS_OFFSET, dtype=jnp.int32))
    return {
        "x": x, "c": c, "positions": positions,
        "w_mod": nrm((L, D, 6 * D), 0.3 * D ** -0.5),
        "b_mod": nrm((L, 6 * D), 0.02),
        "norm_mix_g": gain((L, D)),
        "norm_ffn_g": gain((L, D)),
        "w_in": nrm((L, D, IN_COLS), D ** -0.5),
        "qn_g": gain((L, DA_HEAD_DIM)),
        "kn_g": gain((L, DA_HEAD_DIM)),
        "lam_q1": nrm((L, DA_HEAD_DIM), 0.1),
        "lam_k1": nrm((L, DA_HEAD_DIM), 0.1),
        "lam_q2": nrm((L, DA_HEAD_DIM), 0.1),
        "lam_k2": nrm((L, DA_HEAD_DIM), 0.1),
        "subln_g": gain((L, 2 * DA_HEAD_DIM)),
        "w_proj_a": nrm((L, DA_WIDTH, D), DA_WIDTH ** -0.5),
        "w_dw": nrm((L, CONV_WIDTH, CONV_CH), CONV_WIDTH ** -0.5),
        "b_dw": nrm((L, CONV_CH), 0.02),
        "conv_ln_g": gain((L, CONV_CH)),
        "conv_ln_b": nrm((L, CONV_CH), 0.02),
        "w_proj_b": nrm((L, CONV_CH, D), CONV_CH ** -0.5),
        "b_proj_b": nrm((L, D), 0.02),
        "w_proj_c": nrm((L, SB_WIDTH, D), SB_WIDTH ** -0.5),
        "w_out": nrm((L, D, D), D ** -0.5),
        "w_router": nrm((L, D, N_EXPERTS), D ** -0.5),
        "b_router": nrm((L, N_EXPERTS), 0.01),
        "w1": nrm((L, N_EXPERTS, D, EXPERT_FF), D ** -0.5),
        "w3": nrm((L, N_EXPERTS, D, EXPERT_FF), D ** -0.5),
        "w2": nrm((L, N_EXPERTS, EXPERT_FF, D), EXPERT_FF ** -0.5),
        "ws1": nrm((L, D, SHARED_FF), D ** -0.5),
        "ws3": nrm((L, D, SHARED_FF), D ** -0.5),
        "ws2": nrm((L, SHARED_FF, D), SHARED_FF ** -0.5),
    }


def reference(x, c, positions, w_mod, b_mod, norm_mix_g, norm_ffn_g, w_in, qn_g, kn_g,
              lam_q1, lam_k1, lam_q2, lam_k2, subln_g, w_proj_a, w_dw, b_dw, conv_ln_g, conv_ln_b,
              w_proj_b, b_proj_b, w_proj_c, w_out, w_router, b_router, w1, w3, w2, ws1, ws3, ws2):
    cos, sin = rope_tables(positions, DA_HEAD_DIM)
    c_act = jax.nn.silu(c)
    for l in range(DEPTH):
        lambda_init = 0.8 - 0.6 * math.exp(-0.3 * l)
        mod = c_act @ w_mod[l] + b_mod[l]
        sh_m, sc_m, g_m, sh_f, sc_f, g_f = [m[:, None, :] for m in jnp.split(mod, 6, axis=-1)]
        h = rms_norm(x, norm_mix_g[l]) * (1.0 + sc_m) + sh_m
        x = x + g_m * mixer(h, cos, sin, lambda_init, w_in[l], qn_g[l], kn_g[l], lam_q1[l], lam_k1[l],
                            lam_q2[l], lam_k2[l], subln_g[l], w_proj_a[l], w_dw[l], b_dw[l],
                            conv_ln_g[l], conv_ln_b[l], w_proj_b[l], b_proj_b[l], w_proj_c[l], w_out[l])
        h = rms_norm(x, norm_ffn_g[l]) * (1.0 + sc_f) + sh_f
        x = x + g_f * moe_ffn(h, w_router[l], b_router[l], w1[l], w3[l], w2[l], ws1[l], ws3[l], ws2[l])
    return x
```

```python
import math
from contextlib import ExitStack
import numpy as np
import concourse.bass as bass
import concourse.mybir as mybir
from concourse.bass_utils import run_bass_kernel_spmd

F32 = mybir.dt.float32
BF16 = mybir.dt.bfloat16
I32 = mybir.dt.int32
ALU = mybir.AluOpType
AF = mybir.ActivationFunctionType
AX = mybir.AxisListType

ENGS = ("pe", "act", "dve", "pool", "sp")
SEM_LIM = 30000
DMA_RING = 8

D = 1024
NE = 64
FF = 256
EPS = 1e-6


class Res:
    __slots__ = ("w", "r")

    def __init__(self):
        self.w = None
        self.r = []


class Op:
    __slots__ = ("eng", "fn", "deps", "signal", "k", "is_dma", "slot", "dval")

    def __init__(self, eng, fn, is_dma=False):
        self.eng = eng
        self.fn = fn
        self.deps = set()
        self.signal = False
        self.k = None
        self.is_dma = is_dma
        self.slot = None
        self.dval = None


class _Rec:
    def __getattr__(self, name):
        return lambda *a, **k: (name, a, k)


_REC = _Rec()


class Prog:
    def __init__(self, nc, es):
        self.nc = nc
        self.sems = {e: [es.enter_context(nc.semaphore(f"s_{e}_{i}")) for i in range(6)] for e in ENGS if e != "sp"}
        self.nsig = {e: 0 for e in ENGS}
        self.dq = ("sp", "act", "pool")
        self.dsems = {e: [es.enter_context(nc.semaphore(f"d_{e}_{i}")) for i in range(DMA_RING)] for e in self.dq}
        self.ndma = {e: 0 for e in self.dq}
        self.ring_last = {e: [None] * DMA_RING for e in self.dq}
        self.waited = {e: {} for e in ENGS}
        self.ops = []
        self.last_op = {e: None for e in ENGS}
        self.stage_dmas = []

    def _add(self, op, reads, writes):
        deps = set()
        for r in reads:
            if r.w is not None:
                deps.add(r.w)
        for w in writes:
            if w.w is not None:
                deps.add(w.w)
            for o in w.r:
                deps.add(o)
        for d in deps:
            if d is op:
                continue
            if (not d.is_dma) and d.eng == op.eng and op.eng == "pe" and not op.is_dma:
                continue
            op.deps.add(d)
            if not d.is_dma:
                d.signal = True
        for r in reads:
            r.r.append(op)
        for w in writes:
            w.w = op
            w.r = []
        self.ops.append(op)
        if not op.is_dma:
            self.last_op[op.eng] = op
        return op

    def op(self, eng, fn, reads=(), writes=()):
        return self._add(Op(eng, fn(_REC)), reads, writes)

    def dma(self, q, out, in_, reads=(), writes=()):
        op = Op(q, ("dma_start", (), dict(out=out, in_=in_)), is_dma=True)
        i = self.ndma[q]
        self.ndma[q] += 1
        op.slot = i % DMA_RING
        op.dval = 16 * (i // DMA_RING + 1)
        prev = self.ring_last[q][op.slot]
        if prev is not None:
            op.deps.add(prev)
        self.ring_last[q][op.slot] = op
        self.stage_dmas.append(op)
        return self._add(op, reads, writes)

    def raw(self, eng, fn, reads=(), writes=(), is_dma=False):
        op = Op(eng, fn, is_dma=is_dma)
        if is_dma:
            i = self.ndma[eng]
            self.ndma[eng] += 1
            op.slot = i % DMA_RING
            op.dval = 16 * (i // DMA_RING + 1)
            prev = self.ring_last[eng][op.slot]
            if prev is not None:
                op.deps.add(prev)
            self.ring_last[eng][op.slot] = op
            self.stage_dmas.append(op)
        return self._add(op, reads, writes)

    def barrier(self):
        lasts = [o for o in self.last_op.values() if o is not None]
        dmas = list(self.stage_dmas)
        for e in ENGS:
            op = Op(e, None)
            for d in lasts:
                if d.eng != e:
                    op.deps.add(d)
                    d.signal = True
            for d in dmas:
                op.deps.add(d)
            self.ops.append(op)
        self.stage_dmas = []
        self.last_op = {e: None for e in ENGS}

    def emit(self):
        nc = self.nc
        self.barrier()
        ops = self.ops
        self.ops = []
        for o in ops:
            if o.signal and not o.is_dma and o.k is None:
                o.k = self.nsig[o.eng]
                self.nsig[o.eng] += 1
        per = {e: [o for o in ops if o.eng == e] for e in ENGS}
        engobj = {"pe": "tensor", "act": "scalar", "dve": "vector", "pool": "gpsimd", "sp": "sync"}

        def run(e, eng):
            waited = self.waited[e]
            for o in per[e]:
                need = {}
                for d in o.deps:
                    if d.is_dma:
                        key = ("d", d.eng, d.slot)
                        sem = self.dsems[d.eng][d.slot]
                        val = d.dval
                    else:
                        key = ("c", d.eng, d.k // SEM_LIM)
                        sem = self.sems[d.eng][d.k // SEM_LIM]
                        val = d.k % SEM_LIM + 1
                    if waited.get(key, 0) >= val:
                        continue
                    if key not in need or need[key][1] < val:
                        need[key] = (sem, val)
                for key, (sem, val) in need.items():
                    eng.wait_ge(sem, val)
                    waited[key] = val
                if o.fn is None:
                    continue
                if callable(o.fn):
                    ins = o.fn(eng)
                else:
                    name, a, k = o.fn
                    ins = getattr(eng, name)(*a, **k)
                if o.is_dma:
                    ins.then_inc(self.dsems[o.eng][o.slot], 16)
                elif o.signal:
                    ins.then_inc(self.sems[o.eng][o.k // SEM_LIM], 1)

        with nc.allow_non_contiguous_dma(reason="small strided parameter loads"):
            with nc.Block() as block:
                for e in ENGS:
                    if not per[e]:
                        continue
                    getattr(block, engobj[e])(lambda eng, e=e: run(e, eng))


class Stage:
    count = 0

    def __init__(self, P):
        self.P = P
        self.nc = P.nc
        self.es = ExitStack()
        self.n = 0
        Stage.count += 1
        self.sid = Stage.count

    def sb(self, shape, dt):
        self.n += 1
        t = self.es.enter_context(self.nc.sbuf_tensor(f"t{self.sid}_{self.n}", list(shape), dt))
        return t, Res()

    def ps(self, shape, dt):
        self.n += 1
        t = self.es.enter_context(self.nc.psum_tensor(f"p{self.sid}_{self.n}", list(shape), dt))
        return t, Res()

    def done(self, name=None):
        if name:
            with self.nc.named_scope(name):
                self.P.emit()
        else:
            self.P.emit()
        self.es.close()


def col(ap1d):
    return ap1d.rearrange("(p o) -> p o", o=1)


def make_consts():
    p = np.arange(128)
    ident = np.eye(128, dtype=np.float32)
    blk64 = ((p[:, None] // 64) == (p[None, :] // 64)).astype(np.float32) / 64.0
    rotT = np.zeros((128, 128), np.float32)
    for k in range(128):
        if k % 64 >= 32:
            rotT[k, k - 32] = -1.0
        else:
            rotT[k, k + 32] = 1.0
    ones = np.ones((128, 128), np.float32)
    ustrict = (p[:, None] > p[None, :]).astype(np.float32)
    uincl = (p[:, None] >= p[None, :]).astype(np.float32)
    ulow = (p[:, None] < p[None, :]).astype(np.float32)
    pincl = (p[:, None] <= p[None, :]).astype(np.float32)
    iota = p.astype(np.float32)
    qq = np.arange(512)
    maskc = np.stack([((qq[None, :] - j * 128 - p[:, None]) >= 0) for j in range(4)]).astype(np.float32)
    masks = np.stack([((qq[None, :] - j * 128 - p[:, None]) > 0) for j in range(4)]).astype(np.float32)
    inv = (10000.0 ** (-np.arange(0, 64, 2, dtype=np.float32) / np.float32(64))).astype(np.float32)
    invc = inv[p % 32].astype(np.float32)
    return dict(k_ident=ident, k_blk64=blk64, k_rotT=rotT, k_ones=ones, k_ustrict=ustrict, k_uincl=uincl, k_ulow=ulow, k_pincl=pincl, k_iota=iota,
                k_maskc=maskc, k_masks=masks, k_invc=invc)


W_SHAPES = dict(
    w_mod=(D, 6 * D), b_mod=(6 * D,), norm_mix_g=(D,), norm_ffn_g=(D,), w_in=(D, 7168),
    qn_g=(64,), kn_g=(64,), lam_q1=(64,), lam_k1=(64,), lam_q2=(64,), lam_k2=(64,), subln_g=(128,),
    w_proj_a=(512, D), w_dw=(31, 512), b_dw=(512,), conv_ln_g=(512,), conv_ln_b=(512,),
    w_proj_b=(512, D), b_proj_b=(D,), w_proj_c=(512, D), w_out=(D, D), w_router=(D, NE), b_router=(NE,),
    w1=(NE, D, FF), w3=(NE, D, FF), w2=(NE, FF, D), ws1=(D, FF), ws3=(D, FF), ws2=(FF, D),
)


def build(S, L=2, dbg=(), stages=None):
    NT = S // 128
    NQ = S // 512
    nc = bass.Bass("TRN2", target_bir_lowering=False)
    I = {}
    I["x"] = nc.dram_tensor("x", [S, D], F32, kind="ExternalInput").ap()
    I["c"] = nc.dram_tensor("c", [D], F32, kind="ExternalInput").ap()
    I["pos"] = nc.dram_tensor("pos", [S], I32, kind="ExternalInput").ap()
    dense_moe = stages is not None and "moe" in stages
    for k, shp in W_SHAPES.items():
        if k in ("w1", "w3", "w2") and not dense_moe:
            continue
        I[k] = nc.dram_tensor(k, [L] + list(shp), F32, kind="ExternalInput").ap()
    for k, v in make_consts().items():
        I[k] = nc.dram_tensor(k, list(v.shape), F32, kind="ExternalInput").ap()
    for k in ("w1h", "w3h", "w2h"):
        I[k] = nc.dram_tensor(k, [L * NE * 128, 2048], F32, kind="ExternalInput").ap()
    out = nc.dram_tensor("out", [S, D], F32, kind="ExternalOutput").ap()
    NB = S * 8 // 512 + NE
    NSLOT = NB * 512

    def scratch(name, shape, dt):
        kind = "ExternalOutput" if name in dbg else "Internal"
        return nc.dram_tensor(name, list(shape), dt, kind=kind).ap()

    mod_d = scratch("mod_d", [L, 6 * D], F32)
    cos_d = scratch("cos_d", [128, S], F32)
    sin_d = scratch("sin_d", [128, S], F32)
    hT_d = scratch("hT_d", [D, S], BF16)
    qaT_d = scratch("qaT_d", [512, S], BF16)
    kaT_d = scratch("kaT_d", [512, S], BF16)
    va_d = scratch("va_d", [S, 512], BF16)
    uT_d = scratch("uT_d", [512, S], BF16)
    qcT_d = scratch("qcT_d", [512, S], BF16)
    kcT_d = scratch("kcT_d", [512, S], BF16)
    vc_d = scratch("vc_d", [S, 512], BF16)
    oaT_d = scratch("oaT_d", [512, S], BF16)
    cvT_d = scratch("cvT_d", [512, S], BF16)
    ocT_d = scratch("ocT_d", [512, S], BF16)
    x1_d = scratch("x1_d", [S, D], F32)
    x2_d = scratch("x2_d", [S, D], F32)
    h2T_d = scratch("h2T_d", [D, S], BF16)
    gT_d = scratch("gT_d", [NE, S], F32)
    gate_d = scratch("gate_d", [S, NE], F32)
    pos_d = scratch("pos_d", [S, NE], F32)
    h2_d = scratch("h2_d", [S, D], BF16)
    sbase_d = scratch("sbase_d", [NE], F32)
    eb_d = scratch("eb_d", [128], F32)
    slotk_d = scratch("slotk_d", [S, 8], I32)
    gk_d = scratch("gk_d", [S, 8], F32)
    xs_d = scratch("xs_d", [NSLOT, D], BF16)
    ys_d = scratch("ys_d", [NSLOT, D], BF16)
    ysh_d = scratch("ysh_d", [S, D], BF16)

    def kp(ap2d):
        return ap2d.rearrange("(c p) n -> p c n", p=128)

    es = ExitStack()
    P = Prog(nc, es)

    def stage_mod():
        st = Stage(P)
        cT, r_cT = st.sb([128, 8], F32)
        cA, r_cA = st.sb([128, 8], F32)
        wm = [st.sb([128, 8, 512], F32) for _ in range(2)]
        bm, r_bm = st.sb([1, L * 6 * D], F32)
        mr, r_mr = st.sb([1, L * 6 * D], F32)
        pm = [st.ps([1, 512], F32) for _ in range(2)]
        P.dma("sp", cT[:], I["c"].rearrange("(c p) -> p c", p=128), writes=[r_cT])
        P.dma("sp", bm[:], I["b_mod"].rearrange("(o l) n -> o (l n)", o=1), writes=[r_bm])
        P.op("act", lambda e: e.activation(out=cA[:], in_=cT[:], func=AF.Silu), reads=[r_cT], writes=[r_cA])
        i = 0
        for l in range(L):
            for blk in range(12):
                w_t, r_w = wm[i % 2]
                p_t, r_p = pm[i % 2]
                P.dma("sp", w_t[:], kp(I["w_mod"][l])[:, :, blk * 512:(blk + 1) * 512], writes=[r_w])
                for c in range(8):
                    P.op("pe", lambda e, c=c, w_t=w_t, p_t=p_t: e.matmul(p_t[:], lhsT=cA[:, c:c + 1], rhs=w_t[:, c, :], start=(c == 0), stop=(c == 7)),
                         reads=[r_cA, r_w], writes=[r_p])
                o = l * 6 * D + blk * 512
                P.op("dve", lambda e, o=o, p_t=p_t: e.tensor_tensor(out=mr[:, o:o + 512], in0=p_t[:], in1=bm[:, o:o + 512], op=ALU.add),
                     reads=[r_p, r_bm], writes=[r_mr])
                i += 1
        P.dma("sp", mod_d.rearrange("(o l) n -> o (l n)", o=1), mr[:], reads=[r_mr])
        st.done("mod")

    def stage_rope():
        st = Stage(P)
        pi_t, r_pi = st.sb([128, S], I32)
        pf, r_pf = st.sb([128, S], F32)
        inv, r_inv = st.sb([128, 1], F32)
        ang, r_ang = st.sb([128, S], F32)
        u, r_u = st.sb([128, S], F32)
        ki, r_ki = st.sb([128, S], I32)
        kf, r_kf = st.sb([128, S], F32)
        m, r_m = st.sb([128, S], F32)
        res_t, r_res = st.sb([128, S], F32)
        zero, r_zero = st.sb([128, 1], F32)
        TWO_PI = 2.0 * math.pi
        C1 = 6.28125
        C2 = TWO_PI - C1
        P.dma("sp", pi_t[:], I["pos"].partition_broadcast(128), writes=[r_pi])
        P.dma("sp", inv[:], col(I["k_invc"]), writes=[r_inv])
        P.op("pool", lambda e: e.memset(zero[:], 0.0), writes=[r_zero])
        P.op("dve", lambda e: e.tensor_copy(out=pf[:], in_=pi_t[:]), reads=[r_pi], writes=[r_pf])
        for which, dst in ((0, sin_d), (1, cos_d)):
            shift = 0.0 if which == 0 else math.pi / 2
            P.op("dve", lambda e, shift=shift: e.tensor_scalar(out=ang[:], in0=pf[:], scalar1=inv[:, 0:1], scalar2=shift, op0=ALU.mult, op1=ALU.add),
                 reads=[r_pf, r_inv], writes=[r_ang])
            P.op("dve", lambda e: e.tensor_scalar(out=u[:], in0=ang[:], scalar1=1.0 / TWO_PI, scalar2=None, op0=ALU.mult),
                 reads=[r_ang], writes=[r_u])
            P.op("dve", lambda e: e.tensor_copy(out=ki[:], in_=u[:]), reads=[r_u], writes=[r_ki])
            P.op("dve", lambda e: e.tensor_copy(out=kf[:], in_=ki[:]), reads=[r_ki], writes=[r_kf])
            P.op("dve", lambda e: e.scalar_tensor_tensor(out=u[:], in0=kf[:], scalar=-C1, in1=ang[:], op0=ALU.mult, op1=ALU.add),
                 reads=[r_kf, r_ang], writes=[r_u])
            P.op("dve", lambda e: e.scalar_tensor_tensor(out=u[:], in0=kf[:], scalar=-C2, in1=u[:], op0=ALU.mult, op1=ALU.add),
                 reads=[r_kf, r_u], writes=[r_u])
            P.op("dve", lambda e: e.tensor_scalar(out=m[:], in0=u[:], scalar1=math.pi, scalar2=-TWO_PI, op0=ALU.is_gt, op1=ALU.mult),
                 reads=[r_u], writes=[r_m])
            P.op("dve", lambda e: e.tensor_tensor(out=u[:], in0=u[:], in1=m[:], op=ALU.add), reads=[r_u, r_m], writes=[r_u])
            P.op("dve", lambda e: e.tensor_scalar(out=m[:], in0=u[:], scalar1=-math.pi, scalar2=TWO_PI, op0=ALU.is_lt, op1=ALU.mult),
                 reads=[r_u], writes=[r_m])
            P.op("dve", lambda e: e.tensor_tensor(out=u[:], in0=u[:], in1=m[:], op=ALU.add), reads=[r_u, r_m], writes=[r_u])
            P.op("act", lambda e: e.activation(out=res_t[:], in_=u[:], func=AF.Sin, bias=zero[:, 0:1]), reads=[r_u, r_zero], writes=[r_res])
            P.dma("sp", dst, res_t[:], reads=[r_res])
        st.done("rope")

    def stage_norm(l, x_src, g_name, sc_off, sh_off, hT_dst, router):
        st = Stage(P)
        gB, r_gB = st.sb([128, D], F32)
        scB, r_scB = st.sb([128, D], F32)
        shB, r_shB = st.sb([128, D], F32)
        A, r_A = st.sb([128, D], F32)
        epsb, r_eps = st.sb([128, 1], F32)
        identb, r_idb = st.sb([128, 128], BF16)
        xt = [st.sb([128, D], F32) for _ in range(4)]
        sq, r_sq = st.sb([128, D], F32)
        ss, r_ss = st.sb([128, 1], F32)
        rstd, r_rstd = st.sb([128, 1], F32)
        hf, r_hf = st.sb([128, D], F32)
        hb, r_hb = st.sb([128, D], BF16)
        hTs = [st.sb([128, 8, 128], BF16) for _ in range(2)]
        pt = [st.ps([128, 8, 128], BF16) for _ in range(2)]
        P.dma("sp", gB[:], I[g_name][l].partition_broadcast(128), writes=[r_gB])
        P.dma("sp", scB[:], mod_d[l, sc_off:sc_off + D].partition_broadcast(128), writes=[r_scB])
        P.dma("sp", shB[:], mod_d[l, sh_off:sh_off + D].partition_broadcast(128), writes=[r_shB])
        P.dma("pool", identb[:], I["k_ident"], writes=[r_idb])
        P.op("pool", lambda e: e.memset(epsb[:], EPS), writes=[r_eps])
        P.op("dve", lambda e: e.scalar_tensor_tensor(out=A[:], in0=scB[:], scalar=1.0, in1=gB[:], op0=ALU.add, op1=ALU.mult),
             reads=[r_scB, r_gB], writes=[r_A])
        if router:
            identf, r_idf = st.sb([128, 128], F32)
            wr, r_wr = st.sb([128, 8, NE], F32)
            brB, r_brB = st.sb([128, NE], F32)
            h32, r_h32 = st.sb([128, D], F32)
            hTf, r_hTf = st.sb([128, 8, 128], F32)
            ptf = [st.ps([128, 4, 128], F32) for _ in range(2)]
            plg, r_plg = st.ps([128, NE], F32)
            PC, r_PC = st.ps([128, NE], F32)
            pinc, r_pinc = st.sb([128, 128], F32)
            pgtm, r_pgtm = st.sb([128, 128], F32)
            iotac, r_iotac = st.sb([128, 1], F32)
            posb = [st.sb([128, NE], F32) for _ in range(4)]
            selb = [st.sb([128, NE], F32) for _ in range(4)]
            P.dma("sp", pinc[:], I["k_pincl"], writes=[r_pinc])
            P.dma("sp", pgtm[:], I["k_ustrict"], writes=[r_pgtm])
            P.dma("sp", iotac[:], col(I["k_iota"]), writes=[r_iotac])
            sc_t, r_sc = st.sb([128, NE], F32)
            bi, r_bi = st.sb([128, NE], F32)
            tmp, r_tmp = st.sb([128, NE], F32)
            m1, r_m1 = st.sb([128, 8], F32)
            m2, r_m2 = st.sb([128, 8], F32)
            gs, r_gs = st.sb([128, 8], F32)
            t8, r_t8 = st.sb([128, 8], F32)
            pen, r_pen = st.sb([128, 8], F32)
            sel, r_sel = st.sb([128, NE], F32)
            ssum, r_ssum = st.sb([128, 1], F32)
            gate, r_gate = st.sb([128, NE], F32)
            gTs, r_gTs = st.sb([NE, 128], F32)
            P.dma("sp", identf[:], I["k_ident"], writes=[r_idf])
            P.dma("sp", wr[:], kp(I["w_router"][l]), writes=[r_wr])
            P.dma("sp", brB[:], I["b_router"][l].partition_broadcast(128), writes=[r_brB])
        sq2 = [(sq, r_sq)] + [st.sb([128, D], F32) for _ in range(3)]
        ss2 = [(ss, r_ss)] + [st.sb([128, 1], F32) for _ in range(3)]
        rstd2 = [(rstd, r_rstd)] + [st.sb([128, 1], F32) for _ in range(3)]
        hf2 = [(hf, r_hf)] + [st.sb([128, D], F32) for _ in range(3)]
        hb2 = [(hb, r_hb)] + [st.sb([128, D], BF16) for _ in range(3)]
        if router:
            h322 = [(h32, r_h32)] + [st.sb([128, D], F32) for _ in range(3)]
            dup_sc = [(sc_t, r_sc), st.sb([128, NE], F32)]
            dup_bi = [(bi, r_bi), st.sb([128, NE], F32)]
            dup_tmp = [(tmp, r_tmp), st.sb([128, NE], F32)]
            dup_m1 = [(m1, r_m1), st.sb([128, 8], F32)]
            dup_m2 = [(m2, r_m2), st.sb([128, 8], F32)]
            dup_gs = [(gs, r_gs), st.sb([128, 8], F32)]
            dup_t8 = [(t8, r_t8), st.sb([128, 8], F32)]
            dup_pen = [(pen, r_pen), st.sb([128, 8], F32)]
            dup_sel = [(sel, r_sel), st.sb([128, NE], F32)]
            dup_ssum = [(ssum, r_ssum), st.sb([128, 1], F32)]
            dup_gate = [(gate, r_gate), st.sb([128, NE], F32)]
            dup_gTs = [(gTs, r_gTs), st.sb([NE, 128], F32)]
            dup_plg = [(plg, r_plg), st.ps([128, NE], F32)]
            plg2 = dup_plg
            onec, r_onec = st.sb([128, 1], F32)
            P.op("pool", lambda e: e.memset(onec[:], 1.0), writes=[r_onec])

        def ph1(t):
            x_t, r_x = xt[t % 4]
            sq_t, r_sq_ = sq2[t % 4]
            ss_t, r_ss_ = ss2[t % 4]
            rs_t, r_rs_ = rstd2[t % 4]
            hf_t, r_hf_ = hf2[t % 4]
            hb_t, r_hb_ = hb2[t % 4]
            P.dma("sp", x_t[:], x_src[t * 128:(t + 1) * 128, :], writes=[r_x])
            P.op("act", lambda e: e.activation(out=sq_t[:], in_=x_t[:], func=AF.Square, scale=float(D ** -0.5), accum_out=ss_t[:]),
                 reads=[r_x], writes=[r_sq_, r_ss_])
            P.op("act", lambda e: e.activation(out=rs_t[:], in_=ss_t[:], func=AF.Ln, bias=epsb[:, 0:1]), reads=[r_ss_, r_eps], writes=[r_rs_])
            P.op("act", lambda e: e.activation(out=rs_t[:], in_=rs_t[:], func=AF.Exp, scale=-0.5), reads=[r_rs_], writes=[r_rs_])
            P.op("dve", lambda e: e.scalar_tensor_tensor(out=hf_t[:], in0=x_t[:], scalar=rs_t[:, 0:1], in1=A[:], op0=ALU.mult, op1=ALU.mult),
                 reads=[r_x, r_rs_, r_A], writes=[r_hf_])
            P.op("pool", lambda e: e.tensor_tensor(out=hb_t[:], in0=hf_t[:], in1=shB[:], op=ALU.add), reads=[r_hf_, r_shB], writes=[r_hb_])
            if router:
                h32_t, r_h32_ = h322[t % 4]
                P.op("dve", lambda e: e.tensor_tensor(out=h32_t[:], in0=hf_t[:], in1=shB[:], op=ALU.add), reads=[r_hf_, r_shB], writes=[r_h32_])

        def ph2a(t):
            hb_t, r_hb_ = hb2[t % 4]
            p_t, r_p = pt[t % 2]
            h_t, r_h = hTs[t % 2]
            for c in range(8):
                P.op("pe", lambda e: e.transpose(out=p_t[:, c, :], in_=hb_t[:, c * 128:(c + 1) * 128], identity=identb[:]),
                     reads=[r_hb_, r_idb], writes=[r_p])
            P.op("act", lambda e: e.copy(out=h_t[:], in_=p_t[:]), reads=[r_p], writes=[r_h])
            P.dma("sp", kp(hT_dst)[:, :, t * 128:(t + 1) * 128], h_t[:], reads=[r_h])
            if router:
                P.dma("sp", h2_d[t * 128:(t + 1) * 128, :], hb_t[:], reads=[r_hb_])
                plg, r_plg = plg2[t % 2]
                h32_t, r_h32_ = h322[t % 4]
                for half in range(2):
                    pf_t, r_pf = ptf[half]
                    for c in range(4):
                        cc = half * 4 + c
                        P.op("pe", lambda e: e.transpose(out=pf_t[:, c, :], in_=h32_t[:, cc * 128:(cc + 1) * 128], identity=identf[:]),
                             reads=[r_h32_, r_idf], writes=[r_pf])
                    P.op("dve", lambda e: e.tensor_copy(out=hTf[:, half * 4:(half + 1) * 4, :], in_=pf_t[:]), reads=[r_pf], writes=[r_hTf])
                for c in range(8):
                    P.op("pe", lambda e: e.matmul(plg[:], lhsT=hTf[:, c, :], rhs=wr[:, c, :], start=(c == 0), stop=(c == 7)),
                         reads=[r_hTf, r_wr], writes=[r_plg])

        def ph2b(t):
            sc_t, r_sc = dup_sc[t % 2]
            bi, r_bi = dup_bi[t % 2]
            tmp, r_tmp = dup_tmp[t % 2]
            m1, r_m1 = dup_m1[t % 2]
            m2, r_m2 = dup_m2[t % 2]
            gs, r_gs = dup_gs[t % 2]
            t8, r_t8 = dup_t8[t % 2]
            pen, r_pen = dup_pen[t % 2]
            sel, r_sel = dup_sel[t % 2]
            ssum, r_ssum = dup_ssum[t % 2]
            gate, r_gate = dup_gate[t % 2]
            gTs, r_gTs = dup_gTs[t % 2]
            plg, r_plg = dup_plg[t % 2]
            P.op("act", lambda e: e.activation(out=sc_t[:], in_=plg[:], func=AF.Exp, scale=-1.0), reads=[r_plg], writes=[r_sc])
            yield
            P.op("dve", lambda e: e.tensor_scalar(out=sc_t[:], in0=sc_t[:], scalar1=1.0, scalar2=None, op0=ALU.add), reads=[r_sc], writes=[r_sc])
            yield
            P.op("dve", lambda e: e.reciprocal(out=sc_t[:], in_=sc_t[:]), reads=[r_sc], writes=[r_sc])
            yield
            P.op("dve", lambda e: e.tensor_tensor(out=bi[:], in0=sc_t[:], in1=brB[:], op=ALU.add), reads=[r_sc, r_brB], writes=[r_bi])
            yield
            bi3 = bi[:].rearrange("p (g e) -> p g e", g=8)
            tmp3 = tmp[:].rearrange("p (g e) -> p g e", g=8)
            P.op("dve", lambda e: e.tensor_reduce(out=m1[:], in_=bi3, axis=AX.X, op=ALU.max), reads=[r_bi], writes=[r_m1])
            yield
            P.op("dve", lambda e: e.tensor_tensor(out=tmp3, in0=bi3, in1=m1[:].unsqueeze(2).to_broadcast([128, 8, 8]), op=ALU.is_equal),
                 reads=[r_bi, r_m1], writes=[r_tmp])
            yield
            P.op("dve", lambda e: e.scalar_tensor_tensor(out=tmp[:], in0=tmp[:], scalar=-1e30, in1=bi[:], op0=ALU.mult, op1=ALU.add),
                 reads=[r_tmp, r_bi], writes=[r_tmp])
            yield
            P.op("dve", lambda e: e.tensor_reduce(out=m2[:], in_=tmp3, axis=AX.X, op=ALU.max), reads=[r_tmp], writes=[r_m2])
            yield
            P.op("dve", lambda e: e.tensor_tensor(out=gs[:], in0=m1[:], in1=m2[:], op=ALU.add), reads=[r_m1, r_m2], writes=[r_gs])
            yield
            P.op("dve", lambda e: e.max(out=t8[:], in_=gs[:]), reads=[r_gs], writes=[r_t8])
            yield
            P.op("dve", lambda e: e.tensor_scalar(out=pen[:], in0=gs[:], scalar1=t8[:, 3:4], scalar2=-1e30, op0=ALU.is_lt, op1=ALU.mult),
                 reads=[r_gs, r_t8], writes=[r_pen])
            yield
            P.op("dve", lambda e: e.tensor_tensor(out=tmp3, in0=bi3, in1=pen[:].unsqueeze(2).to_broadcast([128, 8, 8]), op=ALU.add),
                 reads=[r_bi, r_pen], writes=[r_tmp])
            yield
            P.op("dve", lambda e: e.max(out=t8[:], in_=tmp[:]), reads=[r_tmp], writes=[r_t8])
            yield
            P.op("dve", lambda e: e.tensor_scalar(out=sel[:], in0=tmp[:], scalar1=t8[:, 7:8], scalar2=None, op0=ALU.is_ge),
                 reads=[r_tmp, r_t8], writes=[r_sel])
            yield
            P.op("dve", lambda e: e.tensor_tensor(out=sel[:], in0=sel[:], in1=sc_t[:], op=ALU.mult), reads=[r_sel, r_sc], writes=[r_sel])
            yield
            P.op("dve", lambda e: e.tensor_reduce(out=ssum[:], in_=sel[:], axis=AX.X, op=ALU.add), reads=[r_sel], writes=[r_ssum])
            yield
            P.op("dve", lambda e: e.tensor_scalar(out=ssum[:], in0=ssum[:], scalar1=1e-20, scalar2=None, op0=ALU.add), reads=[r_ssum], writes=[r_ssum])
            yield
            P.op("dve", lambda e: e.reciprocal(out=ssum[:], in_=ssum[:]), reads=[r_ssum], writes=[r_ssum])
            yield
            P.op("dve", lambda e: e.tensor_scalar(out=gate[:], in0=sel[:], scalar1=ssum[:, 0:1], scalar2=2.5, op0=ALU.mult, op1=ALU.mult),
                 reads=[r_sel, r_ssum], writes=[r_gate])
            yield
            P.op("dve", lambda e: e.tensor_scalar(out=selb[t % 4][0][:], in0=gate[:], scalar1=0.0, scalar2=None, op0=ALU.is_gt),
                 reads=[r_gate], writes=[selb[t % 4][1]])
            yield
            P.dma("sp", gate_d[t * 128:(t + 1) * 128, :], gate[:], reads=[r_gate])
            yield

        def drain(*gens):
            gens = list(gens)
            while gens:
                for g in list(gens):
                    try:
                        next(g)
                    except StopIteration:
                        gens.remove(g)

        prev_tl = []

        def pc_ops(tl_):
            for tt_ in tl_:
                s_t, r_s = selb[tt_ % 4]
                p_t, r_p = posb[tt_ % 4]
                P.op("pe", lambda e: e.matmul(PC[:], lhsT=pinc[:], rhs=s_t[:], start=(tt_ == 0), stop=False, skip_group_check=True),
                     reads=[r_pinc, r_s], writes=[r_PC])
                P.op("act", lambda e: e.copy(out=p_t[:], in_=PC[:]), reads=[r_PC], writes=[r_p])
                P.op("pe", lambda e: e.matmul(PC[:], lhsT=pgtm[:], rhs=s_t[:], start=False, stop=(tt_ == NT - 1), skip_group_check=True),
                     reads=[r_pgtm, r_s, r_p], writes=[r_PC])
                P.dma("sp", pos_d[tt_ * 128:(tt_ + 1) * 128, :], p_t[:], reads=[r_p])

        for t0_ in range(min(4, NT)):
            ph1(t0_)
        for t in range(0, NT, 2):
            tl = [t] + ([t + 1] if t + 1 < NT else [])
            for tt_ in tl:
                ph2a(tt_)
            if t + 4 < NT:
                ph1(t + 4)
            if t + 5 < NT:
                ph1(t + 5)
            if router:
                pc_ops(prev_tl)
                prev_tl = tl
                drain(*[ph2b(tt_) for tt_ in tl])
        if router:
            pc_ops(prev_tl)
        if router:
            cnt, r_cnt = st.sb([128, NE], F32)
            nbt, r_nbt = st.sb([128, NE], F32)
            ca, r_ca = st.sb([128, NE], F32)
            cb, r_cb = st.sb([128, NE], F32)
            ebc, r_ebc = st.sb([128, 1], F32)
            P.op("act", lambda e: e.copy(out=cnt[:], in_=PC[:]), reads=[r_PC], writes=[r_cnt])
            P.op("dve", lambda e: e.tensor_scalar(out=nbt[:], in0=cnt[:], scalar1=0.0, scalar2=None, op0=ALU.is_gt), reads=[r_cnt], writes=[r_nbt])
            for j_ in range(1, 8):
                P.op("dve", lambda e: e.scalar_tensor_tensor(out=nbt[:], in0=cnt[:], scalar=512.0 * j_, in1=nbt[:], op0=ALU.is_gt, op1=ALU.add),
                     reads=[r_cnt, r_nbt], writes=[r_nbt])
            P.op("dve", lambda e: e.tensor_copy(out=ca[:], in_=nbt[:]), reads=[r_nbt], writes=[r_ca])
            src, r_src, dst_, r_dst = ca, r_ca, cb, r_cb
            k_ = 1
            while k_ < NE:
                P.op("dve", lambda e: e.tensor_copy(out=dst_[:, 0:k_], in_=src[:, 0:k_]), reads=[r_src], writes=[r_dst])
                P.op("dve", lambda e: e.tensor_tensor(out=dst_[:, k_:NE], in0=src[:, k_:NE], in1=src[:, 0:NE - k_], op=ALU.add), reads=[r_src], writes=[r_dst])
                src, r_src, dst_, r_dst = dst_, r_dst, src, r_src
                k_ *= 2
            bend, r_bend = src, r_src
            P.op("dve", lambda e: e.tensor_scalar(out=dst_[:], in0=bend[:], scalar1=iotac[:, 0:1], scalar2=None, op0=ALU.is_le),
                 reads=[r_bend, r_iotac], writes=[r_dst])
            P.op("dve", lambda e: e.tensor_reduce(out=ebc[:], in_=dst_[:], axis=AX.X, op=ALU.add), reads=[r_dst], writes=[r_ebc])
            P.op("dve", lambda e: e.tensor_scalar(out=ebc[:], in0=ebc[:], scalar1=float(NE - 1), scalar2=None, op0=ALU.min), reads=[r_ebc], writes=[r_ebc])
            P.dma("sp", col(eb_d), ebc[:], reads=[r_ebc])
            P.op("dve", lambda e: e.tensor_tensor(out=cnt[:], in0=bend[:], in1=nbt[:], op=ALU.subtract), reads=[r_bend, r_nbt], writes=[r_cnt])
            P.op("dve", lambda e: e.tensor_scalar(out=cnt[:], in0=cnt[:], scalar1=512.0, scalar2=None, op0=ALU.mult), reads=[r_cnt], writes=[r_cnt])
            P.dma("sp", sbase_d.rearrange("(o n) -> o n", o=1), cnt[0:1, :], reads=[r_cnt])
        st.done(f"norm{int(router)}_{l}")

    def stage_proj(l):
        st = Stage(P)
        NCOL = 4096
        w, r_w = st.sb([128, 8, NCOL], BF16)
        blk, r_blk = st.sb([128, 128], BF16)
        rot, r_rot = st.sb([128, 128], BF16)
        epsb, r_eps = st.sb([128, 1], F32)
        gq, r_gq = st.sb([128, 1], F32)
        gk, r_gk = st.sb([128, 1], F32)
        hT = [st.sb([128, 8, 512], BF16) for _ in range(2)]
        cosT = [st.sb([128, 512], F32) for _ in range(2)]
        sinT = [st.sb([128, 512], F32) for _ in range(2)]
        pp = [st.ps([128, 512], F32) for _ in range(4)]
        pms, r_pms = st.ps([128, 512], F32)
        prot, r_prot = st.ps([128, 512], F32)
        sqb, r_sqb = st.sb([128, 512], BF16)
        rs, r_rs = st.sb([128, 512], F32)
        qn, r_qn = st.sb([128, 512], BF16)
        t1, r_t1 = st.sb([128, 512], F32)
        t2, r_t2 = st.sb([128, 512], F32)
        sg, r_sg = st.sb([128, 512], F32)
        ob = [st.sb([128, 512], BF16) for _ in range(4)]
        for c in range(8):
            P.dma("pool", w[:, c, :], I["w_in"][l][c * 128:(c + 1) * 128, 0:NCOL], writes=[r_w])
        P.dma("pool", blk[:], I["k_blk64"], writes=[r_blk])
        P.dma("pool", rot[:], I["k_rotT"], writes=[r_rot])
        P.op("pool", lambda e: e.memset(epsb[:], EPS), writes=[r_eps])
        for hh in range(2):
            P.dma("sp", gq[hh * 64:(hh + 1) * 64, :], col(I["qn_g"][l]), writes=[r_gq])
            P.dma("sp", gk[hh * 64:(hh + 1) * 64, :], col(I["kn_g"][l]), writes=[r_gk])
        pp = pp + [st.ps([128, 512], F32) for _ in range(2)]
        NPP = len(pp)
        loaded = set()

        def load_tile(tt):
            if tt in loaded or tt >= NQ:
                return
            loaded.add(tt)
            ts_ = slice(tt * 512, (tt + 1) * 512)
            P.dma("sp", hT[tt % 2][0][:], kp(hT_d)[:, :, ts_], writes=[hT[tt % 2][1]])
            P.dma("sp", cosT[tt % 2][0][:], cos_d[:, ts_], writes=[cosT[tt % 2][1]])
            P.dma("sp", sinT[tt % 2][0][:], sin_d[:, ts_], writes=[sinT[tt % 2][1]])

        units = []
        for tt in range(NQ):
            for which in range(2):
                for j in range(4):
                    units.append(("qk", tt, (which, j)))
            for j in range(4):
                units.append(("glu", tt, (j,)))
            for which in range(2):
                for j in range(4):
                    units.append(("cp", tt, (which, j)))
            for which in range(2):
                for sub in range(4):
                    units.append(("tm", tt, (which, sub)))
        pidx = []
        cur = 0
        for kind, tt, args in units:
            nb_ = 2 if kind == "glu" else 1
            pidx.append([(cur + i) % NPP for i in range(nb_)])
            cur += nb_
        state = {"oi": 0}

        def fmm(tt, col0, p_t, r_p):
            h_t, r_h = hT[tt % 2]
            for c in range(8):
                P.op("pe", lambda e: e.matmul(p_t[:], lhsT=w[:, c, col0:col0 + 128], rhs=h_t[:, c, :], start=(c == 0), stop=(c == 7)),
                     reads=[r_w, r_h], writes=[r_p])

        def F(u):
            kind, tt, args = units[u]
            load_tile(tt)
            bufs = [pp[i] for i in pidx[u]]
            if kind == "qk":
                which, j = args
                fmm(tt, which * 512 + j * 128, *bufs[0])
            elif kind == "glu":
                (j,) = args
                fmm(tt, 1536 + j * 128, *bufs[0])
                fmm(tt, 2048 + j * 128, *bufs[1])
            elif kind == "cp":
                which, j = args
                fmm(tt, (2560 if which == 0 else 3072) + j * 128, *bufs[0])
            else:
                which, sub = args
                base = 1024 if which == 0 else 3584
                h_t, r_h = hT[tt % 2]
                p_t, r_p = bufs[0]
                for c in range(8):
                    P.op("pe", lambda e: e.matmul(p_t[:], lhsT=h_t[:, c, sub * 128:(sub + 1) * 128], rhs=w[:, c, base:base + 512], start=(c == 0), stop=(c == 7)),
                         reads=[r_w, r_h], writes=[r_p])

        def nxt_ob():
            o = ob[state["oi"] % 4]
            state["oi"] += 1
            return o

        def G(u):
            kind, tt, args = units[u]
            ts_ = slice(tt * 512, (tt + 1) * 512)
            bufs = [pp[i] for i in pidx[u]]
            c_t, r_c = cosT[tt % 2]
            s_t, r_s = sinT[tt % 2]
            if kind == "qk":
                which, j = args
                g_t, r_g, dst = (gq, r_gq, qaT_d) if which == 0 else (gk, r_gk, kaT_d)
                p_t, r_p = bufs[0]
                P.op("act", lambda e: e.activation(out=sqb[:], in_=p_t[:], func=AF.Square), reads=[r_p], writes=[r_sqb])
                P.op("pe", lambda e: e.matmul(pms[:], lhsT=blk[:], rhs=sqb[:], start=True, stop=True), reads=[r_blk, r_sqb], writes=[r_pms])
                P.op("act", lambda e: e.activation(out=rs[:], in_=pms[:], func=AF.Ln, bias=epsb[:, 0:1]), reads=[r_pms, r_eps], writes=[r_rs])
                P.op("act", lambda e: e.activation(out=rs[:], in_=rs[:], func=AF.Exp, scale=-0.5), reads=[r_rs], writes=[r_rs])
                P.op("dve", lambda e: e.scalar_tensor_tensor(out=qn[:], in0=p_t[:], scalar=g_t[:, 0:1], in1=rs[:], op0=ALU.mult, op1=ALU.mult),
                     reads=[r_p, r_g, r_rs], writes=[r_qn])
                P.op("pe", lambda e: e.matmul(prot[:], lhsT=rot[:], rhs=qn[:], start=True, stop=True), reads=[r_rot, r_qn], writes=[r_prot])
                P.op("pool", lambda e: e.tensor_tensor(out=t1[:], in0=qn[:], in1=c_t[:], op=ALU.mult), reads=[r_qn, r_c], writes=[r_t1])
                P.op("dve", lambda e: e.tensor_tensor(out=t2[:], in0=prot[:], in1=s_t[:], op=ALU.mult), reads=[r_prot, r_s], writes=[r_t2])
                o_t, r_o = nxt_ob()
                P.op("dve", lambda e: e.tensor_tensor(out=o_t[:], in0=t1[:], in1=t2[:], op=ALU.add), reads=[r_t1, r_t2], writes=[r_o])
                P.dma("sp", dst[j * 128:(j + 1) * 128, ts_], o_t[:], reads=[r_o])
            elif kind == "glu":
                (j,) = args
                (pa, r_pa), (pg, r_pg) = bufs
                P.op("act", lambda e: e.activation(out=sg[:], in_=pg[:], func=AF.Sigmoid), reads=[r_pg], writes=[r_sg])
                o_t, r_o = nxt_ob()
                P.op("dve", lambda e: e.tensor_tensor(out=o_t[:], in0=pa[:], in1=sg[:], op=ALU.mult), reads=[r_pa, r_sg], writes=[r_o])
                P.dma("sp", uT_d[j * 128:(j + 1) * 128, ts_], o_t[:], reads=[r_o])
            elif kind == "cp":
                which, j = args
                dst, qscale = (qcT_d, 0.125) if which == 0 else (kcT_d, 1.0)
                p_t, r_p = bufs[0]
                o_t, r_o = nxt_ob()
                P.op("act", lambda e: e.mul(out=o_t[:], in_=p_t[:], mul=qscale), reads=[r_p], writes=[r_o])
                P.dma("sp", dst[j * 128:(j + 1) * 128, ts_], o_t[:], reads=[r_o])
            else:
                which, sub = args
                dst = va_d if which == 0 else vc_d
                p_t, r_p = bufs[0]
                o_t, r_o = nxt_ob()
                P.op("dve" if sub % 2 == 0 else "act", (lambda e: e.tensor_copy(out=o_t[:], in_=p_t[:])) if sub % 2 == 0 else (lambda e: e.copy(out=o_t[:], in_=p_t[:])),
                     reads=[r_p], writes=[r_o])
                r0 = tt * 512 + sub * 128
                P.dma("sp", dst[r0:r0 + 128, :], o_t[:], reads=[r_o])

        nU = len(units)
        AHEAD = 2
        for u in range(min(AHEAD, nU)):
            F(u)
        for u in range(nU):
            if u + AHEAD < nU:
                F(u + AHEAD)
            G(u)
        st.done(f"proj{l}")

    def stage_da(l):
        lambda_init = 0.8 - 0.6 * math.exp(-0.3 * l)
        st = Stage(P)
        msk, r_msk = st.sb([128, 4, 512], BF16)
        ones, r_ones = st.sb([128, 128], BF16)
        o128, r_o128 = st.sb([128, 128], BF16)
        epsb, r_eps = st.sb([128, 1], F32)
        lv = [st.sb([128, 64], F32) for _ in range(4)]
        lp, r_lp = st.sb([128, 64], F32)
        ld, r_ld = st.sb([128, 2], F32)
        nlam, r_nlam = st.sb([128, 1], F32)
        gcol, r_gcol = st.sb([128, 1], F32)
        qT = [[st.sb([128, S], BF16) for _ in range(2)] for _ in range(2)]
        kT = [st.sb([128, S], BF16) for _ in range(2)]
        vv = [st.sb([128, NT, 128], BF16) for _ in range(2)]
        pz = [st.ps([128, 512], F32) for _ in range(2)]
        pO = [st.ps([128, 512], F32) for _ in range(2)]
        pL = [st.ps([128, 512], F32) for _ in range(2)]
        for hp_ in range(2):
            for m_ in range(2):
                zr = slice((1 - m_) * 64, (1 - m_) * 64 + 64)
                P.op("pool", lambda e: e.memset(qT[hp_][m_][0][zr, :], 0.0), writes=[qT[hp_][m_][1]])
        pms, r_pms = st.ps([128, 512], F32)
        E = [st.sb([128, 512], BF16) for _ in range(4)]
        rL = [st.sb([128, 512], F32) for _ in range(2)]
        tO = [st.sb([128, 512], F32) for _ in range(2)]
        o_t, r_o = st.sb([128, 512], F32)
        sqb, r_sqb = st.sb([128, 512], BF16)
        rs, r_rs = st.sb([128, 512], F32)
        ob = [st.sb([128, 512], BF16) for _ in range(2)]
        P.dma("pool", msk[:], I["k_maskc"].rearrange("j p q -> p j q"), writes=[r_msk])
        if l == 0:
            zt, r_zt = st.sb([128, 4096], BF16)
            P.op("pool", lambda e: e.memset(zt[:], 0.0), writes=[r_zt])
        zfill = {"next": 0}

        def zero_fill_some(n_):
            if l != 0:
                return
            for _ in range(n_):
                zi_ = zfill["next"]
                if zi_ >= NSLOT // 512:
                    return
                zfill["next"] += 1
                P.dma("pool", xs_d[zi_ * 512:(zi_ + 1) * 512, :].rearrange("(p r) d -> p (r d)", p=128), zt[:], reads=[r_zt])
        P.op("pool", lambda e: e.memset(ones[:], 1.0), writes=[r_ones])
        P.op("pool", lambda e: e.memset(o128[:], 1.0 / 128), writes=[r_o128])
        P.op("pool", lambda e: e.memset(epsb[:], EPS), writes=[r_eps])
        for i, nm in enumerate(("lam_q1", "lam_k1", "lam_q2", "lam_k2")):
            P.dma("sp", lv[i][0][:], I[nm][l].partition_broadcast(128), writes=[lv[i][1]])
        for i in range(2):
            P.op("dve", lambda e, i=i: e.tensor_tensor(out=lp[:], in0=lv[2 * i][0][:], in1=lv[2 * i + 1][0][:], op=ALU.mult),
                 reads=[lv[2 * i][1], lv[2 * i + 1][1]], writes=[r_lp])
            P.op("dve", lambda e, i=i: e.tensor_reduce(out=ld[:, i:i + 1], in_=lp[:], axis=AX.X, op=ALU.add), reads=[r_lp], writes=[r_ld])
        P.op("act", lambda e: e.activation(out=ld[:], in_=ld[:], func=AF.Exp), reads=[r_ld], writes=[r_ld])
        P.op("dve", lambda e: e.tensor_tensor(out=nlam[:], in0=ld[:, 1:2], in1=ld[:, 0:1], op=ALU.subtract), reads=[r_ld], writes=[r_nlam])
        P.op("dve", lambda e: e.tensor_scalar(out=nlam[:], in0=nlam[:], scalar1=-lambda_init, scalar2=None, op0=ALU.add), reads=[r_nlam], writes=[r_nlam])
        P.dma("sp", gcol[:], col(I["subln_g"][l]), writes=[r_gcol])
        P.op("dve", lambda e: e.tensor_scalar(out=gcol[:], in0=gcol[:], scalar1=1.0 - lambda_init, scalar2=None, op0=ALU.mult), reads=[r_gcol], writes=[r_gcol])
        oi = 0
        pz3 = pz + [st.ps([128, 512], F32)]
        units = []
        for hd in range(4):
            for Qi in range(NQ):
                for m in range(2):
                    for kt in range(4 * Qi + 4):
                        units.append((hd, Qi, m, kt))
        loaded = set()

        def load_head(hd):
            if hd in loaded or hd >= 4:
                return
            loaded.add(hd)
            k_t, r_k = kT[hd % 2]
            v_t, r_v = vv[hd % 2]
            for m_ in range(2):
                rr_ = slice(m_ * 64, m_ * 64 + 64)
                P.dma("sp", qT[hd % 2][m_][0][rr_, :], qaT_d[hd * 128 + m_ * 64:hd * 128 + m_ * 64 + 64, :], writes=[qT[hd % 2][m_][1]])
            P.dma("sp", k_t[:], kaT_d[hd * 128:(hd + 1) * 128, :], writes=[r_k])
            P.dma("sp", v_t[:], va_d[:, hd * 128:(hd + 1) * 128].rearrange("(t p) e -> p t e", p=128), writes=[r_v])

        def phA(i):
            hd, Qi, m, kt = units[i]
            load_head(hd)
            q_t, r_q = qT[hd % 2][m]
            k_t, r_k = kT[hd % 2]
            z_t, r_z = pz3[i % 3]
            qs = slice(Qi * 512, (Qi + 1) * 512)
            P.op("pe", lambda e: e.matmul(z_t[:], lhsT=k_t[:, kt * 128:(kt + 1) * 128], rhs=q_t[:, qs], start=True, stop=True),
                 reads=[r_k, r_q], writes=[r_z])

        def phB(i):
            nonlocal oi
            hd, Qi, m, kt = units[i]
            v_t, r_v = vv[hd % 2]
            z_t, r_z = pz3[i % 3]
            e_t, r_e = E[i % 4]
            pO_t, r_pO = pO[m]
            pL_t, r_pL = pL[m]
            nk = 4 * Qi + 4
            j = kt - 4 * Qi
            qs = slice(Qi * 512, (Qi + 1) * 512)
            P.op("act", lambda e: e.activation(out=e_t[:], in_=z_t[:], func=AF.Exp, scale=0.125), reads=[r_z], writes=[r_e])
            if j >= 0:
                P.op("dve", lambda e: e.tensor_tensor(out=e_t[:], in0=e_t[:], in1=msk[:, j, :], op=ALU.mult), reads=[r_e, r_msk], writes=[r_e])
            P.op("pe", lambda e: e.matmul(pO_t[:], lhsT=v_t[:, kt, :], rhs=e_t[:], start=(kt == 0), stop=(kt == nk - 1)),
                 reads=[r_v, r_e], writes=[r_pO])
            P.op("pe", lambda e: e.matmul(pL_t[:], lhsT=ones[:], rhs=e_t[:], start=(kt == 0), stop=(kt == nk - 1)),
                 reads=[r_ones, r_e], writes=[r_pL])
            if kt == nk - 1:
                P.op("act", lambda e: e.activation(out=rL[m][0][:], in_=pL_t[:], func=AF.Ln), reads=[r_pL], writes=[rL[m][1]])
                P.op("act", lambda e: e.activation(out=rL[m][0][:], in_=rL[m][0][:], func=AF.Exp, scale=-1.0), reads=[rL[m][1]], writes=[rL[m][1]])
                P.op("dve", lambda e: e.tensor_tensor(out=tO[m][0][:], in0=pO_t[:], in1=rL[m][0][:], op=ALU.mult), reads=[r_pO, rL[m][1]], writes=[tO[m][1]])
                if m == 1:
                    P.op("dve", lambda e: e.scalar_tensor_tensor(out=o_t[:], in0=tO[1][0][:], scalar=nlam[:, 0:1], in1=tO[0][0][:], op0=ALU.mult, op1=ALU.add),
                         reads=[tO[0][1], tO[1][1], r_nlam], writes=[r_o])
                    P.op("act", lambda e: e.activation(out=sqb[:], in_=o_t[:], func=AF.Square), reads=[r_o], writes=[r_sqb])
                    P.op("pe", lambda e: e.matmul(pms[:], lhsT=o128[:], rhs=sqb[:], start=True, stop=True), reads=[r_o128, r_sqb], writes=[r_pms])
                    P.op("act", lambda e: e.activation(out=rs[:], in_=pms[:], func=AF.Ln, bias=epsb[:, 0:1]), reads=[r_pms, r_eps], writes=[r_rs])
                    P.op("act", lambda e: e.activation(out=rs[:], in_=rs[:], func=AF.Exp, scale=-0.5), reads=[r_rs], writes=[r_rs])
                    b_t, r_b = ob[oi % 2]
                    oi += 1
                    P.op("dve", lambda e: e.scalar_tensor_tensor(out=b_t[:], in0=o_t[:], scalar=gcol[:, 0:1], in1=rs[:], op0=ALU.mult, op1=ALU.mult),
                         reads=[r_o, r_gcol, r_rs], writes=[r_b])
                    P.dma("sp", oaT_d[hd * 128:(hd + 1) * 128, qs], b_t[:], reads=[r_b])

        n = len(units)
        for i in range(min(2, n)):
            phA(i)
        for i in range(n):
            if i + 2 < n:
                phA(i + 2)
            phB(i)
            if i % 4 == 3:
                zero_fill_some(1)
        zero_fill_some(NSLOT)
        st.done(f"da{l}")

    def stage_sb(l):
        st = Stage(P)
        mskb, r_mskb = st.sb([128, 4, 512], BF16)
        uinc, r_uinc = st.sb([128, 128], BF16)
        ulow, r_ulow = st.sb([128, 128], BF16)
        onec, r_onec = st.sb([128, 1], F32)
        qT = [[st.sb([128, S], BF16) for _ in range(2)] for _ in range(2)]
        kT = [st.sb([128, S], BF16) for _ in range(2)]
        kN = [st.sb([128, S], BF16) for _ in range(2)]
        vv = [st.sb([128, NT, 128], BF16) for _ in range(2)]
        pz = [st.ps([128, 512], F32) for _ in range(2)]
        PT = [st.ps([128, 512], F32) for _ in range(2)]
        pO = [st.ps([128, 512], F32) for _ in range(2)]
        ex = [st.ps([128, 512], F32) for _ in range(2)]
        sp_ = [st.sb([128, 512], F32) for _ in range(2)]
        lom = [st.sb([128, 512], BF16) for _ in range(3)]
        ab = [st.sb([128, 512], BF16) for _ in range(3)]
        ob = [st.sb([128, 512], BF16) for _ in range(2)]
        for cp_ in range(2):
            for hh_ in range(2):
                zr = slice((1 - hh_) * 64, (1 - hh_) * 64 + 64)
                P.op("pool", lambda e: e.memset(qT[cp_][hh_][0][zr, :], 0.0), writes=[qT[cp_][hh_][1]])
        P.dma("pool", mskb[:], I["k_masks"].rearrange("j p q -> p j q"), writes=[r_mskb])
        P.dma("pool", uinc[:], I["k_uincl"], writes=[r_uinc])
        P.dma("pool", ulow[:], I["k_ulow"], writes=[r_ulow])

        P.op("pool", lambda e: e.memset(onec[:], 1.0), writes=[r_onec])
        oi = 0
        units = []
        for hp in range(4):
            for Qi in range(NQ):
                for kt in range(4 * Qi + 3, -1, -1):
                    for hh in range(2):
                        units.append((2 * hp + hh, Qi, kt))
        n = len(units)
        loaded = set()

        def load_pair(ch):
            if ch in loaded:
                return
            loaded.add(ch)
            for hh_ in range(2):
                rr_ = slice(hh_ * 64, hh_ * 64 + 64)
                P.dma("sp", qT[ch % 2][hh_][0][rr_, :], qcT_d[ch * 128 + hh_ * 64:ch * 128 + hh_ * 64 + 64, :], writes=[qT[ch % 2][hh_][1]])
            P.dma("sp", kT[ch % 2][0][:], kcT_d[ch * 128:(ch + 1) * 128, :], writes=[kT[ch % 2][1]])
            P.dma("sp", vv[ch % 2][0][:], vc_d[:, ch * 128:(ch + 1) * 128].rearrange("(t p) e -> p t e", p=128), writes=[vv[ch % 2][1]])
            P.op("pool", lambda e: e.tensor_scalar(out=kN[ch % 2][0][:], in0=kT[ch % 2][0][:], scalar1=-1.0, scalar2=None, op0=ALU.mult),
                 reads=[kT[ch % 2][1]], writes=[kN[ch % 2][1]])

        def info(i):
            h, Qi, kt = units[i]
            ch = h // 2
            return h, Qi, kt, ch, kt - 4 * Qi, 4 * Qi + 4, slice(Qi * 512, (Qi + 1) * 512), slice(kt * 128, (kt + 1) * 128)

        def phA(i):
            h, Qi, kt, ch, j, nk, qs, ks = info(i)
            load_pair(ch)
            q_t, r_q = qT[ch % 2][h % 2]
            k_t, r_k = kT[ch % 2]
            z_t, r_z = pz[i % 2]
            P.op("pe", lambda e: e.matmul(z_t[:], lhsT=k_t[:, ks], rhs=q_t[:, qs], start=True, stop=True), reads=[r_k, r_q], writes=[r_z])

        def phB(i):
            h, Qi, kt, ch, j, nk, qs, ks = info(i)
            z_t, r_z = pz[i % 2]
            e_t, r_e = ex[i % 2]
            p_t, r_p = sp_[i % 2]
            l_t, r_l = lom[i % 3]
            P.op("act", lambda e: e.activation(out=e_t[:], in_=z_t[:], func=AF.Exp, scale=-1.0), reads=[r_z], writes=[r_e])
            P.op("act", lambda e: e.activation(out=p_t[:], in_=e_t[:], func=AF.Ln, bias=onec[:, 0:1]), reads=[r_e, r_onec], writes=[r_p])
            P.op("dve", lambda e: e.scalar_tensor_tensor(out=l_t[:], in0=z_t[:], scalar=-1.0, in1=p_t[:], op0=ALU.mult, op1=ALU.subtract),
                 reads=[r_z, r_p], writes=[r_l])
            if j >= 0:
                P.op("dve", lambda e: e.tensor_tensor(out=l_t[:], in0=l_t[:], in1=mskb[:, j, :], op=ALU.mult), reads=[r_l, r_mskb], writes=[r_l])

        def phC(i):
            h, Qi, kt, ch, j, nk, qs, ks = info(i)
            q_t, r_q = qT[ch % 2][h % 2]
            k_t, r_k = kT[ch % 2]
            l_t, r_l = lom[i % 3]
            T_t, r_T = PT[h % 2]
            P.op("pe", lambda e: e.matmul(T_t[:], lhsT=uinc[:], rhs=l_t[:], start=(kt == nk - 1), stop=False, skip_group_check=True),
                 reads=[r_uinc, r_l], writes=[r_T])
            P.op("pe", lambda e: e.matmul(T_t[:], lhsT=k_t[:, ks], rhs=q_t[:, qs], start=False, stop=False, skip_group_check=True),
                 reads=[r_k, r_q], writes=[r_T])

        def phD_act(i):
            h, Qi, kt, ch, j, nk, qs, ks = info(i)
            T_t, r_T = PT[h % 2]
            a_t, r_a = ab[i % 3]
            P.op("act", lambda e: e.activation(out=a_t[:], in_=T_t[:], func=AF.Exp), reads=[r_T], writes=[r_a])
            if j >= 0:
                P.op("dve", lambda e: e.tensor_tensor(out=a_t[:], in0=a_t[:], in1=mskb[:, j, :], op=ALU.mult), reads=[r_a, r_mskb], writes=[r_a])

        def phD_pe(i):
            nonlocal oi
            h, Qi, kt, ch, j, nk, qs, ks = info(i)
            q_t, r_q = qT[ch % 2][h % 2]
            kn_t, r_kn = kN[ch % 2]
            v_t, r_v = vv[ch % 2]
            l_t, r_l = lom[i % 3]
            T_t, r_T = PT[h % 2]
            a_t, r_a = ab[i % 3]
            pO_t, r_pO = pO[h % 2]
            pr = slice((h % 2) * 64, (h % 2) * 64 + 64)
            if kt > 0:
                P.op("pe", lambda e: e.matmul(T_t[:], lhsT=ulow[:], rhs=l_t[:], start=False, stop=False, skip_group_check=True),
                     reads=[r_ulow, r_l, r_a], writes=[r_T])
                P.op("pe", lambda e: e.matmul(T_t[:], lhsT=kn_t[:, ks], rhs=q_t[:, qs], start=False, stop=(kt == 1), skip_group_check=True),
                     reads=[r_kn, r_q], writes=[r_T])
            P.op("pe", lambda e: e.matmul(pO_t[:], lhsT=v_t[:, kt, :], rhs=a_t[:], start=(kt == nk - 1), stop=(kt == 0)),
                 reads=[r_v, r_a], writes=[r_pO])
            if kt == 0:
                b_t, r_b = ob[oi % 2]
                oi += 1
                P.op("act", lambda e: e.copy(out=b_t[pr, :], in_=pO_t[pr, :]), reads=[r_pO], writes=[r_b])
                P.dma("sp", ocT_d[h * 64:(h + 1) * 64, qs], b_t[pr, :], reads=[r_b])

        for it in range(-3, n):
            if 0 <= it < n:
                phD_act(it)
            if 0 <= it + 3 < n:
                phA(it + 3)
            if 0 <= it + 2 < n:
                phB(it + 2)
            if 0 <= it + 1 < n:
                phC(it + 1)
            if 0 <= it < n:
                phD_pe(it)
        st.done(f"sb{l}")

    def stage_cv(l):
        st = Stage(P)
        up, r_up = st.sb([128, 4, 30 + S], BF16)
        wcol, r_wcol = st.sb([128, 4, 31], F32)
        identf, r_idf = st.sb([128, 128], F32)
        dg = [st.sb([128, 31, 128], BF16) for _ in range(4)]
        bcol, r_bcol = st.sb([128, 4], F32)
        gcol, r_gcol = st.sb([128, 4], F32)
        lbcol, r_lbcol = st.sb([128, 4], F32)
        o512, r_o512 = st.sb([128, 128], F32)
        epsb, r_eps = st.sb([128, 1], F32)
        pc = [st.ps([128, 512], F32) for _ in range(4)]
        pmean, r_pmean = st.ps([128, 512], F32)
        pex2, r_pex2 = st.ps([128, 512], F32)
        cv32, r_cv = st.sb([128, 4, 512], F32)
        sq32, r_sq = st.sb([128, 4, 512], F32)
        mean, r_mean = st.sb([128, 512], F32)
        msq, r_msq = st.sb([128, 512], F32)
        rs, r_rs = st.sb([128, 512], F32)
        y = [st.sb([128, 512], F32) for _ in range(2)]
        ob = [st.sb([128, 512], BF16) for _ in range(2)]
        P.op("pool", lambda e: e.memset(up[:, :, 0:30], 0.0), writes=[r_up])
        for j in range(4):
            P.dma("sp", up[:, j, 30:30 + S], uT_d[j * 128:(j + 1) * 128, :], writes=[r_up])
            P.dma("sp", wcol[:, j, :], I["w_dw"][l][:, j * 128:(j + 1) * 128].rearrange("k p -> p k"), writes=[r_wcol])
        P.dma("sp", identf[:], I["k_ident"], writes=[r_idf])
        P.dma("sp", bcol[:], I["b_dw"][l].rearrange("(j p) -> p j", p=128), writes=[r_bcol])
        P.dma("sp", gcol[:], I["conv_ln_g"][l].rearrange("(j p) -> p j", p=128), writes=[r_gcol])
        P.dma("sp", lbcol[:], I["conv_ln_b"][l].rearrange("(j p) -> p j", p=128), writes=[r_lbcol])
        P.op("pool", lambda e: e.memset(o512[:], 1.0 / 512), writes=[r_o512])
        P.op("pool", lambda e: e.memset(epsb[:], EPS), writes=[r_eps])
        junk, r_junk = st.sb([128, 1], F32)
        for j in range(4):
            rr = []
            for k in range(31):
                eng = "dve" if k % 2 == 0 else "pool"
                r1 = Res()
                rr.append(r1)
                P.op(eng, lambda e: e.tensor_scalar(out=dg[j][0][:, k, :], in0=identf[:], scalar1=wcol[:, j, k:k + 1], scalar2=None, op0=ALU.mult),
                     reads=[r_idf, r_wcol], writes=[r1])
            P.op("dve", lambda e: e.memset(junk[:], 0.0), reads=rr, writes=[dg[j][1], r_junk])
        yi = 0
        for tt in range(NQ):
            for j in range(4):
                p_t, r_p = pc[j]
                for k in range(31):
                    P.op("pe", lambda e: e.matmul(p_t[:], lhsT=dg[j][0][:, k, :], rhs=up[:, j, tt * 512 + k:tt * 512 + k + 512], start=(k == 0), stop=(k == 30)),
                         reads=[dg[j][1], r_up], writes=[r_p])
                P.op("dve", lambda e: e.tensor_scalar(out=cv32[:, j, :], in0=p_t[:], scalar1=bcol[:, j:j + 1], scalar2=None, op0=ALU.add),
                     reads=[r_p, r_bcol], writes=[r_cv])
                P.op("pool", lambda e: e.tensor_tensor(out=sq32[:, j, :], in0=cv32[:, j, :], in1=cv32[:, j, :], op=ALU.mult), reads=[r_cv], writes=[r_sq])
            for j in range(4):
                P.op("pe", lambda e: e.matmul(pmean[:], lhsT=o512[:], rhs=cv32[:, j, :], start=(j == 0), stop=(j == 3)), reads=[r_o512, r_cv], writes=[r_pmean])
            for j in range(4):
                P.op("pe", lambda e: e.matmul(pex2[:], lhsT=o512[:], rhs=sq32[:, j, :], start=(j == 0), stop=(j == 3)), reads=[r_o512, r_sq], writes=[r_pex2])
            P.op("act", lambda e: e.copy(out=mean[:], in_=pmean[:]), reads=[r_pmean], writes=[r_mean])
            P.op("pool", lambda e: e.tensor_tensor(out=msq[:], in0=mean[:], in1=mean[:], op=ALU.mult), reads=[r_mean], writes=[r_msq])
            P.op("dve", lambda e: e.tensor_tensor(out=rs[:], in0=pex2[:], in1=msq[:], op=ALU.subtract), reads=[r_pex2, r_msq], writes=[r_rs])
            P.op("act", lambda e: e.activation(out=rs[:], in_=rs[:], func=AF.Ln, bias=epsb[:, 0:1]), reads=[r_rs, r_eps], writes=[r_rs])
            P.op("act", lambda e: e.activation(out=rs[:], in_=rs[:], func=AF.Exp, scale=-0.5), reads=[r_rs], writes=[r_rs])
            for j in range(4):
                y_t, r_y = y[yi % 2]
                b_t, r_b = ob[yi % 2]
                yi += 1
                P.op("pool", lambda e: e.tensor_tensor(out=y_t[:], in0=cv32[:, j, :], in1=mean[:], op=ALU.subtract), reads=[r_cv, r_mean], writes=[r_y])
                P.op("dve", lambda e: e.tensor_tensor(out=y_t[:], in0=y_t[:], in1=rs[:], op=ALU.mult), reads=[r_y, r_rs], writes=[r_y])
                P.op("act", lambda e: e.activation(out=b_t[:], in_=y_t[:], func=AF.Silu, scale=gcol[:, j:j + 1], bias=lbcol[:, j:j + 1]),
                     reads=[r_y, r_gcol, r_lbcol], writes=[r_b])
                P.dma("sp", cvT_d[j * 128:(j + 1) * 128, tt * 512:(tt + 1) * 512], b_t[:], reads=[r_b])
        st.done(f"cv{l}")

    def stage_mg(l, x_src):
        st = Stage(P)
        wg, r_wg = st.sb([128, 8, 3072], BF16)
        wp = [st.sb([128, 4, D], BF16) for _ in range(3)]
        wo, r_wo = st.sb([128, 8, D], BF16)
        bpb, r_bpb = st.sb([128, 8], F32)
        gmB, r_gmB = st.sb([128, D], F32)
        hT = [st.sb([128, 8, 512], BF16) for _ in range(2)]
        obr = [[st.sb([128, 4, 512], BF16) for _ in range(2)] for _ in range(3)]
        mT, r_mT = st.sb([128, 8, 512], BF16)
        py = [st.ps([128, 512], F32) for _ in range(2)]
        pg = [st.ps([128, 512], F32) for _ in range(2)]
        po = [st.ps([128, 512], F32) for _ in range(2)]
        sg = [st.sb([128, 512], F32) for _ in range(2)]
        mb = [st.sb([128, 512], F32) for _ in range(2)]
        acc, r_acc = st.sb([128, 512], F32)
        xt = [st.sb([128, D], F32) for _ in range(2)]
        tmp, r_tmp = st.sb([128, 512], F32)
        xn = [st.sb([128, D], F32) for _ in range(2)]
        for c in range(8):
            P.dma("pool", wg[:, c, :], I["w_in"][l][c * 128:(c + 1) * 128, 4096:7168], writes=[r_wg])
        for b, nm in enumerate(("w_proj_a", "w_proj_b", "w_proj_c")):
            P.dma("pool", wp[b][0][:], kp(I[nm][l]), writes=[wp[b][1]])
        P.dma("pool", wo[:], kp(I["w_out"][l]), writes=[r_wo])
        P.dma("sp", bpb[:], I["b_proj_b"][l].rearrange("(j p) -> p j", p=128), writes=[r_bpb])
        P.dma("sp", gmB[:], mod_d[l, 2 * D:3 * D].partition_broadcast(128), writes=[r_gmB])
        srcs = (oaT_d, cvT_d, ocT_d)
        loaded = set()

        def load_tile(tt):
            if tt in loaded or tt >= NQ:
                return
            loaded.add(tt)
            ts_ = slice(tt * 512, (tt + 1) * 512)
            P.dma("sp", hT[tt % 2][0][:], kp(hT_d)[:, :, ts_], writes=[hT[tt % 2][1]])
            for b_ in range(3):
                P.dma("sp", obr[b_][tt % 2][0][:], kp(srcs[b_])[:, :, ts_], writes=[obr[b_][tt % 2][1]])

        units = [(tt, j, b_) for tt in range(NQ) for j in range(8) for b_ in range(3)]
        state = {"xi": 0}

        def F(u):
            tt, j, b_ = units[u]
            load_tile(tt)
            y_t, r_y = py[u % 2]
            g_t, r_g = pg[u % 2]
            h_t, r_h = hT[tt % 2]
            o_b, r_ob = obr[b_][tt % 2]
            for c in range(4):
                P.op("pe", lambda e: e.matmul(y_t[:], lhsT=wp[b_][0][:, c, j * 128:(j + 1) * 128], rhs=o_b[:, c, :], start=(c == 0), stop=(c == 3)),
                     reads=[wp[b_][1], r_ob], writes=[r_y])
            for c in range(8):
                P.op("pe", lambda e: e.matmul(g_t[:], lhsT=wg[:, c, b_ * D + j * 128:b_ * D + (j + 1) * 128], rhs=h_t[:, c, :], start=(c == 0), stop=(c == 7)),
                     reads=[r_wg, r_h], writes=[r_g])

        def G(u):
            tt, j, b_ = units[u]
            y_t, r_y = py[u % 2]
            g_t, r_g = pg[u % 2]
            s_t, r_s = sg[u % 2]
            m_t, r_m = mb[u % 2]
            P.op("act", lambda e: e.activation(out=s_t[:], in_=g_t[:], func=AF.Sigmoid), reads=[r_g], writes=[r_s])
            if b_ == 0:
                P.op("dve", lambda e: e.tensor_tensor(out=acc[:], in0=y_t[:], in1=s_t[:], op=ALU.mult), reads=[r_y, r_s], writes=[r_acc])
            elif b_ == 1:
                P.op("dve", lambda e: e.scalar_tensor_tensor(out=m_t[:], in0=y_t[:], scalar=bpb[:, j:j + 1], in1=s_t[:], op0=ALU.add, op1=ALU.mult),
                     reads=[r_y, r_bpb, r_s], writes=[r_m])
                P.op("pool", lambda e: e.tensor_tensor(out=acc[:], in0=acc[:], in1=m_t[:], op=ALU.add), reads=[r_acc, r_m], writes=[r_acc])
            else:
                P.op("dve", lambda e: e.tensor_tensor(out=m_t[:], in0=y_t[:], in1=s_t[:], op=ALU.mult), reads=[r_y, r_s], writes=[r_m])
                P.op("dve", lambda e: e.tensor_tensor(out=mT[:, j, :], in0=acc[:], in1=m_t[:], op=ALU.add), reads=[r_acc, r_m], writes=[r_mT])
            if j == 7 and b_ == 2:
                OUT(tt)

        def OUT(tt):
            for sub in range(4):
                x_t, r_x = xt[state["xi"] % 2]
                n_t, r_n = xn[state["xi"] % 2]
                state["xi"] += 1
                r0 = tt * 512 + sub * 128
                P.dma("sp", x_t[:], x_src[r0:r0 + 128, :], writes=[r_x])
                for half in range(2):
                    o_t, r_o = po[half]
                    hs = slice(half * 512, (half + 1) * 512)
                    for c in range(8):
                        P.op("pe", lambda e: e.matmul(o_t[:], lhsT=mT[:, c, sub * 128:(sub + 1) * 128], rhs=wo[:, c, hs], start=(c == 0), stop=(c == 7)),
                             reads=[r_mT, r_wo], writes=[r_o])
                    P.op("dve", lambda e: e.tensor_tensor(out=tmp[:], in0=o_t[:], in1=gmB[:, hs], op=ALU.mult), reads=[r_o, r_gmB], writes=[r_tmp])
                    P.op("pool", lambda e: e.tensor_tensor(out=n_t[:, hs], in0=tmp[:], in1=x_t[:, hs], op=ALU.add), reads=[r_tmp, r_x], writes=[r_n])
                P.dma("sp", x1_d[r0:r0 + 128, :], n_t[:], reads=[r_n])

        nU = len(units)
        F(0)
        for u in range(nU):
            if u + 1 < nU:
                F(u + 1)
            G(u)
        st.done(f"mg{l}")

    def stage_moe(l, dst):
        TS = min(S, 2048)
        NTS = TS // 128
        st = Stage(P)
        hT, r_hT = st.sb([128, 8, TS], BF16)
        acc, r_acc_all = st.sb([128, NTS, D], F32)
        r_acc = [[Res() for _ in range(2)] for _ in range(NTS)]
        w1b = [st.sb([128, 8, FF], BF16) for _ in range(2)]
        w3b = [st.sb([128, 8, FF], BF16) for _ in range(2)]
        w2b = [st.sb([128, 2, D], BF16) for _ in range(2)]
        gB = [st.sb([128, TS], F32) for _ in range(2)]
        gfB, r_gfB = st.sb([128, D], F32)
        pa = [st.ps([128, 512], F32) for _ in range(2)]
        pb = [st.ps([128, 512], F32) for _ in range(2)]
        po = [st.ps([128, 512], F32) for _ in range(4)]
        sa = [st.sb([128, 512], F32) for _ in range(2)]
        tb = [st.sb([128, 512], F32) for _ in range(2)]
        hid = [st.sb([128, 2, 512], BF16) for _ in range(2)]
        xt = [st.sb([128, D], F32) for _ in range(2)]
        xn = [st.sb([128, D], F32) for _ in range(2)]
        P.dma("sp", gfB[:], mod_d[l, 5 * D:6 * D].partition_broadcast(128), writes=[r_gfB])
        oi = 0
        for sti in range(S // TS):
            t0 = sti * TS
            P.dma("sp", hT[:], kp(h2T_d)[:, :, t0:t0 + TS], writes=[r_hT])
            units = [(ex, tt) for ex in range(NE + 1) for tt in range(TS // 512)]
            loaded = set()

            def load_w(ex):
                if ex in loaded or ex > NE:
                    return
                loaded.add(ex)
                w1_t, r_w1 = w1b[ex % 2]
                w3_t, r_w3 = w3b[ex % 2]
                w2_t, r_w2 = w2b[ex % 2]
                g_t, r_g = gB[ex % 2]
                if ex < NE:
                    P.dma("pool", w1_t[:], kp(I["w1"][l, ex]), writes=[r_w1])
                    P.dma("pool", w3_t[:], kp(I["w3"][l, ex]), writes=[r_w3])
                    P.dma("pool", w2_t[:], kp(I["w2"][l, ex]), writes=[r_w2])
                    P.dma("sp", g_t[:], gT_d[ex, t0:t0 + TS].partition_broadcast(128), writes=[r_g])
                else:
                    P.dma("pool", w1_t[:], kp(I["ws1"][l]), writes=[r_w1])
                    P.dma("pool", w3_t[:], kp(I["ws3"][l]), writes=[r_w3])
                    P.dma("pool", w2_t[:], kp(I["ws2"][l]), writes=[r_w2])
                    P.op("dve", lambda e: e.memset(g_t[:], 1.0), writes=[r_g])

            def up(i):
                ex, tt = units[i]
                load_w(ex)
                w1_t, r_w1 = w1b[ex % 2]
                w3_t, r_w3 = w3b[ex % 2]
                g_t, r_g = gB[ex % 2]
                ts_ = slice(tt * 512, (tt + 1) * 512)
                h_t, r_h = hid[i % 2]
                for f in range(2):
                    a_t, r_a = pa[f]
                    b_t, r_b = pb[f]
                    s_t, r_s = sa[f]
                    t_t, r_t = tb[f]
                    for c in range(8):
                        P.op("pe", lambda e: e.matmul(a_t[:], lhsT=w1_t[:, c, f * 128:(f + 1) * 128], rhs=hT[:, c, ts_], start=(c == 0), stop=(c == 7)),
                             reads=[r_w1, r_hT], writes=[r_a])
                    for c in range(8):
                        P.op("pe", lambda e: e.matmul(b_t[:], lhsT=w3_t[:, c, f * 128:(f + 1) * 128], rhs=hT[:, c, ts_], start=(c == 0), stop=(c == 7)),
                             reads=[r_w3, r_hT], writes=[r_b])
                    P.op("act", lambda e: e.activation(out=s_t[:], in_=a_t[:], func=AF.Silu), reads=[r_a], writes=[r_s])
                    P.op("dve", lambda e: e.tensor_tensor(out=t_t[:], in0=b_t[:], in1=g_t[:, ts_], op=ALU.mult), reads=[r_b, r_g], writes=[r_t])
                    P.op("dve", lambda e: e.tensor_tensor(out=h_t[:, f, :], in0=s_t[:], in1=t_t[:], op=ALU.mult), reads=[r_s, r_t], writes=[r_h])

            def down(i):
                nonlocal oi
                ex, tt = units[i]
                w2_t, r_w2 = w2b[ex % 2]
                h_t, r_h = hid[i % 2]
                for sub in range(4):
                    ti = tt * 4 + sub
                    for half in range(2):
                        o_t, r_o = po[oi % 4]
                        oi += 1
                        hs = slice(half * 512, (half + 1) * 512)
                        for f in range(2):
                            P.op("pe", lambda e: e.matmul(o_t[:], lhsT=h_t[:, f, sub * 128:(sub + 1) * 128], rhs=w2_t[:, f, hs], start=(f == 0), stop=(f == 1)),
                                 reads=[r_h, r_w2], writes=[r_o])
                        if ex == 0:
                            P.op("dve", lambda e: e.tensor_copy(out=acc[:, ti, hs], in_=o_t[:]), reads=[r_o], writes=[r_acc[ti][half]])
                        else:
                            P.op("dve", lambda e: e.tensor_tensor(out=acc[:, ti, hs], in0=o_t[:], in1=acc[:, ti, hs], op=ALU.add),
                                 reads=[r_o, r_acc[ti][half]], writes=[r_acc[ti][half]])

            n = len(units)
            load_w(0)
            load_w(1)
            up(0)
            for i in range(n):
                if i + 1 < n:
                    up(i + 1)
                down(i)
                if i + 1 < n and units[i + 1][0] != units[i][0]:
                    load_w(units[i][0] + 2)
            for ti in range(NTS):
                x_t, r_x = xt[ti % 2]
                n_t, r_n = xn[ti % 2]
                r0 = t0 + ti * 128
                P.dma("sp", x_t[:], x1_d[r0:r0 + 128, :], writes=[r_x])
                P.op("dve", lambda e: e.tensor_tensor(out=n_t[:], in0=acc[:, ti, :], in1=gfB[:], op=ALU.mult), reads=[r_acc[ti][0], r_acc[ti][1], r_gfB], writes=[r_n])
                P.op("pool", lambda e: e.tensor_tensor(out=n_t[:], in0=n_t[:], in1=x_t[:], op=ALU.add), reads=[r_n, r_x], writes=[r_n])
                P.dma("sp", dst[r0:r0 + 128, :], n_t[:], reads=[r_n])
        st.done(f"moe{l}")

    def stage_route(l):
        st = Stage(P)
        sbB, r_sbB = st.sb([128, NE], F32)
        P.dma("sp", sbB[:], sbase_d.partition_broadcast(128), writes=[r_sbB])
        pos = [st.sb([128, NE], F32) for _ in range(2)]
        gat = [st.sb([128, NE], F32) for _ in range(2)]
        hrow = [st.sb([128, D], BF16) for _ in range(2)]
        a_ = [st.sb([128, NE], F32) for _ in range(2)]
        sel_ = [st.sb([128, NE], F32) for _ in range(2)]
        t8 = [st.sb([128, 8], F32) for _ in range(2)]
        si = [st.sb([128, 8], I32) for _ in range(2)]
        gk = [st.sb([128, 8], F32) for _ in range(2)]
        junk = [st.sb([128, NE], F32) for _ in range(2)]

        def chain(t):
            b = t % 2
            rows = slice(t * 128, (t + 1) * 128)
            P.dma("sp", pos[b][0][:], pos_d[rows, :], writes=[pos[b][1]])
            P.dma("sp", gat[b][0][:], gate_d[rows, :], writes=[gat[b][1]])
            P.dma("sp", hrow[b][0][:], h2_d[rows, :], writes=[hrow[b][1]])
            yield
            P.op("dve", lambda e: e.tensor_scalar(out=sel_[b][0][:], in0=gat[b][0][:], scalar1=0.0, scalar2=None, op0=ALU.is_gt),
                 reads=[gat[b][1]], writes=[sel_[b][1]])
            yield
            P.op("dve", lambda e: e.tensor_tensor(out=a_[b][0][:], in0=pos[b][0][:], in1=sbB[:], op=ALU.add), reads=[pos[b][1], r_sbB], writes=[a_[b][1]])
            yield
            P.op("dve", lambda e: e.tensor_tensor(out=a_[b][0][:], in0=a_[b][0][:], in1=sel_[b][0][:], op=ALU.mult), reads=[a_[b][1], sel_[b][1]], writes=[a_[b][1]])
            yield
            P.op("dve", lambda e: e.tensor_scalar(out=a_[b][0][:], in0=a_[b][0][:], scalar1=-1.0, scalar2=None, op0=ALU.add), reads=[a_[b][1]], writes=[a_[b][1]])
            yield
            P.op("dve", lambda e: e.max(out=t8[b][0][:], in_=a_[b][0][:]), reads=[a_[b][1]], writes=[t8[b][1]])
            yield
            P.op("dve", lambda e: e.tensor_copy(out=si[b][0][:], in_=t8[b][0][:]), reads=[t8[b][1]], writes=[si[b][1]])
            yield
            for k in range(8):
                P.op("dve", lambda e: e.scalar_tensor_tensor(out=junk[b][0][:], in0=a_[b][0][:], scalar=t8[b][0][:, k:k + 1], in1=gat[b][0][:], op0=ALU.is_equal, op1=ALU.mult),
                     reads=[a_[b][1], t8[b][1], gat[b][1]], writes=[junk[b][1]])
                yield
                P.op("dve", lambda e: e.tensor_reduce(out=gk[b][0][:, k:k + 1], in_=junk[b][0][:], axis=AX.X, op=ALU.add), reads=[junk[b][1]], writes=[gk[b][1]])
                yield
            P.dma("sp", slotk_d[rows, :], si[b][0][:], reads=[si[b][1]])
            P.dma("sp", gk_d[rows, :], gk[b][0][:], reads=[gk[b][1]])
            for k in range(8):
                def fs(eng, b=b, k=k):
                    return eng.indirect_dma_start(out=xs_d[:, :], out_offset=bass.IndirectOffsetOnAxis(ap=si[b][0][:, k:k + 1], axis=0),
                                                  in_=hrow[b][0][:, :], in_offset=None)
                P.raw("pool", fs, reads=[si[b][1], hrow[b][1]], is_dma=True)
            yield

        def drain(*gens):
            gens = list(gens)
            while gens:
                for g in list(gens):
                    try:
                        next(g)
                    except StopIteration:
                        gens.remove(g)

        for t in range(0, NT, 2):
            drain(*[chain(tt_) for tt_ in range(t, min(t + 2, NT))])
        st.done(f"route{l}")

    def stage_moe2(l):
        st = Stage(P)
        identb, r_idb = st.sb([128, 128], BF16)
        ebB, r_ebB = st.sb([128, 128], F32)
        iotac, r_iotac = st.sb([128, 1], F32)
        idxw, r_idxw = st.sb([128, 128], I32)
        w1b = [st.sb([128, 8, FF], BF16) for _ in range(3)]
        w3b = [st.sb([128, 8, FF], BF16) for _ in range(3)]
        w2b = [st.sb([128, 2, D], BF16) for _ in range(3)]
        xtok = [st.sb([128, 4, D], BF16) for _ in range(3)]
        XT = [st.sb([128, 8, 512], BF16) for _ in range(3)]
        hid = [st.sb([128, 2, 512], BF16) for _ in range(2)]
        sa = [st.sb([128, 512], F32) for _ in range(2)]
        ysb = [st.sb([128, 4, D], BF16) for _ in range(2)]
        pt = [st.ps([128, 8, 128], BF16) for _ in range(2)]
        pa = [st.ps([128, 512], F32) for _ in range(2)]
        pb = [st.ps([128, 512], F32) for _ in range(2)]
        po = [st.ps([128, 512], F32) for _ in range(2)]
        P.dma("pool", identb[:], I["k_ident"], writes=[r_idb])
        P.dma("sp", ebB[:], eb_d.partition_broadcast(128), writes=[r_ebB])
        P.dma("sp", iotac[:], col(I["k_iota"]), writes=[r_iotac])
        P.op("dve", lambda e: e.tensor_scalar(out=ebB[:], in0=ebB[:], scalar1=128.0, scalar2=float(l * NE * 128), op0=ALU.mult, op1=ALU.add),
             reads=[r_ebB], writes=[r_ebB])
        P.op("dve", lambda e: e.tensor_scalar(out=ebB[:], in0=ebB[:], scalar1=iotac[:, 0:1], scalar2=None, op0=ALU.add), reads=[r_ebB, r_iotac], writes=[r_ebB])
        P.op("dve", lambda e: e.tensor_copy(out=idxw[:], in_=ebB[:]), reads=[r_ebB], writes=[r_idxw])
        NU = NB + NQ
        state = {"oi": 0}

        def load(u):
            if u >= NU:
                return
            bf = u % 3
            if u < NB:
                for nm, (w_t, r_w) in (("w1h", w1b[bf]), ("w3h", w3b[bf]), ("w2h", w2b[bf])):
                    def fg(eng, nm=nm, w_t=w_t, u=u):
                        return eng.indirect_dma_start(out=w_t[:].rearrange("p c f -> p (c f)"), out_offset=None, in_=I[nm][:, :],
                                                      in_offset=bass.IndirectOffsetOnAxis(ap=idxw[:, u:u + 1], axis=0))
                    P.raw("pool", fg, reads=[r_idxw], writes=[r_w], is_dma=True)
                P.dma("sp", xtok[bf][0][:], xs_d[u * 512:(u + 1) * 512, :].rearrange("(s p) d -> p s d", p=128), writes=[xtok[bf][1]])
            else:
                if u in (NB, NB + 1, NB + 2):
                    P.dma("pool", w1b[bf][0][:], kp(I["ws1"][l]), writes=[w1b[bf][1]])
                    P.dma("pool", w3b[bf][0][:], kp(I["ws3"][l]), writes=[w3b[bf][1]])
                    P.dma("pool", w2b[bf][0][:], kp(I["ws2"][l]), writes=[w2b[bf][1]])
                tt = u - NB
                P.dma("sp", XT[bf][0][:], kp(h2T_d)[:, :, tt * 512:(tt + 1) * 512], writes=[XT[bf][1]])

        def tr(u):
            if u >= NB:
                return
            bf = u % 3
            x_t, r_x = xtok[bf]
            X_t, r_X = XT[bf]
            for sub in range(4):
                p_t, r_p = pt[sub % 2]
                for c in range(8):
                    P.op("pe", lambda e: e.transpose(out=p_t[:, c, :], in_=x_t[:, sub, c * 128:(c + 1) * 128], identity=identb[:]),
                         reads=[r_x, r_idb], writes=[r_p])
                if sub % 2 == 0:
                    P.op("act", lambda e: e.copy(out=X_t[:, :, sub * 128:(sub + 1) * 128], in_=p_t[:]), reads=[r_p], writes=[r_X])
                else:
                    P.op("dve", lambda e: e.tensor_copy(out=X_t[:, :, sub * 128:(sub + 1) * 128], in_=p_t[:]), reads=[r_p], writes=[r_X])

        def up(u):
            bf = u % 3
            w1_t, r_w1 = w1b[bf]
            w3_t, r_w3 = w3b[bf]
            X_t, r_X = XT[bf]
            h_t, r_h = hid[u % 2]
            for f in range(2):
                a_t, r_a = pa[f]
                b_t, r_b = pb[f]
                s_t, r_s = sa[f]
                for c in range(8):
                    P.op("pe", lambda e: e.matmul(a_t[:], lhsT=w1_t[:, c, f * 128:(f + 1) * 128], rhs=X_t[:, c, :], start=(c == 0), stop=(c == 7)),
                         reads=[r_w1, r_X], writes=[r_a])
                for c in range(8):
                    P.op("pe", lambda e: e.matmul(b_t[:], lhsT=w3_t[:, c, f * 128:(f + 1) * 128], rhs=X_t[:, c, :], start=(c == 0), stop=(c == 7)),
                         reads=[r_w3, r_X], writes=[r_b])
                P.op("act", lambda e: e.activation(out=s_t[:], in_=a_t[:], func=AF.Silu), reads=[r_a], writes=[r_s])
                P.op("dve", lambda e: e.tensor_tensor(out=h_t[:, f, :], in0=b_t[:], in1=s_t[:], op=ALU.mult), reads=[r_b, r_s], writes=[r_h])

        def down(u):
            w2_t, r_w2 = w2b[u % 3]
            h_t, r_h = hid[u % 2]
            y_t, r_y = ysb[u % 2]
            for sub in range(4):
                for half in range(2):
                    o_t, r_o = po[state["oi"] % 2]
                    state["oi"] += 1
                    hs = slice(half * 512, (half + 1) * 512)
                    for f in range(2):
                        P.op("pe", lambda e: e.matmul(o_t[:], lhsT=h_t[:, f, sub * 128:(sub + 1) * 128], rhs=w2_t[:, f, hs], start=(f == 0), stop=(f == 1)),
                             reads=[r_h, r_w2], writes=[r_o])
                    if half == 0:
                        P.op("act", lambda e: e.copy(out=y_t[:, sub, hs], in_=o_t[:]), reads=[r_o], writes=[r_y])
                    else:
                        P.op("dve", lambda e: e.tensor_copy(out=y_t[:, sub, hs], in_=o_t[:]), reads=[r_o], writes=[r_y])
            if u < NB:
                P.dma("sp", ys_d[u * 512:(u + 1) * 512, :].rearrange("(s p) d -> p s d", p=128), y_t[:], reads=[r_y])
            else:
                tt = u - NB
                P.dma("sp", ysh_d[tt * 512:(tt + 1) * 512, :].rearrange("(s p) d -> p s d", p=128), y_t[:], reads=[r_y])

        load(0)
        load(1)
        tr(0)
        up(0)
        for u in range(NU):
            load(u + 2)
            if u + 1 < NU:
                tr(u + 1)
                up(u + 1)
            down(u)
        st.done(f"moe{l}")

    def stage_comb(l, dst):
        st = Stage(P)
        gfB, r_gfB = st.sb([128, D], F32)
        P.dma("sp", gfB[:], mod_d[l, 5 * D:6 * D].partition_broadcast(128), writes=[r_gfB])
        si = [st.sb([128, 8], I32) for _ in range(2)]
        gk = [st.sb([128, 8], F32) for _ in range(2)]
        xt = [st.sb([128, D], F32) for _ in range(2)]
        ysh = [st.sb([128, D], BF16) for _ in range(2)]
        yg = [[st.sb([128, D], BF16) for _ in range(8)] for _ in range(2)]
        acc = [st.sb([128, D], F32) for _ in range(2)]
        def fetch(t):
            if t >= NT:
                return
            b = t % 2
            rows = slice(t * 128, (t + 1) * 128)
            P.dma("sp", si[b][0][:], slotk_d[rows, :], writes=[si[b][1]])
            P.dma("sp", gk[b][0][:], gk_d[rows, :], writes=[gk[b][1]])
            P.dma("sp", xt[b][0][:], x1_d[rows, :], writes=[xt[b][1]])
            P.dma("sp", ysh[b][0][:], ysh_d[rows, :], writes=[ysh[b][1]])
            for k in range(8):
                def fg(eng, b=b, k=k):
                    return eng.indirect_dma_start(out=yg[b][k][0][:, :], out_offset=None, in_=ys_d[:, :],
                                                  in_offset=bass.IndirectOffsetOnAxis(ap=si[b][0][:, k:k + 1], axis=0))
                P.raw("pool", fg, reads=[si[b][1]], writes=[yg[b][k][1]], is_dma=True)

        def comp(t):
            b = t % 2
            rows = slice(t * 128, (t + 1) * 128)
            a_t, r_a = acc[b]
            P.op("dve", lambda e: e.scalar_tensor_tensor(out=a_t[:], in0=yg[b][0][0][:], scalar=gk[b][0][:, 0:1], in1=ysh[b][0][:], op0=ALU.mult, op1=ALU.add),
                 reads=[yg[b][0][1], gk[b][1], ysh[b][1]], writes=[r_a])
            for k in range(1, 8):
                P.op("dve", lambda e: e.scalar_tensor_tensor(out=a_t[:], in0=yg[b][k][0][:], scalar=gk[b][0][:, k:k + 1], in1=a_t[:], op0=ALU.mult, op1=ALU.add),
                     reads=[yg[b][k][1], gk[b][1], r_a], writes=[r_a])
            P.op("dve", lambda e: e.tensor_tensor(out=a_t[:], in0=a_t[:], in1=gfB[:], op=ALU.mult), reads=[r_a, r_gfB], writes=[r_a])
            P.op("dve", lambda e: e.tensor_tensor(out=a_t[:], in0=a_t[:], in1=xt[b][0][:], op=ALU.add), reads=[r_a, xt[b][1]], writes=[r_a])
            P.dma("sp", dst[rows, :], a_t[:], reads=[r_a])

        fetch(0)
        for t in range(NT):
            comp_deferred = t
            fetch(t + 1)
            comp(t)
        st.done(f"comb{l}")

    todo = stages if stages is not None else ("mod", "rope", "norm1", "proj")
    if "mod" in todo:
        stage_mod()
    if "rope" in todo:
        stage_rope()
    for l in range(L):
        x_in = I["x"] if l == 0 else x2_d
        if "norm1" in todo:
            stage_norm(l, x_in, "norm_mix_g", 1 * D, 0 * D, hT_d, router=False)
        if "proj" in todo:
            stage_proj(l)
        if "da" in todo:
            stage_da(l)
        if "sb" in todo:
            stage_sb(l)
        if "cv" in todo:
            stage_cv(l)
        if "mg" in todo:
            stage_mg(l, x_in)
        if "norm2" in todo:
            stage_norm(l, x1_d, "norm_ffn_g", 4 * D, 3 * D, h2T_d, router=True)
        if "moe" in todo:
            stage_moe(l, out if l == L - 1 else x2_d)
        if "smoe" in todo:
            stage_route(l)
            stage_moe2(l)
            stage_comb(l, out if l == L - 1 else x2_d)
    es.close()
    return nc


ALL_STAGES = ("mod", "rope", "norm1", "proj", "da", "sb", "cv", "mg", "norm2", "smoe")
_CACHE = {}


def kernel(**inputs):
    x = np.ascontiguousarray(np.asarray(inputs["x"], dtype=np.float32))
    B, S, _ = x.shape
    L = int(np.asarray(inputs["w_mod"]).shape[0])
    key = (S, L)
    if key not in _CACHE:
        _CACHE[key] = build(S, L=L, stages=ALL_STAGES)
    nc = _CACHE[key]
    consts = make_consts()
    shared = {k: np.ascontiguousarray(np.asarray(inputs[k], dtype=np.float32)) for k in W_SHAPES if k not in ("w1", "w3", "w2")}
    shared.update(relayout_experts(inputs, L))
    c = np.asarray(inputs["c"], dtype=np.float32)
    pos = np.asarray(inputs["positions"]).astype(np.int32)
    in_maps = []
    for b in range(B):
        m = {"x": x[b], "c": np.ascontiguousarray(c[b]), "pos": np.ascontiguousarray(pos[b])}
        m.update(shared)
        m.update(consts)
        in_maps.append(m)
    res = run_bass_kernel_spmd(nc, in_maps, core_ids=list(range(B)))
    return np.stack([np.asarray(r["out"], dtype=np.float32) for r in res.results], axis=0)


def relayout_experts(W, L):
    o = {}
    w1 = np.asarray(W["w1"], dtype=np.float32)[:L]
    w3 = np.asarray(W["w3"], dtype=np.float32)[:L]
    w2 = np.asarray(W["w2"], dtype=np.float32)[:L]
    o["w1h"] = np.ascontiguousarray(w1.reshape(L, NE, 8, 128, FF).transpose(0, 1, 3, 2, 4)).reshape(L * NE * 128, 8 * FF)
    o["w3h"] = np.ascontiguousarray(w3.reshape(L, NE, 8, 128, FF).transpose(0, 1, 3, 2, 4)).reshape(L * NE * 128, 8 * FF)
    o["w2h"] = np.ascontiguousarray(w2.reshape(L, NE, 2, 128, D).transpose(0, 1, 3, 2, 4)).reshape(L * NE * 128, 2 * D)
    return o
```

```python
import math
from contextlib import ExitStack
import numpy as np
import concourse.bass as bass
import concourse.mybir as mybir
from concourse.bass_utils import run_bass_kernel_spmd

F32 = mybir.dt.float32
BF16 = mybir.dt.bfloat16
I32 = mybir.dt.int32
ALU = mybir.AluOpType
AF = mybir.ActivationFunctionType
AX = mybir.AxisListType

ENGS = ("pe", "act", "dve", "pool", "sp")
SEM_LIM = 30000
DMA_RING = 8

D = 1024
NE = 64
FF = 256
EPS = 1e-6


class Res:
    __slots__ = ("w", "r")

    def __init__(self):
        self.w = None
        self.r = []


class Op:
    __slots__ = ("eng", "fn", "deps", "signal", "k", "is_dma", "slot", "dval")

    def __init__(self, eng, fn, is_dma=False):
        self.eng = eng
        self.fn = fn
        self.deps = set()
        self.signal = False
        self.k = None
        self.is_dma = is_dma
        self.slot = None
        self.dval = None


class _Rec:
    def __getattr__(self, name):
        return lambda *a, **k: (name, a, k)


_REC = _Rec()


class Prog:
    def __init__(self, nc, es):
        self.nc = nc
        self.sems = {e: [es.enter_context(nc.semaphore(f"s_{e}_{i}")) for i in range(6)] for e in ENGS if e != "sp"}
        self.nsig = {e: 0 for e in ENGS}
        self.dq = ("sp", "act", "pool")
        self.dsems = {e: [es.enter_context(nc.semaphore(f"d_{e}_{i}")) for i in range(DMA_RING)] for e in self.dq}
        self.ndma = {e: 0 for e in self.dq}
        self.ring_last = {e: [None] * DMA_RING for e in self.dq}
        self.waited = {e: {} for e in ENGS}
        self.ops = []
        self.last_op = {e: None for e in ENGS}
        self.stage_dmas = []

    def _add(self, op, reads, writes):
        deps = set()
        for r in reads:
            if r.w is not None:
                deps.add(r.w)
        for w in writes:
            if w.w is not None:
                deps.add(w.w)
            for o in w.r:
                deps.add(o)
        for d in deps:
            if d is op:
                continue
            if (not d.is_dma) and d.eng == op.eng and op.eng == "pe" and not op.is_dma:
                continue
            op.deps.add(d)
            if not d.is_dma:
                d.signal = True
        for r in reads:
            r.r.append(op)
        for w in writes:
            w.w = op
            w.r = []
        self.ops.append(op)
        if not op.is_dma:
            self.last_op[op.eng] = op
        return op

    def op(self, eng, fn, reads=(), writes=()):
        return self._add(Op(eng, fn(_REC)), reads, writes)

    def dma(self, q, out, in_, reads=(), writes=()):
        op = Op(q, ("dma_start", (), dict(out=out, in_=in_)), is_dma=True)
        i = self.ndma[q]
        self.ndma[q] += 1
        op.slot = i % DMA_RING
        op.dval = 16 * (i // DMA_RING + 1)
        prev = self.ring_last[q][op.slot]
        if prev is not None:
            op.deps.add(prev)
        self.ring_last[q][op.slot] = op
        self.stage_dmas.append(op)
        return self._add(op, reads, writes)

    def raw(self, eng, fn, reads=(), writes=(), is_dma=False):
        op = Op(eng, fn, is_dma=is_dma)
        if is_dma:
            i = self.ndma[eng]
            self.ndma[eng] += 1
            op.slot = i % DMA_RING
            op.dval = 16 * (i // DMA_RING + 1)
            prev = self.ring_last[eng][op.slot]
            if prev is not None:
                op.deps.add(prev)
            self.ring_last[eng][op.slot] = op
            self.stage_dmas.append(op)
        return self._add(op, reads, writes)

    def barrier(self):
        lasts = [o for o in self.last_op.values() if o is not None]
        dmas = list(self.stage_dmas)
        for e in ENGS:
            op = Op(e, None)
            for d in lasts:
                if d.eng != e:
                    op.deps.add(d)
                    d.signal = True
            for d in dmas:
                op.deps.add(d)
            self.ops.append(op)
        self.stage_dmas = []
        self.last_op = {e: None for e in ENGS}

    def emit(self):
        nc = self.nc
        self.barrier()
        ops = self.ops
        self.ops = []
        for o in ops:
            if o.signal and not o.is_dma and o.k is None:
                o.k = self.nsig[o.eng]
                self.nsig[o.eng] += 1
        per = {e: [o for o in ops if o.eng == e] for e in ENGS}
        engobj = {"pe": "tensor", "act": "scalar", "dve": "vector", "pool": "gpsimd", "sp": "sync"}

        def run(e, eng):
            waited = self.waited[e]
            for o in per[e]:
                need = {}
                for d in o.deps:
                    if d.is_dma:
                        key = ("d", d.eng, d.slot)
                        sem = self.dsems[d.eng][d.slot]
                        val = d.dval
                    else:
                        key = ("c", d.eng, d.k // SEM_LIM)
                        sem = self.sems[d.eng][d.k // SEM_LIM]
                        val = d.k % SEM_LIM + 1
                    if waited.get(key, 0) >= val:
                        continue
                    if key not in need or need[key][1] < val:
                        need[key] = (sem, val)
                for key, (sem, val) in need.items():
                    eng.wait_ge(sem, val)
                    waited[key] = val
                if o.fn is None:
                    continue
                if callable(o.fn):
                    ins = o.fn(eng)
                else:
                    name, a, k = o.fn
                    ins = getattr(eng, name)(*a, **k)
                if o.is_dma:
                    ins.then_inc(self.dsems[o.eng][o.slot], 16)
                elif o.signal:
                    ins.then_inc(self.sems[o.eng][o.k // SEM_LIM], 1)

        with nc.allow_non_contiguous_dma(reason="small strided parameter loads"):
            with nc.Block() as block:
                for e in ENGS:
                    if not per[e]:
                        continue
                    getattr(block, engobj[e])(lambda eng, e=e: run(e, eng))


class Stage:
    count = 0

    def __init__(self, P):
        self.P = P
        self.nc = P.nc
        self.es = ExitStack()
        self.n = 0
        Stage.count += 1
        self.sid = Stage.count

    def sb(self, shape, dt):
        self.n += 1
        t = self.es.enter_context(self.nc.sbuf_tensor(f"t{self.sid}_{self.n}", list(shape), dt))
        return t, Res()

    def ps(self, shape, dt):
        self.n += 1
        t = self.es.enter_context(self.nc.psum_tensor(f"p{self.sid}_{self.n}", list(shape), dt))
        return t, Res()

    def done(self, name=None):
        if name:
            with self.nc.named_scope(name):
                self.P.emit()
        else:
            self.P.emit()
        self.es.close()


def col(ap1d):
    return ap1d.rearrange("(p o) -> p o", o=1)


def make_consts():
    p = np.arange(128)
    ident = np.eye(128, dtype=np.float32)
    blk64 = ((p[:, None] // 64) == (p[None, :] // 64)).astype(np.float32) / 64.0
    rotT = np.zeros((128, 128), np.float32)
    for k in range(128):
        if k % 64 >= 32:
            rotT[k, k - 32] = -1.0
        else:
            rotT[k, k + 32] = 1.0
    ones = np.ones((128, 128), np.float32)
    ustrict = (p[:, None] > p[None, :]).astype(np.float32)
    uincl = (p[:, None] >= p[None, :]).astype(np.float32)
    ulow = (p[:, None] < p[None, :]).astype(np.float32)
    pincl = (p[:, None] <= p[None, :]).astype(np.float32)
    iota = p.astype(np.float32)
    qq = np.arange(512)
    maskc = np.stack([((qq[None, :] - j * 128 - p[:, None]) >= 0) for j in range(4)]).astype(np.float32)
    masks = np.stack([((qq[None, :] - j * 128 - p[:, None]) > 0) for j in range(4)]).astype(np.float32)
    inv = (10000.0 ** (-np.arange(0, 64, 2, dtype=np.float32) / np.float32(64))).astype(np.float32)
    invc = inv[p % 32].astype(np.float32)
    return dict(k_ident=ident, k_blk64=blk64, k_rotT=rotT, k_ones=ones, k_ustrict=ustrict, k_uincl=uincl, k_ulow=ulow, k_pincl=pincl, k_iota=iota,
                k_maskc=maskc, k_masks=masks, k_invc=invc)


W_SHAPES = dict(
    w_mod=(D, 6 * D), b_mod=(6 * D,), norm_mix_g=(D,), norm_ffn_g=(D,), w_in=(D, 7168),
    qn_g=(64,), kn_g=(64,), lam_q1=(64,), lam_k1=(64,), lam_q2=(64,), lam_k2=(64,), subln_g=(128,),
    w_proj_a=(512, D), w_dw=(31, 512), b_dw=(512,), conv_ln_g=(512,), conv_ln_b=(512,),
    w_proj_b=(512, D), b_proj_b=(D,), w_proj_c=(512, D), w_out=(D, D), w_router=(D, NE), b_router=(NE,),
    w1=(NE, D, FF), w3=(NE, D, FF), w2=(NE, FF, D), ws1=(D, FF), ws3=(D, FF), ws2=(FF, D),
)


def build(S, L=2, dbg=(), stages=None):
    NT = S // 128
    NQ = S // 512
    nc = bass.Bass("TRN2", target_bir_lowering=False)
    I = {}
    I["x"] = nc.dram_tensor("x", [S, D], F32, kind="ExternalInput").ap()
    I["c"] = nc.dram_tensor("c", [D], F32, kind="ExternalInput").ap()
    I["pos"] = nc.dram_tensor("pos", [S], I32, kind="ExternalInput").ap()
    dense_moe = stages is not None and "moe" in stages
    for k, shp in W_SHAPES.items():
        if k in ("w1", "w3", "w2") and not dense_moe:
            continue
        I[k] = nc.dram_tensor(k, [L] + list(shp), F32, kind="ExternalInput").ap()
    for k, v in make_consts().items():
        I[k] = nc.dram_tensor(k, list(v.shape), F32, kind="ExternalInput").ap()
    for k in ("w1h", "w3h", "w2h"):
        I[k] = nc.dram_tensor(k, [L * NE * 128, 2048], F32, kind="ExternalInput").ap()
    out = nc.dram_tensor("out", [S, D], F32, kind="ExternalOutput").ap()
    NB = S * 8 // 512 + NE
    NSLOT = NB * 512

    def scratch(name, shape, dt):
        kind = "ExternalOutput" if name in dbg else "Internal"
        return nc.dram_tensor(name, list(shape), dt, kind=kind).ap()

    mod_d = scratch("mod_d", [L, 6 * D], F32)
    cos_d = scratch("cos_d", [128, S], F32)
    sin_d = scratch("sin_d", [128, S], F32)
    hT_d = scratch("hT_d", [D, S], BF16)
    qaT_d = scratch("qaT_d", [512, S], BF16)
    kaT_d = scratch("kaT_d", [512, S], BF16)
    va_d = scratch("va_d", [S, 512], BF16)
    uT_d = scratch("uT_d", [512, S], BF16)
    qcT_d = scratch("qcT_d", [512, S], BF16)
    kcT_d = scratch("kcT_d", [512, S], BF16)
    vc_d = scratch("vc_d", [S, 512], BF16)
    oaT_d = scratch("oaT_d", [512, S], BF16)
    cvT_d = scratch("cvT_d", [512, S], BF16)
    ocT_d = scratch("ocT_d", [512, S], BF16)
    x1_d = scratch("x1_d", [S, D], F32)
    x2_d = scratch("x2_d", [S, D], F32)
    h2T_d = scratch("h2T_d", [D, S], BF16)
    gT_d = scratch("gT_d", [NE, S], F32)
    gate_d = scratch("gate_d", [S, NE], F32)
    pos_d = scratch("pos_d", [S, NE], F32)
    h2_d = scratch("h2_d", [S, D], BF16)
    sbase_d = scratch("sbase_d", [NE], F32)
    eb_d = scratch("eb_d", [128], F32)
    slotk_d = scratch("slotk_d", [S, 8], I32)
    gk_d = scratch("gk_d", [S, 8], F32)
    xs_d = scratch("xs_d", [NSLOT, D], BF16)
    ys_d = scratch("ys_d", [NSLOT, D], BF16)
    ysh_d = scratch("ysh_d", [S, D], BF16)

    def kp(ap2d):
        return ap2d.rearrange("(c p) n -> p c n", p=128)

    es = ExitStack()
    P = Prog(nc, es)

    def stage_mod():
        st = Stage(P)
        cT, r_cT = st.sb([128, 8], F32)
        cA, r_cA = st.sb([128, 8], F32)
        wm = [st.sb([128, 8, 512], F32) for _ in range(2)]
        bm, r_bm = st.sb([1, L * 6 * D], F32)
        mr, r_mr = st.sb([1, L * 6 * D], F32)
        pm = [st.ps([1, 512], F32) for _ in range(2)]
        P.dma("sp", cT[:], I["c"].rearrange("(c p) -> p c", p=128), writes=[r_cT])
        P.dma("sp", bm[:], I["b_mod"].rearrange("(o l) n -> o (l n)", o=1), writes=[r_bm])
        P.op("act", lambda e: e.activation(out=cA[:], in_=cT[:], func=AF.Silu), reads=[r_cT], writes=[r_cA])
        i = 0
        for l in range(L):
            for blk in range(12):
                w_t, r_w = wm[i % 2]
                p_t, r_p = pm[i % 2]
                P.dma("sp", w_t[:], kp(I["w_mod"][l])[:, :, blk * 512:(blk + 1) * 512], writes=[r_w])
                for c in range(8):
                    P.op("pe", lambda e, c=c, w_t=w_t, p_t=p_t: e.matmul(p_t[:], lhsT=cA[:, c:c + 1], rhs=w_t[:, c, :], start=(c == 0), stop=(c == 7)),
                         reads=[r_cA, r_w], writes=[r_p])
                o = l * 6 * D + blk * 512
                P.op("dve", lambda e, o=o, p_t=p_t: e.tensor_tensor(out=mr[:, o:o + 512], in0=p_t[:], in1=bm[:, o:o + 512], op=ALU.add),
                     reads=[r_p, r_bm], writes=[r_mr])
                i += 1
        P.dma("sp", mod_d.rearrange("(o l) n -> o (l n)", o=1), mr[:], reads=[r_mr])
        st.done("mod")

    def stage_rope():
        st = Stage(P)
        pi_t, r_pi = st.sb([128, S], I32)
        pf, r_pf = st.sb([128, S], F32)
        inv, r_inv = st.sb([128, 1], F32)
        ang, r_ang = st.sb([128, S], F32)
        u, r_u = st.sb([128, S], F32)
        ki, r_ki = st.sb([128, S], I32)
        kf, r_kf = st.sb([128, S], F32)
        m, r_m = st.sb([128, S], F32)
        res_t, r_res = st.sb([128, S], F32)
        zero, r_zero = st.sb([128, 1], F32)
        TWO_PI = 2.0 * math.pi
        C1 = 6.28125
        C2 = TWO_PI - C1
        P.dma("sp", pi_t[:], I["pos"].partition_broadcast(128), writes=[r_pi])
        P.dma("sp", inv[:], col(I["k_invc"]), writes=[r_inv])
        P.op("pool", lambda e: e.memset(zero[:], 0.0), writes=[r_zero])
        P.op("dve", lambda e: e.tensor_copy(out=pf[:], in_=pi_t[:]), reads=[r_pi], writes=[r_pf])
        for which, dst in ((0, sin_d), (1, cos_d)):
            shift = 0.0 if which == 0 else math.pi / 2
            P.op("dve", lambda e, shift=shift: e.tensor_scalar(out=ang[:], in0=pf[:], scalar1=inv[:, 0:1], scalar2=shift, op0=ALU.mult, op1=ALU.add),
                 reads=[r_pf, r_inv], writes=[r_ang])
            P.op("dve", lambda e: e.tensor_scalar(out=u[:], in0=ang[:], scalar1=1.0 / TWO_PI, scalar2=None, op0=ALU.mult),
                 reads=[r_ang], writes=[r_u])
            P.op("dve", lambda e: e.tensor_copy(out=ki[:], in_=u[:]), reads=[r_u], writes=[r_ki])
            P.op("dve", lambda e: e.tensor_copy(out=kf[:], in_=ki[:]), reads=[r_ki], writes=[r_kf])
            P.op("dve", lambda e: e.scalar_tensor_tensor(out=u[:], in0=kf[:], scalar=-C1, in1=ang[:], op0=ALU.mult, op1=ALU.add),
                 reads=[r_kf, r_ang], writes=[r_u])
            P.op("dve", lambda e: e.scalar_tensor_tensor(out=u[:], in0=kf[:], scalar=-C2, in1=u[:], op0=ALU.mult, op1=ALU.add),
                 reads=[r_kf, r_u], writes=[r_u])
            P.op("dve", lambda e: e.tensor_scalar(out=m[:], in0=u[:], scalar1=math.pi, scalar2=-TWO_PI, op0=ALU.is_gt, op1=ALU.mult),
                 reads=[r_u], writes=[r_m])
            P.op("dve", lambda e: e.tensor_tensor(out=u[:], in0=u[:], in1=m[:], op=ALU.add), reads=[r_u, r_m], writes=[r_u])
            P.op("dve", lambda e: e.tensor_scalar(out=m[:], in0=u[:], scalar1=-math.pi, scalar2=TWO_PI, op0=ALU.is_lt, op1=ALU.mult),
                 reads=[r_u], writes=[r_m])
            P.op("dve", lambda e: e.tensor_tensor(out=u[:], in0=u[:], in1=m[:], op=ALU.add), reads=[r_u, r_m], writes=[r_u])
            P.op("act", lambda e: e.activation(out=res_t[:], in_=u[:], func=AF.Sin, bias=zero[:, 0:1]), reads=[r_u, r_zero], writes=[r_res])
            P.dma("sp", dst, res_t[:], reads=[r_res])
        st.done("rope")

    def stage_norm(l, x_src, g_name, sc_off, sh_off, hT_dst, router):
        st = Stage(P)
        gB, r_gB = st.sb([128, D], F32)
        scB, r_scB = st.sb([128, D], F32)
        shB, r_shB = st.sb([128, D], F32)
        A, r_A = st.sb([128, D], F32)
        epsb, r_eps = st.sb([128, 1], F32)
        identb, r_idb = st.sb([128, 128], BF16)
        xt = [st.sb([128, D], F32) for _ in range(4)]
        sq, r_sq = st.sb([128, D], F32)
        ss, r_ss = st.sb([128, 1], F32)
        rstd, r_rstd = st.sb([128, 1], F32)
        hf, r_hf = st.sb([128, D], F32)
        hb, r_hb = st.sb([128, D], BF16)
        hTs = [st.sb([128, 8, 128], BF16) for _ in range(2)]
        pt = [st.ps([128, 8, 128], BF16) for _ in range(2)]
        P.dma("sp", gB[:], I[g_name][l].partition_broadcast(128), writes=[r_gB])
        P.dma("sp", scB[:], mod_d[l, sc_off:sc_off + D].partition_broadcast(128), writes=[r_scB])
        P.dma("sp", shB[:], mod_d[l, sh_off:sh_off + D].partition_broadcast(128), writes=[r_shB])
        P.dma("pool", identb[:], I["k_ident"], writes=[r_idb])
        P.op("pool", lambda e: e.memset(epsb[:], EPS), writes=[r_eps])
        P.op("dve", lambda e: e.scalar_tensor_tensor(out=A[:], in0=scB[:], scalar=1.0, in1=gB[:], op0=ALU.add, op1=ALU.mult),
             reads=[r_scB, r_gB], writes=[r_A])
        if router:
            identf, r_idf = st.sb([128, 128], F32)
            wr, r_wr = st.sb([128, 8, NE], F32)
            brB, r_brB = st.sb([128, NE], F32)
            h32, r_h32 = st.sb([128, D], F32)
            hTf, r_hTf = st.sb([128, 8, 128], F32)
            ptf = [st.ps([128, 4, 128], F32) for _ in range(2)]
            plg, r_plg = st.ps([128, NE], F32)
            PC, r_PC = st.ps([128, NE], F32)
            pinc, r_pinc = st.sb([128, 128], F32)
            pgtm, r_pgtm = st.sb([128, 128], F32)
            iotac, r_iotac = st.sb([128, 1], F32)
            posb = [st.sb([128, NE], F32) for _ in range(4)]
            selb = [st.sb([128, NE], F32) for _ in range(4)]
            P.dma("sp", pinc[:], I["k_pincl"], writes=[r_pinc])
            P.dma("sp", pgtm[:], I["k_ustrict"], writes=[r_pgtm])
            P.dma("sp", iotac[:], col(I["k_iota"]), writes=[r_iotac])
            sc_t, r_sc = st.sb([128, NE], F32)
            bi, r_bi = st.sb([128, NE], F32)
            tmp, r_tmp = st.sb([128, NE], F32)
            m1, r_m1 = st.sb([128, 8], F32)
            m2, r_m2 = st.sb([128, 8], F32)
            gs, r_gs = st.sb([128, 8], F32)
            t8, r_t8 = st.sb([128, 8], F32)
            pen, r_pen = st.sb([128, 8], F32)
            sel, r_sel = st.sb([128, NE], F32)
            ssum, r_ssum = st.sb([128, 1], F32)
            gate, r_gate = st.sb([128, NE], F32)
            gTs, r_gTs = st.sb([NE, 128], F32)
            P.dma("sp", identf[:], I["k_ident"], writes=[r_idf])
            P.dma("sp", wr[:], kp(I["w_router"][l]), writes=[r_wr])
            P.dma("sp", brB[:], I["b_router"][l].partition_broadcast(128), writes=[r_brB])
        sq2 = [(sq, r_sq)] + [st.sb([128, D], F32) for _ in range(3)]
        ss2 = [(ss, r_ss)] + [st.sb([128, 1], F32) for _ in range(3)]
        rstd2 = [(rstd, r_rstd)] + [st.sb([128, 1], F32) for _ in range(3)]
        hf2 = [(hf, r_hf)] + [st.sb([128, D], F32) for _ in range(3)]
        hb2 = [(hb, r_hb)] + [st.sb([128, D], BF16) for _ in range(3)]
        if router:
            h322 = [(h32, r_h32)] + [st.sb([128, D], F32) for _ in range(3)]
            dup_sc = [(sc_t, r_sc), st.sb([128, NE], F32)]
            dup_bi = [(bi, r_bi), st.sb([128, NE], F32)]
            dup_tmp = [(tmp, r_tmp), st.sb([128, NE], F32)]
            dup_m1 = [(m1, r_m1), st.sb([128, 8], F32)]
            dup_m2 = [(m2, r_m2), st.sb([128, 8], F32)]
            dup_gs = [(gs, r_gs), st.sb([128, 8], F32)]
            dup_t8 = [(t8, r_t8), st.sb([128, 8], F32)]
            dup_pen = [(pen, r_pen), st.sb([128, 8], F32)]
            dup_sel = [(sel, r_sel), st.sb([128, NE], F32)]
            dup_ssum = [(ssum, r_ssum), st.sb([128, 1], F32)]
            dup_gate = [(gate, r_gate), st.sb([128, NE], F32)]
            dup_gTs = [(gTs, r_gTs), st.sb([NE, 128], F32)]
            dup_plg = [(plg, r_plg), st.ps([128, NE], F32)]
            plg2 = dup_plg
            onec, r_onec = st.sb([128, 1], F32)
            P.op("pool", lambda e: e.memset(onec[:], 1.0), writes=[r_onec])

        def ph1(t):
            x_t, r_x = xt[t % 4]
            sq_t, r_sq_ = sq2[t % 4]
            ss_t, r_ss_ = ss2[t % 4]
            rs_t, r_rs_ = rstd2[t % 4]
            hf_t, r_hf_ = hf2[t % 4]
            hb_t, r_hb_ = hb2[t % 4]
            P.dma("sp", x_t[:], x_src[t * 128:(t + 1) * 128, :], writes=[r_x])
            P.op("act", lambda e: e.activation(out=sq_t[:], in_=x_t[:], func=AF.Square, scale=float(D ** -0.5), accum_out=ss_t[:]),
                 reads=[r_x], writes=[r_sq_, r_ss_])
            P.op("act", lambda e: e.activation(out=rs_t[:], in_=ss_t[:], func=AF.Ln, bias=epsb[:, 0:1]), reads=[r_ss_, r_eps], writes=[r_rs_])
            P.op("act", lambda e: e.activation(out=rs_t[:], in_=rs_t[:], func=AF.Exp, scale=-0.5), reads=[r_rs_], writes=[r_rs_])
            P.op("dve", lambda e: e.scalar_tensor_tensor(out=hf_t[:], in0=x_t[:], scalar=rs_t[:, 0:1], in1=A[:], op0=ALU.mult, op1=ALU.mult),
                 reads=[r_x, r_rs_, r_A], writes=[r_hf_])
            P.op("pool", lambda e: e.tensor_tensor(out=hb_t[:], in0=hf_t[:], in1=shB[:], op=ALU.add), reads=[r_hf_, r_shB], writes=[r_hb_])
            if router:
                h32_t, r_h32_ = h322[t % 4]
                P.op("dve", lambda e: e.tensor_tensor(out=h32_t[:], in0=hf_t[:], in1=shB[:], op=ALU.add), reads=[r_hf_, r_shB], writes=[r_h32_])

        def ph2a(t):
            hb_t, r_hb_ = hb2[t % 4]
            p_t, r_p = pt[t % 2]
            h_t, r_h = hTs[t % 2]
            for c in range(8):
                P.op("pe", lambda e: e.transpose(out=p_t[:, c, :], in_=hb_t[:, c * 128:(c + 1) * 128], identity=identb[:]),
                     reads=[r_hb_, r_idb], writes=[r_p])
            P.op("act", lambda e: e.copy(out=h_t[:], in_=p_t[:]), reads=[r_p], writes=[r_h])
            P.dma("sp", kp(hT_dst)[:, :, t * 128:(t + 1) * 128], h_t[:], reads=[r_h])
            if router:
                P.dma("sp", h2_d[t * 128:(t + 1) * 128, :], hb_t[:], reads=[r_hb_])
                plg, r_plg = plg2[t % 2]
                h32_t, r_h32_ = h322[t % 4]
                for half in range(2):
                    pf_t, r_pf = ptf[half]
                    for c in range(4):
                        cc = half * 4 + c
                        P.op("pe", lambda e: e.transpose(out=pf_t[:, c, :], in_=h32_t[:, cc * 128:(cc + 1) * 128], identity=identf[:]),
                             reads=[r_h32_, r_idf], writes=[r_pf])
                    P.op("dve", lambda e: e.tensor_copy(out=hTf[:, half * 4:(half + 1) * 4, :], in_=pf_t[:]), reads=[r_pf], writes=[r_hTf])
                for c in range(8):
                    P.op("pe", lambda e: e.matmul(plg[:], lhsT=hTf[:, c, :], rhs=wr[:, c, :], start=(c == 0), stop=(c == 7)),
                         reads=[r_hTf, r_wr], writes=[r_plg])

        def ph2b(t):
            sc_t, r_sc = dup_sc[t % 2]
            bi, r_bi = dup_bi[t % 2]
            tmp, r_tmp = dup_tmp[t % 2]
            m1, r_m1 = dup_m1[t % 2]
            m2, r_m2 = dup_m2[t % 2]
            gs, r_gs = dup_gs[t % 2]
            t8, r_t8 = dup_t8[t % 2]
            pen, r_pen = dup_pen[t % 2]
            sel, r_sel = dup_sel[t % 2]
            ssum, r_ssum = dup_ssum[t % 2]
            gate, r_gate = dup_gate[t % 2]
            gTs, r_gTs = dup_gTs[t % 2]
            plg, r_plg = dup_plg[t % 2]
            P.op("act", lambda e: e.activation(out=sc_t[:], in_=plg[:], func=AF.Exp, scale=-1.0), reads=[r_plg], writes=[r_sc])
            yield
            P.op("dve", lambda e: e.tensor_scalar(out=sc_t[:], in0=sc_t[:], scalar1=1.0, scalar2=None, op0=ALU.add), reads=[r_sc], writes=[r_sc])
            yield
            P.op("dve", lambda e: e.reciprocal(out=sc_t[:], in_=sc_t[:]), reads=[r_sc], writes=[r_sc])
            yield
            P.op("dve", lambda e: e.tensor_tensor(out=bi[:], in0=sc_t[:], in1=brB[:], op=ALU.add), reads=[r_sc, r_brB], writes=[r_bi])
            yield
            bi3 = bi[:].rearrange("p (g e) -> p g e", g=8)
            tmp3 = tmp[:].rearrange("p (g e) -> p g e", g=8)
            P.op("dve", lambda e: e.tensor_reduce(out=m1[:], in_=bi3, axis=AX.X, op=ALU.max), reads=[r_bi], writes=[r_m1])
            yield
            P.op("dve", lambda e: e.tensor_tensor(out=tmp3, in0=bi3, in1=m1[:].unsqueeze(2).to_broadcast([128, 8, 8]), op=ALU.is_equal),
                 reads=[r_bi, r_m1], writes=[r_tmp])
            yield
            P.op("dve", lambda e: e.scalar_tensor_tensor(out=tmp[:], in0=tmp[:], scalar=-1e30, in1=bi[:], op0=ALU.mult, op1=ALU.add),
                 reads=[r_tmp, r_bi], writes=[r_tmp])
            yield
            P.op("dve", lambda e: e.tensor_reduce(out=m2[:], in_=tmp3, axis=AX.X, op=ALU.max), reads=[r_tmp], writes=[r_m2])
            yield
            P.op("dve", lambda e: e.tensor_tensor(out=gs[:], in0=m1[:], in1=m2[:], op=ALU.add), reads=[r_m1, r_m2], writes=[r_gs])
            yield
            P.op("dve", lambda e: e.max(out=t8[:], in_=gs[:]), reads=[r_gs], writes=[r_t8])
            yield
            P.op("dve", lambda e: e.tensor_scalar(out=pen[:], in0=gs[:], scalar1=t8[:, 3:4], scalar2=-1e30, op0=ALU.is_lt, op1=ALU.mult),
                 reads=[r_gs, r_t8], writes=[r_pen])
            yield
            P.op("dve", lambda e: e.tensor_tensor(out=tmp3, in0=bi3, in1=pen[:].unsqueeze(2).to_broadcast([128, 8, 8]), op=ALU.add),
                 reads=[r_bi, r_pen], writes=[r_tmp])
            yield
            P.op("dve", lambda e: e.max(out=t8[:], in_=tmp[:]), reads=[r_tmp], writes=[r_t8])
            yield
            P.op("dve", lambda e: e.tensor_scalar(out=sel[:], in0=tmp[:], scalar1=t8[:, 7:8], scalar2=None, op0=ALU.is_ge),
                 reads=[r_tmp, r_t8], writes=[r_sel])
            yield
            P.op("dve", lambda e: e.tensor_tensor(out=sel[:], in0=sel[:], in1=sc_t[:], op=ALU.mult), reads=[r_sel, r_sc], writes=[r_sel])
            yield
            P.op("dve", lambda e: e.tensor_reduce(out=ssum[:], in_=sel[:], axis=AX.X, op=ALU.add), reads=[r_sel], writes=[r_ssum])
            yield
            P.op("dve", lambda e: e.tensor_scalar(out=ssum[:], in0=ssum[:], scalar1=1e-20, scalar2=None, op0=ALU.add), reads=[r_ssum], writes=[r_ssum])
            yield
            P.op("dve", lambda e: e.reciprocal(out=ssum[:], in_=ssum[:]), reads=[r_ssum], writes=[r_ssum])
            yield
            P.op("dve", lambda e: e.tensor_scalar(out=gate[:], in0=sel[:], scalar1=ssum[:, 0:1], scalar2=2.5, op0=ALU.mult, op1=ALU.mult),
                 reads=[r_sel, r_ssum], writes=[r_gate])
            yield
            P.op("dve", lambda e: e.tensor_scalar(out=selb[t % 4][0][:], in0=gate[:], scalar1=0.0, scalar2=None, op0=ALU.is_gt),
                 reads=[r_gate], writes=[selb[t % 4][1]])
            yield
            P.dma("sp", gate_d[t * 128:(t + 1) * 128, :], gate[:], reads=[r_gate])
            yield

        def drain(*gens):
            gens = list(gens)
            while gens:
                for g in list(gens):
                    try:
                        next(g)
                    except StopIteration:
                        gens.remove(g)

        prev_tl = []

        def pc_ops(tl_):
            for tt_ in tl_:
                s_t, r_s = selb[tt_ % 4]
                p_t, r_p = posb[tt_ % 4]
                P.op("pe", lambda e: e.matmul(PC[:], lhsT=pinc[:], rhs=s_t[:], start=(tt_ == 0), stop=False, skip_group_check=True),
                     reads=[r_pinc, r_s], writes=[r_PC])
                P.op("act", lambda e: e.copy(out=p_t[:], in_=PC[:]), reads=[r_PC], writes=[r_p])
                P.op("pe", lambda e: e.matmul(PC[:], lhsT=pgtm[:], rhs=s_t[:], start=False, stop=(tt_ == NT - 1), skip_group_check=True),
                     reads=[r_pgtm, r_s, r_p], writes=[r_PC])
                P.dma("sp", pos_d[tt_ * 128:(tt_ + 1) * 128, :], p_t[:], reads=[r_p])

        for t0_ in range(min(4, NT)):
            ph1(t0_)
        for t in range(0, NT, 2):
            tl = [t] + ([t + 1] if t + 1 < NT else [])
            for tt_ in tl:
                ph2a(tt_)
            if t + 4 < NT:
                ph1(t + 4)
            if t + 5 < NT:
                ph1(t + 5)
            if router:
                pc_ops(prev_tl)
                prev_tl = tl
                drain(*[ph2b(tt_) for tt_ in tl])
        if router:
            pc_ops(prev_tl)
        if router:
            cnt, r_cnt = st.sb([128, NE], F32)
            nbt, r_nbt = st.sb([128, NE], F32)
            ca, r_ca = st.sb([128, NE], F32)
            cb, r_cb = st.sb([128, NE], F32)
            ebc, r_ebc = st.sb([128, 1], F32)
            P.op("act", lambda e: e.copy(out=cnt[:], in_=PC[:]), reads=[r_PC], writes=[r_cnt])
            P.op("dve", lambda e: e.tensor_scalar(out=nbt[:], in0=cnt[:], scalar1=0.0, scalar2=None, op0=ALU.is_gt), reads=[r_cnt], writes=[r_nbt])
            for j_ in range(1, 8):
                P.op("dve", lambda e: e.scalar_tensor_tensor(out=nbt[:], in0=cnt[:], scalar=512.0 * j_, in1=nbt[:], op0=ALU.is_gt, op1=ALU.add),
                     reads=[r_cnt, r_nbt], writes=[r_nbt])
            P.op("dve", lambda e: e.tensor_copy(out=ca[:], in_=nbt[:]), reads=[r_nbt], writes=[r_ca])
            src, r_src, dst_, r_dst = ca, r_ca, cb, r_cb
            k_ = 1
            while k_ < NE:
                P.op("dve", lambda e: e.tensor_copy(out=dst_[:, 0:k_], in_=src[:, 0:k_]), reads=[r_src], writes=[r_dst])
                P.op("dve", lambda e: e.tensor_tensor(out=dst_[:, k_:NE], in0=src[:, k_:NE], in1=src[:, 0:NE - k_], op=ALU.add), reads=[r_src], writes=[r_dst])
                src, r_src, dst_, r_dst = dst_, r_dst, src, r_src
                k_ *= 2
            bend, r_bend = src, r_src
            P.op("dve", lambda e: e.tensor_scalar(out=dst_[:], in0=bend[:], scalar1=iotac[:, 0:1], scalar2=None, op0=ALU.is_le),
                 reads=[r_bend, r_iotac], writes=[r_dst])
            P.op("dve", lambda e: e.tensor_reduce(out=ebc[:], in_=dst_[:], axis=AX.X, op=ALU.add), reads=[r_dst], writes=[r_ebc])
            P.op("dve", lambda e: e.tensor_scalar(out=ebc[:], in0=ebc[:], scalar1=float(NE - 1), scalar2=None, op0=ALU.min), reads=[r_ebc], writes=[r_ebc])
            P.dma("sp", col(eb_d), ebc[:], reads=[r_ebc])
            P.op("dve", lambda e: e.tensor_tensor(out=cnt[:], in0=bend[:], in1=nbt[:], op=ALU.subtract), reads=[r_bend, r_nbt], writes=[r_cnt])
            P.op("dve", lambda e: e.tensor_scalar(out=cnt[:], in0=cnt[:], scalar1=512.0, scalar2=None, op0=ALU.mult), reads=[r_cnt], writes=[r_cnt])
            P.dma("sp", sbase_d.rearrange("(o n) -> o n", o=1), cnt[0:1, :], reads=[r_cnt])
        st.done(f"norm{int(router)}_{l}")

    def stage_proj(l):
        st = Stage(P)
        NCOL = 4096
        w, r_w = st.sb([128, 8, NCOL], BF16)
        blk, r_blk = st.sb([128, 128], BF16)
        rot, r_rot = st.sb([128, 128], BF16)
        epsb, r_eps = st.sb([128, 1], F32)
        gq, r_gq = st.sb([128, 1], F32)
        gk, r_gk = st.sb([128, 1], F32)
        hT = [st.sb([128, 8, 512], BF16) for _ in range(2)]
        cosT = [st.sb([128, 512], F32) for _ in range(2)]
        sinT = [st.sb([128, 512], F32) for _ in range(2)]
        pp = [st.ps([128, 512], F32) for _ in range(4)]
        pms, r_pms = st.ps([128, 512], F32)
        prot, r_prot = st.ps([128, 512], F32)
        sqb, r_sqb = st.sb([128, 512], BF16)
        rs, r_rs = st.sb([128, 512], F32)
        qn, r_qn = st.sb([128, 512], BF16)
        t1, r_t1 = st.sb([128, 512], F32)
        t2, r_t2 = st.sb([128, 512], F32)
        sg, r_sg = st.sb([128, 512], F32)
        ob = [st.sb([128, 512], BF16) for _ in range(4)]
        for c in range(8):
            P.dma("pool", w[:, c, :], I["w_in"][l][c * 128:(c + 1) * 128, 0:NCOL], writes=[r_w])
        P.dma("pool", blk[:], I["k_blk64"], writes=[r_blk])
        P.dma("pool", rot[:], I["k_rotT"], writes=[r_rot])
        P.op("pool", lambda e: e.memset(epsb[:], EPS), writes=[r_eps])
        for hh in range(2):
            P.dma("sp", gq[hh * 64:(hh + 1) * 64, :], col(I["qn_g"][l]), writes=[r_gq])
            P.dma("sp", gk[hh * 64:(hh + 1) * 64, :], col(I["kn_g"][l]), writes=[r_gk])
        pp = pp + [st.ps([128, 512], F32) for _ in range(2)]
        NPP = len(pp)
        loaded = set()

        def load_tile(tt):
            if tt in loaded or tt >= NQ:
                return
            loaded.add(tt)
            ts_ = slice(tt * 512, (tt + 1) * 512)
            P.dma("sp", hT[tt % 2][0][:], kp(hT_d)[:, :, ts_], writes=[hT[tt % 2][1]])
            P.dma("sp", cosT[tt % 2][0][:], cos_d[:, ts_], writes=[cosT[tt % 2][1]])
            P.dma("sp", sinT[tt % 2][0][:], sin_d[:, ts_], writes=[sinT[tt % 2][1]])

        units = []
        for tt in range(NQ):
            for which in range(2):
                for j in range(4):
                    units.append(("qk", tt, (which, j)))
            for j in range(4):
                units.append(("glu", tt, (j,)))
            for which in range(2):
                for j in range(4):
                    units.append(("cp", tt, (which, j)))
            for which in range(2):
                for sub in range(4):
                    units.append(("tm", tt, (which, sub)))
        pidx = []
        cur = 0
        for kind, tt, args in units:
            nb_ = 2 if kind == "glu" else 1
            pidx.append([(cur + i) % NPP for i in range(nb_)])
            cur += nb_
        state = {"oi": 0}

        def fmm(tt, col0, p_t, r_p):
            h_t, r_h = hT[tt % 2]
            for c in range(8):
                P.op("pe", lambda e: e.matmul(p_t[:], lhsT=w[:, c, col0:col0 + 128], rhs=h_t[:, c, :], start=(c == 0), stop=(c == 7)),
                     reads=[r_w, r_h], writes=[r_p])

        def F(u):
            kind, tt, args = units[u]
            load_tile(tt)
            bufs = [pp[i] for i in pidx[u]]
            if kind == "qk":
                which, j = args
                fmm(tt, which * 512 + j * 128, *bufs[0])
            elif kind == "glu":
                (j,) = args
                fmm(tt, 1536 + j * 128, *bufs[0])
                fmm(tt, 2048 + j * 128, *bufs[1])
            elif kind == "cp":
                which, j = args
                fmm(tt, (2560 if which == 0 else 3072) + j * 128, *bufs[0])
            else:
                which, sub = args
                base = 1024 if which == 0 else 3584
                h_t, r_h = hT[tt % 2]
                p_t, r_p = bufs[0]
                for c in range(8):
                    P.op("pe", lambda e: e.matmul(p_t[:], lhsT=h_t[:, c, sub * 128:(sub + 1) * 128], rhs=w[:, c, base:base + 512], start=(c == 0), stop=(c == 7)),
                         reads=[r_w, r_h], writes=[r_p])

        def nxt_ob():
            o = ob[state["oi"] % 4]
            state["oi"] += 1
            return o

        def G(u):
            kind, tt, args = units[u]
            ts_ = slice(tt * 512, (tt + 1) * 512)
            bufs = [pp[i] for i in pidx[u]]
            c_t, r_c = cosT[tt % 2]
            s_t, r_s = sinT[tt % 2]
            if kind == "qk":
                which, j = args
                g_t, r_g, dst = (gq, r_gq, qaT_d) if which == 0 else (gk, r_gk, kaT_d)
                p_t, r_p = bufs[0]
                P.op("act", lambda e: e.activation(out=sqb[:], in_=p_t[:], func=AF.Square), reads=[r_p], writes=[r_sqb])
                P.op("pe", lambda e: e.matmul(pms[:], lhsT=blk[:], rhs=sqb[:], start=True, stop=True), reads=[r_blk, r_sqb], writes=[r_pms])
                P.op("act", lambda e: e.activation(out=rs[:], in_=pms[:], func=AF.Ln, bias=epsb[:, 0:1]), reads=[r_pms, r_eps], writes=[r_rs])
                P.op("act", lambda e: e.activation(out=rs[:], in_=rs[:], func=AF.Exp, scale=-0.5), reads=[r_rs], writes=[r_rs])
                P.op("dve", lambda e: e.scalar_tensor_tensor(out=qn[:], in0=p_t[:], scalar=g_t[:, 0:1], in1=rs[:], op0=ALU.mult, op1=ALU.mult),
                     reads=[r_p, r_g, r_rs], writes=[r_qn])
                P.op("pe", lambda e: e.matmul(prot[:], lhsT=rot[:], rhs=qn[:], start=True, stop=True), reads=[r_rot, r_qn], writes=[r_prot])
                P.op("pool", lambda e: e.tensor_tensor(out=t1[:], in0=qn[:], in1=c_t[:], op=ALU.mult), reads=[r_qn, r_c], writes=[r_t1])
                P.op("dve", lambda e: e.tensor_tensor(out=t2[:], in0=prot[:], in1=s_t[:], op=ALU.mult), reads=[r_prot, r_s], writes=[r_t2])
                o_t, r_o = nxt_ob()
                P.op("dve", lambda e: e.tensor_tensor(out=o_t[:], in0=t1[:], in1=t2[:], op=ALU.add), reads=[r_t1, r_t2], writes=[r_o])
                P.dma("sp", dst[j * 128:(j + 1) * 128, ts_], o_t[:], reads=[r_o])
            elif kind == "glu":
                (j,) = args
                (pa, r_pa), (pg, r_pg) = bufs
                P.op("act", lambda e: e.activation(out=sg[:], in_=pg[:], func=AF.Sigmoid), reads=[r_pg], writes=[r_sg])
                o_t, r_o = nxt_ob()
                P.op("dve", lambda e: e.tensor_tensor(out=o_t[:], in0=pa[:], in1=sg[:], op=ALU.mult), reads=[r_pa, r_sg], writes=[r_o])
                P.dma("sp", uT_d[j * 128:(j + 1) * 128, ts_], o_t[:], reads=[r_o])
            elif kind == "cp":
                which, j = args
                dst, qscale = (qcT_d, 0.125) if which == 0 else (kcT_d, 1.0)
                p_t, r_p = bufs[0]
                o_t, r_o = nxt_ob()
                P.op("act", lambda e: e.mul(out=o_t[:], in_=p_t[:], mul=qscale), reads=[r_p], writes=[r_o])
                P.dma("sp", dst[j * 128:(j + 1) * 128, ts_], o_t[:], reads=[r_o])
            else:
                which, sub = args
                dst = va_d if which == 0 else vc_d
                p_t, r_p = bufs[0]
                o_t, r_o = nxt_ob()
                P.op("dve" if sub % 2 == 0 else "act", (lambda e: e.tensor_copy(out=o_t[:], in_=p_t[:])) if sub % 2 == 0 else (lambda e: e.copy(out=o_t[:], in_=p_t[:])),
                     reads=[r_p], writes=[r_o])
                r0 = tt * 512 + sub * 128
                P.dma("sp", dst[r0:r0 + 128, :], o_t[:], reads=[r_o])

        nU = len(units)
        AHEAD = 2
        for u in range(min(AHEAD, nU)):
            F(u)
        for u in range(nU):
            if u + AHEAD < nU:
                F(u + AHEAD)
            G(u)
        st.done(f"proj{l}")

    def stage_da(l):
        lambda_init = 0.8 - 0.6 * math.exp(-0.3 * l)
        st = Stage(P)
        msk, r_msk = st.sb([128, 4, 512], BF16)
        ones, r_ones = st.sb([128, 128], BF16)
        o128, r_o128 = st.sb([128, 128], BF16)
        epsb, r_eps = st.sb([128, 1], F32)
        lv = [st.sb([128, 64], F32) for _ in range(4)]
        lp, r_lp = st.sb([128, 64], F32)
        ld, r_ld = st.sb([128, 2], F32)
        nlam, r_nlam = st.sb([128, 1], F32)
        gcol, r_gcol = st.sb([128, 1], F32)
        qT = [[st.sb([128, S], BF16) for _ in range(2)] for _ in range(2)]
        kT = [st.sb([128, S], BF16) for _ in range(2)]
        vv = [st.sb([128, NT, 128], BF16) for _ in range(2)]
        pz = [st.ps([128, 512], F32) for _ in range(2)]
        pO = [st.ps([128, 512], F32) for _ in range(2)]
        pL = [st.ps([128, 512], F32) for _ in range(2)]
        for hp_ in range(2):
            for m_ in range(2):
                zr = slice((1 - m_) * 64, (1 - m_) * 64 + 64)
                P.op("pool", lambda e: e.memset(qT[hp_][m_][0][zr, :], 0.0), writes=[qT[hp_][m_][1]])
        pms, r_pms = st.ps([128, 512], F32)
        E = [st.sb([128, 512], BF16) for _ in range(4)]
        rL = [st.sb([128, 512], F32) for _ in range(2)]
        tO = [st.sb([128, 512], F32) for _ in range(2)]
        o_t, r_o = st.sb([128, 512], F32)
        sqb, r_sqb = st.sb([128, 512], BF16)
        rs, r_rs = st.sb([128, 512], F32)
        ob = [st.sb([128, 512], BF16) for _ in range(2)]
        P.dma("pool", msk[:], I["k_maskc"].rearrange("j p q -> p j q"), writes=[r_msk])
        if l == 0:
            zt, r_zt = st.sb([128, 4096], BF16)
            P.op("pool", lambda e: e.memset(zt[:], 0.0), writes=[r_zt])
        zfill = {"next": 0}

        def zero_fill_some(n_):
            if l != 0:
                return
            for _ in range(n_):
                zi_ = zfill["next"]
                if zi_ >= NSLOT // 512:
                    return
                zfill["next"] += 1
                P.dma("pool", xs_d[zi_ * 512:(zi_ + 1) * 512, :].rearrange("(p r) d -> p (r d)", p=128), zt[:], reads=[r_zt])
        P.op("pool", lambda e: e.memset(ones[:], 1.0), writes=[r_ones])
        P.op("pool", lambda e: e.memset(o128[:], 1.0 / 128), writes=[r_o128])
        P.op("pool", lambda e: e.memset(epsb[:], EPS), writes=[r_eps])
        for i, nm in enumerate(("lam_q1", "lam_k1", "lam_q2", "lam_k2")):
            P.dma("sp", lv[i][0][:], I[nm][l].partition_broadcast(128), writes=[lv[i][1]])
        for i in range(2):
            P.op("dve", lambda e, i=i: e.tensor_tensor(out=lp[:], in0=lv[2 * i][0][:], in1=lv[2 * i + 1][0][:], op=ALU.mult),
                 reads=[lv[2 * i][1], lv[2 * i + 1][1]], writes=[r_lp])
            P.op("dve", lambda e, i=i: e.tensor_reduce(out=ld[:, i:i + 1], in_=lp[:], axis=AX.X, op=ALU.add), reads=[r_lp], writes=[r_ld])
        P.op("act", lambda e: e.activation(out=ld[:], in_=ld[:], func=AF.Exp), reads=[r_ld], writes=[r_ld])
        P.op("dve", lambda e: e.tensor_tensor(out=nlam[:], in0=ld[:, 1:2], in1=ld[:, 0:1], op=ALU.subtract), reads=[r_ld], writes=[r_nlam])
        P.op("dve", lambda e: e.tensor_scalar(out=nlam[:], in0=nlam[:], scalar1=-lambda_init, scalar2=None, op0=ALU.add), reads=[r_nlam], writes=[r_nlam])
        P.dma("sp", gcol[:], col(I["subln_g"][l]), writes=[r_gcol])
        P.op("dve", lambda e: e.tensor_scalar(out=gcol[:], in0=gcol[:], scalar1=1.0 - lambda_init, scalar2=None, op0=ALU.mult), reads=[r_gcol], writes=[r_gcol])
        oi = 0
        pz3 = pz + [st.ps([128, 512], F32)]
        units = []
        for hd in range(4):
            for Qi in range(NQ):
                for m in range(2):
                    for kt in range(4 * Qi + 4):
                        units.append((hd, Qi, m, kt))
        loaded = set()

        def load_head(hd):
            if hd in loaded or hd >= 4:
                return
            loaded.add(hd)
            k_t, r_k = kT[hd % 2]
            v_t, r_v = vv[hd % 2]
            for m_ in range(2):
                rr_ = slice(m_ * 64, m_ * 64 + 64)
                P.dma("sp", qT[hd % 2][m_][0][rr_, :], qaT_d[hd * 128 + m_ * 64:hd * 128 + m_ * 64 + 64, :], writes=[qT[hd % 2][m_][1]])
            P.dma("sp", k_t[:], kaT_d[hd * 128:(hd + 1) * 128, :], writes=[r_k])
            P.dma("sp", v_t[:], va_d[:, hd * 128:(hd + 1) * 128].rearrange("(t p) e -> p t e", p=128), writes=[r_v])

        def phA(i):
            hd, Qi, m, kt = units[i]
            load_head(hd)
            q_t, r_q = qT[hd % 2][m]
            k_t, r_k = kT[hd % 2]
            z_t, r_z = pz3[i % 3]
            qs = slice(Qi * 512, (Qi + 1) * 512)
            P.op("pe", lambda e: e.matmul(z_t[:], lhsT=k_t[:, kt * 128:(kt + 1) * 128], rhs=q_t[:, qs], start=True, stop=True),
                 reads=[r_k, r_q], writes=[r_z])

        def phB(i):
            nonlocal oi
            hd, Qi, m, kt = units[i]
            v_t, r_v = vv[hd % 2]
            z_t, r_z = pz3[i % 3]
            e_t, r_e = E[i % 4]
            pO_t, r_pO = pO[m]
            pL_t, r_pL = pL[m]
            nk = 4 * Qi + 4
            j = kt - 4 * Qi
            qs = slice(Qi * 512, (Qi + 1) * 512)
            P.op("act", lambda e: e.activation(out=e_t[:], in_=z_t[:], func=AF.Exp, scale=0.125), reads=[r_z], writes=[r_e])
            if j >= 0:
                P.op("dve", lambda e: e.tensor_tensor(out=e_t[:], in0=e_t[:], in1=msk[:, j, :], op=ALU.mult), reads=[r_e, r_msk], writes=[r_e])
            P.op("pe", lambda e: e.matmul(pO_t[:], lhsT=v_t[:, kt, :], rhs=e_t[:], start=(kt == 0), stop=(kt == nk - 1)),
                 reads=[r_v, r_e], writes=[r_pO])
            P.op("pe", lambda e: e.matmul(pL_t[:], lhsT=ones[:], rhs=e_t[:], start=(kt == 0), stop=(kt == nk - 1)),
                 reads=[r_ones, r_e], writes=[r_pL])
            if kt == nk - 1:
                P.op("act", lambda e: e.activation(out=rL[m][0][:], in_=pL_t[:], func=AF.Ln), reads=[r_pL], writes=[rL[m][1]])
                P.op("act", lambda e: e.activation(out=rL[m][0][:], in_=rL[m][0][:], func=AF.Exp, scale=-1.0), reads=[rL[m][1]], writes=[rL[m][1]])
                P.op("dve", lambda e: e.tensor_tensor(out=tO[m][0][:], in0=pO_t[:], in1=rL[m][0][:], op=ALU.mult), reads=[r_pO, rL[m][1]], writes=[tO[m][1]])
                if m == 1:
                    P.op("dve", lambda e: e.scalar_tensor_tensor(out=o_t[:], in0=tO[1][0][:], scalar=nlam[:, 0:1], in1=tO[0][0][:], op0=ALU.mult, op1=ALU.add),
                         reads=[tO[0][1], tO[1][1], r_nlam], writes=[r_o])
                    P.op("act", lambda e: e.activation(out=sqb[:], in_=o_t[:], func=AF.Square), reads=[r_o], writes=[r_sqb])
                    P.op("pe", lambda e: e.matmul(pms[:], lhsT=o128[:], rhs=sqb[:], start=True, stop=True), reads=[r_o128, r_sqb], writes=[r_pms])
                    P.op("act", lambda e: e.activation(out=rs[:], in_=pms[:], func=AF.Ln, bias=epsb[:, 0:1]), reads=[r_pms, r_eps], writes=[r_rs])
                    P.op("act", lambda e: e.activation(out=rs[:], in_=rs[:], func=AF.Exp, scale=-0.5), reads=[r_rs], writes=[r_rs])
                    b_t, r_b = ob[oi % 2]
                    oi += 1
                    P.op("dve", lambda e: e.scalar_tensor_tensor(out=b_t[:], in0=o_t[:], scalar=gcol[:, 0:1], in1=rs[:], op0=ALU.mult, op1=ALU.mult),
                         reads=[r_o, r_gcol, r_rs], writes=[r_b])
                    P.dma("sp", oaT_d[hd * 128:(hd + 1) * 128, qs], b_t[:], reads=[r_b])

        n = len(units)
        for i in range(min(2, n)):
            phA(i)
        for i in range(n):
            if i + 2 < n:
                phA(i + 2)
            phB(i)
            if i % 4 == 3:
                zero_fill_some(1)
        zero_fill_some(NSLOT)
        st.done(f"da{l}")

    def stage_sb(l):
        st = Stage(P)
        mskb, r_mskb = st.sb([128, 4, 512], BF16)
        uinc, r_uinc = st.sb([128, 128], BF16)
        ulow, r_ulow = st.sb([128, 128], BF16)
        onec, r_onec = st.sb([128, 1], F32)
        qT = [[st.sb([128, S], BF16) for _ in range(2)] for _ in range(2)]
        kT = [st.sb([128, S], BF16) for _ in range(2)]
        kN = [st.sb([128, S], BF16) for _ in range(2)]
        vv = [st.sb([128, NT, 128], BF16) for _ in range(2)]
        pz = [st.ps([128, 2, 512], F32) for _ in range(2)]
        PT, r_PT = st.ps([128, 2, 512], F32)
        pO, r_pO = st.ps([128, 2, 512], F32)
        ex = [st.sb([128, 2, 512], F32) for _ in range(2)]
        sp_ = [st.sb([128, 2, 512], F32) for _ in range(2)]
        lom = [st.sb([128, 2, 512], BF16) for _ in range(3)]
        ab = [st.sb([128, 2, 512], BF16) for _ in range(3)]
        ob = [st.sb([128, 512], BF16) for _ in range(2)]
        for cp_ in range(2):
            for hh_ in range(2):
                zr = slice((1 - hh_) * 64, (1 - hh_) * 64 + 64)
                P.op("pool", lambda e: e.memset(qT[cp_][hh_][0][zr, :], 0.0), writes=[qT[cp_][hh_][1]])
        P.dma("pool", mskb[:], I["k_masks"].rearrange("j p q -> p j q"), writes=[r_mskb])
        P.dma("pool", uinc[:], I["k_uincl"], writes=[r_uinc])
        P.dma("pool", ulow[:], I["k_ulow"], writes=[r_ulow])
        P.op("pool", lambda e: e.memset(onec[:], 1.0), writes=[r_onec])
        state = {"oi": 0}
        units = [(ch, Qi, kt) for ch in range(4) for Qi in range(NQ) for kt in range(4 * Qi + 3, -1, -1)]
        n = len(units)
        loaded = set()

        def load_pair(ch):
            if ch in loaded:
                return
            loaded.add(ch)
            for hh_ in range(2):
                rr_ = slice(hh_ * 64, hh_ * 64 + 64)
                P.dma("sp", qT[ch % 2][hh_][0][rr_, :], qcT_d[ch * 128 + hh_ * 64:ch * 128 + hh_ * 64 + 64, :], writes=[qT[ch % 2][hh_][1]])
            P.dma("sp", kT[ch % 2][0][:], kcT_d[ch * 128:(ch + 1) * 128, :], writes=[kT[ch % 2][1]])
            P.dma("sp", vv[ch % 2][0][:], vc_d[:, ch * 128:(ch + 1) * 128].rearrange("(t p) e -> p t e", p=128), writes=[vv[ch % 2][1]])
            P.op("pool", lambda e: e.tensor_scalar(out=kN[ch % 2][0][:], in0=kT[ch % 2][0][:], scalar1=-1.0, scalar2=None, op0=ALU.mult),
                 reads=[kT[ch % 2][1]], writes=[kN[ch % 2][1]])

        def info(i):
            ch, Qi, kt = units[i]
            return ch, Qi, kt, kt - 4 * Qi, 4 * Qi + 4, slice(Qi * 512, (Qi + 1) * 512), slice(kt * 128, (kt + 1) * 128)

        def mask2(j):
            return mskb[:, j:j + 1, :].to_broadcast([128, 2, 512])

        def phA(i):
            ch, Qi, kt, j, nk, qs, ks = info(i)
            load_pair(ch)
            k_t, r_k = kT[ch % 2]
            z_t, r_z = pz[i % 2]
            for hh in range(2):
                q_t, r_q = qT[ch % 2][hh]
                P.op("pe", lambda e: e.matmul(z_t[:, hh, :], lhsT=k_t[:, ks], rhs=q_t[:, qs], start=True, stop=True), reads=[r_k, r_q], writes=[r_z])

        def phB(i):
            ch, Qi, kt, j, nk, qs, ks = info(i)
            z_t, r_z = pz[i % 2]
            e_t, r_e = ex[i % 2]
            p_t, r_p = sp_[i % 2]
            l_t, r_l = lom[i % 3]
            P.op("act", lambda e: e.activation(out=e_t[:], in_=z_t[:], func=AF.Exp, scale=-1.0), reads=[r_z], writes=[r_e])
            P.op("act", lambda e: e.activation(out=p_t[:], in_=e_t[:], func=AF.Ln, bias=onec[:, 0:1]), reads=[r_e, r_onec], writes=[r_p])
            P.op("dve", lambda e: e.scalar_tensor_tensor(out=l_t[:], in0=z_t[:], scalar=-1.0, in1=p_t[:], op0=ALU.mult, op1=ALU.subtract),
                 reads=[r_z, r_p], writes=[r_l])
            if j >= 0:
                P.op("dve", lambda e: e.tensor_tensor(out=l_t[:], in0=l_t[:], in1=mask2(j), op=ALU.mult), reads=[r_l, r_mskb], writes=[r_l])

        def phC(i):
            ch, Qi, kt, j, nk, qs, ks = info(i)
            k_t, r_k = kT[ch % 2]
            l_t, r_l = lom[i % 3]
            for hh in range(2):
                q_t, r_q = qT[ch % 2][hh]
                P.op("pe", lambda e: e.matmul(PT[:, hh, :], lhsT=uinc[:], rhs=l_t[:, hh, :], start=(kt == nk - 1), stop=False, skip_group_check=True),
                     reads=[r_uinc, r_l], writes=[r_PT])
                P.op("pe", lambda e: e.matmul(PT[:, hh, :], lhsT=k_t[:, ks], rhs=q_t[:, qs], start=False, stop=False, skip_group_check=True),
                     reads=[r_k, r_q], writes=[r_PT])

        def phD_act(i):
            ch, Qi, kt, j, nk, qs, ks = info(i)
            a_t, r_a = ab[i % 3]
            P.op("act", lambda e: e.activation(out=a_t[:], in_=PT[:], func=AF.Exp), reads=[r_PT], writes=[r_a])
            if j >= 0:
                P.op("dve", lambda e: e.tensor_tensor(out=a_t[:], in0=a_t[:], in1=mask2(j), op=ALU.mult), reads=[r_a, r_mskb], writes=[r_a])

        def phD_corr(i):
            ch, Qi, kt, j, nk, qs, ks = info(i)
            kn_t, r_kn = kN[ch % 2]
            l_t, r_l = lom[i % 3]
            a_t, r_a = ab[i % 3]
            if kt > 0:
                for hh in range(2):
                    q_t, r_q = qT[ch % 2][hh]
                    P.op("pe", lambda e: e.matmul(PT[:, hh, :], lhsT=ulow[:], rhs=l_t[:, hh, :], start=False, stop=False, skip_group_check=True),
                         reads=[r_ulow, r_l, r_a], writes=[r_PT])
                    P.op("pe", lambda e: e.matmul(PT[:, hh, :], lhsT=kn_t[:, ks], rhs=q_t[:, qs], start=False, stop=(kt == 1), skip_group_check=True),
                         reads=[r_kn, r_q], writes=[r_PT])

        def phD_pv(i):
            ch, Qi, kt, j, nk, qs, ks = info(i)
            v_t, r_v = vv[ch % 2]
            a_t, r_a = ab[i % 3]
            for hh in range(2):
                P.op("pe", lambda e: e.matmul(pO[:, hh, :], lhsT=v_t[:, kt, :], rhs=a_t[:, hh, :], start=(kt == nk - 1), stop=(kt == 0)),
                     reads=[r_v, r_a], writes=[r_pO])
            if kt == 0:
                b_t, r_b = ob[state["oi"] % 2]
                state["oi"] += 1
                for hh in range(2):
                    pr = slice(hh * 64, hh * 64 + 64)
                    P.op("act", lambda e: e.copy(out=b_t[pr, :], in_=pO[pr, hh, :]), reads=[r_pO], writes=[r_b])
                P.dma("sp", ocT_d[ch * 128:(ch + 1) * 128, qs], b_t[:], reads=[r_b])

        for it in range(-3, n):
            if 0 <= it < n:
                phD_act(it)
            if 0 <= it + 3 < n:
                phA(it + 3)
            if 0 <= it + 2 < n:
                phB(it + 2)
            if 0 <= it < n:
                phD_corr(it)
            if 0 <= it + 1 < n:
                phC(it + 1)
            if 0 <= it < n:
                phD_pv(it)
        st.done(f"sb{l}")

    def stage_cv(l):
        st = Stage(P)
        up, r_up = st.sb([128, 4, 30 + S], BF16)
        wcol, r_wcol = st.sb([128, 4, 31], F32)
        identf, r_idf = st.sb([128, 128], F32)
        dg = [st.sb([128, 31, 128], BF16) for _ in range(4)]
        bcol, r_bcol = st.sb([128, 4], F32)
        gcol, r_gcol = st.sb([128, 4], F32)
        lbcol, r_lbcol = st.sb([128, 4], F32)
        o512, r_o512 = st.sb([128, 128], F32)
        epsb, r_eps = st.sb([128, 1], F32)
        pc = [st.ps([128, 512], F32) for _ in range(4)]
        pmean, r_pmean = st.ps([128, 512], F32)
        pex2, r_pex2 = st.ps([128, 512], F32)
        cv32, r_cv = st.sb([128, 4, 512], F32)
        sq32, r_sq = st.sb([128, 4, 512], F32)
        mean, r_mean = st.sb([128, 512], F32)
        msq, r_msq = st.sb([128, 512], F32)
        rs, r_rs = st.sb([128, 512], F32)
        y = [st.sb([128, 512], F32) for _ in range(2)]
        ob = [st.sb([128, 512], BF16) for _ in range(2)]
        P.op("pool", lambda e: e.memset(up[:, :, 0:30], 0.0), writes=[r_up])
        for j in range(4):
            P.dma("sp", up[:, j, 30:30 + S], uT_d[j * 128:(j + 1) * 128, :], writes=[r_up])
            P.dma("sp", wcol[:, j, :], I["w_dw"][l][:, j * 128:(j + 1) * 128].rearrange("k p -> p k"), writes=[r_wcol])
        P.dma("sp", identf[:], I["k_ident"], writes=[r_idf])
        P.dma("sp", bcol[:], I["b_dw"][l].rearrange("(j p) -> p j", p=128), writes=[r_bcol])
        P.dma("sp", gcol[:], I["conv_ln_g"][l].rearrange("(j p) -> p j", p=128), writes=[r_gcol])
        P.dma("sp", lbcol[:], I["conv_ln_b"][l].rearrange("(j p) -> p j", p=128), writes=[r_lbcol])
        P.op("pool", lambda e: e.memset(o512[:], 1.0 / 512), writes=[r_o512])
        P.op("pool", lambda e: e.memset(epsb[:], EPS), writes=[r_eps])
        junk, r_junk = st.sb([128, 1], F32)
        for j in range(4):
            rr = []
            for k in range(31):
                eng = "dve" if k % 2 == 0 else "pool"
                r1 = Res()
                rr.append(r1)
                P.op(eng, lambda e: e.tensor_scalar(out=dg[j][0][:, k, :], in0=identf[:], scalar1=wcol[:, j, k:k + 1], scalar2=None, op0=ALU.mult),
                     reads=[r_idf, r_wcol], writes=[r1])
            P.op("dve", lambda e: e.memset(junk[:], 0.0), reads=rr, writes=[dg[j][1], r_junk])
        yi = 0
        for tt in range(NQ):
            for j in range(4):
                p_t, r_p = pc[j]
                for k in range(31):
                    P.op("pe", lambda e: e.matmul(p_t[:], lhsT=dg[j][0][:, k, :], rhs=up[:, j, tt * 512 + k:tt * 512 + k + 512], start=(k == 0), stop=(k == 30)),
                         reads=[dg[j][1], r_up], writes=[r_p])
                P.op("dve", lambda e: e.tensor_scalar(out=cv32[:, j, :], in0=p_t[:], scalar1=bcol[:, j:j + 1], scalar2=None, op0=ALU.add),
                     reads=[r_p, r_bcol], writes=[r_cv])
                P.op("pool", lambda e: e.tensor_tensor(out=sq32[:, j, :], in0=cv32[:, j, :], in1=cv32[:, j, :], op=ALU.mult), reads=[r_cv], writes=[r_sq])
            for j in range(4):
                P.op("pe", lambda e: e.matmul(pmean[:], lhsT=o512[:], rhs=cv32[:, j, :], start=(j == 0), stop=(j == 3)), reads=[r_o512, r_cv], writes=[r_pmean])
            for j in range(4):
                P.op("pe", lambda e: e.matmul(pex2[:], lhsT=o512[:], rhs=sq32[:, j, :], start=(j == 0), stop=(j == 3)), reads=[r_o512, r_sq], writes=[r_pex2])
            P.op("act", lambda e: e.copy(out=mean[:], in_=pmean[:]), reads=[r_pmean], writes=[r_mean])
            P.op("pool", lambda e: e.tensor_tensor(out=msq[:], in0=mean[:], in1=mean[:], op=ALU.mult), reads=[r_mean], writes=[r_msq])
            P.op("dve", lambda e: e.tensor_tensor(out=rs[:], in0=pex2[:], in1=msq[:], op=ALU.subtract), reads=[r_pex2, r_msq], writes=[r_rs])
            P.op("act", lambda e: e.activation(out=rs[:], in_=rs[:], func=AF.Ln, bias=epsb[:, 0:1]), reads=[r_rs, r_eps], writes=[r_rs])
            P.op("act", lambda e: e.activation(out=rs[:], in_=rs[:], func=AF.Exp, scale=-0.5), reads=[r_rs], writes=[r_rs])
            for j in range(4):
                y_t, r_y = y[yi % 2]
                b_t, r_b = ob[yi % 2]
                yi += 1
                P.op("pool", lambda e: e.tensor_tensor(out=y_t[:], in0=cv32[:, j, :], in1=mean[:], op=ALU.subtract), reads=[r_cv, r_mean], writes=[r_y])
                P.op("dve", lambda e: e.tensor_tensor(out=y_t[:], in0=y_t[:], in1=rs[:], op=ALU.mult), reads=[r_y, r_rs], writes=[r_y])
                P.op("act", lambda e: e.activation(out=b_t[:], in_=y_t[:], func=AF.Silu, scale=gcol[:, j:j + 1], bias=lbcol[:, j:j + 1]),
                     reads=[r_y, r_gcol, r_lbcol], writes=[r_b])
                P.dma("sp", cvT_d[j * 128:(j + 1) * 128, tt * 512:(tt + 1) * 512], b_t[:], reads=[r_b])
        st.done(f"cv{l}")

    def stage_mg(l, x_src):
        st = Stage(P)
        wg, r_wg = st.sb([128, 8, 3072], BF16)
        wp = [st.sb([128, 4, D], BF16) for _ in range(3)]
        wo, r_wo = st.sb([128, 8, D], BF16)
        bpb, r_bpb = st.sb([128, 8], F32)
        gmB, r_gmB = st.sb([128, D], F32)
        hT = [st.sb([128, 8, 512], BF16) for _ in range(2)]
        obr = [[st.sb([128, 4, 512], BF16) for _ in range(2)] for _ in range(3)]
        mT, r_mT = st.sb([128, 8, 512], BF16)
        py = [st.ps([128, 512], F32) for _ in range(2)]
        pg = [st.ps([128, 512], F32) for _ in range(2)]
        po = [st.ps([128, 512], F32) for _ in range(2)]
        sg = [st.sb([128, 512], F32) for _ in range(2)]
        mb = [st.sb([128, 512], F32) for _ in range(2)]
        acc, r_acc = st.sb([128, 512], F32)
        xt = [st.sb([128, D], F32) for _ in range(2)]
        tmp, r_tmp = st.sb([128, 512], F32)
        xn = [st.sb([128, D], F32) for _ in range(2)]
        for c in range(8):
            P.dma("pool", wg[:, c, :], I["w_in"][l][c * 128:(c + 1) * 128, 4096:7168], writes=[r_wg])
        for b, nm in enumerate(("w_proj_a", "w_proj_b", "w_proj_c")):
            P.dma("pool", wp[b][0][:], kp(I[nm][l]), writes=[wp[b][1]])
        P.dma("pool", wo[:], kp(I["w_out"][l]), writes=[r_wo])
        P.dma("sp", bpb[:], I["b_proj_b"][l].rearrange("(j p) -> p j", p=128), writes=[r_bpb])
        P.dma("sp", gmB[:], mod_d[l, 2 * D:3 * D].partition_broadcast(128), writes=[r_gmB])
        srcs = (oaT_d, cvT_d, ocT_d)
        loaded = set()

        def load_tile(tt):
            if tt in loaded or tt >= NQ:
                return
            loaded.add(tt)
            ts_ = slice(tt * 512, (tt + 1) * 512)
            P.dma("sp", hT[tt % 2][0][:], kp(hT_d)[:, :, ts_], writes=[hT[tt % 2][1]])
            for b_ in range(3):
                P.dma("sp", obr[b_][tt % 2][0][:], kp(srcs[b_])[:, :, ts_], writes=[obr[b_][tt % 2][1]])

        units = [(tt, j, b_) for tt in range(NQ) for j in range(8) for b_ in range(3)]
        state = {"xi": 0}

        def F(u):
            tt, j, b_ = units[u]
            load_tile(tt)
            y_t, r_y = py[u % 2]
            g_t, r_g = pg[u % 2]
            h_t, r_h = hT[tt % 2]
            o_b, r_ob = obr[b_][tt % 2]
            for c in range(4):
                P.op("pe", lambda e: e.matmul(y_t[:], lhsT=wp[b_][0][:, c, j * 128:(j + 1) * 128], rhs=o_b[:, c, :], start=(c == 0), stop=(c == 3)),
                     reads=[wp[b_][1], r_ob], writes=[r_y])
            for c in range(8):
                P.op("pe", lambda e: e.matmul(g_t[:], lhsT=wg[:, c, b_ * D + j * 128:b_ * D + (j + 1) * 128], rhs=h_t[:, c, :], start=(c == 0), stop=(c == 7)),
                     reads=[r_wg, r_h], writes=[r_g])

        def G(u):
            tt, j, b_ = units[u]
            y_t, r_y = py[u % 2]
            g_t, r_g = pg[u % 2]
            s_t, r_s = sg[u % 2]
            m_t, r_m = mb[u % 2]
            P.op("act", lambda e: e.activation(out=s_t[:], in_=g_t[:], func=AF.Sigmoid), reads=[r_g], writes=[r_s])
            if b_ == 0:
                P.op("dve", lambda e: e.tensor_tensor(out=acc[:], in0=y_t[:], in1=s_t[:], op=ALU.mult), reads=[r_y, r_s], writes=[r_acc])
            elif b_ == 1:
                P.op("dve", lambda e: e.scalar_tensor_tensor(out=m_t[:], in0=y_t[:], scalar=bpb[:, j:j + 1], in1=s_t[:], op0=ALU.add, op1=ALU.mult),
                     reads=[r_y, r_bpb, r_s], writes=[r_m])
                P.op("pool", lambda e: e.tensor_tensor(out=acc[:], in0=acc[:], in1=m_t[:], op=ALU.add), reads=[r_acc, r_m], writes=[r_acc])
            else:
                P.op("dve", lambda e: e.tensor_tensor(out=m_t[:], in0=y_t[:], in1=s_t[:], op=ALU.mult), reads=[r_y, r_s], writes=[r_m])
                P.op("dve", lambda e: e.tensor_tensor(out=mT[:, j, :], in0=acc[:], in1=m_t[:], op=ALU.add), reads=[r_acc, r_m], writes=[r_mT])
            if j == 7 and b_ == 2:
                OUT(tt)

        def OUT(tt):
            for sub in range(4):
                x_t, r_x = xt[state["xi"] % 2]
                n_t, r_n = xn[state["xi"] % 2]
                state["xi"] += 1
                r0 = tt * 512 + sub * 128
                P.dma("sp", x_t[:], x_src[r0:r0 + 128, :], writes=[r_x])
                for half in range(2):
                    o_t, r_o = po[half]
                    hs = slice(half * 512, (half + 1) * 512)
                    for c in range(8):
                        P.op("pe", lambda e: e.matmul(o_t[:], lhsT=mT[:, c, sub * 128:(sub + 1) * 128], rhs=wo[:, c, hs], start=(c == 0), stop=(c == 7)),
                             reads=[r_mT, r_wo], writes=[r_o])
                    P.op("dve", lambda e: e.tensor_tensor(out=tmp[:], in0=o_t[:], in1=gmB[:, hs], op=ALU.mult), reads=[r_o, r_gmB], writes=[r_tmp])
                    P.op("pool", lambda e: e.tensor_tensor(out=n_t[:, hs], in0=tmp[:], in1=x_t[:, hs], op=ALU.add), reads=[r_tmp, r_x], writes=[r_n])
                P.dma("sp", x1_d[r0:r0 + 128, :], n_t[:], reads=[r_n])

        nU = len(units)
        F(0)
        for u in range(nU):
            if u + 1 < nU:
                F(u + 1)
            G(u)
        st.done(f"mg{l}")

    def stage_moe(l, dst):
        TS = min(S, 2048)
        NTS = TS // 128
        st = Stage(P)
        hT, r_hT = st.sb([128, 8, TS], BF16)
        acc, r_acc_all = st.sb([128, NTS, D], F32)
        r_acc = [[Res() for _ in range(2)] for _ in range(NTS)]
        w1b = [st.sb([128, 8, FF], BF16) for _ in range(2)]
        w3b = [st.sb([128, 8, FF], BF16) for _ in range(2)]
        w2b = [st.sb([128, 2, D], BF16) for _ in range(2)]
        gB = [st.sb([128, TS], F32) for _ in range(2)]
        gfB, r_gfB = st.sb([128, D], F32)
        pa = [st.ps([128, 512], F32) for _ in range(2)]
        pb = [st.ps([128, 512], F32) for _ in range(2)]
        po = [st.ps([128, 512], F32) for _ in range(4)]
        sa = [st.sb([128, 512], F32) for _ in range(2)]
        tb = [st.sb([128, 512], F32) for _ in range(2)]
        hid = [st.sb([128, 2, 512], BF16) for _ in range(2)]
        xt = [st.sb([128, D], F32) for _ in range(2)]
        xn = [st.sb([128, D], F32) for _ in range(2)]
        P.dma("sp", gfB[:], mod_d[l, 5 * D:6 * D].partition_broadcast(128), writes=[r_gfB])
        oi = 0
        for sti in range(S // TS):
            t0 = sti * TS
            P.dma("sp", hT[:], kp(h2T_d)[:, :, t0:t0 + TS], writes=[r_hT])
            units = [(ex, tt) for ex in range(NE + 1) for tt in range(TS // 512)]
            loaded = set()

            def load_w(ex):
                if ex in loaded or ex > NE:
                    return
                loaded.add(ex)
                w1_t, r_w1 = w1b[ex % 2]
                w3_t, r_w3 = w3b[ex % 2]
                w2_t, r_w2 = w2b[ex % 2]
                g_t, r_g = gB[ex % 2]
                if ex < NE:
                    P.dma("pool", w1_t[:], kp(I["w1"][l, ex]), writes=[r_w1])
                    P.dma("pool", w3_t[:], kp(I["w3"][l, ex]), writes=[r_w3])
                    P.dma("pool", w2_t[:], kp(I["w2"][l, ex]), writes=[r_w2])
                    P.dma("sp", g_t[:], gT_d[ex, t0:t0 + TS].partition_broadcast(128), writes=[r_g])
                else:
                    P.dma("pool", w1_t[:], kp(I["ws1"][l]), writes=[r_w1])
                    P.dma("pool", w3_t[:], kp(I["ws3"][l]), writes=[r_w3])
                    P.dma("pool", w2_t[:], kp(I["ws2"][l]), writes=[r_w2])
                    P.op("dve", lambda e: e.memset(g_t[:], 1.0), writes=[r_g])

            def up(i):
                ex, tt = units[i]
                load_w(ex)
                w1_t, r_w1 = w1b[ex % 2]
                w3_t, r_w3 = w3b[ex % 2]
                g_t, r_g = gB[ex % 2]
                ts_ = slice(tt * 512, (tt + 1) * 512)
                h_t, r_h = hid[i % 2]
                for f in range(2):
                    a_t, r_a = pa[f]
                    b_t, r_b = pb[f]
                    s_t, r_s = sa[f]
                    t_t, r_t = tb[f]
                    for c in range(8):
                        P.op("pe", lambda e: e.matmul(a_t[:], lhsT=w1_t[:, c, f * 128:(f + 1) * 128], rhs=hT[:, c, ts_], start=(c == 0), stop=(c == 7)),
                             reads=[r_w1, r_hT], writes=[r_a])
                    for c in range(8):
                        P.op("pe", lambda e: e.matmul(b_t[:], lhsT=w3_t[:, c, f * 128:(f + 1) * 128], rhs=hT[:, c, ts_], start=(c == 0), stop=(c == 7)),
                             reads=[r_w3, r_hT], writes=[r_b])
                    P.op("act", lambda e: e.activation(out=s_t[:], in_=a_t[:], func=AF.Silu), reads=[r_a], writes=[r_s])
                    P.op("dve", lambda e: e.tensor_tensor(out=t_t[:], in0=b_t[:], in1=g_t[:, ts_], op=ALU.mult), reads=[r_b, r_g], writes=[r_t])
                    P.op("dve", lambda e: e.tensor_tensor(out=h_t[:, f, :], in0=s_t[:], in1=t_t[:], op=ALU.mult), reads=[r_s, r_t], writes=[r_h])

            def down(i):
                nonlocal oi
                ex, tt = units[i]
                w2_t, r_w2 = w2b[ex % 2]
                h_t, r_h = hid[i % 2]
                for sub in range(4):
                    ti = tt * 4 + sub
                    for half in range(2):
                        o_t, r_o = po[oi % 4]
                        oi += 1
                        hs = slice(half * 512, (half + 1) * 512)
                        for f in range(2):
                            P.op("pe", lambda e: e.matmul(o_t[:], lhsT=h_t[:, f, sub * 128:(sub + 1) * 128], rhs=w2_t[:, f, hs], start=(f == 0), stop=(f == 1)),
                                 reads=[r_h, r_w2], writes=[r_o])
                        if ex == 0:
                            P.op("dve", lambda e: e.tensor_copy(out=acc[:, ti, hs], in_=o_t[:]), reads=[r_o], writes=[r_acc[ti][half]])
                        else:
                            P.op("dve", lambda e: e.tensor_tensor(out=acc[:, ti, hs], in0=o_t[:], in1=acc[:, ti, hs], op=ALU.add),
                                 reads=[r_o, r_acc[ti][half]], writes=[r_acc[ti][half]])

            n = len(units)
            load_w(0)
            load_w(1)
            up(0)
            for i in range(n):
                if i + 1 < n:
                    up(i + 1)
                down(i)
                if i + 1 < n and units[i + 1][0] != units[i][0]:
                    load_w(units[i][0] + 2)
            for ti in range(NTS):
                x_t, r_x = xt[ti % 2]
                n_t, r_n = xn[ti % 2]
                r0 = t0 + ti * 128
                P.dma("sp", x_t[:], x1_d[r0:r0 + 128, :], writes=[r_x])
                P.op("dve", lambda e: e.tensor_tensor(out=n_t[:], in0=acc[:, ti, :], in1=gfB[:], op=ALU.mult), reads=[r_acc[ti][0], r_acc[ti][1], r_gfB], writes=[r_n])
                P.op("pool", lambda e: e.tensor_tensor(out=n_t[:], in0=n_t[:], in1=x_t[:], op=ALU.add), reads=[r_n, r_x], writes=[r_n])
                P.dma("sp", dst[r0:r0 + 128, :], n_t[:], reads=[r_n])
        st.done(f"moe{l}")

    def stage_route(l):
        st = Stage(P)
        sbB, r_sbB = st.sb([128, NE], F32)
        P.dma("sp", sbB[:], sbase_d.partition_broadcast(128), writes=[r_sbB])
        pos = [st.sb([128, NE], F32) for _ in range(2)]
        gat = [st.sb([128, NE], F32) for _ in range(2)]
        hrow = [st.sb([128, D], BF16) for _ in range(2)]
        a_ = [st.sb([128, NE], F32) for _ in range(2)]
        sel_ = [st.sb([128, NE], F32) for _ in range(2)]
        t8 = [st.sb([128, 8], F32) for _ in range(2)]
        si = [st.sb([128, 8], I32) for _ in range(2)]
        gk = [st.sb([128, 8], F32) for _ in range(2)]
        junk = [st.sb([128, NE], F32) for _ in range(2)]

        def chain(t):
            b = t % 2
            rows = slice(t * 128, (t + 1) * 128)
            P.dma("sp", pos[b][0][:], pos_d[rows, :], writes=[pos[b][1]])
            P.dma("sp", gat[b][0][:], gate_d[rows, :], writes=[gat[b][1]])
            P.dma("sp", hrow[b][0][:], h2_d[rows, :], writes=[hrow[b][1]])
            yield
            P.op("dve", lambda e: e.tensor_scalar(out=sel_[b][0][:], in0=gat[b][0][:], scalar1=0.0, scalar2=None, op0=ALU.is_gt),
                 reads=[gat[b][1]], writes=[sel_[b][1]])
            yield
            P.op("dve", lambda e: e.tensor_tensor(out=a_[b][0][:], in0=pos[b][0][:], in1=sbB[:], op=ALU.add), reads=[pos[b][1], r_sbB], writes=[a_[b][1]])
            yield
            P.op("dve", lambda e: e.tensor_tensor(out=a_[b][0][:], in0=a_[b][0][:], in1=sel_[b][0][:], op=ALU.mult), reads=[a_[b][1], sel_[b][1]], writes=[a_[b][1]])
            yield
            P.op("dve", lambda e: e.tensor_scalar(out=a_[b][0][:], in0=a_[b][0][:], scalar1=-1.0, scalar2=None, op0=ALU.add), reads=[a_[b][1]], writes=[a_[b][1]])
            yield
            P.op("dve", lambda e: e.max(out=t8[b][0][:], in_=a_[b][0][:]), reads=[a_[b][1]], writes=[t8[b][1]])
            yield
            P.op("dve", lambda e: e.tensor_copy(out=si[b][0][:], in_=t8[b][0][:]), reads=[t8[b][1]], writes=[si[b][1]])
            yield
            for k in range(8):
                P.op("dve", lambda e: e.scalar_tensor_tensor(out=junk[b][0][:], in0=a_[b][0][:], scalar=t8[b][0][:, k:k + 1], in1=gat[b][0][:], op0=ALU.is_equal, op1=ALU.mult),
                     reads=[a_[b][1], t8[b][1], gat[b][1]], writes=[junk[b][1]])
                yield
                P.op("dve", lambda e: e.tensor_reduce(out=gk[b][0][:, k:k + 1], in_=junk[b][0][:], axis=AX.X, op=ALU.add), reads=[junk[b][1]], writes=[gk[b][1]])
                yield
            P.dma("sp", slotk_d[rows, :], si[b][0][:], reads=[si[b][1]])
            P.dma("sp", gk_d[rows, :], gk[b][0][:], reads=[gk[b][1]])
            for k in range(8):
                def fs(eng, b=b, k=k):
                    return eng.indirect_dma_start(out=xs_d[:, :], out_offset=bass.IndirectOffsetOnAxis(ap=si[b][0][:, k:k + 1], axis=0),
                                                  in_=hrow[b][0][:, :], in_offset=None)
                P.raw("pool", fs, reads=[si[b][1], hrow[b][1]], is_dma=True)
            yield

        def drain(*gens):
            gens = list(gens)
            while gens:
                for g in list(gens):
                    try:
                        next(g)
                    except StopIteration:
                        gens.remove(g)

        for t in range(0, NT, 2):
            drain(*[chain(tt_) for tt_ in range(t, min(t + 2, NT))])
        st.done(f"route{l}")

    def stage_moe2(l):
        st = Stage(P)
        identb, r_idb = st.sb([128, 128], BF16)
        ebB, r_ebB = st.sb([128, 128], F32)
        iotac, r_iotac = st.sb([128, 1], F32)
        idxw, r_idxw = st.sb([128, 128], I32)
        w1b = [st.sb([128, 8, FF], BF16) for _ in range(3)]
        w3b = [st.sb([128, 8, FF], BF16) for _ in range(3)]
        w2b = [st.sb([128, 2, D], BF16) for _ in range(3)]
        xtok = [st.sb([128, 4, D], BF16) for _ in range(3)]
        XT = [st.sb([128, 8, 512], BF16) for _ in range(3)]
        hid = [st.sb([128, 2, 512], BF16) for _ in range(2)]
        sa = [st.sb([128, 512], F32) for _ in range(2)]
        ysb = [st.sb([128, 4, D], BF16) for _ in range(2)]
        pt = [st.ps([128, 8, 128], BF16) for _ in range(2)]
        pa = [st.ps([128, 512], F32) for _ in range(2)]
        pb = [st.ps([128, 512], F32) for _ in range(2)]
        po = [st.ps([128, 512], F32) for _ in range(2)]
        P.dma("pool", identb[:], I["k_ident"], writes=[r_idb])
        P.dma("sp", ebB[:], eb_d.partition_broadcast(128), writes=[r_ebB])
        P.dma("sp", iotac[:], col(I["k_iota"]), writes=[r_iotac])
        P.op("dve", lambda e: e.tensor_scalar(out=ebB[:], in0=ebB[:], scalar1=128.0, scalar2=float(l * NE * 128), op0=ALU.mult, op1=ALU.add),
             reads=[r_ebB], writes=[r_ebB])
        P.op("dve", lambda e: e.tensor_scalar(out=ebB[:], in0=ebB[:], scalar1=iotac[:, 0:1], scalar2=None, op0=ALU.add), reads=[r_ebB, r_iotac], writes=[r_ebB])
        P.op("dve", lambda e: e.tensor_copy(out=idxw[:], in_=ebB[:]), reads=[r_ebB], writes=[r_idxw])
        NU = NB + NQ
        state = {"oi": 0}

        def load(u):
            if u >= NU:
                return
            bf = u % 3
            if u < NB:
                for nm, (w_t, r_w) in (("w1h", w1b[bf]), ("w3h", w3b[bf]), ("w2h", w2b[bf])):
                    def fg(eng, nm=nm, w_t=w_t, u=u):
                        return eng.indirect_dma_start(out=w_t[:].rearrange("p c f -> p (c f)"), out_offset=None, in_=I[nm][:, :],
                                                      in_offset=bass.IndirectOffsetOnAxis(ap=idxw[:, u:u + 1], axis=0))
                    P.raw("pool", fg, reads=[r_idxw], writes=[r_w], is_dma=True)
                P.dma("sp", xtok[bf][0][:], xs_d[u * 512:(u + 1) * 512, :].rearrange("(s p) d -> p s d", p=128), writes=[xtok[bf][1]])
            else:
                if u in (NB, NB + 1, NB + 2):
                    P.dma("pool", w1b[bf][0][:], kp(I["ws1"][l]), writes=[w1b[bf][1]])
                    P.dma("pool", w3b[bf][0][:], kp(I["ws3"][l]), writes=[w3b[bf][1]])
                    P.dma("pool", w2b[bf][0][:], kp(I["ws2"][l]), writes=[w2b[bf][1]])
                tt = u - NB
                P.dma("sp", XT[bf][0][:], kp(h2T_d)[:, :, tt * 512:(tt + 1) * 512], writes=[XT[bf][1]])

        def tr(u):
            if u >= NB:
                return
            bf = u % 3
            x_t, r_x = xtok[bf]
            X_t, r_X = XT[bf]
            for sub in range(4):
                p_t, r_p = pt[sub % 2]
                for c in range(8):
                    P.op("pe", lambda e: e.transpose(out=p_t[:, c, :], in_=x_t[:, sub, c * 128:(c + 1) * 128], identity=identb[:]),
                         reads=[r_x, r_idb], writes=[r_p])
                if sub % 2 == 0:
                    P.op("act", lambda e: e.copy(out=X_t[:, :, sub * 128:(sub + 1) * 128], in_=p_t[:]), reads=[r_p], writes=[r_X])
                else:
                    P.op("dve", lambda e: e.tensor_copy(out=X_t[:, :, sub * 128:(sub + 1) * 128], in_=p_t[:]), reads=[r_p], writes=[r_X])

        def up(u):
            bf = u % 3
            w1_t, r_w1 = w1b[bf]
            w3_t, r_w3 = w3b[bf]
            X_t, r_X = XT[bf]
            h_t, r_h = hid[u % 2]
            for f in range(2):
                a_t, r_a = pa[f]
                b_t, r_b = pb[f]
                s_t, r_s = sa[f]
                for c in range(8):
                    P.op("pe", lambda e: e.matmul(a_t[:], lhsT=w1_t[:, c, f * 128:(f + 1) * 128], rhs=X_t[:, c, :], start=(c == 0), stop=(c == 7)),
                         reads=[r_w1, r_X], writes=[r_a])
                for c in range(8):
                    P.op("pe", lambda e: e.matmul(b_t[:], lhsT=w3_t[:, c, f * 128:(f + 1) * 128], rhs=X_t[:, c, :], start=(c == 0), stop=(c == 7)),
                         reads=[r_w3, r_X], writes=[r_b])
                P.op("act", lambda e: e.activation(out=s_t[:], in_=a_t[:], func=AF.Silu), reads=[r_a], writes=[r_s])
                P.op("dve", lambda e: e.tensor_tensor(out=h_t[:, f, :], in0=b_t[:], in1=s_t[:], op=ALU.mult), reads=[r_b, r_s], writes=[r_h])

        def down(u):
            w2_t, r_w2 = w2b[u % 3]
            h_t, r_h = hid[u % 2]
            y_t, r_y = ysb[u % 2]
            for sub in range(4):
                for half in range(2):
                    o_t, r_o = po[state["oi"] % 2]
                    state["oi"] += 1
                    hs = slice(half * 512, (half + 1) * 512)
                    for f in range(2):
                        P.op("pe", lambda e: e.matmul(o_t[:], lhsT=h_t[:, f, sub * 128:(sub + 1) * 128], rhs=w2_t[:, f, hs], start=(f == 0), stop=(f == 1)),
                             reads=[r_h, r_w2], writes=[r_o])
                    if half == 0:
                        P.op("act", lambda e: e.copy(out=y_t[:, sub, hs], in_=o_t[:]), reads=[r_o], writes=[r_y])
                    else:
                        P.op("dve", lambda e: e.tensor_copy(out=y_t[:, sub, hs], in_=o_t[:]), reads=[r_o], writes=[r_y])
            if u < NB:
                P.dma("sp", ys_d[u * 512:(u + 1) * 512, :].rearrange("(s p) d -> p s d", p=128), y_t[:], reads=[r_y])
            else:
                tt = u - NB
                P.dma("sp", ysh_d[tt * 512:(tt + 1) * 512, :].rearrange("(s p) d -> p s d", p=128), y_t[:], reads=[r_y])

        load(0)
        load(1)
        tr(0)
        up(0)
        for u in range(NU):
            load(u + 2)
            if u + 1 < NU:
                tr(u + 1)
                up(u + 1)
            down(u)
        st.done(f"moe{l}")

    def stage_comb(l, dst):
        st = Stage(P)
        gfB, r_gfB = st.sb([128, D], F32)
        P.dma("sp", gfB[:], mod_d[l, 5 * D:6 * D].partition_broadcast(128), writes=[r_gfB])
        si = [st.sb([128, 8], I32) for _ in range(2)]
        gk = [st.sb([128, 8], F32) for _ in range(2)]
        xt = [st.sb([128, D], F32) for _ in range(2)]
        ysh = [st.sb([128, D], BF16) for _ in range(2)]
        yg = [[st.sb([128, D], BF16) for _ in range(8)] for _ in range(2)]
        acc = [st.sb([128, D], F32) for _ in range(2)]
        def fetch(t):
            if t >= NT:
                return
            b = t % 2
            rows = slice(t * 128, (t + 1) * 128)
            P.dma("sp", si[b][0][:], slotk_d[rows, :], writes=[si[b][1]])
            P.dma("sp", gk[b][0][:], gk_d[rows, :], writes=[gk[b][1]])
            P.dma("sp", xt[b][0][:], x1_d[rows, :], writes=[xt[b][1]])
            P.dma("sp", ysh[b][0][:], ysh_d[rows, :], writes=[ysh[b][1]])
            for k in range(8):
                def fg(eng, b=b, k=k):
                    return eng.indirect_dma_start(out=yg[b][k][0][:, :], out_offset=None, in_=ys_d[:, :],
                                                  in_offset=bass.IndirectOffsetOnAxis(ap=si[b][0][:, k:k + 1], axis=0))
                P.raw("pool", fg, reads=[si[b][1]], writes=[yg[b][k][1]], is_dma=True)

        def comp(t):
            b = t % 2
            rows = slice(t * 128, (t + 1) * 128)
            a_t, r_a = acc[b]
            P.op("dve", lambda e: e.scalar_tensor_tensor(out=a_t[:], in0=yg[b][0][0][:], scalar=gk[b][0][:, 0:1], in1=ysh[b][0][:], op0=ALU.mult, op1=ALU.add),
                 reads=[yg[b][0][1], gk[b][1], ysh[b][1]], writes=[r_a])
            for k in range(1, 8):
                P.op("dve", lambda e: e.scalar_tensor_tensor(out=a_t[:], in0=yg[b][k][0][:], scalar=gk[b][0][:, k:k + 1], in1=a_t[:], op0=ALU.mult, op1=ALU.add),
                     reads=[yg[b][k][1], gk[b][1], r_a], writes=[r_a])
            P.op("dve", lambda e: e.tensor_tensor(out=a_t[:], in0=a_t[:], in1=gfB[:], op=ALU.mult), reads=[r_a, r_gfB], writes=[r_a])
            P.op("dve", lambda e: e.tensor_tensor(out=a_t[:], in0=a_t[:], in1=xt[b][0][:], op=ALU.add), reads=[r_a, xt[b][1]], writes=[r_a])
            P.dma("sp", dst[rows, :], a_t[:], reads=[r_a])

        fetch(0)
        for t in range(NT):
            comp_deferred = t
            fetch(t + 1)
            comp(t)
        st.done(f"comb{l}")

    todo = stages if stages is not None else ("mod", "rope", "norm1", "proj")
    if "mod" in todo:
        stage_mod()
    if "rope" in todo:
        stage_rope()
    for l in range(L):
        x_in = I["x"] if l == 0 else x2_d
        if "norm1" in todo:
            stage_norm(l, x_in, "norm_mix_g", 1 * D, 0 * D, hT_d, router=False)
        if "proj" in todo:
            stage_proj(l)
        if "da" in todo:
            stage_da(l)
        if "sb" in todo:
            stage_sb(l)
        if "cv" in todo:
            stage_cv(l)
        if "mg" in todo:
            stage_mg(l, x_in)
        if "norm2" in todo:
            stage_norm(l, x1_d, "norm_ffn_g", 4 * D, 3 * D, h2T_d, router=True)
        if "moe" in todo:
            stage_moe(l, out if l == L - 1 else x2_d)
        if "smoe" in todo:
            stage_route(l)
            stage_moe2(l)
            stage_comb(l, out if l == L - 1 else x2_d)
    es.close()
    return nc


ALL_STAGES = ("mod", "rope", "norm1", "proj", "da", "sb", "cv", "mg", "norm2", "smoe")
_CACHE = {}


def kernel(**inputs):
    x = np.ascontiguousarray(np.asarray(inputs["x"], dtype=np.float32))
    B, S, _ = x.shape
    L = int(np.asarray(inputs["w_mod"]).shape[0])
    key = (S, L)
    if key not in _CACHE:
        _CACHE[key] = build(S, L=L, stages=ALL_STAGES)
    nc = _CACHE[key]
    consts = make_consts()
    shared = {k: np.ascontiguousarray(np.asarray(inputs[k], dtype=np.float32)) for k in W_SHAPES if k not in ("w1", "w3", "w2")}
    shared.update(relayout_experts(inputs, L))
    c = np.asarray(inputs["c"], dtype=np.float32)
    pos = np.asarray(inputs["positions"]).astype(np.int32)
    in_maps = []
    for b in range(B):
        m = {"x": x[b], "c": np.ascontiguousarray(c[b]), "pos": np.ascontiguousarray(pos[b])}
        m.update(shared)
        m.update(consts)
        in_maps.append(m)
    res = run_bass_kernel_spmd(nc, in_maps, core_ids=list(range(B)))
    return np.stack([np.asarray(r["out"], dtype=np.float32) for r in res.results], axis=0)


def relayout_experts(W, L):
    o = {}
    w1 = np.asarray(W["w1"], dtype=np.float32)[:L]
    w3 = np.asarray(W["w3"], dtype=np.float32)[:L]
    w2 = np.asarray(W["w2"], dtype=np.float32)[:L]
    o["w1h"] = np.ascontiguousarray(w1.reshape(L, NE, 8, 128, FF).transpose(0, 1, 3, 2, 4)).reshape(L * NE * 128, 8 * FF)
    o["w3h"] = np.ascontiguousarray(w3.reshape(L, NE, 8, 128, FF).transpose(0, 1, 3, 2, 4)).reshape(L * NE * 128, 8 * FF)
    o["w2h"] = np.ascontiguousarray(w2.reshape(L, NE, 2, 128, D).transpose(0, 1, 3, 2, 4)).reshape(L * NE * 128, 2 * D)
    return o
```

```python
import math
from contextlib import ExitStack
import numpy as np
import concourse.bass as bass
import concourse.mybir as mybir
from concourse.bass_utils import run_bass_kernel_spmd

F32 = mybir.dt.float32
BF16 = mybir.dt.bfloat16
I32 = mybir.dt.int32
ALU = mybir.AluOpType
AF = mybir.ActivationFunctionType
AX = mybir.AxisListType

ENGS = ("pe", "act", "dve", "pool", "sp")
SEM_LIM = 30000
DMA_RING = 8

D = 1024
NE = 64
FF = 256
EPS = 1e-6


class Res:
    __slots__ = ("w", "r")

    def __init__(self):
        self.w = None
        self.r = []


class Op:
    __slots__ = ("eng", "fn", "deps", "signal", "k", "is_dma", "slot", "dval")

    def __init__(self, eng, fn, is_dma=False):
        self.eng = eng
        self.fn = fn
        self.deps = set()
        self.signal = False
        self.k = None
        self.is_dma = is_dma
        self.slot = None
        self.dval = None


class _Rec:
    def __getattr__(self, name):
        return lambda *a, **k: (name, a, k)


_REC = _Rec()


class Prog:
    def __init__(self, nc, es):
        self.nc = nc
        self.sems = {e: [es.enter_context(nc.semaphore(f"s_{e}_{i}")) for i in range(6)] for e in ENGS if e != "sp"}
        self.nsig = {e: 0 for e in ENGS}
        self.dq = ("sp", "act", "pool")
        self.dsems = {e: [es.enter_context(nc.semaphore(f"d_{e}_{i}")) for i in range(DMA_RING)] for e in self.dq}
        self.ndma = {e: 0 for e in self.dq}
        self.ring_last = {e: [None] * DMA_RING for e in self.dq}
        self.waited = {e: {} for e in ENGS}
        self.ops = []
        self.last_op = {e: None for e in ENGS}
        self.stage_dmas = []

    def _add(self, op, reads, writes):
        deps = set()
        for r in reads:
            if r.w is not None:
                deps.add(r.w)
        for w in writes:
            if w.w is not None:
                deps.add(w.w)
            for o in w.r:
                deps.add(o)
        for d in deps:
            if d is op:
                continue
            if (not d.is_dma) and d.eng == op.eng and op.eng == "pe" and not op.is_dma:
                continue
            op.deps.add(d)
            if not d.is_dma:
                d.signal = True
        for r in reads:
            r.r.append(op)
        for w in writes:
            w.w = op
            w.r = []
        self.ops.append(op)
        if not op.is_dma:
            self.last_op[op.eng] = op
        return op

    def op(self, eng, fn, reads=(), writes=()):
        return self._add(Op(eng, fn(_REC)), reads, writes)

    def dma(self, q, out, in_, reads=(), writes=()):
        op = Op(q, ("dma_start", (), dict(out=out, in_=in_)), is_dma=True)
        i = self.ndma[q]
        self.ndma[q] += 1
        op.slot = i % DMA_RING
        op.dval = 16 * (i // DMA_RING + 1)
        prev = self.ring_last[q][op.slot]
        if prev is not None:
            op.deps.add(prev)
        self.ring_last[q][op.slot] = op
        self.stage_dmas.append(op)
        return self._add(op, reads, writes)

    def raw(self, eng, fn, reads=(), writes=(), is_dma=False):
        op = Op(eng, fn, is_dma=is_dma)
        if is_dma:
            i = self.ndma[eng]
            self.ndma[eng] += 1
            op.slot = i % DMA_RING
            op.dval = 16 * (i // DMA_RING + 1)
            prev = self.ring_last[eng][op.slot]
            if prev is not None:
                op.deps.add(prev)
            self.ring_last[eng][op.slot] = op
            self.stage_dmas.append(op)
        return self._add(op, reads, writes)

    def barrier(self):
        lasts = [o for o in self.last_op.values() if o is not None]
        dmas = list(self.stage_dmas)
        for e in ENGS:
            op = Op(e, None)
            for d in lasts:
                if d.eng != e:
                    op.deps.add(d)
                    d.signal = True
            for d in dmas:
                op.deps.add(d)
            self.ops.append(op)
        self.stage_dmas = []
        self.last_op = {e: None for e in ENGS}

    def emit(self):
        nc = self.nc
        self.barrier()
        ops = self.ops
        self.ops = []
        for o in ops:
            if o.signal and not o.is_dma and o.k is None:
                o.k = self.nsig[o.eng]
                self.nsig[o.eng] += 1
        per = {e: [o for o in ops if o.eng == e] for e in ENGS}
        engobj = {"pe": "tensor", "act": "scalar", "dve": "vector", "pool": "gpsimd", "sp": "sync"}

        def run(e, eng):
            waited = self.waited[e]
            for o in per[e]:
                need = {}
                for d in o.deps:
                    if d.is_dma:
                        key = ("d", d.eng, d.slot)
                        sem = self.dsems[d.eng][d.slot]
                        val = d.dval
                    else:
                        key = ("c", d.eng, d.k // SEM_LIM)
                        sem = self.sems[d.eng][d.k // SEM_LIM]
                        val = d.k % SEM_LIM + 1
                    if waited.get(key, 0) >= val:
                        continue
                    if key not in need or need[key][1] < val:
                        need[key] = (sem, val)
                for key, (sem, val) in need.items():
                    eng.wait_ge(sem, val)
                    waited[key] = val
                if o.fn is None:
                    continue
                if callable(o.fn):
                    ins = o.fn(eng)
                else:
                    name, a, k = o.fn
                    ins = getattr(eng, name)(*a, **k)
                if o.is_dma:
                    ins.then_inc(self.dsems[o.eng][o.slot], 16)
                elif o.signal:
                    ins.then_inc(self.sems[o.eng][o.k // SEM_LIM], 1)

        with nc.allow_non_contiguous_dma(reason="small strided parameter loads"):
            with nc.Block() as block:
                for e in ENGS:
                    if not per[e]:
                        continue
                    getattr(block, engobj[e])(lambda eng, e=e: run(e, eng))


class Stage:
    count = 0

    def __init__(self, P):
        self.P = P
        self.nc = P.nc
        self.es = ExitStack()
        self.n = 0
        Stage.count += 1
        self.sid = Stage.count

    def sb(self, shape, dt):
        self.n += 1
        t = self.es.enter_context(self.nc.sbuf_tensor(f"t{self.sid}_{self.n}", list(shape), dt))
        return t, Res()

    def ps(self, shape, dt):
        self.n += 1
        t = self.es.enter_context(self.nc.psum_tensor(f"p{self.sid}_{self.n}", list(shape), dt))
        return t, Res()

    def done(self, name=None):
        if name:
            with self.nc.named_scope(name):
                self.P.emit()
        else:
            self.P.emit()
        self.es.close()


def col(ap1d):
    return ap1d.rearrange("(p o) -> p o", o=1)


def make_consts():
    p = np.arange(128)
    ident = np.eye(128, dtype=np.float32)
    blk64 = ((p[:, None] // 64) == (p[None, :] // 64)).astype(np.float32) / 64.0
    rotT = np.zeros((128, 128), np.float32)
    for k in range(128):
        if k % 64 >= 32:
            rotT[k, k - 32] = -1.0
        else:
            rotT[k, k + 32] = 1.0
    ones = np.ones((128, 128), np.float32)
    ustrict = (p[:, None] > p[None, :]).astype(np.float32)
    uincl = (p[:, None] >= p[None, :]).astype(np.float32)
    ulow = (p[:, None] < p[None, :]).astype(np.float32)
    pincl = (p[:, None] <= p[None, :]).astype(np.float32)
    iota = p.astype(np.float32)
    qq = np.arange(512)
    maskc = np.stack([((qq[None, :] - j * 128 - p[:, None]) >= 0) for j in range(4)]).astype(np.float32)
    masks = np.stack([((qq[None, :] - j * 128 - p[:, None]) > 0) for j in range(4)]).astype(np.float32)
    inv = (10000.0 ** (-np.arange(0, 64, 2, dtype=np.float32) / np.float32(64))).astype(np.float32)
    invc = inv[p % 32].astype(np.float32)
    return dict(k_ident=ident, k_blk64=blk64, k_rotT=rotT, k_ones=ones, k_ustrict=ustrict, k_uincl=uincl, k_ulow=ulow, k_pincl=pincl, k_iota=iota,
                k_maskc=maskc, k_masks=masks, k_invc=invc)


W_SHAPES = dict(
    w_mod=(D, 6 * D), b_mod=(6 * D,), norm_mix_g=(D,), norm_ffn_g=(D,), w_in=(D, 7168),
    qn_g=(64,), kn_g=(64,), lam_q1=(64,), lam_k1=(64,), lam_q2=(64,), lam_k2=(64,), subln_g=(128,),
    w_proj_a=(512, D), w_dw=(31, 512), b_dw=(512,), conv_ln_g=(512,), conv_ln_b=(512,),
    w_proj_b=(512, D), b_proj_b=(D,), w_proj_c=(512, D), w_out=(D, D), w_router=(D, NE), b_router=(NE,),
    w1=(NE, D, FF), w3=(NE, D, FF), w2=(NE, FF, D), ws1=(D, FF), ws3=(D, FF), ws2=(FF, D),
)


def build(S, L=2, dbg=(), stages=None):
    NT = S // 128
    NQ = S // 512
    nc = bass.Bass("TRN2", target_bir_lowering=False)
    I = {}
    I["x"] = nc.dram_tensor("x", [S, D], F32, kind="ExternalInput").ap()
    I["c"] = nc.dram_tensor("c", [D], F32, kind="ExternalInput").ap()
    I["pos"] = nc.dram_tensor("pos", [S], I32, kind="ExternalInput").ap()
    dense_moe = stages is not None and "moe" in stages
    for k, shp in W_SHAPES.items():
        if k in ("w1", "w3", "w2") and not dense_moe:
            continue
        I[k] = nc.dram_tensor(k, [L] + list(shp), F32, kind="ExternalInput").ap()
    for k, v in make_consts().items():
        I[k] = nc.dram_tensor(k, list(v.shape), F32, kind="ExternalInput").ap()
    for k in ("w1h", "w3h", "w2h"):
        I[k] = nc.dram_tensor(k, [L * NE * 128, 2048], F32, kind="ExternalInput").ap()
    out = nc.dram_tensor("out", [S, D], F32, kind="ExternalOutput").ap()
    NB = S * 8 // 512 + NE
    NSLOT = NB * 512

    def scratch(name, shape, dt):
        kind = "ExternalOutput" if name in dbg else "Internal"
        return nc.dram_tensor(name, list(shape), dt, kind=kind).ap()

    mod_d = scratch("mod_d", [L, 6 * D], F32)
    cos_d = scratch("cos_d", [128, S], F32)
    sin_d = scratch("sin_d", [128, S], F32)
    hT_d = scratch("hT_d", [D, S], BF16)
    qaT_d = scratch("qaT_d", [512, S], BF16)
    kaT_d = scratch("kaT_d", [512, S], BF16)
    va_d = scratch("va_d", [S, 512], BF16)
    uT_d = scratch("uT_d", [512, S], BF16)
    qcT_d = scratch("qcT_d", [512, S], BF16)
    kcT_d = scratch("kcT_d", [512, S], BF16)
    vc_d = scratch("vc_d", [S, 512], BF16)
    oaT_d = scratch("oaT_d", [512, S], BF16)
    cvT_d = scratch("cvT_d", [512, S], BF16)
    ocT_d = scratch("ocT_d", [512, S], BF16)
    x1_d = scratch("x1_d", [S, D], F32)
    x2_d = scratch("x2_d", [S, D], F32)
    h2T_d = scratch("h2T_d", [D, S], BF16)
    gT_d = scratch("gT_d", [NE, S], F32)
    gate_d = scratch("gate_d", [S, NE], F32)
    pos_d = scratch("pos_d", [S, NE], F32)
    h2_d = scratch("h2_d", [S, D], BF16)
    sbase_d = scratch("sbase_d", [NE], F32)
    eb_d = scratch("eb_d", [128], F32)
    slotk_d = scratch("slotk_d", [S, 8], I32)
    gk_d = scratch("gk_d", [S, 8], F32)
    xs_d = scratch("xs_d", [NSLOT, D], BF16)
    ys_d = scratch("ys_d", [NSLOT, D], BF16)
    ysh_d = scratch("ysh_d", [S, D], BF16)

    def kp(ap2d):
        return ap2d.rearrange("(c p) n -> p c n", p=128)

    es = ExitStack()
    P = Prog(nc, es)

    def stage_mod():
        st = Stage(P)
        cT, r_cT = st.sb([128, 8], F32)
        cA, r_cA = st.sb([128, 8], F32)
        wm = [st.sb([128, 8, 512], F32) for _ in range(2)]
        bm, r_bm = st.sb([1, L * 6 * D], F32)
        mr, r_mr = st.sb([1, L * 6 * D], F32)
        pm = [st.ps([1, 512], F32) for _ in range(2)]
        P.dma("sp", cT[:], I["c"].rearrange("(c p) -> p c", p=128), writes=[r_cT])
        P.dma("sp", bm[:], I["b_mod"].rearrange("(o l) n -> o (l n)", o=1), writes=[r_bm])
        P.op("act", lambda e: e.activation(out=cA[:], in_=cT[:], func=AF.Silu), reads=[r_cT], writes=[r_cA])
        i = 0
        for l in range(L):
            for blk in range(12):
                w_t, r_w = wm[i % 2]
                p_t, r_p = pm[i % 2]
                P.dma("sp", w_t[:], kp(I["w_mod"][l])[:, :, blk * 512:(blk + 1) * 512], writes=[r_w])
                for c in range(8):
                    P.op("pe", lambda e, c=c, w_t=w_t, p_t=p_t: e.matmul(p_t[:], lhsT=cA[:, c:c + 1], rhs=w_t[:, c, :], start=(c == 0), stop=(c == 7)),
                         reads=[r_cA, r_w], writes=[r_p])
                o = l * 6 * D + blk * 512
                P.op("dve", lambda e, o=o, p_t=p_t: e.tensor_tensor(out=mr[:, o:o + 512], in0=p_t[:], in1=bm[:, o:o + 512], op=ALU.add),
                     reads=[r_p, r_bm], writes=[r_mr])
                i += 1
        P.dma("sp", mod_d.rearrange("(o l) n -> o (l n)", o=1), mr[:], reads=[r_mr])
        st.done("mod")

    def stage_rope():
        st = Stage(P)
        pi_t, r_pi = st.sb([128, S], I32)
        pf, r_pf = st.sb([128, S], F32)
        inv, r_inv = st.sb([128, 1], F32)
        ang, r_ang = st.sb([128, S], F32)
        u, r_u = st.sb([128, S], F32)
        ki, r_ki = st.sb([128, S], I32)
        kf, r_kf = st.sb([128, S], F32)
        m, r_m = st.sb([128, S], F32)
        res_t, r_res = st.sb([128, S], F32)
        zero, r_zero = st.sb([128, 1], F32)
        TWO_PI = 2.0 * math.pi
        C1 = 6.28125
        C2 = TWO_PI - C1
        P.dma("sp", pi_t[:], I["pos"].partition_broadcast(128), writes=[r_pi])
        P.dma("sp", inv[:], col(I["k_invc"]), writes=[r_inv])
        P.op("pool", lambda e: e.memset(zero[:], 0.0), writes=[r_zero])
        P.op("dve", lambda e: e.tensor_copy(out=pf[:], in_=pi_t[:]), reads=[r_pi], writes=[r_pf])
        for which, dst in ((0, sin_d), (1, cos_d)):
            shift = 0.0 if which == 0 else math.pi / 2
            P.op("dve", lambda e, shift=shift: e.tensor_scalar(out=ang[:], in0=pf[:], scalar1=inv[:, 0:1], scalar2=shift, op0=ALU.mult, op1=ALU.add),
                 reads=[r_pf, r_inv], writes=[r_ang])
            P.op("dve", lambda e: e.tensor_scalar(out=u[:], in0=ang[:], scalar1=1.0 / TWO_PI, scalar2=None, op0=ALU.mult),
                 reads=[r_ang], writes=[r_u])
            P.op("dve", lambda e: e.tensor_copy(out=ki[:], in_=u[:]), reads=[r_u], writes=[r_ki])
            P.op("dve", lambda e: e.tensor_copy(out=kf[:], in_=ki[:]), reads=[r_ki], writes=[r_kf])
            P.op("dve", lambda e: e.scalar_tensor_tensor(out=u[:], in0=kf[:], scalar=-C1, in1=ang[:], op0=ALU.mult, op1=ALU.add),
                 reads=[r_kf, r_ang], writes=[r_u])
            P.op("dve", lambda e: e.scalar_tensor_tensor(out=u[:], in0=kf[:], scalar=-C2, in1=u[:], op0=ALU.mult, op1=ALU.add),
                 reads=[r_kf, r_u], writes=[r_u])
            P.op("dve", lambda e: e.tensor_scalar(out=m[:], in0=u[:], scalar1=math.pi, scalar2=-TWO_PI, op0=ALU.is_gt, op1=ALU.mult),
                 reads=[r_u], writes=[r_m])
            P.op("dve", lambda e: e.tensor_tensor(out=u[:], in0=u[:], in1=m[:], op=ALU.add), reads=[r_u, r_m], writes=[r_u])
            P.op("dve", lambda e: e.tensor_scalar(out=m[:], in0=u[:], scalar1=-math.pi, scalar2=TWO_PI, op0=ALU.is_lt, op1=ALU.mult),
                 reads=[r_u], writes=[r_m])
            P.op("dve", lambda e: e.tensor_tensor(out=u[:], in0=u[:], in1=m[:], op=ALU.add), reads=[r_u, r_m], writes=[r_u])
            P.op("act", lambda e: e.activation(out=res_t[:], in_=u[:], func=AF.Sin, bias=zero[:, 0:1]), reads=[r_u, r_zero], writes=[r_res])
            P.dma("sp", dst, res_t[:], reads=[r_res])
        st.done("rope")

    def stage_norm(l, x_src, g_name, sc_off, sh_off, hT_dst, router):
        st = Stage(P)
        gB, r_gB = st.sb([128, D], F32)
        scB, r_scB = st.sb([128, D], F32)
        shB, r_shB = st.sb([128, D], F32)
        A, r_A = st.sb([128, D], F32)
        epsb, r_eps = st.sb([128, 1], F32)
        identb, r_idb = st.sb([128, 128], BF16)
        xt = [st.sb([128, D], F32) for _ in range(4)]
        sq, r_sq = st.sb([128, D], F32)
        ss, r_ss = st.sb([128, 1], F32)
        rstd, r_rstd = st.sb([128, 1], F32)
        hf, r_hf = st.sb([128, D], F32)
        hb, r_hb = st.sb([128, D], BF16)
        hTs = [st.sb([128, 8, 128], BF16) for _ in range(2)]
        pt = [st.ps([128, 8, 128], BF16) for _ in range(2)]
        P.dma("sp", gB[:], I[g_name][l].partition_broadcast(128), writes=[r_gB])
        P.dma("sp", scB[:], mod_d[l, sc_off:sc_off + D].partition_broadcast(128), writes=[r_scB])
        P.dma("sp", shB[:], mod_d[l, sh_off:sh_off + D].partition_broadcast(128), writes=[r_shB])
        P.dma("pool", identb[:], I["k_ident"], writes=[r_idb])
        P.op("pool", lambda e: e.memset(epsb[:], EPS), writes=[r_eps])
        P.op("dve", lambda e: e.scalar_tensor_tensor(out=A[:], in0=scB[:], scalar=1.0, in1=gB[:], op0=ALU.add, op1=ALU.mult),
             reads=[r_scB, r_gB], writes=[r_A])
        if router:
            identf, r_idf = st.sb([128, 128], F32)
            wr, r_wr = st.sb([128, 8, NE], F32)
            brB, r_brB = st.sb([128, NE], F32)
            h32, r_h32 = st.sb([128, D], F32)
            hTf, r_hTf = st.sb([128, 8, 128], F32)
            ptf = [st.ps([128, 4, 128], F32) for _ in range(2)]
            plg, r_plg = st.ps([128, NE], F32)
            PC, r_PC = st.ps([128, NE], F32)
            pinc, r_pinc = st.sb([128, 128], F32)
            pgtm, r_pgtm = st.sb([128, 128], F32)
            iotac, r_iotac = st.sb([128, 1], F32)
            posb = [st.sb([128, NE], F32) for _ in range(4)]
            selb = [st.sb([128, NE], F32) for _ in range(4)]
            P.dma("sp", pinc[:], I["k_pincl"], writes=[r_pinc])
            P.dma("sp", pgtm[:], I["k_ustrict"], writes=[r_pgtm])
            P.dma("sp", iotac[:], col(I["k_iota"]), writes=[r_iotac])
            sc_t, r_sc = st.sb([128, NE], F32)
            bi, r_bi = st.sb([128, NE], F32)
            tmp, r_tmp = st.sb([128, NE], F32)
            m1, r_m1 = st.sb([128, 8], F32)
            m2, r_m2 = st.sb([128, 8], F32)
            gs, r_gs = st.sb([128, 8], F32)
            t8, r_t8 = st.sb([128, 8], F32)
            pen, r_pen = st.sb([128, 8], F32)
            sel, r_sel = st.sb([128, NE], F32)
            ssum, r_ssum = st.sb([128, 1], F32)
            gate, r_gate = st.sb([128, NE], F32)
            gTs, r_gTs = st.sb([NE, 128], F32)
            P.dma("sp", identf[:], I["k_ident"], writes=[r_idf])
            P.dma("sp", wr[:], kp(I["w_router"][l]), writes=[r_wr])
            P.dma("sp", brB[:], I["b_router"][l].partition_broadcast(128), writes=[r_brB])
        sq2 = [(sq, r_sq)] + [st.sb([128, D], F32) for _ in range(3)]
        ss2 = [(ss, r_ss)] + [st.sb([128, 1], F32) for _ in range(3)]
        rstd2 = [(rstd, r_rstd)] + [st.sb([128, 1], F32) for _ in range(3)]
        hf2 = [(hf, r_hf)] + [st.sb([128, D], F32) for _ in range(3)]
        hb2 = [(hb, r_hb)] + [st.sb([128, D], BF16) for _ in range(3)]
        if router:
            h322 = [(h32, r_h32)] + [st.sb([128, D], F32) for _ in range(3)]
            dup_sc = [(sc_t, r_sc), st.sb([128, NE], F32)]
            dup_bi = [(bi, r_bi), st.sb([128, NE], F32)]
            dup_tmp = [(tmp, r_tmp), st.sb([128, NE], F32)]
            dup_m1 = [(m1, r_m1), st.sb([128, 8], F32)]
            dup_m2 = [(m2, r_m2), st.sb([128, 8], F32)]
            dup_gs = [(gs, r_gs), st.sb([128, 8], F32)]
            dup_t8 = [(t8, r_t8), st.sb([128, 8], F32)]
            dup_pen = [(pen, r_pen), st.sb([128, 8], F32)]
            dup_sel = [(sel, r_sel), st.sb([128, NE], F32)]
            dup_ssum = [(ssum, r_ssum), st.sb([128, 1], F32)]
            dup_gate = [(gate, r_gate), st.sb([128, NE], F32)]
            dup_gTs = [(gTs, r_gTs), st.sb([NE, 128], F32)]
            dup_plg = [(plg, r_plg), st.ps([128, NE], F32)]
            plg2 = dup_plg
            onec, r_onec = st.sb([128, 1], F32)
            P.op("pool", lambda e: e.memset(onec[:], 1.0), writes=[r_onec])

        def ph1(t):
            x_t, r_x = xt[t % 4]
            sq_t, r_sq_ = sq2[t % 4]
            ss_t, r_ss_ = ss2[t % 4]
            rs_t, r_rs_ = rstd2[t % 4]
            hf_t, r_hf_ = hf2[t % 4]
            hb_t, r_hb_ = hb2[t % 4]
            P.dma("sp", x_t[:], x_src[t * 128:(t + 1) * 128, :], writes=[r_x])
            P.op("act", lambda e: e.activation(out=sq_t[:], in_=x_t[:], func=AF.Square, scale=float(D ** -0.5), accum_out=ss_t[:]),
                 reads=[r_x], writes=[r_sq_, r_ss_])
            P.op("act", lambda e: e.activation(out=rs_t[:], in_=ss_t[:], func=AF.Ln, bias=epsb[:, 0:1]), reads=[r_ss_, r_eps], writes=[r_rs_])
            P.op("act", lambda e: e.activation(out=rs_t[:], in_=rs_t[:], func=AF.Exp, scale=-0.5), reads=[r_rs_], writes=[r_rs_])
            P.op("dve", lambda e: e.scalar_tensor_tensor(out=hf_t[:], in0=x_t[:], scalar=rs_t[:, 0:1], in1=A[:], op0=ALU.mult, op1=ALU.mult),
                 reads=[r_x, r_rs_, r_A], writes=[r_hf_])
            P.op("pool", lambda e: e.tensor_tensor(out=hb_t[:], in0=hf_t[:], in1=shB[:], op=ALU.add), reads=[r_hf_, r_shB], writes=[r_hb_])
            if router:
                h32_t, r_h32_ = h322[t % 4]
                P.op("dve", lambda e: e.tensor_tensor(out=h32_t[:], in0=hf_t[:], in1=shB[:], op=ALU.add), reads=[r_hf_, r_shB], writes=[r_h32_])

        def ph2a(t):
            hb_t, r_hb_ = hb2[t % 4]
            p_t, r_p = pt[t % 2]
            h_t, r_h = hTs[t % 2]
            for c in range(8):
                P.op("pe", lambda e: e.transpose(out=p_t[:, c, :], in_=hb_t[:, c * 128:(c + 1) * 128], identity=identb[:]),
                     reads=[r_hb_, r_idb], writes=[r_p])
            P.op("act", lambda e: e.copy(out=h_t[:], in_=p_t[:]), reads=[r_p], writes=[r_h])
            P.dma("sp", kp(hT_dst)[:, :, t * 128:(t + 1) * 128], h_t[:], reads=[r_h])
            if router:
                P.dma("sp", h2_d[t * 128:(t + 1) * 128, :], hb_t[:], reads=[r_hb_])
                plg, r_plg = plg2[t % 2]
                h32_t, r_h32_ = h322[t % 4]
                for half in range(2):
                    pf_t, r_pf = ptf[half]
                    for c in range(4):
                        cc = half * 4 + c
                        P.op("pe", lambda e: e.transpose(out=pf_t[:, c, :], in_=h32_t[:, cc * 128:(cc + 1) * 128], identity=identf[:]),
                             reads=[r_h32_, r_idf], writes=[r_pf])
                    P.op("dve", lambda e: e.tensor_copy(out=hTf[:, half * 4:(half + 1) * 4, :], in_=pf_t[:]), reads=[r_pf], writes=[r_hTf])
                for c in range(8):
                    P.op("pe", lambda e: e.matmul(plg[:], lhsT=hTf[:, c, :], rhs=wr[:, c, :], start=(c == 0), stop=(c == 7)),
                         reads=[r_hTf, r_wr], writes=[r_plg])

        def ph2b(t):
            sc_t, r_sc = dup_sc[t % 2]
            bi, r_bi = dup_bi[t % 2]
            tmp, r_tmp = dup_tmp[t % 2]
            m1, r_m1 = dup_m1[t % 2]
            m2, r_m2 = dup_m2[t % 2]
            gs, r_gs = dup_gs[t % 2]
            t8, r_t8 = dup_t8[t % 2]
            pen, r_pen = dup_pen[t % 2]
            sel, r_sel = dup_sel[t % 2]
            ssum, r_ssum = dup_ssum[t % 2]
            gate, r_gate = dup_gate[t % 2]
            gTs, r_gTs = dup_gTs[t % 2]
            plg, r_plg = dup_plg[t % 2]
            P.op("act", lambda e: e.activation(out=sc_t[:], in_=plg[:], func=AF.Exp, scale=-1.0), reads=[r_plg], writes=[r_sc])
            yield
            P.op("dve", lambda e: e.tensor_scalar(out=sc_t[:], in0=sc_t[:], scalar1=1.0, scalar2=None, op0=ALU.add), reads=[r_sc], writes=[r_sc])
            yield
            P.op("dve", lambda e: e.reciprocal(out=sc_t[:], in_=sc_t[:]), reads=[r_sc], writes=[r_sc])
            yield
            P.op("dve", lambda e: e.tensor_tensor(out=bi[:], in0=sc_t[:], in1=brB[:], op=ALU.add), reads=[r_sc, r_brB], writes=[r_bi])
            yield
            bi3 = bi[:].rearrange("p (g e) -> p g e", g=8)
            tmp3 = tmp[:].rearrange("p (g e) -> p g e", g=8)
            P.op("dve", lambda e: e.tensor_reduce(out=m1[:], in_=bi3, axis=AX.X, op=ALU.max), reads=[r_bi], writes=[r_m1])
            yield
            P.op("dve", lambda e: e.tensor_tensor(out=tmp3, in0=bi3, in1=m1[:].unsqueeze(2).to_broadcast([128, 8, 8]), op=ALU.is_equal),
                 reads=[r_bi, r_m1], writes=[r_tmp])
            yield
            P.op("dve", lambda e: e.scalar_tensor_tensor(out=tmp[:], in0=tmp[:], scalar=-1e30, in1=bi[:], op0=ALU.mult, op1=ALU.add),
                 reads=[r_tmp, r_bi], writes=[r_tmp])
            yield
            P.op("dve", lambda e: e.tensor_reduce(out=m2[:], in_=tmp3, axis=AX.X, op=ALU.max), reads=[r_tmp], writes=[r_m2])
            yield
            P.op("dve", lambda e: e.tensor_tensor(out=gs[:], in0=m1[:], in1=m2[:], op=ALU.add), reads=[r_m1, r_m2], writes=[r_gs])
            yield
            P.op("dve", lambda e: e.max(out=t8[:], in_=gs[:]), reads=[r_gs], writes=[r_t8])
            yield
            P.op("dve", lambda e: e.tensor_scalar(out=pen[:], in0=gs[:], scalar1=t8[:, 3:4], scalar2=-1e30, op0=ALU.is_lt, op1=ALU.mult),
                 reads=[r_gs, r_t8], writes=[r_pen])
            yield
            P.op("dve", lambda e: e.tensor_tensor(out=tmp3, in0=bi3, in1=pen[:].unsqueeze(2).to_broadcast([128, 8, 8]), op=ALU.add),
                 reads=[r_bi, r_pen], writes=[r_tmp])
            yield
            P.op("dve", lambda e: e.max(out=t8[:], in_=tmp[:]), reads=[r_tmp], writes=[r_t8])
            yield
            P.op("dve", lambda e: e.tensor_scalar(out=sel[:], in0=tmp[:], scalar1=t8[:, 7:8], scalar2=None, op0=ALU.is_ge),
                 reads=[r_tmp, r_t8], writes=[r_sel])
            yield
            P.op("dve", lambda e: e.tensor_tensor(out=sel[:], in0=sel[:], in1=sc_t[:], op=ALU.mult), reads=[r_sel, r_sc], writes=[r_sel])
            yield
            P.op("dve", lambda e: e.tensor_reduce(out=ssum[:], in_=sel[:], axis=AX.X, op=ALU.add), reads=[r_sel], writes=[r_ssum])
            yield
            P.op("dve", lambda e: e.tensor_scalar(out=ssum[:], in0=ssum[:], scalar1=1e-20, scalar2=None, op0=ALU.add), reads=[r_ssum], writes=[r_ssum])
            yield
            P.op("dve", lambda e: e.reciprocal(out=ssum[:], in_=ssum[:]), reads=[r_ssum], writes=[r_ssum])
            yield
            P.op("dve", lambda e: e.tensor_scalar(out=gate[:], in0=sel[:], scalar1=ssum[:, 0:1], scalar2=2.5, op0=ALU.mult, op1=ALU.mult),
                 reads=[r_sel, r_ssum], writes=[r_gate])
            yield
            P.op("dve", lambda e: e.tensor_scalar(out=selb[t % 4][0][:], in0=gate[:], scalar1=0.0, scalar2=None, op0=ALU.is_gt),
                 reads=[r_gate], writes=[selb[t % 4][1]])
            yield
            P.dma("sp", gate_d[t * 128:(t + 1) * 128, :], gate[:], reads=[r_gate])
            yield

        def drain(*gens):
            gens = list(gens)
            while gens:
                for g in list(gens):
                    try:
                        next(g)
                    except StopIteration:
                        gens.remove(g)

        prev_tl = []

        def pc_ops(tl_):
            for tt_ in tl_:
                s_t, r_s = selb[tt_ % 4]
                p_t, r_p = posb[tt_ % 4]
                P.op("pe", lambda e: e.matmul(PC[:], lhsT=pinc[:], rhs=s_t[:], start=(tt_ == 0), stop=False, skip_group_check=True),
                     reads=[r_pinc, r_s], writes=[r_PC])
                P.op("act", lambda e: e.copy(out=p_t[:], in_=PC[:]), reads=[r_PC], writes=[r_p])
                P.op("pe", lambda e: e.matmul(PC[:], lhsT=pgtm[:], rhs=s_t[:], start=False, stop=(tt_ == NT - 1), skip_group_check=True),
                     reads=[r_pgtm, r_s, r_p], writes=[r_PC])
                P.dma("sp", pos_d[tt_ * 128:(tt_ + 1) * 128, :], p_t[:], reads=[r_p])

        for t0_ in range(min(4, NT)):
            ph1(t0_)
        for t in range(0, NT, 2):
            tl = [t] + ([t + 1] if t + 1 < NT else [])
            for tt_ in tl:
                ph2a(tt_)
            if t + 4 < NT:
                ph1(t + 4)
            if t + 5 < NT:
                ph1(t + 5)
            if router:
                pc_ops(prev_tl)
                prev_tl = tl
                drain(*[ph2b(tt_) for tt_ in tl])
        if router:
            pc_ops(prev_tl)
        if router:
            cnt, r_cnt = st.sb([128, NE], F32)
            nbt, r_nbt = st.sb([128, NE], F32)
            ca, r_ca = st.sb([128, NE], F32)
            cb, r_cb = st.sb([128, NE], F32)
            ebc, r_ebc = st.sb([128, 1], F32)
            P.op("act", lambda e: e.copy(out=cnt[:], in_=PC[:]), reads=[r_PC], writes=[r_cnt])
            P.op("dve", lambda e: e.tensor_scalar(out=nbt[:], in0=cnt[:], scalar1=0.0, scalar2=None, op0=ALU.is_gt), reads=[r_cnt], writes=[r_nbt])
            for j_ in range(1, 8):
                P.op("dve", lambda e: e.scalar_tensor_tensor(out=nbt[:], in0=cnt[:], scalar=512.0 * j_, in1=nbt[:], op0=ALU.is_gt, op1=ALU.add),
                     reads=[r_cnt, r_nbt], writes=[r_nbt])
            P.op("dve", lambda e: e.tensor_copy(out=ca[:], in_=nbt[:]), reads=[r_nbt], writes=[r_ca])
            src, r_src, dst_, r_dst = ca, r_ca, cb, r_cb
            k_ = 1
            while k_ < NE:
                P.op("dve", lambda e: e.tensor_copy(out=dst_[:, 0:k_], in_=src[:, 0:k_]), reads=[r_src], writes=[r_dst])
                P.op("dve", lambda e: e.tensor_tensor(out=dst_[:, k_:NE], in0=src[:, k_:NE], in1=src[:, 0:NE - k_], op=ALU.add), reads=[r_src], writes=[r_dst])
                src, r_src, dst_, r_dst = dst_, r_dst, src, r_src
                k_ *= 2
            bend, r_bend = src, r_src
            P.op("dve", lambda e: e.tensor_scalar(out=dst_[:], in0=bend[:], scalar1=iotac[:, 0:1], scalar2=None, op0=ALU.is_le),
                 reads=[r_bend, r_iotac], writes=[r_dst])
            P.op("dve", lambda e: e.tensor_reduce(out=ebc[:], in_=dst_[:], axis=AX.X, op=ALU.add), reads=[r_dst], writes=[r_ebc])
            P.op("dve", lambda e: e.tensor_scalar(out=ebc[:], in0=ebc[:], scalar1=float(NE - 1), scalar2=None, op0=ALU.min), reads=[r_ebc], writes=[r_ebc])
            P.dma("sp", col(eb_d), ebc[:], reads=[r_ebc])
            P.op("dve", lambda e: e.tensor_tensor(out=cnt[:], in0=bend[:], in1=nbt[:], op=ALU.subtract), reads=[r_bend, r_nbt], writes=[r_cnt])
            P.op("dve", lambda e: e.tensor_scalar(out=cnt[:], in0=cnt[:], scalar1=512.0, scalar2=None, op0=ALU.mult), reads=[r_cnt], writes=[r_cnt])
            P.dma("sp", sbase_d.rearrange("(o n) -> o n", o=1), cnt[0:1, :], reads=[r_cnt])
        st.done(f"norm{int(router)}_{l}")

    def stage_proj(l):
        st = Stage(P)
        NCOL = 4096
        w, r_w = st.sb([128, 8, NCOL], BF16)
        blk, r_blk = st.sb([128, 128], BF16)
        rot, r_rot = st.sb([128, 128], BF16)
        epsb, r_eps = st.sb([128, 1], F32)
        gq, r_gq = st.sb([128, 1], F32)
        gk, r_gk = st.sb([128, 1], F32)
        hT = [st.sb([128, 8, 512], BF16) for _ in range(2)]
        cosT = [st.sb([128, 512], F32) for _ in range(2)]
        sinT = [st.sb([128, 512], F32) for _ in range(2)]
        pp = [st.ps([128, 512], F32) for _ in range(4)]
        pms, r_pms = st.ps([128, 512], F32)
        prot, r_prot = st.ps([128, 512], F32)
        sqb, r_sqb = st.sb([128, 512], BF16)
        rs, r_rs = st.sb([128, 512], F32)
        qn, r_qn = st.sb([128, 512], BF16)
        t1, r_t1 = st.sb([128, 512], F32)
        t2, r_t2 = st.sb([128, 512], F32)
        sg, r_sg = st.sb([128, 512], F32)
        ob = [st.sb([128, 512], BF16) for _ in range(4)]
        for c in range(8):
            P.dma("pool", w[:, c, :], I["w_in"][l][c * 128:(c + 1) * 128, 0:NCOL], writes=[r_w])
        P.dma("pool", blk[:], I["k_blk64"], writes=[r_blk])
        P.dma("pool", rot[:], I["k_rotT"], writes=[r_rot])
        P.op("pool", lambda e: e.memset(epsb[:], EPS), writes=[r_eps])
        for hh in range(2):
            P.dma("sp", gq[hh * 64:(hh + 1) * 64, :], col(I["qn_g"][l]), writes=[r_gq])
            P.dma("sp", gk[hh * 64:(hh + 1) * 64, :], col(I["kn_g"][l]), writes=[r_gk])
        pp = pp + [st.ps([128, 512], F32) for _ in range(2)]
        NPP = len(pp)
        loaded = set()

        def load_tile(tt):
            if tt in loaded or tt >= NQ:
                return
            loaded.add(tt)
            ts_ = slice(tt * 512, (tt + 1) * 512)
            P.dma("sp", hT[tt % 2][0][:], kp(hT_d)[:, :, ts_], writes=[hT[tt % 2][1]])
            P.dma("sp", cosT[tt % 2][0][:], cos_d[:, ts_], writes=[cosT[tt % 2][1]])
            P.dma("sp", sinT[tt % 2][0][:], sin_d[:, ts_], writes=[sinT[tt % 2][1]])

        units = []
        for tt in range(NQ):
            for which in range(2):
                for j in range(4):
                    units.append(("qk", tt, (which, j)))
            for j in range(4):
                units.append(("glu", tt, (j,)))
            for which in range(2):
                for j in range(4):
                    units.append(("cp", tt, (which, j)))
            for which in range(2):
                for sub in range(4):
                    units.append(("tm", tt, (which, sub)))
        pidx = []
        cur = 0
        for kind, tt, args in units:
            nb_ = 2 if kind == "glu" else 1
            pidx.append([(cur + i) % NPP for i in range(nb_)])
            cur += nb_
        state = {"oi": 0}

        def fmm(tt, col0, p_t, r_p):
            h_t, r_h = hT[tt % 2]
            for c in range(8):
                P.op("pe", lambda e: e.matmul(p_t[:], lhsT=w[:, c, col0:col0 + 128], rhs=h_t[:, c, :], start=(c == 0), stop=(c == 7)),
                     reads=[r_w, r_h], writes=[r_p])

        def F(u):
            kind, tt, args = units[u]
            load_tile(tt)
            bufs = [pp[i] for i in pidx[u]]
            if kind == "qk":
                which, j = args
                fmm(tt, which * 512 + j * 128, *bufs[0])
            elif kind == "glu":
                (j,) = args
                fmm(tt, 1536 + j * 128, *bufs[0])
                fmm(tt, 2048 + j * 128, *bufs[1])
            elif kind == "cp":
                which, j = args
                fmm(tt, (2560 if which == 0 else 3072) + j * 128, *bufs[0])
            else:
                which, sub = args
                base = 1024 if which == 0 else 3584
                h_t, r_h = hT[tt % 2]
                p_t, r_p = bufs[0]
                for c in range(8):
                    P.op("pe", lambda e: e.matmul(p_t[:], lhsT=h_t[:, c, sub * 128:(sub + 1) * 128], rhs=w[:, c, base:base + 512], start=(c == 0), stop=(c == 7)),
                         reads=[r_w, r_h], writes=[r_p])

        def nxt_ob():
            o = ob[state["oi"] % 4]
            state["oi"] += 1
            return o

        def G(u):
            kind, tt, args = units[u]
            ts_ = slice(tt * 512, (tt + 1) * 512)
            bufs = [pp[i] for i in pidx[u]]
            c_t, r_c = cosT[tt % 2]
            s_t, r_s = sinT[tt % 2]
            if kind == "qk":
                which, j = args
                g_t, r_g, dst = (gq, r_gq, qaT_d) if which == 0 else (gk, r_gk, kaT_d)
                p_t, r_p = bufs[0]
                P.op("act", lambda e: e.activation(out=sqb[:], in_=p_t[:], func=AF.Square), reads=[r_p], writes=[r_sqb])
                P.op("pe", lambda e: e.matmul(pms[:], lhsT=blk[:], rhs=sqb[:], start=True, stop=True), reads=[r_blk, r_sqb], writes=[r_pms])
                P.op("act", lambda e: e.activation(out=rs[:], in_=pms[:], func=AF.Ln, bias=epsb[:, 0:1]), reads=[r_pms, r_eps], writes=[r_rs])
                P.op("act", lambda e: e.activation(out=rs[:], in_=rs[:], func=AF.Exp, scale=-0.5), reads=[r_rs], writes=[r_rs])
                P.op("dve", lambda e: e.scalar_tensor_tensor(out=qn[:], in0=p_t[:], scalar=g_t[:, 0:1], in1=rs[:], op0=ALU.mult, op1=ALU.mult),
                     reads=[r_p, r_g, r_rs], writes=[r_qn])
                P.op("pe", lambda e: e.matmul(prot[:], lhsT=rot[:], rhs=qn[:], start=True, stop=True), reads=[r_rot, r_qn], writes=[r_prot])
                P.op("pool", lambda e: e.tensor_tensor(out=t1[:], in0=qn[:], in1=c_t[:], op=ALU.mult), reads=[r_qn, r_c], writes=[r_t1])
                P.op("dve", lambda e: e.tensor_tensor(out=t2[:], in0=prot[:], in1=s_t[:], op=ALU.mult), reads=[r_prot, r_s], writes=[r_t2])
                o_t, r_o = nxt_ob()
                P.op("dve", lambda e: e.tensor_tensor(out=o_t[:], in0=t1[:], in1=t2[:], op=ALU.add), reads=[r_t1, r_t2], writes=[r_o])
                P.dma("sp", dst[j * 128:(j + 1) * 128, ts_], o_t[:], reads=[r_o])
            elif kind == "glu":
                (j,) = args
                (pa, r_pa), (pg, r_pg) = bufs
                P.op("act", lambda e: e.activation(out=sg[:], in_=pg[:], func=AF.Sigmoid), reads=[r_pg], writes=[r_sg])
                o_t, r_o = nxt_ob()
                P.op("dve", lambda e: e.tensor_tensor(out=o_t[:], in0=pa[:], in1=sg[:], op=ALU.mult), reads=[r_pa, r_sg], writes=[r_o])
                P.dma("sp", uT_d[j * 128:(j + 1) * 128, ts_], o_t[:], reads=[r_o])
            elif kind == "cp":
                which, j = args
                dst, qscale = (qcT_d, 0.125) if which == 0 else (kcT_d, 1.0)
                p_t, r_p = bufs[0]
                o_t, r_o = nxt_ob()
                P.op("act", lambda e: e.mul(out=o_t[:], in_=p_t[:], mul=qscale), reads=[r_p], writes=[r_o])
                P.dma("sp", dst[j * 128:(j + 1) * 128, ts_], o_t[:], reads=[r_o])
            else:
                which, sub = args
                dst = va_d if which == 0 else vc_d
                p_t, r_p = bufs[0]
                o_t, r_o = nxt_ob()
                P.op("dve" if sub % 2 == 0 else "act", (lambda e: e.tensor_copy(out=o_t[:], in_=p_t[:])) if sub % 2 == 0 else (lambda e: e.copy(out=o_t[:], in_=p_t[:])),
                     reads=[r_p], writes=[r_o])
                r0 = tt * 512 + sub * 128
                P.dma("sp", dst[r0:r0 + 128, :], o_t[:], reads=[r_o])

        nU = len(units)
        AHEAD = 2
        for u in range(min(AHEAD, nU)):
            F(u)
        for u in range(nU):
            if u + AHEAD < nU:
                F(u + AHEAD)
            G(u)
        st.done(f"proj{l}")

    def stage_da(l):
        lambda_init = 0.8 - 0.6 * math.exp(-0.3 * l)
        st = Stage(P)
        msk, r_msk = st.sb([128, 4, 512], BF16)
        ones, r_ones = st.sb([128, 128], BF16)
        o128, r_o128 = st.sb([128, 128], BF16)
        epsb, r_eps = st.sb([128, 1], F32)
        lv = [st.sb([128, 64], F32) for _ in range(4)]
        lp, r_lp = st.sb([128, 64], F32)
        ld, r_ld = st.sb([128, 2], F32)
        nlam, r_nlam = st.sb([128, 1], F32)
        gcol, r_gcol = st.sb([128, 1], F32)
        qT = [[st.sb([128, S], BF16) for _ in range(2)] for _ in range(2)]
        kT = [st.sb([128, S], BF16) for _ in range(2)]
        vv = [st.sb([128, NT, 128], BF16) for _ in range(2)]
        pz = [st.ps([128, 512], F32) for _ in range(2)]
        pO = [st.ps([128, 512], F32) for _ in range(2)]
        pL = [st.ps([128, 512], F32) for _ in range(2)]
        for hp_ in range(2):
            for m_ in range(2):
                zr = slice((1 - m_) * 64, (1 - m_) * 64 + 64)
                P.op("pool", lambda e: e.memset(qT[hp_][m_][0][zr, :], 0.0), writes=[qT[hp_][m_][1]])
        pms, r_pms = st.ps([128, 512], F32)
        E = [st.sb([128, 512], BF16) for _ in range(4)]
        rL = [st.sb([128, 512], F32) for _ in range(2)]
        tO = [st.sb([128, 512], F32) for _ in range(2)]
        o_t, r_o = st.sb([128, 512], F32)
        sqb, r_sqb = st.sb([128, 512], BF16)
        rs, r_rs = st.sb([128, 512], F32)
        ob = [st.sb([128, 512], BF16) for _ in range(2)]
        P.dma("pool", msk[:], I["k_maskc"].rearrange("j p q -> p j q"), writes=[r_msk])
        if l == 0:
            zt, r_zt = st.sb([128, 4096], BF16)
            P.op("pool", lambda e: e.memset(zt[:], 0.0), writes=[r_zt])
        zfill = {"next": 0}

        def zero_fill_some(n_):
            if l != 0:
                return
            for _ in range(n_):
                zi_ = zfill["next"]
                if zi_ >= NSLOT // 512:
                    return
                zfill["next"] += 1
                P.dma("pool", xs_d[zi_ * 512:(zi_ + 1) * 512, :].rearrange("(p r) d -> p (r d)", p=128), zt[:], reads=[r_zt])
        P.op("pool", lambda e: e.memset(ones[:], 1.0), writes=[r_ones])
        P.op("pool", lambda e: e.memset(o128[:], 1.0 / 128), writes=[r_o128])
        P.op("pool", lambda e: e.memset(epsb[:], EPS), writes=[r_eps])
        for i, nm in enumerate(("lam_q1", "lam_k1", "lam_q2", "lam_k2")):
            P.dma("sp", lv[i][0][:], I[nm][l].partition_broadcast(128), writes=[lv[i][1]])
        for i in range(2):
            P.op("dve", lambda e, i=i: e.tensor_tensor(out=lp[:], in0=lv[2 * i][0][:], in1=lv[2 * i + 1][0][:], op=ALU.mult),
                 reads=[lv[2 * i][1], lv[2 * i + 1][1]], writes=[r_lp])
            P.op("dve", lambda e, i=i: e.tensor_reduce(out=ld[:, i:i + 1], in_=lp[:], axis=AX.X, op=ALU.add), reads=[r_lp], writes=[r_ld])
        P.op("act", lambda e: e.activation(out=ld[:], in_=ld[:], func=AF.Exp), reads=[r_ld], writes=[r_ld])
        P.op("dve", lambda e: e.tensor_tensor(out=nlam[:], in0=ld[:, 1:2], in1=ld[:, 0:1], op=ALU.subtract), reads=[r_ld], writes=[r_nlam])
        P.op("dve", lambda e: e.tensor_scalar(out=nlam[:], in0=nlam[:], scalar1=-lambda_init, scalar2=None, op0=ALU.add), reads=[r_nlam], writes=[r_nlam])
        P.dma("sp", gcol[:], col(I["subln_g"][l]), writes=[r_gcol])
        P.op("dve", lambda e: e.tensor_scalar(out=gcol[:], in0=gcol[:], scalar1=1.0 - lambda_init, scalar2=None, op0=ALU.mult), reads=[r_gcol], writes=[r_gcol])
        oi = 0
        pz3 = pz + [st.ps([128, 512], F32)]
        units = []
        for hd in range(4):
            for Qi in range(NQ):
                for m in range(2):
                    for kt in range(4 * Qi + 4):
                        units.append((hd, Qi, m, kt))
        loaded = set()

        def load_head(hd):
            if hd in loaded or hd >= 4:
                return
            loaded.add(hd)
            k_t, r_k = kT[hd % 2]
            v_t, r_v = vv[hd % 2]
            for m_ in range(2):
                rr_ = slice(m_ * 64, m_ * 64 + 64)
                P.dma("sp", qT[hd % 2][m_][0][rr_, :], qaT_d[hd * 128 + m_ * 64:hd * 128 + m_ * 64 + 64, :], writes=[qT[hd % 2][m_][1]])
            P.dma("sp", k_t[:], kaT_d[hd * 128:(hd + 1) * 128, :], writes=[r_k])
            P.dma("sp", v_t[:], va_d[:, hd * 128:(hd + 1) * 128].rearrange("(t p) e -> p t e", p=128), writes=[r_v])

        def phA(i):
            hd, Qi, m, kt = units[i]
            load_head(hd)
            q_t, r_q = qT[hd % 2][m]
            k_t, r_k = kT[hd % 2]
            z_t, r_z = pz3[i % 3]
            qs = slice(Qi * 512, (Qi + 1) * 512)
            P.op("pe", lambda e: e.matmul(z_t[:], lhsT=k_t[:, kt * 128:(kt + 1) * 128], rhs=q_t[:, qs], start=True, stop=True),
                 reads=[r_k, r_q], writes=[r_z])

        def phB(i):
            nonlocal oi
            hd, Qi, m, kt = units[i]
            v_t, r_v = vv[hd % 2]
            z_t, r_z = pz3[i % 3]
            e_t, r_e = E[i % 4]
            pO_t, r_pO = pO[m]
            pL_t, r_pL = pL[m]
            nk = 4 * Qi + 4
            j = kt - 4 * Qi
            qs = slice(Qi * 512, (Qi + 1) * 512)
            P.op("act", lambda e: e.activation(out=e_t[:], in_=z_t[:], func=AF.Exp, scale=0.125), reads=[r_z], writes=[r_e])
            if j >= 0:
                P.op("dve", lambda e: e.tensor_tensor(out=e_t[:], in0=e_t[:], in1=msk[:, j, :], op=ALU.mult), reads=[r_e, r_msk], writes=[r_e])
            P.op("pe", lambda e: e.matmul(pO_t[:], lhsT=v_t[:, kt, :], rhs=e_t[:], start=(kt == 0), stop=(kt == nk - 1)),
                 reads=[r_v, r_e], writes=[r_pO])
            P.op("pe", lambda e: e.matmul(pL_t[:], lhsT=ones[:], rhs=e_t[:], start=(kt == 0), stop=(kt == nk - 1)),
                 reads=[r_ones, r_e], writes=[r_pL])
            if kt == nk - 1:
                P.op("act", lambda e: e.activation(out=rL[m][0][:], in_=pL_t[:], func=AF.Ln), reads=[r_pL], writes=[rL[m][1]])
                P.op("act", lambda e: e.activation(out=rL[m][0][:], in_=rL[m][0][:], func=AF.Exp, scale=-1.0), reads=[rL[m][1]], writes=[rL[m][1]])
                P.op("dve", lambda e: e.tensor_tensor(out=tO[m][0][:], in0=pO_t[:], in1=rL[m][0][:], op=ALU.mult), reads=[r_pO, rL[m][1]], writes=[tO[m][1]])
                if m == 1:
                    P.op("dve", lambda e: e.scalar_tensor_tensor(out=o_t[:], in0=tO[1][0][:], scalar=nlam[:, 0:1], in1=tO[0][0][:], op0=ALU.mult, op1=ALU.add),
                         reads=[tO[0][1], tO[1][1], r_nlam], writes=[r_o])
                    P.op("act", lambda e: e.activation(out=sqb[:], in_=o_t[:], func=AF.Square), reads=[r_o], writes=[r_sqb])
                    P.op("pe", lambda e: e.matmul(pms[:], lhsT=o128[:], rhs=sqb[:], start=True, stop=True), reads=[r_o128, r_sqb], writes=[r_pms])
                    P.op("act", lambda e: e.activation(out=rs[:], in_=pms[:], func=AF.Ln, bias=epsb[:, 0:1]), reads=[r_pms, r_eps], writes=[r_rs])
                    P.op("act", lambda e: e.activation(out=rs[:], in_=rs[:], func=AF.Exp, scale=-0.5), reads=[r_rs], writes=[r_rs])
                    b_t, r_b = ob[oi % 2]
                    oi += 1
                    P.op("dve", lambda e: e.scalar_tensor_tensor(out=b_t[:], in0=o_t[:], scalar=gcol[:, 0:1], in1=rs[:], op0=ALU.mult, op1=ALU.mult),
                         reads=[r_o, r_gcol, r_rs], writes=[r_b])
                    P.dma("sp", oaT_d[hd * 128:(hd + 1) * 128, qs], b_t[:], reads=[r_b])

        n = len(units)
        for i in range(min(2, n)):
            phA(i)
        for i in range(n):
            if i + 2 < n:
                phA(i + 2)
            phB(i)
            if i % 4 == 3:
                zero_fill_some(1)
        zero_fill_some(NSLOT)
        st.done(f"da{l}")

    def stage_sb(l):
        st = Stage(P)
        mskb, r_mskb = st.sb([128, 4, 512], BF16)
        uinc, r_uinc = st.sb([128, 128], BF16)
        ulow, r_ulow = st.sb([128, 128], BF16)
        onec, r_onec = st.sb([128, 1], F32)
        qT = [[st.sb([128, S], BF16) for _ in range(2)] for _ in range(2)]
        kT = [st.sb([128, S], BF16) for _ in range(2)]
        kN = [st.sb([128, S], BF16) for _ in range(2)]
        vv = [st.sb([128, NT, 128], BF16) for _ in range(2)]
        pz = [st.ps([128, 2, 512], F32) for _ in range(2)]
        PT, r_PT = st.ps([128, 2, 512], F32)
        pO, r_pO = st.ps([128, 2, 512], F32)
        ex = [st.sb([128, 2, 512], F32) for _ in range(2)]
        sp_ = [st.sb([128, 2, 512], F32) for _ in range(2)]
        lom = [st.sb([128, 2, 512], BF16) for _ in range(3)]
        ab = [st.sb([128, 2, 512], BF16) for _ in range(3)]
        ob = [st.sb([128, 512], BF16) for _ in range(2)]
        for cp_ in range(2):
            for hh_ in range(2):
                zr = slice((1 - hh_) * 64, (1 - hh_) * 64 + 64)
                P.op("pool", lambda e: e.memset(qT[cp_][hh_][0][zr, :], 0.0), writes=[qT[cp_][hh_][1]])
        P.dma("pool", mskb[:], I["k_masks"].rearrange("j p q -> p j q"), writes=[r_mskb])
        P.dma("pool", uinc[:], I["k_uincl"], writes=[r_uinc])
        P.dma("pool", ulow[:], I["k_ulow"], writes=[r_ulow])
        P.op("pool", lambda e: e.memset(onec[:], 1.0), writes=[r_onec])
        state = {"oi": 0}
        r_PTh = [Res(), Res()]
        abr = [[Res(), Res()] for _ in range(3)]
        units = [(ch, Qi, kt) for ch in range(4) for Qi in range(NQ) for kt in range(4 * Qi + 3, -1, -1)]
        n = len(units)
        loaded = set()

        def load_pair(ch):
            if ch in loaded:
                return
            loaded.add(ch)
            for hh_ in range(2):
                rr_ = slice(hh_ * 64, hh_ * 64 + 64)
                P.dma("sp", qT[ch % 2][hh_][0][rr_, :], qcT_d[ch * 128 + hh_ * 64:ch * 128 + hh_ * 64 + 64, :], writes=[qT[ch % 2][hh_][1]])
            P.dma("sp", kT[ch % 2][0][:], kcT_d[ch * 128:(ch + 1) * 128, :], writes=[kT[ch % 2][1]])
            P.dma("sp", vv[ch % 2][0][:], vc_d[:, ch * 128:(ch + 1) * 128].rearrange("(t p) e -> p t e", p=128), writes=[vv[ch % 2][1]])
            P.op("pool", lambda e: e.tensor_scalar(out=kN[ch % 2][0][:], in0=kT[ch % 2][0][:], scalar1=-1.0, scalar2=None, op0=ALU.mult),
                 reads=[kT[ch % 2][1]], writes=[kN[ch % 2][1]])

        def info(i):
            ch, Qi, kt = units[i]
            return ch, Qi, kt, kt - 4 * Qi, 4 * Qi + 4, slice(Qi * 512, (Qi + 1) * 512), slice(kt * 128, (kt + 1) * 128)

        def mask2(j):
            return mskb[:, j:j + 1, :].to_broadcast([128, 2, 512])

        def phA(i):
            ch, Qi, kt, j, nk, qs, ks = info(i)
            load_pair(ch)
            k_t, r_k = kT[ch % 2]
            z_t, r_z = pz[i % 2]
            for hh in range(2):
                q_t, r_q = qT[ch % 2][hh]
                P.op("pe", lambda e: e.matmul(z_t[:, hh, :], lhsT=k_t[:, ks], rhs=q_t[:, qs], start=True, stop=True), reads=[r_k, r_q], writes=[r_z])

        def phB(i):
            ch, Qi, kt, j, nk, qs, ks = info(i)
            z_t, r_z = pz[i % 2]
            e_t, r_e = ex[i % 2]
            p_t, r_p = sp_[i % 2]
            l_t, r_l = lom[i % 3]
            P.op("act", lambda e: e.activation(out=e_t[:], in_=z_t[:], func=AF.Exp, scale=-1.0), reads=[r_z], writes=[r_e])
            P.op("act", lambda e: e.activation(out=p_t[:], in_=e_t[:], func=AF.Ln, bias=onec[:, 0:1]), reads=[r_e, r_onec], writes=[r_p])
            P.op("dve", lambda e: e.scalar_tensor_tensor(out=l_t[:], in0=z_t[:], scalar=-1.0, in1=p_t[:], op0=ALU.mult, op1=ALU.subtract),
                 reads=[r_z, r_p], writes=[r_l])
            if j >= 0:
                P.op("dve", lambda e: e.tensor_tensor(out=l_t[:], in0=l_t[:], in1=mask2(j), op=ALU.mult), reads=[r_l, r_mskb], writes=[r_l])

        def phC(i, hh):
            ch, Qi, kt, j, nk, qs, ks = info(i)
            k_t, r_k = kT[ch % 2]
            l_t, r_l = lom[i % 3]
            q_t, r_q = qT[ch % 2][hh]
            P.op("pe", lambda e: e.matmul(PT[:, hh, :], lhsT=uinc[:], rhs=l_t[:, hh, :], start=(kt == nk - 1), stop=False, skip_group_check=True),
                 reads=[r_uinc, r_l], writes=[r_PTh[hh]])
            P.op("pe", lambda e: e.matmul(PT[:, hh, :], lhsT=k_t[:, ks], rhs=q_t[:, qs], start=False, stop=False, skip_group_check=True),
                 reads=[r_k, r_q], writes=[r_PTh[hh]])

        def phD_act(i):
            ch, Qi, kt, j, nk, qs, ks = info(i)
            a_t, _ = ab[i % 3]
            for hh in range(2):
                r_a = abr[i % 3][hh]
                P.op("act", lambda e: e.activation(out=a_t[:, hh, :], in_=PT[:, hh, :], func=AF.Exp), reads=[r_PTh[hh]], writes=[r_a])
                if j >= 0:
                    P.op("dve", lambda e: e.tensor_tensor(out=a_t[:, hh, :], in0=a_t[:, hh, :], in1=mskb[:, j, :], op=ALU.mult), reads=[r_a, r_mskb], writes=[r_a])

        def phD_corr(i, hh):
            ch, Qi, kt, j, nk, qs, ks = info(i)
            kn_t, r_kn = kN[ch % 2]
            l_t, r_l = lom[i % 3]
            r_a = abr[i % 3][hh]
            if kt > 0:
                q_t, r_q = qT[ch % 2][hh]
                P.op("pe", lambda e: e.matmul(PT[:, hh, :], lhsT=ulow[:], rhs=l_t[:, hh, :], start=False, stop=False, skip_group_check=True),
                     reads=[r_ulow, r_l, r_a], writes=[r_PTh[hh]])
                P.op("pe", lambda e: e.matmul(PT[:, hh, :], lhsT=kn_t[:, ks], rhs=q_t[:, qs], start=False, stop=(kt == 1), skip_group_check=True),
                     reads=[r_kn, r_q], writes=[r_PTh[hh]])

        def phD_pv(i):
            ch, Qi, kt, j, nk, qs, ks = info(i)
            v_t, r_v = vv[ch % 2]
            a_t, _ = ab[i % 3]
            for hh in range(2):
                P.op("pe", lambda e: e.matmul(pO[:, hh, :], lhsT=v_t[:, kt, :], rhs=a_t[:, hh, :], start=(kt == nk - 1), stop=(kt == 0)),
                     reads=[r_v, abr[i % 3][hh]], writes=[r_pO])
            if kt == 0:
                b_t, r_b = ob[state["oi"] % 2]
                state["oi"] += 1
                for hh in range(2):
                    pr = slice(hh * 64, hh * 64 + 64)
                    P.op("act", lambda e: e.copy(out=b_t[pr, :], in_=pO[pr, hh, :]), reads=[r_pO], writes=[r_b])
                P.dma("sp", ocT_d[ch * 128:(ch + 1) * 128, qs], b_t[:], reads=[r_b])

        for it in range(-3, n):
            if 0 <= it < n:
                phD_act(it)
            if 0 <= it + 3 < n:
                phA(it + 3)
            if 0 <= it + 2 < n:
                phB(it + 2)
            for hh in range(2):
                if 0 <= it < n:
                    phD_corr(it, hh)
                if 0 <= it + 1 < n:
                    phC(it + 1, hh)
            if 0 <= it < n:
                phD_pv(it)
        st.done(f"sb{l}")

    def stage_cv(l):
        st = Stage(P)
        up, r_up = st.sb([128, 4, 30 + S], BF16)
        wcol, r_wcol = st.sb([128, 4, 31], F32)
        identf, r_idf = st.sb([128, 128], F32)
        dg = [st.sb([128, 31, 128], BF16) for _ in range(4)]
        bcol, r_bcol = st.sb([128, 4], F32)
        gcol, r_gcol = st.sb([128, 4], F32)
        lbcol, r_lbcol = st.sb([128, 4], F32)
        o512, r_o512 = st.sb([128, 128], F32)
        epsb, r_eps = st.sb([128, 1], F32)
        pc = [st.ps([128, 512], F32) for _ in range(4)]
        pmean, r_pmean = st.ps([128, 512], F32)
        pex2, r_pex2 = st.ps([128, 512], F32)
        cv32, r_cv = st.sb([128, 4, 512], F32)
        sq32, r_sq = st.sb([128, 4, 512], F32)
        mean, r_mean = st.sb([128, 512], F32)
        msq, r_msq = st.sb([128, 512], F32)
        rs, r_rs = st.sb([128, 512], F32)
        y = [st.sb([128, 512], F32) for _ in range(2)]
        ob = [st.sb([128, 512], BF16) for _ in range(2)]
        P.op("pool", lambda e: e.memset(up[:, :, 0:30], 0.0), writes=[r_up])
        for j in range(4):
            P.dma("sp", up[:, j, 30:30 + S], uT_d[j * 128:(j + 1) * 128, :], writes=[r_up])
            P.dma("sp", wcol[:, j, :], I["w_dw"][l][:, j * 128:(j + 1) * 128].rearrange("k p -> p k"), writes=[r_wcol])
        P.dma("sp", identf[:], I["k_ident"], writes=[r_idf])
        P.dma("sp", bcol[:], I["b_dw"][l].rearrange("(j p) -> p j", p=128), writes=[r_bcol])
        P.dma("sp", gcol[:], I["conv_ln_g"][l].rearrange("(j p) -> p j", p=128), writes=[r_gcol])
        P.dma("sp", lbcol[:], I["conv_ln_b"][l].rearrange("(j p) -> p j", p=128), writes=[r_lbcol])
        P.op("pool", lambda e: e.memset(o512[:], 1.0 / 512), writes=[r_o512])
        P.op("pool", lambda e: e.memset(epsb[:], EPS), writes=[r_eps])
        junk, r_junk = st.sb([128, 1], F32)
        for j in range(4):
            rr = []
            for k in range(31):
                eng = "dve" if k % 2 == 0 else "pool"
                r1 = Res()
                rr.append(r1)
                P.op(eng, lambda e: e.tensor_scalar(out=dg[j][0][:, k, :], in0=identf[:], scalar1=wcol[:, j, k:k + 1], scalar2=None, op0=ALU.mult),
                     reads=[r_idf, r_wcol], writes=[r1])
            P.op("dve", lambda e: e.memset(junk[:], 0.0), reads=rr, writes=[dg[j][1], r_junk])
        yi = 0
        for tt in range(NQ):
            for j in range(4):
                p_t, r_p = pc[j]
                for k in range(31):
                    P.op("pe", lambda e: e.matmul(p_t[:], lhsT=dg[j][0][:, k, :], rhs=up[:, j, tt * 512 + k:tt * 512 + k + 512], start=(k == 0), stop=(k == 30)),
                         reads=[dg[j][1], r_up], writes=[r_p])
                P.op("dve", lambda e: e.tensor_scalar(out=cv32[:, j, :], in0=p_t[:], scalar1=bcol[:, j:j + 1], scalar2=None, op0=ALU.add),
                     reads=[r_p, r_bcol], writes=[r_cv])
                P.op("pool", lambda e: e.tensor_tensor(out=sq32[:, j, :], in0=cv32[:, j, :], in1=cv32[:, j, :], op=ALU.mult), reads=[r_cv], writes=[r_sq])
            for j in range(4):
                P.op("pe", lambda e: e.matmul(pmean[:], lhsT=o512[:], rhs=cv32[:, j, :], start=(j == 0), stop=(j == 3)), reads=[r_o512, r_cv], writes=[r_pmean])
            for j in range(4):
                P.op("pe", lambda e: e.matmul(pex2[:], lhsT=o512[:], rhs=sq32[:, j, :], start=(j == 0), stop=(j == 3)), reads=[r_o512, r_sq], writes=[r_pex2])
            P.op("act", lambda e: e.copy(out=mean[:], in_=pmean[:]), reads=[r_pmean], writes=[r_mean])
            P.op("pool", lambda e: e.tensor_tensor(out=msq[:], in0=mean[:], in1=mean[:], op=ALU.mult), reads=[r_mean], writes=[r_msq])
            P.op("dve", lambda e: e.tensor_tensor(out=rs[:], in0=pex2[:], in1=msq[:], op=ALU.subtract), reads=[r_pex2, r_msq], writes=[r_rs])
            P.op("act", lambda e: e.activation(out=rs[:], in_=rs[:], func=AF.Ln, bias=epsb[:, 0:1]), reads=[r_rs, r_eps], writes=[r_rs])
            P.op("act", lambda e: e.activation(out=rs[:], in_=rs[:], func=AF.Exp, scale=-0.5), reads=[r_rs], writes=[r_rs])
            for j in range(4):
                y_t, r_y = y[yi % 2]
                b_t, r_b = ob[yi % 2]
                yi += 1
                P.op("pool", lambda e: e.tensor_tensor(out=y_t[:], in0=cv32[:, j, :], in1=mean[:], op=ALU.subtract), reads=[r_cv, r_mean], writes=[r_y])
                P.op("dve", lambda e: e.tensor_tensor(out=y_t[:], in0=y_t[:], in1=rs[:], op=ALU.mult), reads=[r_y, r_rs], writes=[r_y])
                P.op("act", lambda e: e.activation(out=b_t[:], in_=y_t[:], func=AF.Silu, scale=gcol[:, j:j + 1], bias=lbcol[:, j:j + 1]),
                     reads=[r_y, r_gcol, r_lbcol], writes=[r_b])
                P.dma("sp", cvT_d[j * 128:(j + 1) * 128, tt * 512:(tt + 1) * 512], b_t[:], reads=[r_b])
        st.done(f"cv{l}")

    def stage_mg(l, x_src):
        st = Stage(P)
        wg, r_wg = st.sb([128, 8, 3072], BF16)
        wp = [st.sb([128, 4, D], BF16) for _ in range(3)]
        wo, r_wo = st.sb([128, 8, D], BF16)
        bpb, r_bpb = st.sb([128, 8], F32)
        gmB, r_gmB = st.sb([128, D], F32)
        hT = [st.sb([128, 8, 512], BF16) for _ in range(2)]
        obr = [[st.sb([128, 4, 512], BF16) for _ in range(2)] for _ in range(3)]
        mT, r_mT = st.sb([128, 8, 512], BF16)
        py = [st.ps([128, 512], F32) for _ in range(2)]
        pg = [st.ps([128, 512], F32) for _ in range(2)]
        po = [st.ps([128, 512], F32) for _ in range(2)]
        sg = [st.sb([128, 512], F32) for _ in range(2)]
        mb = [st.sb([128, 512], F32) for _ in range(2)]
        acc, r_acc = st.sb([128, 512], F32)
        xt = [st.sb([128, D], F32) for _ in range(2)]
        tmp, r_tmp = st.sb([128, 512], F32)
        xn = [st.sb([128, D], F32) for _ in range(2)]
        for c in range(8):
            P.dma("pool", wg[:, c, :], I["w_in"][l][c * 128:(c + 1) * 128, 4096:7168], writes=[r_wg])
        for b, nm in enumerate(("w_proj_a", "w_proj_b", "w_proj_c")):
            P.dma("pool", wp[b][0][:], kp(I[nm][l]), writes=[wp[b][1]])
        P.dma("pool", wo[:], kp(I["w_out"][l]), writes=[r_wo])
        P.dma("sp", bpb[:], I["b_proj_b"][l].rearrange("(j p) -> p j", p=128), writes=[r_bpb])
        P.dma("sp", gmB[:], mod_d[l, 2 * D:3 * D].partition_broadcast(128), writes=[r_gmB])
        srcs = (oaT_d, cvT_d, ocT_d)
        loaded = set()

        def load_tile(tt):
            if tt in loaded or tt >= NQ:
                return
            loaded.add(tt)
            ts_ = slice(tt * 512, (tt + 1) * 512)
            P.dma("sp", hT[tt % 2][0][:], kp(hT_d)[:, :, ts_], writes=[hT[tt % 2][1]])
            for b_ in range(3):
                P.dma("sp", obr[b_][tt % 2][0][:], kp(srcs[b_])[:, :, ts_], writes=[obr[b_][tt % 2][1]])

        units = [(tt, j, b_) for tt in range(NQ) for j in range(8) for b_ in range(3)]
        state = {"xi": 0}

        def F(u):
            tt, j, b_ = units[u]
            load_tile(tt)
            y_t, r_y = py[u % 2]
            g_t, r_g = pg[u % 2]
            h_t, r_h = hT[tt % 2]
            o_b, r_ob = obr[b_][tt % 2]
            for c in range(4):
                P.op("pe", lambda e: e.matmul(y_t[:], lhsT=wp[b_][0][:, c, j * 128:(j + 1) * 128], rhs=o_b[:, c, :], start=(c == 0), stop=(c == 3)),
                     reads=[wp[b_][1], r_ob], writes=[r_y])
            for c in range(8):
                P.op("pe", lambda e: e.matmul(g_t[:], lhsT=wg[:, c, b_ * D + j * 128:b_ * D + (j + 1) * 128], rhs=h_t[:, c, :], start=(c == 0), stop=(c == 7)),
                     reads=[r_wg, r_h], writes=[r_g])

        def G(u):
            tt, j, b_ = units[u]
            y_t, r_y = py[u % 2]
            g_t, r_g = pg[u % 2]
            s_t, r_s = sg[u % 2]
            m_t, r_m = mb[u % 2]
            P.op("act", lambda e: e.activation(out=s_t[:], in_=g_t[:], func=AF.Sigmoid), reads=[r_g], writes=[r_s])
            if b_ == 0:
                P.op("dve", lambda e: e.tensor_tensor(out=acc[:], in0=y_t[:], in1=s_t[:], op=ALU.mult), reads=[r_y, r_s], writes=[r_acc])
            elif b_ == 1:
                P.op("dve", lambda e: e.scalar_tensor_tensor(out=m_t[:], in0=y_t[:], scalar=bpb[:, j:j + 1], in1=s_t[:], op0=ALU.add, op1=ALU.mult),
                     reads=[r_y, r_bpb, r_s], writes=[r_m])
                P.op("pool", lambda e: e.tensor_tensor(out=acc[:], in0=acc[:], in1=m_t[:], op=ALU.add), reads=[r_acc, r_m], writes=[r_acc])
            else:
                P.op("dve", lambda e: e.tensor_tensor(out=m_t[:], in0=y_t[:], in1=s_t[:], op=ALU.mult), reads=[r_y, r_s], writes=[r_m])
                P.op("dve", lambda e: e.tensor_tensor(out=mT[:, j, :], in0=acc[:], in1=m_t[:], op=ALU.add), reads=[r_acc, r_m], writes=[r_mT])
            if j == 7 and b_ == 2:
                OUT(tt)

        def OUT(tt):
            for sub in range(4):
                x_t, r_x = xt[state["xi"] % 2]
                n_t, r_n = xn[state["xi"] % 2]
                state["xi"] += 1
                r0 = tt * 512 + sub * 128
                P.dma("sp", x_t[:], x_src[r0:r0 + 128, :], writes=[r_x])
                for half in range(2):
                    o_t, r_o = po[half]
                    hs = slice(half * 512, (half + 1) * 512)
                    for c in range(8):
                        P.op("pe", lambda e: e.matmul(o_t[:], lhsT=mT[:, c, sub * 128:(sub + 1) * 128], rhs=wo[:, c, hs], start=(c == 0), stop=(c == 7)),
                             reads=[r_mT, r_wo], writes=[r_o])
                    P.op("dve", lambda e: e.tensor_tensor(out=tmp[:], in0=o_t[:], in1=gmB[:, hs], op=ALU.mult), reads=[r_o, r_gmB], writes=[r_tmp])
                    P.op("pool", lambda e: e.tensor_tensor(out=n_t[:, hs], in0=tmp[:], in1=x_t[:, hs], op=ALU.add), reads=[r_tmp, r_x], writes=[r_n])
                P.dma("sp", x1_d[r0:r0 + 128, :], n_t[:], reads=[r_n])

        nU = len(units)
        F(0)
        for u in range(nU):
            if u + 1 < nU:
                F(u + 1)
            G(u)
        st.done(f"mg{l}")

    def stage_moe(l, dst):
        TS = min(S, 2048)
        NTS = TS // 128
        st = Stage(P)
        hT, r_hT = st.sb([128, 8, TS], BF16)
        acc, r_acc_all = st.sb([128, NTS, D], F32)
        r_acc = [[Res() for _ in range(2)] for _ in range(NTS)]
        w1b = [st.sb([128, 8, FF], BF16) for _ in range(2)]
        w3b = [st.sb([128, 8, FF], BF16) for _ in range(2)]
        w2b = [st.sb([128, 2, D], BF16) for _ in range(2)]
        gB = [st.sb([128, TS], F32) for _ in range(2)]
        gfB, r_gfB = st.sb([128, D], F32)
        pa = [st.ps([128, 512], F32) for _ in range(2)]
        pb = [st.ps([128, 512], F32) for _ in range(2)]
        po = [st.ps([128, 512], F32) for _ in range(4)]
        sa = [st.sb([128, 512], F32) for _ in range(2)]
        tb = [st.sb([128, 512], F32) for _ in range(2)]
        hid = [st.sb([128, 2, 512], BF16) for _ in range(2)]
        xt = [st.sb([128, D], F32) for _ in range(2)]
        xn = [st.sb([128, D], F32) for _ in range(2)]
        P.dma("sp", gfB[:], mod_d[l, 5 * D:6 * D].partition_broadcast(128), writes=[r_gfB])
        oi = 0
        for sti in range(S // TS):
            t0 = sti * TS
            P.dma("sp", hT[:], kp(h2T_d)[:, :, t0:t0 + TS], writes=[r_hT])
            units = [(ex, tt) for ex in range(NE + 1) for tt in range(TS // 512)]
            loaded = set()

            def load_w(ex):
                if ex in loaded or ex > NE:
                    return
                loaded.add(ex)
                w1_t, r_w1 = w1b[ex % 2]
                w3_t, r_w3 = w3b[ex % 2]
                w2_t, r_w2 = w2b[ex % 2]
                g_t, r_g = gB[ex % 2]
                if ex < NE:
                    P.dma("pool", w1_t[:], kp(I["w1"][l, ex]), writes=[r_w1])
                    P.dma("pool", w3_t[:], kp(I["w3"][l, ex]), writes=[r_w3])
                    P.dma("pool", w2_t[:], kp(I["w2"][l, ex]), writes=[r_w2])
                    P.dma("sp", g_t[:], gT_d[ex, t0:t0 + TS].partition_broadcast(128), writes=[r_g])
                else:
                    P.dma("pool", w1_t[:], kp(I["ws1"][l]), writes=[r_w1])
                    P.dma("pool", w3_t[:], kp(I["ws3"][l]), writes=[r_w3])
                    P.dma("pool", w2_t[:], kp(I["ws2"][l]), writes=[r_w2])
                    P.op("dve", lambda e: e.memset(g_t[:], 1.0), writes=[r_g])

            def up(i):
                ex, tt = units[i]
                load_w(ex)
                w1_t, r_w1 = w1b[ex % 2]
                w3_t, r_w3 = w3b[ex % 2]
                g_t, r_g = gB[ex % 2]
                ts_ = slice(tt * 512, (tt + 1) * 512)
                h_t, r_h = hid[i % 2]
                for f in range(2):
                    a_t, r_a = pa[f]
                    b_t, r_b = pb[f]
                    s_t, r_s = sa[f]
                    t_t, r_t = tb[f]
                    for c in range(8):
                        P.op("pe", lambda e: e.matmul(a_t[:], lhsT=w1_t[:, c, f * 128:(f + 1) * 128], rhs=hT[:, c, ts_], start=(c == 0), stop=(c == 7)),
                             reads=[r_w1, r_hT], writes=[r_a])
                    for c in range(8):
                        P.op("pe", lambda e: e.matmul(b_t[:], lhsT=w3_t[:, c, f * 128:(f + 1) * 128], rhs=hT[:, c, ts_], start=(c == 0), stop=(c == 7)),
                             reads=[r_w3, r_hT], writes=[r_b])
                    P.op("act", lambda e: e.activation(out=s_t[:], in_=a_t[:], func=AF.Silu), reads=[r_a], writes=[r_s])
                    P.op("dve", lambda e: e.tensor_tensor(out=t_t[:], in0=b_t[:], in1=g_t[:, ts_], op=ALU.mult), reads=[r_b, r_g], writes=[r_t])
                    P.op("dve", lambda e: e.tensor_tensor(out=h_t[:, f, :], in0=s_t[:], in1=t_t[:], op=ALU.mult), reads=[r_s, r_t], writes=[r_h])

            def down(i):
                nonlocal oi
                ex, tt = units[i]
                w2_t, r_w2 = w2b[ex % 2]
                h_t, r_h = hid[i % 2]
                for sub in range(4):
                    ti = tt * 4 + sub
                    for half in range(2):
                        o_t, r_o = po[oi % 4]
                        oi += 1
                        hs = slice(half * 512, (half + 1) * 512)
                        for f in range(2):
                            P.op("pe", lambda e: e.matmul(o_t[:], lhsT=h_t[:, f, sub * 128:(sub + 1) * 128], rhs=w2_t[:, f, hs], start=(f == 0), stop=(f == 1)),
                                 reads=[r_h, r_w2], writes=[r_o])
                        if ex == 0:
                            P.op("dve", lambda e: e.tensor_copy(out=acc[:, ti, hs], in_=o_t[:]), reads=[r_o], writes=[r_acc[ti][half]])
                        else:
                            P.op("dve", lambda e: e.tensor_tensor(out=acc[:, ti, hs], in0=o_t[:], in1=acc[:, ti, hs], op=ALU.add),
                                 reads=[r_o, r_acc[ti][half]], writes=[r_acc[ti][half]])

            n = len(units)
            load_w(0)
            load_w(1)
            up(0)
            for i in range(n):
                if i + 1 < n:
                    up(i + 1)
                down(i)
                if i + 1 < n and units[i + 1][0] != units[i][0]:
                    load_w(units[i][0] + 2)
            for ti in range(NTS):
                x_t, r_x = xt[ti % 2]
                n_t, r_n = xn[ti % 2]
                r0 = t0 + ti * 128
                P.dma("sp", x_t[:], x1_d[r0:r0 + 128, :], writes=[r_x])
                P.op("dve", lambda e: e.tensor_tensor(out=n_t[:], in0=acc[:, ti, :], in1=gfB[:], op=ALU.mult), reads=[r_acc[ti][0], r_acc[ti][1], r_gfB], writes=[r_n])
                P.op("pool", lambda e: e.tensor_tensor(out=n_t[:], in0=n_t[:], in1=x_t[:], op=ALU.add), reads=[r_n, r_x], writes=[r_n])
                P.dma("sp", dst[r0:r0 + 128, :], n_t[:], reads=[r_n])
        st.done(f"moe{l}")

    def stage_route(l):
        st = Stage(P)
        sbB, r_sbB = st.sb([128, NE], F32)
        P.dma("sp", sbB[:], sbase_d.partition_broadcast(128), writes=[r_sbB])
        pos = [st.sb([128, NE], F32) for _ in range(2)]
        gat = [st.sb([128, NE], F32) for _ in range(2)]
        hrow = [st.sb([128, D], BF16) for _ in range(2)]
        a_ = [st.sb([128, NE], F32) for _ in range(2)]
        sel_ = [st.sb([128, NE], F32) for _ in range(2)]
        t8 = [st.sb([128, 8], F32) for _ in range(2)]
        si = [st.sb([128, 8], I32) for _ in range(2)]
        gk = [st.sb([128, 8], F32) for _ in range(2)]
        junk = [st.sb([128, NE], F32) for _ in range(2)]

        def chain(t):
            b = t % 2
            rows = slice(t * 128, (t + 1) * 128)
            P.dma("sp", pos[b][0][:], pos_d[rows, :], writes=[pos[b][1]])
            P.dma("sp", gat[b][0][:], gate_d[rows, :], writes=[gat[b][1]])
            P.dma("sp", hrow[b][0][:], h2_d[rows, :], writes=[hrow[b][1]])
            yield
            P.op("dve", lambda e: e.tensor_scalar(out=sel_[b][0][:], in0=gat[b][0][:], scalar1=0.0, scalar2=None, op0=ALU.is_gt),
                 reads=[gat[b][1]], writes=[sel_[b][1]])
            yield
            P.op("dve", lambda e: e.tensor_tensor(out=a_[b][0][:], in0=pos[b][0][:], in1=sbB[:], op=ALU.add), reads=[pos[b][1], r_sbB], writes=[a_[b][1]])
            yield
            P.op("dve", lambda e: e.tensor_tensor(out=a_[b][0][:], in0=a_[b][0][:], in1=sel_[b][0][:], op=ALU.mult), reads=[a_[b][1], sel_[b][1]], writes=[a_[b][1]])
            yield
            P.op("dve", lambda e: e.tensor_scalar(out=a_[b][0][:], in0=a_[b][0][:], scalar1=-1.0, scalar2=None, op0=ALU.add), reads=[a_[b][1]], writes=[a_[b][1]])
            yield
            P.op("dve", lambda e: e.max(out=t8[b][0][:], in_=a_[b][0][:]), reads=[a_[b][1]], writes=[t8[b][1]])
            yield
            P.op("dve", lambda e: e.tensor_copy(out=si[b][0][:], in_=t8[b][0][:]), reads=[t8[b][1]], writes=[si[b][1]])
            yield
            for k in range(8):
                P.op("dve", lambda e: e.scalar_tensor_tensor(out=junk[b][0][:], in0=a_[b][0][:], scalar=t8[b][0][:, k:k + 1], in1=gat[b][0][:], op0=ALU.is_equal, op1=ALU.mult),
                     reads=[a_[b][1], t8[b][1], gat[b][1]], writes=[junk[b][1]])
                yield
                P.op("dve", lambda e: e.tensor_reduce(out=gk[b][0][:, k:k + 1], in_=junk[b][0][:], axis=AX.X, op=ALU.add), reads=[junk[b][1]], writes=[gk[b][1]])
                yield
            P.dma("sp", slotk_d[rows, :], si[b][0][:], reads=[si[b][1]])
            P.dma("sp", gk_d[rows, :], gk[b][0][:], reads=[gk[b][1]])
            for k in range(8):
                def fs(eng, b=b, k=k):
                    return eng.indirect_dma_start(out=xs_d[:, :], out_offset=bass.IndirectOffsetOnAxis(ap=si[b][0][:, k:k + 1], axis=0),
                                                  in_=hrow[b][0][:, :], in_offset=None)
                P.raw("pool", fs, reads=[si[b][1], hrow[b][1]], is_dma=True)
            yield

        def drain(*gens):
            gens = list(gens)
            while gens:
                for g in list(gens):
                    try:
                        next(g)
                    except StopIteration:
                        gens.remove(g)

        for t in range(0, NT, 2):
            drain(*[chain(tt_) for tt_ in range(t, min(t + 2, NT))])
        st.done(f"route{l}")

    def stage_moe2(l):
        st = Stage(P)
        identb, r_idb = st.sb([128, 128], BF16)
        ebB, r_ebB = st.sb([128, 128], F32)
        iotac, r_iotac = st.sb([128, 1], F32)
        idxw, r_idxw = st.sb([128, 128], I32)
        w1b = [st.sb([128, 8, FF], BF16) for _ in range(3)]
        w3b = [st.sb([128, 8, FF], BF16) for _ in range(3)]
        w2b = [st.sb([128, 2, D], BF16) for _ in range(3)]
        xtok = [st.sb([128, 4, D], BF16) for _ in range(3)]
        XT = [st.sb([128, 8, 512], BF16) for _ in range(3)]
        hid = [st.sb([128, 2, 512], BF16) for _ in range(2)]
        sa = [st.sb([128, 512], F32) for _ in range(2)]
        ysb = [st.sb([128, 4, D], BF16) for _ in range(2)]
        pt = [st.ps([128, 8, 128], BF16) for _ in range(2)]
        pa = [st.ps([128, 512], F32) for _ in range(2)]
        pb = [st.ps([128, 512], F32) for _ in range(2)]
        po = [st.ps([128, 512], F32) for _ in range(2)]
        P.dma("pool", identb[:], I["k_ident"], writes=[r_idb])
        P.dma("sp", ebB[:], eb_d.partition_broadcast(128), writes=[r_ebB])
        P.dma("sp", iotac[:], col(I["k_iota"]), writes=[r_iotac])
        P.op("dve", lambda e: e.tensor_scalar(out=ebB[:], in0=ebB[:], scalar1=128.0, scalar2=float(l * NE * 128), op0=ALU.mult, op1=ALU.add),
             reads=[r_ebB], writes=[r_ebB])
        P.op("dve", lambda e: e.tensor_scalar(out=ebB[:], in0=ebB[:], scalar1=iotac[:, 0:1], scalar2=None, op0=ALU.add), reads=[r_ebB, r_iotac], writes=[r_ebB])
        P.op("dve", lambda e: e.tensor_copy(out=idxw[:], in_=ebB[:]), reads=[r_ebB], writes=[r_idxw])
        NU = NB + NQ
        state = {"oi": 0}

        def load(u):
            if u >= NU:
                return
            bf = u % 3
            if u < NB:
                for nm, (w_t, r_w) in (("w1h", w1b[bf]), ("w3h", w3b[bf]), ("w2h", w2b[bf])):
                    def fg(eng, nm=nm, w_t=w_t, u=u):
                        return eng.indirect_dma_start(out=w_t[:].rearrange("p c f -> p (c f)"), out_offset=None, in_=I[nm][:, :],
                                                      in_offset=bass.IndirectOffsetOnAxis(ap=idxw[:, u:u + 1], axis=0))
                    P.raw("pool", fg, reads=[r_idxw], writes=[r_w], is_dma=True)
                P.dma("sp", xtok[bf][0][:], xs_d[u * 512:(u + 1) * 512, :].rearrange("(s p) d -> p s d", p=128), writes=[xtok[bf][1]])
            else:
                if u in (NB, NB + 1, NB + 2):
                    P.dma("pool", w1b[bf][0][:], kp(I["ws1"][l]), writes=[w1b[bf][1]])
                    P.dma("pool", w3b[bf][0][:], kp(I["ws3"][l]), writes=[w3b[bf][1]])
                    P.dma("pool", w2b[bf][0][:], kp(I["ws2"][l]), writes=[w2b[bf][1]])
                tt = u - NB
                P.dma("sp", XT[bf][0][:], kp(h2T_d)[:, :, tt * 512:(tt + 1) * 512], writes=[XT[bf][1]])

        def tr(u):
            if u >= NB:
                return
            bf = u % 3
            x_t, r_x = xtok[bf]
            X_t, r_X = XT[bf]
            for sub in range(4):
                p_t, r_p = pt[sub % 2]
                for c in range(8):
                    P.op("pe", lambda e: e.transpose(out=p_t[:, c, :], in_=x_t[:, sub, c * 128:(c + 1) * 128], identity=identb[:]),
                         reads=[r_x, r_idb], writes=[r_p])
                if sub % 2 == 0:
                    P.op("act", lambda e: e.copy(out=X_t[:, :, sub * 128:(sub + 1) * 128], in_=p_t[:]), reads=[r_p], writes=[r_X])
                else:
                    P.op("dve", lambda e: e.tensor_copy(out=X_t[:, :, sub * 128:(sub + 1) * 128], in_=p_t[:]), reads=[r_p], writes=[r_X])

        def up(u):
            bf = u % 3
            w1_t, r_w1 = w1b[bf]
            w3_t, r_w3 = w3b[bf]
            X_t, r_X = XT[bf]
            h_t, r_h = hid[u % 2]
            for f in range(2):
                a_t, r_a = pa[f]
                b_t, r_b = pb[f]
                s_t, r_s = sa[f]
                for c in range(8):
                    P.op("pe", lambda e: e.matmul(a_t[:], lhsT=w1_t[:, c, f * 128:(f + 1) * 128], rhs=X_t[:, c, :], start=(c == 0), stop=(c == 7)),
                         reads=[r_w1, r_X], writes=[r_a])
                for c in range(8):
                    P.op("pe", lambda e: e.matmul(b_t[:], lhsT=w3_t[:, c, f * 128:(f + 1) * 128], rhs=X_t[:, c, :], start=(c == 0), stop=(c == 7)),
                         reads=[r_w3, r_X], writes=[r_b])
                P.op("act", lambda e: e.activation(out=s_t[:], in_=a_t[:], func=AF.Silu), reads=[r_a], writes=[r_s])
                P.op("dve", lambda e: e.tensor_tensor(out=h_t[:, f, :], in0=b_t[:], in1=s_t[:], op=ALU.mult), reads=[r_b, r_s], writes=[r_h])

        def down(u):
            w2_t, r_w2 = w2b[u % 3]
            h_t, r_h = hid[u % 2]
            y_t, r_y = ysb[u % 2]
            for sub in range(4):
                for half in range(2):
                    o_t, r_o = po[state["oi"] % 2]
                    state["oi"] += 1
                    hs = slice(half * 512, (half + 1) * 512)
                    for f in range(2):
                        P.op("pe", lambda e: e.matmul(o_t[:], lhsT=h_t[:, f, sub * 128:(sub + 1) * 128], rhs=w2_t[:, f, hs], start=(f == 0), stop=(f == 1)),
                             reads=[r_h, r_w2], writes=[r_o])
                    if half == 0:
                        P.op("act", lambda e: e.copy(out=y_t[:, sub, hs], in_=o_t[:]), reads=[r_o], writes=[r_y])
                    else:
                        P.op("dve", lambda e: e.tensor_copy(out=y_t[:, sub, hs], in_=o_t[:]), reads=[r_o], writes=[r_y])
            if u < NB:
                P.dma("sp", ys_d[u * 512:(u + 1) * 512, :].rearrange("(s p) d -> p s d", p=128), y_t[:], reads=[r_y])
            else:
                tt = u - NB
                P.dma("sp", ysh_d[tt * 512:(tt + 1) * 512, :].rearrange("(s p) d -> p s d", p=128), y_t[:], reads=[r_y])

        load(0)
        load(1)
        tr(0)
        up(0)
        for u in range(NU):
            load(u + 2)
            if u + 1 < NU:
                tr(u + 1)
                up(u + 1)
            down(u)
        st.done(f"moe{l}")

    def stage_comb(l, dst):
        st = Stage(P)
        gfB, r_gfB = st.sb([128, D], F32)
        P.dma("sp", gfB[:], mod_d[l, 5 * D:6 * D].partition_broadcast(128), writes=[r_gfB])
        si = [st.sb([128, 8], I32) for _ in range(2)]
        gk = [st.sb([128, 8], F32) for _ in range(2)]
        xt = [st.sb([128, D], F32) for _ in range(2)]
        ysh = [st.sb([128, D], BF16) for _ in range(2)]
        yg = [[st.sb([128, D], BF16) for _ in range(8)] for _ in range(2)]
        acc = [st.sb([128, D], F32) for _ in range(2)]
        def fetch(t):
            if t >= NT:
                return
            b = t % 2
            rows = slice(t * 128, (t + 1) * 128)
            P.dma("sp", si[b][0][:], slotk_d[rows, :], writes=[si[b][1]])
            P.dma("sp", gk[b][0][:], gk_d[rows, :], writes=[gk[b][1]])
            P.dma("sp", xt[b][0][:], x1_d[rows, :], writes=[xt[b][1]])
            P.dma("sp", ysh[b][0][:], ysh_d[rows, :], writes=[ysh[b][1]])
            for k in range(8):
                def fg(eng, b=b, k=k):
                    return eng.indirect_dma_start(out=yg[b][k][0][:, :], out_offset=None, in_=ys_d[:, :],
                                                  in_offset=bass.IndirectOffsetOnAxis(ap=si[b][0][:, k:k + 1], axis=0))
                P.raw("pool", fg, reads=[si[b][1]], writes=[yg[b][k][1]], is_dma=True)

        def comp(t):
            b = t % 2
            rows = slice(t * 128, (t + 1) * 128)
            a_t, r_a = acc[b]
            P.op("dve", lambda e: e.scalar_tensor_tensor(out=a_t[:], in0=yg[b][0][0][:], scalar=gk[b][0][:, 0:1], in1=ysh[b][0][:], op0=ALU.mult, op1=ALU.add),
                 reads=[yg[b][0][1], gk[b][1], ysh[b][1]], writes=[r_a])
            for k in range(1, 8):
                P.op("dve", lambda e: e.scalar_tensor_tensor(out=a_t[:], in0=yg[b][k][0][:], scalar=gk[b][0][:, k:k + 1], in1=a_t[:], op0=ALU.mult, op1=ALU.add),
                     reads=[yg[b][k][1], gk[b][1], r_a], writes=[r_a])
            P.op("dve", lambda e: e.tensor_tensor(out=a_t[:], in0=a_t[:], in1=gfB[:], op=ALU.mult), reads=[r_a, r_gfB], writes=[r_a])
            P.op("dve", lambda e: e.tensor_tensor(out=a_t[:], in0=a_t[:], in1=xt[b][0][:], op=ALU.add), reads=[r_a, xt[b][1]], writes=[r_a])
            P.dma("sp", dst[rows, :], a_t[:], reads=[r_a])

        fetch(0)
        for t in range(NT):
            comp_deferred = t
            fetch(t + 1)
            comp(t)
        st.done(f"comb{l}")

    todo = stages if stages is not None else ("mod", "rope", "norm1", "proj")
    if "mod" in todo:
        stage_mod()
    if "rope" in todo:
        stage_rope()
    for l in range(L):
        x_in = I["x"] if l == 0 else x2_d
        if "norm1" in todo:
            stage_norm(l, x_in, "norm_mix_g", 1 * D, 0 * D, hT_d, router=False)
        if "proj" in todo:
            stage_proj(l)
        if "da" in todo:
            stage_da(l)
        if "sb" in todo:
            stage_sb(l)
        if "cv" in todo:
            stage_cv(l)
        if "mg" in todo:
            stage_mg(l, x_in)
        if "norm2" in todo:
            stage_norm(l, x1_d, "norm_ffn_g", 4 * D, 3 * D, h2T_d, router=True)
        if "moe" in todo:
            stage_moe(l, out if l == L - 1 else x2_d)
        if "smoe" in todo:
            stage_route(l)
            stage_moe2(l)
            stage_comb(l, out if l == L - 1 else x2_d)
    es.close()
    return nc


ALL_STAGES = ("mod", "rope", "norm1", "proj", "da", "sb", "cv", "mg", "norm2", "smoe")
_CACHE = {}


def kernel(**inputs):
    x = np.ascontiguousarray(np.asarray(inputs["x"], dtype=np.float32))
    B, S, _ = x.shape
    L = int(np.asarray(inputs["w_mod"]).shape[0])
    key = (S, L)
    if key not in _CACHE:
        _CACHE[key] = build(S, L=L, stages=ALL_STAGES)
    nc = _CACHE[key]
    consts = make_consts()
    shared = {k: np.ascontiguousarray(np.asarray(inputs[k], dtype=np.float32)) for k in W_SHAPES if k not in ("w1", "w3", "w2")}
    shared.update(relayout_experts(inputs, L))
    c = np.asarray(inputs["c"], dtype=np.float32)
    pos = np.asarray(inputs["positions"]).astype(np.int32)
    in_maps = []
    for b in range(B):
        m = {"x": x[b], "c": np.ascontiguousarray(c[b]), "pos": np.ascontiguousarray(pos[b])}
        m.update(shared)
        m.update(consts)
        in_maps.append(m)
    res = run_bass_kernel_spmd(nc, in_maps, core_ids=list(range(B)))
    return np.stack([np.asarray(r["out"], dtype=np.float32) for r in res.results], axis=0)


def relayout_experts(W, L):
    o = {}
    w1 = np.asarray(W["w1"], dtype=np.float32)[:L]
    w3 = np.asarray(W["w3"], dtype=np.float32)[:L]
    w2 = np.asarray(W["w2"], dtype=np.float32)[:L]
    o["w1h"] = np.ascontiguousarray(w1.reshape(L, NE, 8, 128, FF).transpose(0, 1, 3, 2, 4)).reshape(L * NE * 128, 8 * FF)
    o["w3h"] = np.ascontiguousarray(w3.reshape(L, NE, 8, 128, FF).transpose(0, 1, 3, 2, 4)).reshape(L * NE * 128, 8 * FF)
    o["w2h"] = np.ascontiguousarray(w2.reshape(L, NE, 2, 128, D).transpose(0, 1, 3, 2, 4)).reshape(L * NE * 128, 2 * D)
    return o
```

```python
import math
from contextlib import ExitStack
import numpy as np
import concourse.bass as bass
import concourse.mybir as mybir
from concourse.bass_utils import run_bass_kernel_spmd

F32 = mybir.dt.float32
BF16 = mybir.dt.bfloat16
I32 = mybir.dt.int32
ALU = mybir.AluOpType
AF = mybir.ActivationFunctionType
AX = mybir.AxisListType

ENGS = ("pe", "act", "dve", "pool", "sp")
SEM_LIM = 30000
DMA_RING = 8

D = 1024
NE = 64
FF = 256
EPS = 1e-6


class Res:
    __slots__ = ("w", "r")

    def __init__(self):
        self.w = None
        self.r = []


class Op:
    __slots__ = ("eng", "fn", "deps", "signal", "k", "is_dma", "slot", "dval")

    def __init__(self, eng, fn, is_dma=False):
        self.eng = eng
        self.fn = fn
        self.deps = set()
        self.signal = False
        self.k = None
        self.is_dma = is_dma
        self.slot = None
        self.dval = None


class _Rec:
    def __getattr__(self, name):
        return lambda *a, **k: (name, a, k)


_REC = _Rec()


class Prog:
    def __init__(self, nc, es):
        self.nc = nc
        self.sems = {e: [es.enter_context(nc.semaphore(f"s_{e}_{i}")) for i in range(6)] for e in ENGS if e != "sp"}
        self.nsig = {e: 0 for e in ENGS}
        self.dq = ("sp", "act", "pool")
        self.dsems = {e: [es.enter_context(nc.semaphore(f"d_{e}_{i}")) for i in range(DMA_RING)] for e in self.dq}
        self.ndma = {e: 0 for e in self.dq}
        self.ring_last = {e: [None] * DMA_RING for e in self.dq}
        self.waited = {e: {} for e in ENGS}
        self.ops = []
        self.last_op = {e: None for e in ENGS}
        self.stage_dmas = []

    def _add(self, op, reads, writes):
        deps = set()
        for r in reads:
            if r.w is not None:
                deps.add(r.w)
        for w in writes:
            if w.w is not None:
                deps.add(w.w)
            for o in w.r:
                deps.add(o)
        for d in deps:
            if d is op:
                continue
            if (not d.is_dma) and d.eng == op.eng and op.eng == "pe" and not op.is_dma:
                continue
            op.deps.add(d)
            if not d.is_dma:
                d.signal = True
        for r in reads:
            r.r.append(op)
        for w in writes:
            w.w = op
            w.r = []
        self.ops.append(op)
        if not op.is_dma:
            self.last_op[op.eng] = op
        return op

    def op(self, eng, fn, reads=(), writes=()):
        return self._add(Op(eng, fn(_REC)), reads, writes)

    def dma(self, q, out, in_, reads=(), writes=()):
        op = Op(q, ("dma_start", (), dict(out=out, in_=in_)), is_dma=True)
        i = self.ndma[q]
        self.ndma[q] += 1
        op.slot = i % DMA_RING
        op.dval = 16 * (i // DMA_RING + 1)
        prev = self.ring_last[q][op.slot]
        if prev is not None:
            op.deps.add(prev)
        self.ring_last[q][op.slot] = op
        self.stage_dmas.append(op)
        return self._add(op, reads, writes)

    def raw(self, eng, fn, reads=(), writes=(), is_dma=False):
        op = Op(eng, fn, is_dma=is_dma)
        if is_dma:
            i = self.ndma[eng]
            self.ndma[eng] += 1
            op.slot = i % DMA_RING
            op.dval = 16 * (i // DMA_RING + 1)
            prev = self.ring_last[eng][op.slot]
            if prev is not None:
                op.deps.add(prev)
            self.ring_last[eng][op.slot] = op
            self.stage_dmas.append(op)
        return self._add(op, reads, writes)

    def barrier(self):
        lasts = [o for o in self.last_op.values() if o is not None]
        dmas = list(self.stage_dmas)
        for e in ENGS:
            op = Op(e, None)
            for d in lasts:
                if d.eng != e:
                    op.deps.add(d)
                    d.signal = True
            for d in dmas:
                op.deps.add(d)
            self.ops.append(op)
        self.stage_dmas = []
        self.last_op = {e: None for e in ENGS}

    def emit(self):
        nc = self.nc
        self.barrier()
        ops = self.ops
        self.ops = []
        for o in ops:
            if o.signal and not o.is_dma and o.k is None:
                o.k = self.nsig[o.eng]
                self.nsig[o.eng] += 1
        per = {e: [o for o in ops if o.eng == e] for e in ENGS}
        engobj = {"pe": "tensor", "act": "scalar", "dve": "vector", "pool": "gpsimd", "sp": "sync"}

        def run(e, eng):
            waited = self.waited[e]
            for o in per[e]:
                need = {}
                for d in o.deps:
                    if d.is_dma:
                        key = ("d", d.eng, d.slot)
                        sem = self.dsems[d.eng][d.slot]
                        val = d.dval
                    else:
                        key = ("c", d.eng, d.k // SEM_LIM)
                        sem = self.sems[d.eng][d.k // SEM_LIM]
                        val = d.k % SEM_LIM + 1
                    if waited.get(key, 0) >= val:
                        continue
                    if key not in need or need[key][1] < val:
                        need[key] = (sem, val)
                for key, (sem, val) in need.items():
                    eng.wait_ge(sem, val)
                    waited[key] = val
                if o.fn is None:
                    continue
                if callable(o.fn):
                    ins = o.fn(eng)
                else:
                    name, a, k = o.fn
                    ins = getattr(eng, name)(*a, **k)
                if o.is_dma:
                    ins.then_inc(self.dsems[o.eng][o.slot], 16)
                elif o.signal:
                    ins.then_inc(self.sems[o.eng][o.k // SEM_LIM], 1)

        with nc.allow_non_contiguous_dma(reason="small strided parameter loads"):
            with nc.Block() as block:
                for e in ENGS:
                    if not per[e]:
                        continue
                    getattr(block, engobj[e])(lambda eng, e=e: run(e, eng))


class Stage:
    count = 0

    def __init__(self, P):
        self.P = P
        self.nc = P.nc
        self.es = ExitStack()
        self.n = 0
        Stage.count += 1
        self.sid = Stage.count

    def sb(self, shape, dt):
        self.n += 1
        t = self.es.enter_context(self.nc.sbuf_tensor(f"t{self.sid}_{self.n}", list(shape), dt))
        return t, Res()

    def ps(self, shape, dt):
        self.n += 1
        t = self.es.enter_context(self.nc.psum_tensor(f"p{self.sid}_{self.n}", list(shape), dt))
        return t, Res()

    def done(self, name=None):
        if name:
            with self.nc.named_scope(name):
                self.P.emit()
        else:
            self.P.emit()
        self.es.close()


def col(ap1d):
    return ap1d.rearrange("(p o) -> p o", o=1)


def make_consts():
    p = np.arange(128)
    ident = np.eye(128, dtype=np.float32)
    blk64 = ((p[:, None] // 64) == (p[None, :] // 64)).astype(np.float32) / 64.0
    rotT = np.zeros((128, 128), np.float32)
    for k in range(128):
        if k % 64 >= 32:
            rotT[k, k - 32] = -1.0
        else:
            rotT[k, k + 32] = 1.0
    ones = np.ones((128, 128), np.float32)
    ustrict = (p[:, None] > p[None, :]).astype(np.float32)
    uincl = (p[:, None] >= p[None, :]).astype(np.float32)
    ulow = (p[:, None] < p[None, :]).astype(np.float32)
    pincl = (p[:, None] <= p[None, :]).astype(np.float32)
    iota = p.astype(np.float32)
    qq = np.arange(512)
    maskc = np.stack([((qq[None, :] - j * 128 - p[:, None]) >= 0) for j in range(4)]).astype(np.float32)
    masks = np.stack([((qq[None, :] - j * 128 - p[:, None]) > 0) for j in range(4)]).astype(np.float32)
    inv = (10000.0 ** (-np.arange(0, 64, 2, dtype=np.float32) / np.float32(64))).astype(np.float32)
    invc = inv[p % 32].astype(np.float32)
    return dict(k_ident=ident, k_blk64=blk64, k_rotT=rotT, k_ones=ones, k_ustrict=ustrict, k_uincl=uincl, k_ulow=ulow, k_pincl=pincl, k_iota=iota,
                k_maskc=maskc, k_masks=masks, k_invc=invc)


W_SHAPES = dict(
    w_mod=(D, 6 * D), b_mod=(6 * D,), norm_mix_g=(D,), norm_ffn_g=(D,), w_in=(D, 7168),
    qn_g=(64,), kn_g=(64,), lam_q1=(64,), lam_k1=(64,), lam_q2=(64,), lam_k2=(64,), subln_g=(128,),
    w_proj_a=(512, D), w_dw=(31, 512), b_dw=(512,), conv_ln_g=(512,), conv_ln_b=(512,),
    w_proj_b=(512, D), b_proj_b=(D,), w_proj_c=(512, D), w_out=(D, D), w_router=(D, NE), b_router=(NE,),
    w1=(NE, D, FF), w3=(NE, D, FF), w2=(NE, FF, D), ws1=(D, FF), ws3=(D, FF), ws2=(FF, D),
)


def build(S, L=2, dbg=(), stages=None):
    NT = S // 128
    NQ = S // 512
    nc = bass.Bass("TRN2", target_bir_lowering=False)
    I = {}
    I["x"] = nc.dram_tensor("x", [S, D], F32, kind="ExternalInput").ap()
    I["c"] = nc.dram_tensor("c", [D], F32, kind="ExternalInput").ap()
    I["pos"] = nc.dram_tensor("pos", [S], I32, kind="ExternalInput").ap()
    dense_moe = stages is not None and "moe" in stages
    for k, shp in W_SHAPES.items():
        if k in ("w1", "w3", "w2") and not dense_moe:
            continue
        I[k] = nc.dram_tensor(k, [L] + list(shp), F32, kind="ExternalInput").ap()
    for k, v in make_consts().items():
        I[k] = nc.dram_tensor(k, list(v.shape), F32, kind="ExternalInput").ap()
    for k in ("w1h", "w3h", "w2h"):
        I[k] = nc.dram_tensor(k, [L * NE * 128, 2048], F32, kind="ExternalInput").ap()
    out = nc.dram_tensor("out", [S, D], F32, kind="ExternalOutput").ap()
    NB = S * 8 // 512 + NE
    NSLOT = NB * 512

    def scratch(name, shape, dt):
        kind = "ExternalOutput" if name in dbg else "Internal"
        return nc.dram_tensor(name, list(shape), dt, kind=kind).ap()

    mod_d = scratch("mod_d", [L, 6 * D], F32)
    cos_d = scratch("cos_d", [128, S], F32)
    sin_d = scratch("sin_d", [128, S], F32)
    hT_d = scratch("hT_d", [D, S], BF16)
    qaT_d = scratch("qaT_d", [512, S], BF16)
    kaT_d = scratch("kaT_d", [512, S], BF16)
    va_d = scratch("va_d", [S, 512], BF16)
    uT_d = scratch("uT_d", [512, S], BF16)
    qcT_d = scratch("qcT_d", [512, S], BF16)
    kcT_d = scratch("kcT_d", [512, S], BF16)
    vc_d = scratch("vc_d", [S, 512], BF16)
    oaT_d = scratch("oaT_d", [512, S], BF16)
    cvT_d = scratch("cvT_d", [512, S], BF16)
    ocT_d = scratch("ocT_d", [512, S], BF16)
    x1_d = scratch("x1_d", [S, D], F32)
    x2_d = scratch("x2_d", [S, D], F32)
    h2T_d = scratch("h2T_d", [D, S], BF16)
    gT_d = scratch("gT_d", [NE, S], F32)
    gate_d = scratch("gate_d", [S, NE], F32)
    pos_d = scratch("pos_d", [S, NE], F32)
    h2_d = scratch("h2_d", [S, D], BF16)
    sbase_d = scratch("sbase_d", [NE], F32)
    eb_d = scratch("eb_d", [128], F32)
    slotk_d = scratch("slotk_d", [S, 8], I32)
    gk_d = scratch("gk_d", [S, 8], F32)
    xs_d = scratch("xs_d", [NSLOT, D], BF16)
    ys_d = scratch("ys_d", [NSLOT, D], BF16)
    ysh_d = scratch("ysh_d", [S, D], BF16)

    def kp(ap2d):
        return ap2d.rearrange("(c p) n -> p c n", p=128)

    es = ExitStack()
    P = Prog(nc, es)

    def stage_mod():
        st = Stage(P)
        cT, r_cT = st.sb([128, 8], F32)
        cA, r_cA = st.sb([128, 8], F32)
        wm = [st.sb([128, 8, 512], F32) for _ in range(2)]
        bm, r_bm = st.sb([1, L * 6 * D], F32)
        mr, r_mr = st.sb([1, L * 6 * D], F32)
        pm = [st.ps([1, 512], F32) for _ in range(2)]
        P.dma("sp", cT[:], I["c"].rearrange("(c p) -> p c", p=128), writes=[r_cT])
        P.dma("sp", bm[:], I["b_mod"].rearrange("(o l) n -> o (l n)", o=1), writes=[r_bm])
        P.op("act", lambda e: e.activation(out=cA[:], in_=cT[:], func=AF.Silu), reads=[r_cT], writes=[r_cA])
        i = 0
        for l in range(L):
            for blk in range(12):
                w_t, r_w = wm[i % 2]
                p_t, r_p = pm[i % 2]
                P.dma("sp", w_t[:], kp(I["w_mod"][l])[:, :, blk * 512:(blk + 1) * 512], writes=[r_w])
                for c in range(8):
                    P.op("pe", lambda e, c=c, w_t=w_t, p_t=p_t: e.matmul(p_t[:], lhsT=cA[:, c:c + 1], rhs=w_t[:, c, :], start=(c == 0), stop=(c == 7)),
                         reads=[r_cA, r_w], writes=[r_p])
                o = l * 6 * D + blk * 512
                P.op("dve", lambda e, o=o, p_t=p_t: e.tensor_tensor(out=mr[:, o:o + 512], in0=p_t[:], in1=bm[:, o:o + 512], op=ALU.add),
                     reads=[r_p, r_bm], writes=[r_mr])
                i += 1
        P.dma("sp", mod_d.rearrange("(o l) n -> o (l n)", o=1), mr[:], reads=[r_mr])
        st.done("mod")

    def stage_rope():
        st = Stage(P)
        pi_t, r_pi = st.sb([128, S], I32)
        pf, r_pf = st.sb([128, S], F32)
        inv, r_inv = st.sb([128, 1], F32)
        ang, r_ang = st.sb([128, S], F32)
        u, r_u = st.sb([128, S], F32)
        ki, r_ki = st.sb([128, S], I32)
        kf, r_kf = st.sb([128, S], F32)
        m, r_m = st.sb([128, S], F32)
        res_t, r_res = st.sb([128, S], F32)
        zero, r_zero = st.sb([128, 1], F32)
        TWO_PI = 2.0 * math.pi
        C1 = 6.28125
        C2 = TWO_PI - C1
        P.dma("sp", pi_t[:], I["pos"].partition_broadcast(128), writes=[r_pi])
        P.dma("sp", inv[:], col(I["k_invc"]), writes=[r_inv])
        P.op("pool", lambda e: e.memset(zero[:], 0.0), writes=[r_zero])
        P.op("dve", lambda e: e.tensor_copy(out=pf[:], in_=pi_t[:]), reads=[r_pi], writes=[r_pf])
        for which, dst in ((0, sin_d), (1, cos_d)):
            shift = 0.0 if which == 0 else math.pi / 2
            P.op("dve", lambda e, shift=shift: e.tensor_scalar(out=ang[:], in0=pf[:], scalar1=inv[:, 0:1], scalar2=shift, op0=ALU.mult, op1=ALU.add),
                 reads=[r_pf, r_inv], writes=[r_ang])
            P.op("dve", lambda e: e.tensor_scalar(out=u[:], in0=ang[:], scalar1=1.0 / TWO_PI, scalar2=None, op0=ALU.mult),
                 reads=[r_ang], writes=[r_u])
            P.op("dve", lambda e: e.tensor_copy(out=ki[:], in_=u[:]), reads=[r_u], writes=[r_ki])
            P.op("dve", lambda e: e.tensor_copy(out=kf[:], in_=ki[:]), reads=[r_ki], writes=[r_kf])
            P.op("dve", lambda e: e.scalar_tensor_tensor(out=u[:], in0=kf[:], scalar=-C1, in1=ang[:], op0=ALU.mult, op1=ALU.add),
                 reads=[r_kf, r_ang], writes=[r_u])
            P.op("dve", lambda e: e.scalar_tensor_tensor(out=u[:], in0=kf[:], scalar=-C2, in1=u[:], op0=ALU.mult, op1=ALU.add),
                 reads=[r_kf, r_u], writes=[r_u])
            P.op("dve", lambda e: e.tensor_scalar(out=m[:], in0=u[:], scalar1=math.pi, scalar2=-TWO_PI, op0=ALU.is_gt, op1=ALU.mult),
                 reads=[r_u], writes=[r_m])
            P.op("dve", lambda e: e.tensor_tensor(out=u[:], in0=u[:], in1=m[:], op=ALU.add), reads=[r_u, r_m], writes=[r_u])
            P.op("dve", lambda e: e.tensor_scalar(out=m[:], in0=u[:], scalar1=-math.pi, scalar2=TWO_PI, op0=ALU.is_lt, op1=ALU.mult),
                 reads=[r_u], writes=[r_m])
            P.op("dve", lambda e: e.tensor_tensor(out=u[:], in0=u[:], in1=m[:], op=ALU.add), reads=[r_u, r_m], writes=[r_u])
            P.op("act", lambda e: e.activation(out=res_t[:], in_=u[:], func=AF.Sin, bias=zero[:, 0:1]), reads=[r_u, r_zero], writes=[r_res])
            P.dma("sp", dst, res_t[:], reads=[r_res])
        st.done("rope")

    def stage_norm(l, x_src, g_name, sc_off, sh_off, hT_dst, router):
        st = Stage(P)
        gB, r_gB = st.sb([128, D], F32)
        scB, r_scB = st.sb([128, D], F32)
        shB, r_shB = st.sb([128, D], F32)
        A, r_A = st.sb([128, D], F32)
        epsb, r_eps = st.sb([128, 1], F32)
        identb, r_idb = st.sb([128, 128], BF16)
        xt = [st.sb([128, D], F32) for _ in range(4)]
        sq, r_sq = st.sb([128, D], F32)
        ss, r_ss = st.sb([128, 1], F32)
        rstd, r_rstd = st.sb([128, 1], F32)
        hf, r_hf = st.sb([128, D], F32)
        hb, r_hb = st.sb([128, D], BF16)
        hTs = [st.sb([128, 8, 128], BF16) for _ in range(2)]
        pt = [st.ps([128, 8, 128], BF16) for _ in range(2)]
        P.dma("sp", gB[:], I[g_name][l].partition_broadcast(128), writes=[r_gB])
        P.dma("sp", scB[:], mod_d[l, sc_off:sc_off + D].partition_broadcast(128), writes=[r_scB])
        P.dma("sp", shB[:], mod_d[l, sh_off:sh_off + D].partition_broadcast(128), writes=[r_shB])
        P.dma("pool", identb[:], I["k_ident"], writes=[r_idb])
        P.op("pool", lambda e: e.memset(epsb[:], EPS), writes=[r_eps])
        P.op("dve", lambda e: e.scalar_tensor_tensor(out=A[:], in0=scB[:], scalar=1.0, in1=gB[:], op0=ALU.add, op1=ALU.mult),
             reads=[r_scB, r_gB], writes=[r_A])
        if router:
            identf, r_idf = st.sb([128, 128], F32)
            wr, r_wr = st.sb([128, 8, NE], F32)
            brB, r_brB = st.sb([128, NE], F32)
            h32, r_h32 = st.sb([128, D], F32)
            hTf, r_hTf = st.sb([128, 8, 128], F32)
            ptf = [st.ps([128, 4, 128], F32) for _ in range(2)]
            plg, r_plg = st.ps([128, NE], F32)
            PC, r_PC = st.ps([128, NE], F32)
            pinc, r_pinc = st.sb([128, 128], F32)
            pgtm, r_pgtm = st.sb([128, 128], F32)
            iotac, r_iotac = st.sb([128, 1], F32)
            posb = [st.sb([128, NE], F32) for _ in range(4)]
            selb = [st.sb([128, NE], F32) for _ in range(4)]
            P.dma("sp", pinc[:], I["k_pincl"], writes=[r_pinc])
            P.dma("sp", pgtm[:], I["k_ustrict"], writes=[r_pgtm])
            P.dma("sp", iotac[:], col(I["k_iota"]), writes=[r_iotac])
            sc_t, r_sc = st.sb([128, NE], F32)
            bi, r_bi = st.sb([128, NE], F32)
            tmp, r_tmp = st.sb([128, NE], F32)
            m1, r_m1 = st.sb([128, 8], F32)
            m2, r_m2 = st.sb([128, 8], F32)
            gs, r_gs = st.sb([128, 8], F32)
            t8, r_t8 = st.sb([128, 8], F32)
            pen, r_pen = st.sb([128, 8], F32)
            sel, r_sel = st.sb([128, NE], F32)
            ssum, r_ssum = st.sb([128, 1], F32)
            gate, r_gate = st.sb([128, NE], F32)
            gTs, r_gTs = st.sb([NE, 128], F32)
            P.dma("sp", identf[:], I["k_ident"], writes=[r_idf])
            P.dma("sp", wr[:], kp(I["w_router"][l]), writes=[r_wr])
            P.dma("sp", brB[:], I["b_router"][l].partition_broadcast(128), writes=[r_brB])
        sq2 = [(sq, r_sq)] + [st.sb([128, D], F32) for _ in range(3)]
        ss2 = [(ss, r_ss)] + [st.sb([128, 1], F32) for _ in range(3)]
        rstd2 = [(rstd, r_rstd)] + [st.sb([128, 1], F32) for _ in range(3)]
        hf2 = [(hf, r_hf)] + [st.sb([128, D], F32) for _ in range(3)]
        hb2 = [(hb, r_hb)] + [st.sb([128, D], BF16) for _ in range(3)]
        if router:
            h322 = [(h32, r_h32)] + [st.sb([128, D], F32) for _ in range(3)]
            dup_sc = [(sc_t, r_sc), st.sb([128, NE], F32)]
            dup_bi = [(bi, r_bi), st.sb([128, NE], F32)]
            dup_tmp = [(tmp, r_tmp), st.sb([128, NE], F32)]
            dup_m1 = [(m1, r_m1), st.sb([128, 8], F32)]
            dup_m2 = [(m2, r_m2), st.sb([128, 8], F32)]
            dup_gs = [(gs, r_gs), st.sb([128, 8], F32)]
            dup_t8 = [(t8, r_t8), st.sb([128, 8], F32)]
            dup_pen = [(pen, r_pen), st.sb([128, 8], F32)]
            dup_sel = [(sel, r_sel), st.sb([128, NE], F32)]
            dup_ssum = [(ssum, r_ssum), st.sb([128, 1], F32)]
            dup_gate = [(gate, r_gate), st.sb([128, NE], F32)]
            dup_gTs = [(gTs, r_gTs), st.sb([NE, 128], F32)]
            dup_plg = [(plg, r_plg), st.ps([128, NE], F32)]
            plg2 = dup_plg
            onec, r_onec = st.sb([128, 1], F32)
            P.op("pool", lambda e: e.memset(onec[:], 1.0), writes=[r_onec])

        def ph1(t):
            x_t, r_x = xt[t % 4]
            sq_t, r_sq_ = sq2[t % 4]
            ss_t, r_ss_ = ss2[t % 4]
            rs_t, r_rs_ = rstd2[t % 4]
            hf_t, r_hf_ = hf2[t % 4]
            hb_t, r_hb_ = hb2[t % 4]
            P.dma("sp", x_t[:], x_src[t * 128:(t + 1) * 128, :], writes=[r_x])
            P.op("act", lambda e: e.activation(out=sq_t[:], in_=x_t[:], func=AF.Square, scale=float(D ** -0.5), accum_out=ss_t[:]),
                 reads=[r_x], writes=[r_sq_, r_ss_])
            P.op("act", lambda e: e.activation(out=rs_t[:], in_=ss_t[:], func=AF.Ln, bias=epsb[:, 0:1]), reads=[r_ss_, r_eps], writes=[r_rs_])
            P.op("act", lambda e: e.activation(out=rs_t[:], in_=rs_t[:], func=AF.Exp, scale=-0.5), reads=[r_rs_], writes=[r_rs_])
            P.op("dve", lambda e: e.scalar_tensor_tensor(out=hf_t[:], in0=x_t[:], scalar=rs_t[:, 0:1], in1=A[:], op0=ALU.mult, op1=ALU.mult),
                 reads=[r_x, r_rs_, r_A], writes=[r_hf_])
            P.op("pool", lambda e: e.tensor_tensor(out=hb_t[:], in0=hf_t[:], in1=shB[:], op=ALU.add), reads=[r_hf_, r_shB], writes=[r_hb_])
            if router:
                h32_t, r_h32_ = h322[t % 4]
                P.op("dve", lambda e: e.tensor_tensor(out=h32_t[:], in0=hf_t[:], in1=shB[:], op=ALU.add), reads=[r_hf_, r_shB], writes=[r_h32_])

        def ph2a(t):
            hb_t, r_hb_ = hb2[t % 4]
            p_t, r_p = pt[t % 2]
            h_t, r_h = hTs[t % 2]
            for c in range(8):
                P.op("pe", lambda e: e.transpose(out=p_t[:, c, :], in_=hb_t[:, c * 128:(c + 1) * 128], identity=identb[:]),
                     reads=[r_hb_, r_idb], writes=[r_p])
            P.op("act", lambda e: e.copy(out=h_t[:], in_=p_t[:]), reads=[r_p], writes=[r_h])
            P.dma("sp", kp(hT_dst)[:, :, t * 128:(t + 1) * 128], h_t[:], reads=[r_h])
            if router:
                P.dma("sp", h2_d[t * 128:(t + 1) * 128, :], hb_t[:], reads=[r_hb_])
                plg, r_plg = plg2[t % 2]
                h32_t, r_h32_ = h322[t % 4]
                for half in range(2):
                    pf_t, r_pf = ptf[half]
                    for c in range(4):
                        cc = half * 4 + c
                        P.op("pe", lambda e: e.transpose(out=pf_t[:, c, :], in_=h32_t[:, cc * 128:(cc + 1) * 128], identity=identf[:]),
                             reads=[r_h32_, r_idf], writes=[r_pf])
                    P.op("dve", lambda e: e.tensor_copy(out=hTf[:, half * 4:(half + 1) * 4, :], in_=pf_t[:]), reads=[r_pf], writes=[r_hTf])
                for c in range(8):
                    P.op("pe", lambda e: e.matmul(plg[:], lhsT=hTf[:, c, :], rhs=wr[:, c, :], start=(c == 0), stop=(c == 7)),
                         reads=[r_hTf, r_wr], writes=[r_plg])

        def ph2b(t):
            sc_t, r_sc = dup_sc[t % 2]
            bi, r_bi = dup_bi[t % 2]
            tmp, r_tmp = dup_tmp[t % 2]
            m1, r_m1 = dup_m1[t % 2]
            m2, r_m2 = dup_m2[t % 2]
            gs, r_gs = dup_gs[t % 2]
            t8, r_t8 = dup_t8[t % 2]
            pen, r_pen = dup_pen[t % 2]
            sel, r_sel = dup_sel[t % 2]
            ssum, r_ssum = dup_ssum[t % 2]
            gate, r_gate = dup_gate[t % 2]
            gTs, r_gTs = dup_gTs[t % 2]
            plg, r_plg = dup_plg[t % 2]
            P.op("act", lambda e: e.activation(out=sc_t[:], in_=plg[:], func=AF.Exp, scale=-1.0), reads=[r_plg], writes=[r_sc])
            yield
            P.op("dve", lambda e: e.tensor_scalar(out=sc_t[:], in0=sc_t[:], scalar1=1.0, scalar2=None, op0=ALU.add), reads=[r_sc], writes=[r_sc])
            yield
            P.op("dve", lambda e: e.reciprocal(out=sc_t[:], in_=sc_t[:]), reads=[r_sc], writes=[r_sc])
            yield
            P.op("dve", lambda e: e.tensor_tensor(out=bi[:], in0=sc_t[:], in1=brB[:], op=ALU.add), reads=[r_sc, r_brB], writes=[r_bi])
            yield
            bi3 = bi[:].rearrange("p (g e) -> p g e", g=8)
            tmp3 = tmp[:].rearrange("p (g e) -> p g e", g=8)
            P.op("dve", lambda e: e.tensor_reduce(out=m1[:], in_=bi3, axis=AX.X, op=ALU.max), reads=[r_bi], writes=[r_m1])
            yield
            P.op("dve", lambda e: e.tensor_tensor(out=tmp3, in0=bi3, in1=m1[:].unsqueeze(2).to_broadcast([128, 8, 8]), op=ALU.is_equal),
                 reads=[r_bi, r_m1], writes=[r_tmp])
            yield
            P.op("dve", lambda e: e.scalar_tensor_tensor(out=tmp[:], in0=tmp[:], scalar=-1e30, in1=bi[:], op0=ALU.mult, op1=ALU.add),
                 reads=[r_tmp, r_bi], writes=[r_tmp])
            yield
            P.op("dve", lambda e: e.tensor_reduce(out=m2[:], in_=tmp3, axis=AX.X, op=ALU.max), reads=[r_tmp], writes=[r_m2])
            yield
            P.op("dve", lambda e: e.tensor_tensor(out=gs[:], in0=m1[:], in1=m2[:], op=ALU.add), reads=[r_m1, r_m2], writes=[r_gs])
            yield
            P.op("dve", lambda e: e.max(out=t8[:], in_=gs[:]), reads=[r_gs], writes=[r_t8])
            yield
            P.op("dve", lambda e: e.tensor_scalar(out=pen[:], in0=gs[:], scalar1=t8[:, 3:4], scalar2=-1e30, op0=ALU.is_lt, op1=ALU.mult),
                 reads=[r_gs, r_t8], writes=[r_pen])
            yield
            P.op("dve", lambda e: e.tensor_tensor(out=tmp3, in0=bi3, in1=pen[:].unsqueeze(2).to_broadcast([128, 8, 8]), op=ALU.add),
                 reads=[r_bi, r_pen], writes=[r_tmp])
            yield
            P.op("dve", lambda e: e.max(out=t8[:], in_=tmp[:]), reads=[r_tmp], writes=[r_t8])
            yield
            P.op("dve", lambda e: e.tensor_scalar(out=sel[:], in0=tmp[:], scalar1=t8[:, 7:8], scalar2=None, op0=ALU.is_ge),
                 reads=[r_tmp, r_t8], writes=[r_sel])
            yield
            P.op("dve", lambda e: e.tensor_tensor(out=sel[:], in0=sel[:], in1=sc_t[:], op=ALU.mult), reads=[r_sel, r_sc], writes=[r_sel])
            yield
            P.op("dve", lambda e: e.tensor_reduce(out=ssum[:], in_=sel[:], axis=AX.X, op=ALU.add), reads=[r_sel], writes=[r_ssum])
            yield
            P.op("dve", lambda e: e.tensor_scalar(out=ssum[:], in0=ssum[:], scalar1=1e-20, scalar2=None, op0=ALU.add), reads=[r_ssum], writes=[r_ssum])
            yield
            P.op("dve", lambda e: e.reciprocal(out=ssum[:], in_=ssum[:]), reads=[r_ssum], writes=[r_ssum])
            yield
            P.op("dve", lambda e: e.tensor_scalar(out=gate[:], in0=sel[:], scalar1=ssum[:, 0:1], scalar2=2.5, op0=ALU.mult, op1=ALU.mult),
                 reads=[r_sel, r_ssum], writes=[r_gate])
            yield
            P.op("dve", lambda e: e.tensor_scalar(out=selb[t % 4][0][:], in0=gate[:], scalar1=0.0, scalar2=None, op0=ALU.is_gt),
                 reads=[r_gate], writes=[selb[t % 4][1]])
            yield
            P.dma("sp", gate_d[t * 128:(t + 1) * 128, :], gate[:], reads=[r_gate])
            yield

        def drain(*gens):
            gens = list(gens)
            while gens:
                for g in list(gens):
                    try:
                        next(g)
                    except StopIteration:
                        gens.remove(g)

        prev_tl = []

        def pc_ops(tl_):
            for tt_ in tl_:
                s_t, r_s = selb[tt_ % 4]
                p_t, r_p = posb[tt_ % 4]
                P.op("pe", lambda e: e.matmul(PC[:], lhsT=pinc[:], rhs=s_t[:], start=(tt_ == 0), stop=False, skip_group_check=True),
                     reads=[r_pinc, r_s], writes=[r_PC])
                P.op("act", lambda e: e.copy(out=p_t[:], in_=PC[:]), reads=[r_PC], writes=[r_p])
                P.op("pe", lambda e: e.matmul(PC[:], lhsT=pgtm[:], rhs=s_t[:], start=False, stop=(tt_ == NT - 1), skip_group_check=True),
                     reads=[r_pgtm, r_s, r_p], writes=[r_PC])
                P.dma("sp", pos_d[tt_ * 128:(tt_ + 1) * 128, :], p_t[:], reads=[r_p])

        for t0_ in range(min(4, NT)):
            ph1(t0_)
        for t in range(0, NT, 2):
            tl = [t] + ([t + 1] if t + 1 < NT else [])
            for tt_ in tl:
                ph2a(tt_)
            if t + 4 < NT:
                ph1(t + 4)
            if t + 5 < NT:
                ph1(t + 5)
            if router:
                pc_ops(prev_tl)
                prev_tl = tl
                drain(*[ph2b(tt_) for tt_ in tl])
        if router:
            pc_ops(prev_tl)
        if router:
            cnt, r_cnt = st.sb([128, NE], F32)
            nbt, r_nbt = st.sb([128, NE], F32)
            ca, r_ca = st.sb([128, NE], F32)
            cb, r_cb = st.sb([128, NE], F32)
            ebc, r_ebc = st.sb([128, 1], F32)
            P.op("act", lambda e: e.copy(out=cnt[:], in_=PC[:]), reads=[r_PC], writes=[r_cnt])
            P.op("dve", lambda e: e.tensor_scalar(out=nbt[:], in0=cnt[:], scalar1=0.0, scalar2=None, op0=ALU.is_gt), reads=[r_cnt], writes=[r_nbt])
            for j_ in range(1, 8):
                P.op("dve", lambda e: e.scalar_tensor_tensor(out=nbt[:], in0=cnt[:], scalar=512.0 * j_, in1=nbt[:], op0=ALU.is_gt, op1=ALU.add),
                     reads=[r_cnt, r_nbt], writes=[r_nbt])
            P.op("dve", lambda e: e.tensor_copy(out=ca[:], in_=nbt[:]), reads=[r_nbt], writes=[r_ca])
            src, r_src, dst_, r_dst = ca, r_ca, cb, r_cb
            k_ = 1
            while k_ < NE:
                P.op("dve", lambda e: e.tensor_copy(out=dst_[:, 0:k_], in_=src[:, 0:k_]), reads=[r_src], writes=[r_dst])
                P.op("dve", lambda e: e.tensor_tensor(out=dst_[:, k_:NE], in0=src[:, k_:NE], in1=src[:, 0:NE - k_], op=ALU.add), reads=[r_src], writes=[r_dst])
                src, r_src, dst_, r_dst = dst_, r_dst, src, r_src
                k_ *= 2
            bend, r_bend = src, r_src
            P.op("dve", lambda e: e.tensor_scalar(out=dst_[:], in0=bend[:], scalar1=iotac[:, 0:1], scalar2=None, op0=ALU.is_le),
                 reads=[r_bend, r_iotac], writes=[r_dst])
            P.op("dve", lambda e: e.tensor_reduce(out=ebc[:], in_=dst_[:], axis=AX.X, op=ALU.add), reads=[r_dst], writes=[r_ebc])
            P.op("dve", lambda e: e.tensor_scalar(out=ebc[:], in0=ebc[:], scalar1=float(NE - 1), scalar2=None, op0=ALU.min), reads=[r_ebc], writes=[r_ebc])
            P.dma("sp", col(eb_d), ebc[:], reads=[r_ebc])
            P.op("dve", lambda e: e.tensor_tensor(out=cnt[:], in0=bend[:], in1=nbt[:], op=ALU.subtract), reads=[r_bend, r_nbt], writes=[r_cnt])
            P.op("dve", lambda e: e.tensor_scalar(out=cnt[:], in0=cnt[:], scalar1=512.0, scalar2=None, op0=ALU.mult), reads=[r_cnt], writes=[r_cnt])
            P.dma("sp", sbase_d.rearrange("(o n) -> o n", o=1), cnt[0:1, :], reads=[r_cnt])
        st.done(f"norm{int(router)}_{l}")

    def stage_proj(l):
        st = Stage(P)
        NCOL = 4096
        w, r_w = st.sb([128, 8, NCOL], BF16)
        blk, r_blk = st.sb([128, 128], BF16)
        rot, r_rot = st.sb([128, 128], BF16)
        epsb, r_eps = st.sb([128, 1], F32)
        gq, r_gq = st.sb([128, 1], F32)
        gk, r_gk = st.sb([128, 1], F32)
        hT = [st.sb([128, 8, 512], BF16) for _ in range(2)]
        cosT = [st.sb([128, 512], F32) for _ in range(2)]
        sinT = [st.sb([128, 512], F32) for _ in range(2)]
        pp = [st.ps([128, 512], F32) for _ in range(4)]
        pms, r_pms = st.ps([128, 512], F32)
        prot, r_prot = st.ps([128, 512], F32)
        sqb, r_sqb = st.sb([128, 512], BF16)
        rs, r_rs = st.sb([128, 512], F32)
        qn, r_qn = st.sb([128, 512], BF16)
        t1, r_t1 = st.sb([128, 512], F32)
        t2, r_t2 = st.sb([128, 512], F32)
        sg, r_sg = st.sb([128, 512], F32)
        ob = [st.sb([128, 512], BF16) for _ in range(4)]
        for c in range(8):
            P.dma("pool", w[:, c, :], I["w_in"][l][c * 128:(c + 1) * 128, 0:NCOL], writes=[r_w])
        P.dma("pool", blk[:], I["k_blk64"], writes=[r_blk])
        P.dma("pool", rot[:], I["k_rotT"], writes=[r_rot])
        P.op("pool", lambda e: e.memset(epsb[:], EPS), writes=[r_eps])
        for hh in range(2):
            P.dma("sp", gq[hh * 64:(hh + 1) * 64, :], col(I["qn_g"][l]), writes=[r_gq])
            P.dma("sp", gk[hh * 64:(hh + 1) * 64, :], col(I["kn_g"][l]), writes=[r_gk])
        pp = pp + [st.ps([128, 512], F32) for _ in range(2)]
        NPP = len(pp)
        loaded = set()

        def load_tile(tt):
            if tt in loaded or tt >= NQ:
                return
            loaded.add(tt)
            ts_ = slice(tt * 512, (tt + 1) * 512)
            P.dma("sp", hT[tt % 2][0][:], kp(hT_d)[:, :, ts_], writes=[hT[tt % 2][1]])
            P.dma("sp", cosT[tt % 2][0][:], cos_d[:, ts_], writes=[cosT[tt % 2][1]])
            P.dma("sp", sinT[tt % 2][0][:], sin_d[:, ts_], writes=[sinT[tt % 2][1]])

        units = []
        for tt in range(NQ):
            for which in range(2):
                for j in range(4):
                    units.append(("qk", tt, (which, j)))
            for j in range(4):
                units.append(("glu", tt, (j,)))
            for which in range(2):
                for j in range(4):
                    units.append(("cp", tt, (which, j)))
            for which in range(2):
                for sub in range(4):
                    units.append(("tm", tt, (which, sub)))
        pidx = []
        cur = 0
        for kind, tt, args in units:
            nb_ = 2 if kind == "glu" else 1
            pidx.append([(cur + i) % NPP for i in range(nb_)])
            cur += nb_
        state = {"oi": 0}

        def fmm(tt, col0, p_t, r_p):
            h_t, r_h = hT[tt % 2]
            for c in range(8):
                P.op("pe", lambda e: e.matmul(p_t[:], lhsT=w[:, c, col0:col0 + 128], rhs=h_t[:, c, :], start=(c == 0), stop=(c == 7)),
                     reads=[r_w, r_h], writes=[r_p])

        def F(u):
            kind, tt, args = units[u]
            load_tile(tt)
            bufs = [pp[i] for i in pidx[u]]
            if kind == "qk":
                which, j = args
                fmm(tt, which * 512 + j * 128, *bufs[0])
            elif kind == "glu":
                (j,) = args
                fmm(tt, 1536 + j * 128, *bufs[0])
                fmm(tt, 2048 + j * 128, *bufs[1])
            elif kind == "cp":
                which, j = args
                fmm(tt, (2560 if which == 0 else 3072) + j * 128, *bufs[0])
            else:
                which, sub = args
                base = 1024 if which == 0 else 3584
                h_t, r_h = hT[tt % 2]
                p_t, r_p = bufs[0]
                for c in range(8):
                    P.op("pe", lambda e: e.matmul(p_t[:], lhsT=h_t[:, c, sub * 128:(sub + 1) * 128], rhs=w[:, c, base:base + 512], start=(c == 0), stop=(c == 7)),
                         reads=[r_w, r_h], writes=[r_p])

        def nxt_ob():
            o = ob[state["oi"] % 4]
            state["oi"] += 1
            return o

        def G(u):
            kind, tt, args = units[u]
            ts_ = slice(tt * 512, (tt + 1) * 512)
            bufs = [pp[i] for i in pidx[u]]
            c_t, r_c = cosT[tt % 2]
            s_t, r_s = sinT[tt % 2]
            if kind == "qk":
                which, j = args
                g_t, r_g, dst = (gq, r_gq, qaT_d) if which == 0 else (gk, r_gk, kaT_d)
                p_t, r_p = bufs[0]
                P.op("act", lambda e: e.activation(out=sqb[:], in_=p_t[:], func=AF.Square), reads=[r_p], writes=[r_sqb])
                P.op("pe", lambda e: e.matmul(pms[:], lhsT=blk[:], rhs=sqb[:], start=True, stop=True), reads=[r_blk, r_sqb], writes=[r_pms])
                P.op("act", lambda e: e.activation(out=rs[:], in_=pms[:], func=AF.Ln, bias=epsb[:, 0:1]), reads=[r_pms, r_eps], writes=[r_rs])
                P.op("act", lambda e: e.activation(out=rs[:], in_=rs[:], func=AF.Exp, scale=-0.5), reads=[r_rs], writes=[r_rs])
                P.op("dve", lambda e: e.scalar_tensor_tensor(out=qn[:], in0=p_t[:], scalar=g_t[:, 0:1], in1=rs[:], op0=ALU.mult, op1=ALU.mult),
                     reads=[r_p, r_g, r_rs], writes=[r_qn])
                P.op("pe", lambda e: e.matmul(prot[:], lhsT=rot[:], rhs=qn[:], start=True, stop=True), reads=[r_rot, r_qn], writes=[r_prot])
                P.op("pool", lambda e: e.tensor_tensor(out=t1[:], in0=qn[:], in1=c_t[:], op=ALU.mult), reads=[r_qn, r_c], writes=[r_t1])
                P.op("dve", lambda e: e.tensor_tensor(out=t2[:], in0=prot[:], in1=s_t[:], op=ALU.mult), reads=[r_prot, r_s], writes=[r_t2])
                o_t, r_o = nxt_ob()
                P.op("dve", lambda e: e.tensor_tensor(out=o_t[:], in0=t1[:], in1=t2[:], op=ALU.add), reads=[r_t1, r_t2], writes=[r_o])
                P.dma("sp", dst[j * 128:(j + 1) * 128, ts_], o_t[:], reads=[r_o])
            elif kind == "glu":
                (j,) = args
                (pa, r_pa), (pg, r_pg) = bufs
                P.op("act", lambda e: e.activation(out=sg[:], in_=pg[:], func=AF.Sigmoid), reads=[r_pg], writes=[r_sg])
                o_t, r_o = nxt_ob()
                P.op("dve", lambda e: e.tensor_tensor(out=o_t[:], in0=pa[:], in1=sg[:], op=ALU.mult), reads=[r_pa, r_sg], writes=[r_o])
                P.dma("sp", uT_d[j * 128:(j + 1) * 128, ts_], o_t[:], reads=[r_o])
            elif kind == "cp":
                which, j = args
                dst, qscale = (qcT_d, 0.125) if which == 0 else (kcT_d, 1.0)
                p_t, r_p = bufs[0]
                o_t, r_o = nxt_ob()
                P.op("act", lambda e: e.mul(out=o_t[:], in_=p_t[:], mul=qscale), reads=[r_p], writes=[r_o])
                P.dma("sp", dst[j * 128:(j + 1) * 128, ts_], o_t[:], reads=[r_o])
            else:
                which, sub = args
                dst = va_d if which == 0 else vc_d
                p_t, r_p = bufs[0]
                o_t, r_o = nxt_ob()
                P.op("dve" if sub % 2 == 0 else "act", (lambda e: e.tensor_copy(out=o_t[:], in_=p_t[:])) if sub % 2 == 0 else (lambda e: e.copy(out=o_t[:], in_=p_t[:])),
                     reads=[r_p], writes=[r_o])
                r0 = tt * 512 + sub * 128
                P.dma("sp", dst[r0:r0 + 128, :], o_t[:], reads=[r_o])

        nU = len(units)
        AHEAD = 2
        for u in range(min(AHEAD, nU)):
            F(u)
        for u in range(nU):
            if u + AHEAD < nU:
                F(u + AHEAD)
            G(u)
        st.done(f"proj{l}")

    def stage_da(l):
        lambda_init = 0.8 - 0.6 * math.exp(-0.3 * l)
        st = Stage(P)
        msk, r_msk = st.sb([128, 4, 512], BF16)
        ones, r_ones = st.sb([128, 128], BF16)
        o128, r_o128 = st.sb([128, 128], BF16)
        epsb, r_eps = st.sb([128, 1], F32)
        lv = [st.sb([128, 64], F32) for _ in range(4)]
        lp, r_lp = st.sb([128, 64], F32)
        ld, r_ld = st.sb([128, 2], F32)
        nlam, r_nlam = st.sb([128, 1], F32)
        gcol, r_gcol = st.sb([128, 1], F32)
        qT = [[st.sb([128, S], BF16) for _ in range(2)] for _ in range(2)]
        kT = [st.sb([128, S], BF16) for _ in range(2)]
        vv = [st.sb([128, NT, 128], BF16) for _ in range(2)]
        pz = [st.ps([128, 512], F32) for _ in range(2)]
        pO = [st.ps([128, 512], F32) for _ in range(2)]
        pL = [st.ps([128, 512], F32) for _ in range(2)]
        for hp_ in range(2):
            for m_ in range(2):
                zr = slice((1 - m_) * 64, (1 - m_) * 64 + 64)
                P.op("pool", lambda e: e.memset(qT[hp_][m_][0][zr, :], 0.0), writes=[qT[hp_][m_][1]])
        pms, r_pms = st.ps([128, 512], F32)
        E = [st.sb([128, 512], BF16) for _ in range(4)]
        rL = [st.sb([128, 512], F32) for _ in range(2)]
        tO = [st.sb([128, 512], F32) for _ in range(2)]
        o_t, r_o = st.sb([128, 512], F32)
        sqb, r_sqb = st.sb([128, 512], BF16)
        rs, r_rs = st.sb([128, 512], F32)
        ob = [st.sb([128, 512], BF16) for _ in range(2)]
        P.dma("pool", msk[:], I["k_maskc"].rearrange("j p q -> p j q"), writes=[r_msk])
        if l == 0:
            zt, r_zt = st.sb([128, 4096], BF16)
            P.op("pool", lambda e: e.memset(zt[:], 0.0), writes=[r_zt])
        zfill = {"next": 0}

        def zero_fill_some(n_):
            if l != 0:
                return
            for _ in range(n_):
                zi_ = zfill["next"]
                if zi_ >= NSLOT // 512:
                    return
                zfill["next"] += 1
                P.dma("pool", xs_d[zi_ * 512:(zi_ + 1) * 512, :].rearrange("(p r) d -> p (r d)", p=128), zt[:], reads=[r_zt])
        P.op("pool", lambda e: e.memset(ones[:], 1.0), writes=[r_ones])
        P.op("pool", lambda e: e.memset(o128[:], 1.0 / 128), writes=[r_o128])
        P.op("pool", lambda e: e.memset(epsb[:], EPS), writes=[r_eps])
        for i, nm in enumerate(("lam_q1", "lam_k1", "lam_q2", "lam_k2")):
            P.dma("sp", lv[i][0][:], I[nm][l].partition_broadcast(128), writes=[lv[i][1]])
        for i in range(2):
            P.op("dve", lambda e, i=i: e.tensor_tensor(out=lp[:], in0=lv[2 * i][0][:], in1=lv[2 * i + 1][0][:], op=ALU.mult),
                 reads=[lv[2 * i][1], lv[2 * i + 1][1]], writes=[r_lp])
            P.op("dve", lambda e, i=i: e.tensor_reduce(out=ld[:, i:i + 1], in_=lp[:], axis=AX.X, op=ALU.add), reads=[r_lp], writes=[r_ld])
        P.op("act", lambda e: e.activation(out=ld[:], in_=ld[:], func=AF.Exp), reads=[r_ld], writes=[r_ld])
        P.op("dve", lambda e: e.tensor_tensor(out=nlam[:], in0=ld[:, 1:2], in1=ld[:, 0:1], op=ALU.subtract), reads=[r_ld], writes=[r_nlam])
        P.op("dve", lambda e: e.tensor_scalar(out=nlam[:], in0=nlam[:], scalar1=-lambda_init, scalar2=None, op0=ALU.add), reads=[r_nlam], writes=[r_nlam])
        P.dma("sp", gcol[:], col(I["subln_g"][l]), writes=[r_gcol])
        P.op("dve", lambda e: e.tensor_scalar(out=gcol[:], in0=gcol[:], scalar1=1.0 - lambda_init, scalar2=None, op0=ALU.mult), reads=[r_gcol], writes=[r_gcol])
        oi = 0
        pz3 = pz + [st.ps([128, 512], F32)]
        units = []
        for hd in range(4):
            for Qi in range(NQ):
                for m in range(2):
                    for kt in range(4 * Qi + 4):
                        units.append((hd, Qi, m, kt))
        loaded = set()

        def load_head(hd):
            if hd in loaded or hd >= 4:
                return
            loaded.add(hd)
            k_t, r_k = kT[hd % 2]
            v_t, r_v = vv[hd % 2]
            for m_ in range(2):
                rr_ = slice(m_ * 64, m_ * 64 + 64)
                P.dma("sp", qT[hd % 2][m_][0][rr_, :], qaT_d[hd * 128 + m_ * 64:hd * 128 + m_ * 64 + 64, :], writes=[qT[hd % 2][m_][1]])
            P.dma("sp", k_t[:], kaT_d[hd * 128:(hd + 1) * 128, :], writes=[r_k])
            P.dma("sp", v_t[:], va_d[:, hd * 128:(hd + 1) * 128].rearrange("(t p) e -> p t e", p=128), writes=[r_v])

        def phA(i):
            hd, Qi, m, kt = units[i]
            load_head(hd)
            q_t, r_q = qT[hd % 2][m]
            k_t, r_k = kT[hd % 2]
            z_t, r_z = pz3[i % 3]
            qs = slice(Qi * 512, (Qi + 1) * 512)
            P.op("pe", lambda e: e.matmul(z_t[:], lhsT=k_t[:, kt * 128:(kt + 1) * 128], rhs=q_t[:, qs], start=True, stop=True),
                 reads=[r_k, r_q], writes=[r_z])

        def phB(i):
            nonlocal oi
            hd, Qi, m, kt = units[i]
            v_t, r_v = vv[hd % 2]
            z_t, r_z = pz3[i % 3]
            e_t, r_e = E[i % 4]
            pO_t, r_pO = pO[m]
            pL_t, r_pL = pL[m]
            nk = 4 * Qi + 4
            j = kt - 4 * Qi
            qs = slice(Qi * 512, (Qi + 1) * 512)
            P.op("act", lambda e: e.activation(out=e_t[:], in_=z_t[:], func=AF.Exp, scale=0.125), reads=[r_z], writes=[r_e])
            if j >= 0:
                P.op("dve", lambda e: e.tensor_tensor(out=e_t[:], in0=e_t[:], in1=msk[:, j, :], op=ALU.mult), reads=[r_e, r_msk], writes=[r_e])
            P.op("pe", lambda e: e.matmul(pO_t[:], lhsT=v_t[:, kt, :], rhs=e_t[:], start=(kt == 0), stop=(kt == nk - 1)),
                 reads=[r_v, r_e], writes=[r_pO])
            P.op("pe", lambda e: e.matmul(pL_t[:], lhsT=ones[:], rhs=e_t[:], start=(kt == 0), stop=(kt == nk - 1)),
                 reads=[r_ones, r_e], writes=[r_pL])
            if kt == nk - 1:
                P.op("act", lambda e: e.activation(out=rL[m][0][:], in_=pL_t[:], func=AF.Ln), reads=[r_pL], writes=[rL[m][1]])
                P.op("act", lambda e: e.activation(out=rL[m][0][:], in_=rL[m][0][:], func=AF.Exp, scale=-1.0), reads=[rL[m][1]], writes=[rL[m][1]])
                P.op("dve", lambda e: e.tensor_tensor(out=tO[m][0][:], in0=pO_t[:], in1=rL[m][0][:], op=ALU.mult), reads=[r_pO, rL[m][1]], writes=[tO[m][1]])
                if m == 1:
                    P.op("dve", lambda e: e.scalar_tensor_tensor(out=o_t[:], in0=tO[1][0][:], scalar=nlam[:, 0:1], in1=tO[0][0][:], op0=ALU.mult, op1=ALU.add),
                         reads=[tO[0][1], tO[1][1], r_nlam], writes=[r_o])
                    P.op("act", lambda e: e.activation(out=sqb[:], in_=o_t[:], func=AF.Square), reads=[r_o], writes=[r_sqb])
                    P.op("pe", lambda e: e.matmul(pms[:], lhsT=o128[:], rhs=sqb[:], start=True, stop=True), reads=[r_o128, r_sqb], writes=[r_pms])
                    P.op("act", lambda e: e.activation(out=rs[:], in_=pms[:], func=AF.Ln, bias=epsb[:, 0:1]), reads=[r_pms, r_eps], writes=[r_rs])
                    P.op("act", lambda e: e.activation(out=rs[:], in_=rs[:], func=AF.Exp, scale=-0.5), reads=[r_rs], writes=[r_rs])
                    b_t, r_b = ob[oi % 2]
                    oi += 1
                    P.op("dve", lambda e: e.scalar_tensor_tensor(out=b_t[:], in0=o_t[:], scalar=gcol[:, 0:1], in1=rs[:], op0=ALU.mult, op1=ALU.mult),
                         reads=[r_o, r_gcol, r_rs], writes=[r_b])
                    P.dma("sp", oaT_d[hd * 128:(hd + 1) * 128, qs], b_t[:], reads=[r_b])

        n = len(units)
        for i in range(min(2, n)):
            phA(i)
        for i in range(n):
            if i + 2 < n:
                phA(i + 2)
            phB(i)
            if i % 4 == 3:
                zero_fill_some(1)
        zero_fill_some(NSLOT)
        st.done(f"da{l}")

    def stage_sb(l):
        st = Stage(P)
        mskb, r_mskb = st.sb([128, 4, 512], BF16)
        uinc, r_uinc = st.sb([128, 128], BF16)
        ulow, r_ulow = st.sb([128, 128], BF16)
        onec, r_onec = st.sb([128, 1], F32)
        qT = [[st.sb([128, S], BF16) for _ in range(2)] for _ in range(2)]
        kT = [st.sb([128, S], BF16) for _ in range(2)]
        kN = [st.sb([128, S], BF16) for _ in range(2)]
        vv = [st.sb([128, NT, 128], BF16) for _ in range(2)]
        pz = [st.ps([128, 2, 512], F32) for _ in range(2)]
        PT, r_PT = st.ps([128, 2, 512], F32)
        pO, r_pO = st.ps([128, 2, 512], F32)
        ex = [st.sb([128, 2, 512], F32) for _ in range(2)]
        sp_ = [st.sb([128, 2, 512], F32) for _ in range(2)]
        lom = [st.sb([128, 2, 512], BF16) for _ in range(3)]
        ab = [st.sb([128, 2, 512], BF16) for _ in range(3)]
        ob = [st.sb([128, 512], BF16) for _ in range(2)]
        for cp_ in range(2):
            for hh_ in range(2):
                zr = slice((1 - hh_) * 64, (1 - hh_) * 64 + 64)
                P.op("pool", lambda e: e.memset(qT[cp_][hh_][0][zr, :], 0.0), writes=[qT[cp_][hh_][1]])
        P.dma("pool", mskb[:], I["k_masks"].rearrange("j p q -> p j q"), writes=[r_mskb])
        P.dma("pool", uinc[:], I["k_uincl"], writes=[r_uinc])
        P.dma("pool", ulow[:], I["k_ulow"], writes=[r_ulow])
        P.op("pool", lambda e: e.memset(onec[:], 1.0), writes=[r_onec])
        state = {"oi": 0}
        r_PTh = [Res(), Res()]
        abr = [[Res(), Res()] for _ in range(3)]
        units = [(ch, Qi, kt) for ch in range(4) for Qi in range(NQ) for kt in range(4 * Qi + 3, -1, -1)]
        n = len(units)
        loaded = set()

        def load_pair(ch):
            if ch in loaded:
                return
            loaded.add(ch)
            for hh_ in range(2):
                rr_ = slice(hh_ * 64, hh_ * 64 + 64)
                P.dma("sp", qT[ch % 2][hh_][0][rr_, :], qcT_d[ch * 128 + hh_ * 64:ch * 128 + hh_ * 64 + 64, :], writes=[qT[ch % 2][hh_][1]])
            P.dma("sp", kT[ch % 2][0][:], kcT_d[ch * 128:(ch + 1) * 128, :], writes=[kT[ch % 2][1]])
            P.dma("sp", vv[ch % 2][0][:], vc_d[:, ch * 128:(ch + 1) * 128].rearrange("(t p) e -> p t e", p=128), writes=[vv[ch % 2][1]])
            P.op("pool", lambda e: e.tensor_scalar(out=kN[ch % 2][0][:], in0=kT[ch % 2][0][:], scalar1=-1.0, scalar2=None, op0=ALU.mult),
                 reads=[kT[ch % 2][1]], writes=[kN[ch % 2][1]])

        def info(i):
            ch, Qi, kt = units[i]
            return ch, Qi, kt, kt - 4 * Qi, 4 * Qi + 4, slice(Qi * 512, (Qi + 1) * 512), slice(kt * 128, (kt + 1) * 128)

        def mask2(j):
            return mskb[:, j:j + 1, :].to_broadcast([128, 2, 512])

        def phA(i):
            ch, Qi, kt, j, nk, qs, ks = info(i)
            load_pair(ch)
            k_t, r_k = kT[ch % 2]
            z_t, r_z = pz[i % 2]
            for hh in range(2):
                q_t, r_q = qT[ch % 2][hh]
                P.op("pe", lambda e: e.matmul(z_t[:, hh, :], lhsT=k_t[:, ks], rhs=q_t[:, qs], start=True, stop=True), reads=[r_k, r_q], writes=[r_z])

        def phB(i):
            ch, Qi, kt, j, nk, qs, ks = info(i)
            z_t, r_z = pz[i % 2]
            e_t, r_e = ex[i % 2]
            p_t, r_p = sp_[i % 2]
            l_t, r_l = lom[i % 3]
            P.op("act", lambda e: e.activation(out=e_t[:], in_=z_t[:], func=AF.Exp, scale=-1.0), reads=[r_z], writes=[r_e])
            P.op("act", lambda e: e.activation(out=p_t[:], in_=e_t[:], func=AF.Ln, bias=onec[:, 0:1]), reads=[r_e, r_onec], writes=[r_p])
            P.op("dve", lambda e: e.scalar_tensor_tensor(out=l_t[:], in0=z_t[:], scalar=-1.0, in1=p_t[:], op0=ALU.mult, op1=ALU.subtract),
                 reads=[r_z, r_p], writes=[r_l])
            if j >= 0:
                P.op("dve", lambda e: e.tensor_tensor(out=l_t[:], in0=l_t[:], in1=mask2(j), op=ALU.mult), reads=[r_l, r_mskb], writes=[r_l])

        def phC(i, hh):
            ch, Qi, kt, j, nk, qs, ks = info(i)
            k_t, r_k = kT[ch % 2]
            l_t, r_l = lom[i % 3]
            q_t, r_q = qT[ch % 2][hh]
            P.op("pe", lambda e: e.matmul(PT[:, hh, :], lhsT=uinc[:], rhs=l_t[:, hh, :], start=(kt == nk - 1), stop=False, skip_group_check=True),
                 reads=[r_uinc, r_l], writes=[r_PTh[hh]])
            P.op("pe", lambda e: e.matmul(PT[:, hh, :], lhsT=k_t[:, ks], rhs=q_t[:, qs], start=False, stop=False, skip_group_check=True),
                 reads=[r_k, r_q], writes=[r_PTh[hh]])

        def phD_act(i):
            ch, Qi, kt, j, nk, qs, ks = info(i)
            a_t, _ = ab[i % 3]
            for hh in range(2):
                r_a = abr[i % 3][hh]
                P.op("act", lambda e: e.activation(out=a_t[:, hh, :], in_=PT[:, hh, :], func=AF.Exp), reads=[r_PTh[hh]], writes=[r_a])
                if j >= 0:
                    P.op("dve", lambda e: e.tensor_tensor(out=a_t[:, hh, :], in0=a_t[:, hh, :], in1=mskb[:, j, :], op=ALU.mult), reads=[r_a, r_mskb], writes=[r_a])

        def phD_corr(i, hh):
            ch, Qi, kt, j, nk, qs, ks = info(i)
            kn_t, r_kn = kN[ch % 2]
            l_t, r_l = lom[i % 3]
            r_a = abr[i % 3][hh]
            if kt > 0:
                q_t, r_q = qT[ch % 2][hh]
                P.op("pe", lambda e: e.matmul(PT[:, hh, :], lhsT=ulow[:], rhs=l_t[:, hh, :], start=False, stop=False, skip_group_check=True),
                     reads=[r_ulow, r_l, r_a], writes=[r_PTh[hh]])
                P.op("pe", lambda e: e.matmul(PT[:, hh, :], lhsT=kn_t[:, ks], rhs=q_t[:, qs], start=False, stop=(kt == 1), skip_group_check=True),
                     reads=[r_kn, r_q], writes=[r_PTh[hh]])

        def phD_pv(i):
            ch, Qi, kt, j, nk, qs, ks = info(i)
            v_t, r_v = vv[ch % 2]
            a_t, _ = ab[i % 3]
            for hh in range(2):
                P.op("pe", lambda e: e.matmul(pO[:, hh, :], lhsT=v_t[:, kt, :], rhs=a_t[:, hh, :], start=(kt == nk - 1), stop=(kt == 0)),
                     reads=[r_v, abr[i % 3][hh]], writes=[r_pO])
            if kt == 0:
                b_t, r_b = ob[state["oi"] % 2]
                state["oi"] += 1
                for hh in range(2):
                    pr = slice(hh * 64, hh * 64 + 64)
                    P.op("act", lambda e: e.copy(out=b_t[pr, :], in_=pO[pr, hh, :]), reads=[r_pO], writes=[r_b])
                P.dma("sp", ocT_d[ch * 128:(ch + 1) * 128, qs], b_t[:], reads=[r_b])

        for it in range(-3, n):
            if 0 <= it < n:
                phD_act(it)
            if 0 <= it + 3 < n:
                phA(it + 3)
            if 0 <= it + 2 < n:
                phB(it + 2)
            for hh in range(2):
                if 0 <= it < n:
                    phD_corr(it, hh)
                if 0 <= it + 1 < n:
                    phC(it + 1, hh)
            if 0 <= it < n:
                phD_pv(it)
        st.done(f"sb{l}")

    def stage_cv(l):
        st = Stage(P)
        up, r_up = st.sb([128, 4, 30 + S], BF16)
        wcol, r_wcol = st.sb([128, 4, 31], F32)
        identf, r_idf = st.sb([128, 128], F32)
        dg = [st.sb([128, 31, 128], BF16) for _ in range(4)]
        bcol, r_bcol = st.sb([128, 4], F32)
        gcol, r_gcol = st.sb([128, 4], F32)
        lbcol, r_lbcol = st.sb([128, 4], F32)
        o512, r_o512 = st.sb([128, 128], F32)
        epsb, r_eps = st.sb([128, 1], F32)
        pc = [st.ps([128, 512], F32) for _ in range(4)]
        pmean, r_pmean = st.ps([128, 512], F32)
        pex2, r_pex2 = st.ps([128, 512], F32)
        cv32, r_cv = st.sb([128, 4, 512], F32)
        sq32, r_sq = st.sb([128, 4, 512], F32)
        mean, r_mean = st.sb([128, 512], F32)
        msq, r_msq = st.sb([128, 512], F32)
        rs, r_rs = st.sb([128, 512], F32)
        y = [st.sb([128, 512], F32) for _ in range(2)]
        ob = [st.sb([128, 512], BF16) for _ in range(2)]
        P.op("pool", lambda e: e.memset(up[:, :, 0:30], 0.0), writes=[r_up])
        for j in range(4):
            P.dma("sp", up[:, j, 30:30 + S], uT_d[j * 128:(j + 1) * 128, :], writes=[r_up])
            P.dma("sp", wcol[:, j, :], I["w_dw"][l][:, j * 128:(j + 1) * 128].rearrange("k p -> p k"), writes=[r_wcol])
        P.dma("sp", identf[:], I["k_ident"], writes=[r_idf])
        P.dma("sp", bcol[:], I["b_dw"][l].rearrange("(j p) -> p j", p=128), writes=[r_bcol])
        P.dma("sp", gcol[:], I["conv_ln_g"][l].rearrange("(j p) -> p j", p=128), writes=[r_gcol])
        P.dma("sp", lbcol[:], I["conv_ln_b"][l].rearrange("(j p) -> p j", p=128), writes=[r_lbcol])
        P.op("pool", lambda e: e.memset(o512[:], 1.0 / 512), writes=[r_o512])
        P.op("pool", lambda e: e.memset(epsb[:], EPS), writes=[r_eps])
        junk, r_junk = st.sb([128, 1], F32)
        for j in range(4):
            rr = []
            for k in range(31):
                eng = ("act", "dve", "act", "dve", "pool")[k % 5]
                r1 = Res()
                rr.append(r1)
                if eng == "act":
                    P.op(eng, lambda e: e.mul(out=dg[j][0][:, k, :], in_=identf[:], mul=wcol[:, j, k:k + 1]), reads=[r_idf, r_wcol], writes=[r1])
                else:
                    P.op(eng, lambda e: e.tensor_scalar(out=dg[j][0][:, k, :], in0=identf[:], scalar1=wcol[:, j, k:k + 1], scalar2=None, op0=ALU.mult),
                         reads=[r_idf, r_wcol], writes=[r1])
            P.op("dve", lambda e: e.memset(junk[:], 0.0), reads=rr, writes=[dg[j][1], r_junk])
        yi = 0
        for tt in range(NQ):
            for j in range(4):
                p_t, r_p = pc[j]
                for k in range(31):
                    P.op("pe", lambda e: e.matmul(p_t[:], lhsT=dg[j][0][:, k, :], rhs=up[:, j, tt * 512 + k:tt * 512 + k + 512], start=(k == 0), stop=(k == 30)),
                         reads=[dg[j][1], r_up], writes=[r_p])
                P.op("dve", lambda e: e.tensor_scalar(out=cv32[:, j, :], in0=p_t[:], scalar1=bcol[:, j:j + 1], scalar2=None, op0=ALU.add),
                     reads=[r_p, r_bcol], writes=[r_cv])
                P.op("pool", lambda e: e.tensor_tensor(out=sq32[:, j, :], in0=cv32[:, j, :], in1=cv32[:, j, :], op=ALU.mult), reads=[r_cv], writes=[r_sq])
            for j in range(4):
                P.op("pe", lambda e: e.matmul(pmean[:], lhsT=o512[:], rhs=cv32[:, j, :], start=(j == 0), stop=(j == 3)), reads=[r_o512, r_cv], writes=[r_pmean])
            for j in range(4):
                P.op("pe", lambda e: e.matmul(pex2[:], lhsT=o512[:], rhs=sq32[:, j, :], start=(j == 0), stop=(j == 3)), reads=[r_o512, r_sq], writes=[r_pex2])
            P.op("act", lambda e: e.copy(out=mean[:], in_=pmean[:]), reads=[r_pmean], writes=[r_mean])
            P.op("pool", lambda e: e.tensor_tensor(out=msq[:], in0=mean[:], in1=mean[:], op=ALU.mult), reads=[r_mean], writes=[r_msq])
            P.op("dve", lambda e: e.tensor_tensor(out=rs[:], in0=pex2[:], in1=msq[:], op=ALU.subtract), reads=[r_pex2, r_msq], writes=[r_rs])
            P.op("act", lambda e: e.activation(out=rs[:], in_=rs[:], func=AF.Ln, bias=epsb[:, 0:1]), reads=[r_rs, r_eps], writes=[r_rs])
            P.op("act", lambda e: e.activation(out=rs[:], in_=rs[:], func=AF.Exp, scale=-0.5), reads=[r_rs], writes=[r_rs])
            for j in range(4):
                y_t, r_y = y[yi % 2]
                b_t, r_b = ob[yi % 2]
                yi += 1
                P.op("pool", lambda e: e.tensor_tensor(out=y_t[:], in0=cv32[:, j, :], in1=mean[:], op=ALU.subtract), reads=[r_cv, r_mean], writes=[r_y])
                P.op("dve", lambda e: e.tensor_tensor(out=y_t[:], in0=y_t[:], in1=rs[:], op=ALU.mult), reads=[r_y, r_rs], writes=[r_y])
                P.op("act", lambda e: e.activation(out=b_t[:], in_=y_t[:], func=AF.Silu, scale=gcol[:, j:j + 1], bias=lbcol[:, j:j + 1]),
                     reads=[r_y, r_gcol, r_lbcol], writes=[r_b])
                P.dma("sp", cvT_d[j * 128:(j + 1) * 128, tt * 512:(tt + 1) * 512], b_t[:], reads=[r_b])
        st.done(f"cv{l}")

    def stage_mg(l, x_src):
        st = Stage(P)
        wg, r_wg = st.sb([128, 8, 3072], BF16)
        wp = [st.sb([128, 4, D], BF16) for _ in range(3)]
        wo, r_wo = st.sb([128, 8, D], BF16)
        bpb, r_bpb = st.sb([128, 8], F32)
        gmB, r_gmB = st.sb([128, D], F32)
        hT = [st.sb([128, 8, 512], BF16) for _ in range(2)]
        obr = [[st.sb([128, 4, 512], BF16) for _ in range(2)] for _ in range(3)]
        mT, r_mT = st.sb([128, 8, 512], BF16)
        py = [st.ps([128, 512], F32) for _ in range(2)]
        pg = [st.ps([128, 512], F32) for _ in range(2)]
        po = [st.ps([128, 512], F32) for _ in range(2)]
        sg = [st.sb([128, 512], F32) for _ in range(2)]
        mb = [st.sb([128, 512], F32) for _ in range(2)]
        acc, r_acc = st.sb([128, 512], F32)
        xt = [st.sb([128, D], F32) for _ in range(2)]
        tmp, r_tmp = st.sb([128, 512], F32)
        xn = [st.sb([128, D], F32) for _ in range(2)]
        for c in range(8):
            P.dma("pool", wg[:, c, :], I["w_in"][l][c * 128:(c + 1) * 128, 4096:7168], writes=[r_wg])
        for b, nm in enumerate(("w_proj_a", "w_proj_b", "w_proj_c")):
            P.dma("pool", wp[b][0][:], kp(I[nm][l]), writes=[wp[b][1]])
        P.dma("pool", wo[:], kp(I["w_out"][l]), writes=[r_wo])
        P.dma("sp", bpb[:], I["b_proj_b"][l].rearrange("(j p) -> p j", p=128), writes=[r_bpb])
        P.dma("sp", gmB[:], mod_d[l, 2 * D:3 * D].partition_broadcast(128), writes=[r_gmB])
        srcs = (oaT_d, cvT_d, ocT_d)
        loaded = set()

        def load_tile(tt):
            if tt in loaded or tt >= NQ:
                return
            loaded.add(tt)
            ts_ = slice(tt * 512, (tt + 1) * 512)
            P.dma("sp", hT[tt % 2][0][:], kp(hT_d)[:, :, ts_], writes=[hT[tt % 2][1]])
            for b_ in range(3):
                P.dma("sp", obr[b_][tt % 2][0][:], kp(srcs[b_])[:, :, ts_], writes=[obr[b_][tt % 2][1]])

        units = [(tt, j, b_) for tt in range(NQ) for j in range(8) for b_ in range(3)]
        state = {"xi": 0}

        def F(u):
            tt, j, b_ = units[u]
            load_tile(tt)
            y_t, r_y = py[u % 2]
            g_t, r_g = pg[u % 2]
            h_t, r_h = hT[tt % 2]
            o_b, r_ob = obr[b_][tt % 2]
            for c in range(4):
                P.op("pe", lambda e: e.matmul(y_t[:], lhsT=wp[b_][0][:, c, j * 128:(j + 1) * 128], rhs=o_b[:, c, :], start=(c == 0), stop=(c == 3)),
                     reads=[wp[b_][1], r_ob], writes=[r_y])
            for c in range(8):
                P.op("pe", lambda e: e.matmul(g_t[:], lhsT=wg[:, c, b_ * D + j * 128:b_ * D + (j + 1) * 128], rhs=h_t[:, c, :], start=(c == 0), stop=(c == 7)),
                     reads=[r_wg, r_h], writes=[r_g])

        def G(u):
            tt, j, b_ = units[u]
            y_t, r_y = py[u % 2]
            g_t, r_g = pg[u % 2]
            s_t, r_s = sg[u % 2]
            m_t, r_m = mb[u % 2]
            P.op("act", lambda e: e.activation(out=s_t[:], in_=g_t[:], func=AF.Sigmoid), reads=[r_g], writes=[r_s])
            if b_ == 0:
                P.op("dve", lambda e: e.tensor_tensor(out=acc[:], in0=y_t[:], in1=s_t[:], op=ALU.mult), reads=[r_y, r_s], writes=[r_acc])
            elif b_ == 1:
                P.op("dve", lambda e: e.scalar_tensor_tensor(out=m_t[:], in0=y_t[:], scalar=bpb[:, j:j + 1], in1=s_t[:], op0=ALU.add, op1=ALU.mult),
                     reads=[r_y, r_bpb, r_s], writes=[r_m])
                P.op("pool", lambda e: e.tensor_tensor(out=acc[:], in0=acc[:], in1=m_t[:], op=ALU.add), reads=[r_acc, r_m], writes=[r_acc])
            else:
                P.op("dve", lambda e: e.tensor_tensor(out=m_t[:], in0=y_t[:], in1=s_t[:], op=ALU.mult), reads=[r_y, r_s], writes=[r_m])
                P.op("dve", lambda e: e.tensor_tensor(out=mT[:, j, :], in0=acc[:], in1=m_t[:], op=ALU.add), reads=[r_acc, r_m], writes=[r_mT])
            if j == 7 and b_ == 2:
                OUT(tt)

        def OUT(tt):
            for sub in range(4):
                x_t, r_x = xt[state["xi"] % 2]
                n_t, r_n = xn[state["xi"] % 2]
                state["xi"] += 1
                r0 = tt * 512 + sub * 128
                P.dma("sp", x_t[:], x_src[r0:r0 + 128, :], writes=[r_x])
                for half in range(2):
                    o_t, r_o = po[half]
                    hs = slice(half * 512, (half + 1) * 512)
                    for c in range(8):
                        P.op("pe", lambda e: e.matmul(o_t[:], lhsT=mT[:, c, sub * 128:(sub + 1) * 128], rhs=wo[:, c, hs], start=(c == 0), stop=(c == 7)),
                             reads=[r_mT, r_wo], writes=[r_o])
                    P.op("dve", lambda e: e.tensor_tensor(out=tmp[:], in0=o_t[:], in1=gmB[:, hs], op=ALU.mult), reads=[r_o, r_gmB], writes=[r_tmp])
                    P.op("pool", lambda e: e.tensor_tensor(out=n_t[:, hs], in0=tmp[:], in1=x_t[:, hs], op=ALU.add), reads=[r_tmp, r_x], writes=[r_n])
                P.dma("sp", x1_d[r0:r0 + 128, :], n_t[:], reads=[r_n])

        nU = len(units)
        F(0)
        for u in range(nU):
            if u + 1 < nU:
                F(u + 1)
            G(u)
        st.done(f"mg{l}")

    def stage_moe(l, dst):
        TS = min(S, 2048)
        NTS = TS // 128
        st = Stage(P)
        hT, r_hT = st.sb([128, 8, TS], BF16)
        acc, r_acc_all = st.sb([128, NTS, D], F32)
        r_acc = [[Res() for _ in range(2)] for _ in range(NTS)]
        w1b = [st.sb([128, 8, FF], BF16) for _ in range(2)]
        w3b = [st.sb([128, 8, FF], BF16) for _ in range(2)]
        w2b = [st.sb([128, 2, D], BF16) for _ in range(2)]
        gB = [st.sb([128, TS], F32) for _ in range(2)]
        gfB, r_gfB = st.sb([128, D], F32)
        pa = [st.ps([128, 512], F32) for _ in range(2)]
        pb = [st.ps([128, 512], F32) for _ in range(2)]
        po = [st.ps([128, 512], F32) for _ in range(4)]
        sa = [st.sb([128, 512], F32) for _ in range(2)]
        tb = [st.sb([128, 512], F32) for _ in range(2)]
        hid = [st.sb([128, 2, 512], BF16) for _ in range(2)]
        xt = [st.sb([128, D], F32) for _ in range(2)]
        xn = [st.sb([128, D], F32) for _ in range(2)]
        P.dma("sp", gfB[:], mod_d[l, 5 * D:6 * D].partition_broadcast(128), writes=[r_gfB])
        oi = 0
        for sti in range(S // TS):
            t0 = sti * TS
            P.dma("sp", hT[:], kp(h2T_d)[:, :, t0:t0 + TS], writes=[r_hT])
            units = [(ex, tt) for ex in range(NE + 1) for tt in range(TS // 512)]
            loaded = set()

            def load_w(ex):
                if ex in loaded or ex > NE:
                    return
                loaded.add(ex)
                w1_t, r_w1 = w1b[ex % 2]
                w3_t, r_w3 = w3b[ex % 2]
                w2_t, r_w2 = w2b[ex % 2]
                g_t, r_g = gB[ex % 2]
                if ex < NE:
                    P.dma("pool", w1_t[:], kp(I["w1"][l, ex]), writes=[r_w1])
                    P.dma("pool", w3_t[:], kp(I["w3"][l, ex]), writes=[r_w3])
                    P.dma("pool", w2_t[:], kp(I["w2"][l, ex]), writes=[r_w2])
                    P.dma("sp", g_t[:], gT_d[ex, t0:t0 + TS].partition_broadcast(128), writes=[r_g])
                else:
                    P.dma("pool", w1_t[:], kp(I["ws1"][l]), writes=[r_w1])
                    P.dma("pool", w3_t[:], kp(I["ws3"][l]), writes=[r_w3])
                    P.dma("pool", w2_t[:], kp(I["ws2"][l]), writes=[r_w2])
                    P.op("dve", lambda e: e.memset(g_t[:], 1.0), writes=[r_g])

            def up(i):
                ex, tt = units[i]
                load_w(ex)
                w1_t, r_w1 = w1b[ex % 2]
                w3_t, r_w3 = w3b[ex % 2]
                g_t, r_g = gB[ex % 2]
                ts_ = slice(tt * 512, (tt + 1) * 512)
                h_t, r_h = hid[i % 2]
                for f in range(2):
                    a_t, r_a = pa[f]
                    b_t, r_b = pb[f]
                    s_t, r_s = sa[f]
                    t_t, r_t = tb[f]
                    for c in range(8):
                        P.op("pe", lambda e: e.matmul(a_t[:], lhsT=w1_t[:, c, f * 128:(f + 1) * 128], rhs=hT[:, c, ts_], start=(c == 0), stop=(c == 7)),
                             reads=[r_w1, r_hT], writes=[r_a])
                    for c in range(8):
                        P.op("pe", lambda e: e.matmul(b_t[:], lhsT=w3_t[:, c, f * 128:(f + 1) * 128], rhs=hT[:, c, ts_], start=(c == 0), stop=(c == 7)),
                             reads=[r_w3, r_hT], writes=[r_b])
                    P.op("act", lambda e: e.activation(out=s_t[:], in_=a_t[:], func=AF.Silu), reads=[r_a], writes=[r_s])
                    P.op("dve", lambda e: e.tensor_tensor(out=t_t[:], in0=b_t[:], in1=g_t[:, ts_], op=ALU.mult), reads=[r_b, r_g], writes=[r_t])
                    P.op("dve", lambda e: e.tensor_tensor(out=h_t[:, f, :], in0=s_t[:], in1=t_t[:], op=ALU.mult), reads=[r_s, r_t], writes=[r_h])

            def down(i):
                nonlocal oi
                ex, tt = units[i]
                w2_t, r_w2 = w2b[ex % 2]
                h_t, r_h = hid[i % 2]
                for sub in range(4):
                    ti = tt * 4 + sub
                    for half in range(2):
                        o_t, r_o = po[oi % 4]
                        oi += 1
                        hs = slice(half * 512, (half + 1) * 512)
                        for f in range(2):
                            P.op("pe", lambda e: e.matmul(o_t[:], lhsT=h_t[:, f, sub * 128:(sub + 1) * 128], rhs=w2_t[:, f, hs], start=(f == 0), stop=(f == 1)),
                                 reads=[r_h, r_w2], writes=[r_o])
                        if ex == 0:
                            P.op("dve", lambda e: e.tensor_copy(out=acc[:, ti, hs], in_=o_t[:]), reads=[r_o], writes=[r_acc[ti][half]])
                        else:
                            P.op("dve", lambda e: e.tensor_tensor(out=acc[:, ti, hs], in0=o_t[:], in1=acc[:, ti, hs], op=ALU.add),
                                 reads=[r_o, r_acc[ti][half]], writes=[r_acc[ti][half]])

            n = len(units)
            load_w(0)
            load_w(1)
            up(0)
            for i in range(n):
                if i + 1 < n:
                    up(i + 1)
                down(i)
                if i + 1 < n and units[i + 1][0] != units[i][0]:
                    load_w(units[i][0] + 2)
            for ti in range(NTS):
                x_t, r_x = xt[ti % 2]
                n_t, r_n = xn[ti % 2]
                r0 = t0 + ti * 128
                P.dma("sp", x_t[:], x1_d[r0:r0 + 128, :], writes=[r_x])
                P.op("dve", lambda e: e.tensor_tensor(out=n_t[:], in0=acc[:, ti, :], in1=gfB[:], op=ALU.mult), reads=[r_acc[ti][0], r_acc[ti][1], r_gfB], writes=[r_n])
                P.op("pool", lambda e: e.tensor_tensor(out=n_t[:], in0=n_t[:], in1=x_t[:], op=ALU.add), reads=[r_n, r_x], writes=[r_n])
                P.dma("sp", dst[r0:r0 + 128, :], n_t[:], reads=[r_n])
        st.done(f"moe{l}")

    def stage_route(l):
        st = Stage(P)
        sbB, r_sbB = st.sb([128, NE], F32)
        P.dma("sp", sbB[:], sbase_d.partition_broadcast(128), writes=[r_sbB])
        pos = [st.sb([128, NE], F32) for _ in range(2)]
        gat = [st.sb([128, NE], F32) for _ in range(2)]
        hrow = [st.sb([128, D], BF16) for _ in range(2)]
        a_ = [st.sb([128, NE], F32) for _ in range(2)]
        sel_ = [st.sb([128, NE], F32) for _ in range(2)]
        t8 = [st.sb([128, 8], F32) for _ in range(2)]
        si = [st.sb([128, 8], I32) for _ in range(2)]
        gk = [st.sb([128, 8], F32) for _ in range(2)]
        junk = [st.sb([128, NE], F32) for _ in range(2)]

        def chain(t):
            b = t % 2
            rows = slice(t * 128, (t + 1) * 128)
            P.dma("sp", pos[b][0][:], pos_d[rows, :], writes=[pos[b][1]])
            P.dma("sp", gat[b][0][:], gate_d[rows, :], writes=[gat[b][1]])
            P.dma("sp", hrow[b][0][:], h2_d[rows, :], writes=[hrow[b][1]])
            yield
            P.op("dve", lambda e: e.tensor_scalar(out=sel_[b][0][:], in0=gat[b][0][:], scalar1=0.0, scalar2=None, op0=ALU.is_gt),
                 reads=[gat[b][1]], writes=[sel_[b][1]])
            yield
            P.op("dve", lambda e: e.tensor_tensor(out=a_[b][0][:], in0=pos[b][0][:], in1=sbB[:], op=ALU.add), reads=[pos[b][1], r_sbB], writes=[a_[b][1]])
            yield
            P.op("dve", lambda e: e.tensor_tensor(out=a_[b][0][:], in0=a_[b][0][:], in1=sel_[b][0][:], op=ALU.mult), reads=[a_[b][1], sel_[b][1]], writes=[a_[b][1]])
            yield
            P.op("dve", lambda e: e.tensor_scalar(out=a_[b][0][:], in0=a_[b][0][:], scalar1=-1.0, scalar2=None, op0=ALU.add), reads=[a_[b][1]], writes=[a_[b][1]])
            yield
            P.op("dve", lambda e: e.max(out=t8[b][0][:], in_=a_[b][0][:]), reads=[a_[b][1]], writes=[t8[b][1]])
            yield
            P.op("dve", lambda e: e.tensor_copy(out=si[b][0][:], in_=t8[b][0][:]), reads=[t8[b][1]], writes=[si[b][1]])
            yield
            for k in range(8):
                P.op("dve", lambda e: e.scalar_tensor_tensor(out=junk[b][0][:], in0=a_[b][0][:], scalar=t8[b][0][:, k:k + 1], in1=gat[b][0][:], op0=ALU.is_equal, op1=ALU.mult),
                     reads=[a_[b][1], t8[b][1], gat[b][1]], writes=[junk[b][1]])
                yield
                P.op("dve", lambda e: e.tensor_reduce(out=gk[b][0][:, k:k + 1], in_=junk[b][0][:], axis=AX.X, op=ALU.add), reads=[junk[b][1]], writes=[gk[b][1]])
                yield
            P.dma("sp", slotk_d[rows, :], si[b][0][:], reads=[si[b][1]])
            P.dma("sp", gk_d[rows, :], gk[b][0][:], reads=[gk[b][1]])
            for k in range(8):
                def fs(eng, b=b, k=k):
                    return eng.indirect_dma_start(out=xs_d[:, :], out_offset=bass.IndirectOffsetOnAxis(ap=si[b][0][:, k:k + 1], axis=0),
                                                  in_=hrow[b][0][:, :], in_offset=None)
                P.raw("pool", fs, reads=[si[b][1], hrow[b][1]], is_dma=True)
            yield

        def drain(*gens):
            gens = list(gens)
            while gens:
                for g in list(gens):
                    try:
                        next(g)
                    except StopIteration:
                        gens.remove(g)

        for t in range(0, NT, 2):
            drain(*[chain(tt_) for tt_ in range(t, min(t + 2, NT))])
        st.done(f"route{l}")

    def stage_moe2(l):
        st = Stage(P)
        identb, r_idb = st.sb([128, 128], BF16)
        ebB, r_ebB = st.sb([128, 128], F32)
        iotac, r_iotac = st.sb([128, 1], F32)
        idxw, r_idxw = st.sb([128, 128], I32)
        w1b = [st.sb([128, 8, FF], BF16) for _ in range(3)]
        w3b = [st.sb([128, 8, FF], BF16) for _ in range(3)]
        w2b = [st.sb([128, 2, D], BF16) for _ in range(3)]
        xtok = [st.sb([128, 4, D], BF16) for _ in range(3)]
        XT = [st.sb([128, 8, 512], BF16) for _ in range(3)]
        hid = [st.sb([128, 2, 512], BF16) for _ in range(2)]
        sa = [st.sb([128, 512], F32) for _ in range(2)]
        ysb = [st.sb([128, 4, D], BF16) for _ in range(2)]
        pt = [st.ps([128, 8, 128], BF16) for _ in range(2)]
        pa = [st.ps([128, 512], F32) for _ in range(2)]
        pb = [st.ps([128, 512], F32) for _ in range(2)]
        po = [st.ps([128, 512], F32) for _ in range(2)]
        P.dma("pool", identb[:], I["k_ident"], writes=[r_idb])
        P.dma("sp", ebB[:], eb_d.partition_broadcast(128), writes=[r_ebB])
        P.dma("sp", iotac[:], col(I["k_iota"]), writes=[r_iotac])
        P.op("dve", lambda e: e.tensor_scalar(out=ebB[:], in0=ebB[:], scalar1=128.0, scalar2=float(l * NE * 128), op0=ALU.mult, op1=ALU.add),
             reads=[r_ebB], writes=[r_ebB])
        P.op("dve", lambda e: e.tensor_scalar(out=ebB[:], in0=ebB[:], scalar1=iotac[:, 0:1], scalar2=None, op0=ALU.add), reads=[r_ebB, r_iotac], writes=[r_ebB])
        P.op("dve", lambda e: e.tensor_copy(out=idxw[:], in_=ebB[:]), reads=[r_ebB], writes=[r_idxw])
        NU = NB + NQ
        state = {"oi": 0}

        def load(u):
            if u >= NU:
                return
            bf = u % 3
            if u < NB:
                for nm, (w_t, r_w) in (("w1h", w1b[bf]), ("w3h", w3b[bf]), ("w2h", w2b[bf])):
                    def fg(eng, nm=nm, w_t=w_t, u=u):
                        return eng.indirect_dma_start(out=w_t[:].rearrange("p c f -> p (c f)"), out_offset=None, in_=I[nm][:, :],
                                                      in_offset=bass.IndirectOffsetOnAxis(ap=idxw[:, u:u + 1], axis=0))
                    P.raw("pool", fg, reads=[r_idxw], writes=[r_w], is_dma=True)
                P.dma("sp", xtok[bf][0][:], xs_d[u * 512:(u + 1) * 512, :].rearrange("(s p) d -> p s d", p=128), writes=[xtok[bf][1]])
            else:
                if u in (NB, NB + 1, NB + 2):
                    P.dma("pool", w1b[bf][0][:], kp(I["ws1"][l]), writes=[w1b[bf][1]])
                    P.dma("pool", w3b[bf][0][:], kp(I["ws3"][l]), writes=[w3b[bf][1]])
                    P.dma("pool", w2b[bf][0][:], kp(I["ws2"][l]), writes=[w2b[bf][1]])
                tt = u - NB
                P.dma("sp", XT[bf][0][:], kp(h2T_d)[:, :, tt * 512:(tt + 1) * 512], writes=[XT[bf][1]])

        def tr(u):
            if u >= NB:
                return
            bf = u % 3
            x_t, r_x = xtok[bf]
            X_t, r_X = XT[bf]
            for sub in range(4):
                p_t, r_p = pt[sub % 2]
                for c in range(8):
                    P.op("pe", lambda e: e.transpose(out=p_t[:, c, :], in_=x_t[:, sub, c * 128:(c + 1) * 128], identity=identb[:]),
                         reads=[r_x, r_idb], writes=[r_p])
                if sub % 2 == 0:
                    P.op("act", lambda e: e.copy(out=X_t[:, :, sub * 128:(sub + 1) * 128], in_=p_t[:]), reads=[r_p], writes=[r_X])
                else:
                    P.op("dve", lambda e: e.tensor_copy(out=X_t[:, :, sub * 128:(sub + 1) * 128], in_=p_t[:]), reads=[r_p], writes=[r_X])

        def up(u):
            bf = u % 3
            w1_t, r_w1 = w1b[bf]
            w3_t, r_w3 = w3b[bf]
            X_t, r_X = XT[bf]
            h_t, r_h = hid[u % 2]
            for f in range(2):
                a_t, r_a = pa[f]
                b_t, r_b = pb[f]
                s_t, r_s = sa[f]
                for c in range(8):
                    P.op("pe", lambda e: e.matmul(a_t[:], lhsT=w1_t[:, c, f * 128:(f + 1) * 128], rhs=X_t[:, c, :], start=(c == 0), stop=(c == 7)),
                         reads=[r_w1, r_X], writes=[r_a])
                for c in range(8):
                    P.op("pe", lambda e: e.matmul(b_t[:], lhsT=w3_t[:, c, f * 128:(f + 1) * 128], rhs=X_t[:, c, :], start=(c == 0), stop=(c == 7)),
                         reads=[r_w3, r_X], writes=[r_b])
                P.op("act", lambda e: e.activation(out=s_t[:], in_=a_t[:], func=AF.Silu), reads=[r_a], writes=[r_s])
                P.op("dve", lambda e: e.tensor_tensor(out=h_t[:, f, :], in0=b_t[:], in1=s_t[:], op=ALU.mult), reads=[r_b, r_s], writes=[r_h])

        def down(u):
            w2_t, r_w2 = w2b[u % 3]
            h_t, r_h = hid[u % 2]
            y_t, r_y = ysb[u % 2]
            for sub in range(4):
                for half in range(2):
                    o_t, r_o = po[state["oi"] % 2]
                    state["oi"] += 1
                    hs = slice(half * 512, (half + 1) * 512)
                    for f in range(2):
                        P.op("pe", lambda e: e.matmul(o_t[:], lhsT=h_t[:, f, sub * 128:(sub + 1) * 128], rhs=w2_t[:, f, hs], start=(f == 0), stop=(f == 1)),
                             reads=[r_h, r_w2], writes=[r_o])
                    if half == 0:
                        P.op("act", lambda e: e.copy(out=y_t[:, sub, hs], in_=o_t[:]), reads=[r_o], writes=[r_y])
                    else:
                        P.op("dve", lambda e: e.tensor_copy(out=y_t[:, sub, hs], in_=o_t[:]), reads=[r_o], writes=[r_y])
            if u < NB:
                P.dma("sp", ys_d[u * 512:(u + 1) * 512, :].rearrange("(s p) d -> p s d", p=128), y_t[:], reads=[r_y])
            else:
                tt = u - NB
                P.dma("sp", ysh_d[tt * 512:(tt + 1) * 512, :].rearrange("(s p) d -> p s d", p=128), y_t[:], reads=[r_y])

        load(0)
        load(1)
        tr(0)
        up(0)
        for u in range(NU):
            load(u + 2)
            if u + 1 < NU:
                tr(u + 1)
                up(u + 1)
            down(u)
        st.done(f"moe{l}")

    def stage_comb(l, dst):
        st = Stage(P)
        gfB, r_gfB = st.sb([128, D], F32)
        P.dma("sp", gfB[:], mod_d[l, 5 * D:6 * D].partition_broadcast(128), writes=[r_gfB])
        si = [st.sb([128, 8], I32) for _ in range(2)]
        gk = [st.sb([128, 8], F32) for _ in range(2)]
        xt = [st.sb([128, D], F32) for _ in range(2)]
        ysh = [st.sb([128, D], BF16) for _ in range(2)]
        yg = [[st.sb([128, D], BF16) for _ in range(8)] for _ in range(2)]
        acc = [st.sb([128, D], F32) for _ in range(2)]
        def fetch(t):
            if t >= NT:
                return
            b = t % 2
            rows = slice(t * 128, (t + 1) * 128)
            P.dma("sp", si[b][0][:], slotk_d[rows, :], writes=[si[b][1]])
            P.dma("sp", gk[b][0][:], gk_d[rows, :], writes=[gk[b][1]])
            P.dma("sp", xt[b][0][:], x1_d[rows, :], writes=[xt[b][1]])
            P.dma("sp", ysh[b][0][:], ysh_d[rows, :], writes=[ysh[b][1]])
            for k in range(8):
                def fg(eng, b=b, k=k):
                    return eng.indirect_dma_start(out=yg[b][k][0][:, :], out_offset=None, in_=ys_d[:, :],
                                                  in_offset=bass.IndirectOffsetOnAxis(ap=si[b][0][:, k:k + 1], axis=0))
                P.raw("pool", fg, reads=[si[b][1]], writes=[yg[b][k][1]], is_dma=True)

        def comp(t):
            b = t % 2
            rows = slice(t * 128, (t + 1) * 128)
            a_t, r_a = acc[b]
            P.op("dve", lambda e: e.scalar_tensor_tensor(out=a_t[:], in0=yg[b][0][0][:], scalar=gk[b][0][:, 0:1], in1=ysh[b][0][:], op0=ALU.mult, op1=ALU.add),
                 reads=[yg[b][0][1], gk[b][1], ysh[b][1]], writes=[r_a])
            for k in range(1, 8):
                P.op("dve", lambda e: e.scalar_tensor_tensor(out=a_t[:], in0=yg[b][k][0][:], scalar=gk[b][0][:, k:k + 1], in1=a_t[:], op0=ALU.mult, op1=ALU.add),
                     reads=[yg[b][k][1], gk[b][1], r_a], writes=[r_a])
            P.op("dve", lambda e: e.tensor_tensor(out=a_t[:], in0=a_t[:], in1=gfB[:], op=ALU.mult), reads=[r_a, r_gfB], writes=[r_a])
            P.op("dve", lambda e: e.tensor_tensor(out=a_t[:], in0=a_t[:], in1=xt[b][0][:], op=ALU.add), reads=[r_a, xt[b][1]], writes=[r_a])
            P.dma("sp", dst[rows, :], a_t[:], reads=[r_a])

        fetch(0)
        for t in range(NT):
            comp_deferred = t
            fetch(t + 1)
            comp(t)
        st.done(f"comb{l}")

    todo = stages if stages is not None else ("mod", "rope", "norm1", "proj")
    if "mod" in todo:
        stage_mod()
    if "rope" in todo:
        stage_rope()
    for l in range(L):
        x_in = I["x"] if l == 0 else x2_d
        if "norm1" in todo:
            stage_norm(l, x_in, "norm_mix_g", 1 * D, 0 * D, hT_d, router=False)
        if "proj" in todo:
            stage_proj(l)
        if "da" in todo:
            stage_da(l)
        if "sb" in todo:
            stage_sb(l)
        if "cv" in todo:
            stage_cv(l)
        if "mg" in todo:
            stage_mg(l, x_in)
        if "norm2" in todo:
            stage_norm(l, x1_d, "norm_ffn_g", 4 * D, 3 * D, h2T_d, router=True)
        if "moe" in todo:
            stage_moe(l, out if l == L - 1 else x2_d)
        if "smoe" in todo:
            stage_route(l)
            stage_moe2(l)
            stage_comb(l, out if l == L - 1 else x2_d)
    es.close()
    return nc


ALL_STAGES = ("mod", "rope", "norm1", "proj", "da", "sb", "cv", "mg", "norm2", "smoe")
_CACHE = {}


def kernel(**inputs):
    x = np.ascontiguousarray(np.asarray(inputs["x"], dtype=np.float32))
    B, S, _ = x.shape
    L = int(np.asarray(inputs["w_mod"]).shape[0])
    key = (S, L)
    if key not in _CACHE:
        _CACHE[key] = build(S, L=L, stages=ALL_STAGES)
    nc = _CACHE[key]
    consts = make_consts()
    shared = {k: np.ascontiguousarray(np.asarray(inputs[k], dtype=np.float32)) for k in W_SHAPES if k not in ("w1", "w3", "w2")}
    shared.update(relayout_experts(inputs, L))
    c = np.asarray(inputs["c"], dtype=np.float32)
    pos = np.asarray(inputs["positions"]).astype(np.int32)
    in_maps = []
    for b in range(B):
        m = {"x": x[b], "c": np.ascontiguousarray(c[b]), "pos": np.ascontiguousarray(pos[b])}
        m.update(shared)
        m.update(consts)
        in_maps.append(m)
    res = run_bass_kernel_spmd(nc, in_maps, core_ids=list(range(B)))
    return np.stack([np.asarray(r["out"], dtype=np.float32) for r in res.results], axis=0)


def relayout_experts(W, L):
    o = {}
    w1 = np.asarray(W["w1"], dtype=np.float32)[:L]
    w3 = np.asarray(W["w3"], dtype=np.float32)[:L]
    w2 = np.asarray(W["w2"], dtype=np.float32)[:L]
    o["w1h"] = np.ascontiguousarray(w1.reshape(L, NE, 8, 128, FF).transpose(0, 1, 3, 2, 4)).reshape(L * NE * 128, 8 * FF)
    o["w3h"] = np.ascontiguousarray(w3.reshape(L, NE, 8, 128, FF).transpose(0, 1, 3, 2, 4)).reshape(L * NE * 128, 8 * FF)
    o["w2h"] = np.ascontiguousarray(w2.reshape(L, NE, 2, 128, D).transpose(0, 1, 3, 2, 4)).reshape(L * NE * 128, 2 * D)
    return o
```

```python
import math
from contextlib import ExitStack
import numpy as np
import concourse.bass as bass
import concourse.mybir as mybir
from concourse.bass_utils import run_bass_kernel_spmd

F32 = mybir.dt.float32
BF16 = mybir.dt.bfloat16
I32 = mybir.dt.int32
ALU = mybir.AluOpType
AF = mybir.ActivationFunctionType
AX = mybir.AxisListType

ENGS = ("pe", "act", "dve", "pool", "sp")
SEM_LIM = 30000
DMA_RING = 8

D = 1024
NE = 64
FF = 256
EPS = 1e-6


class Res:
    __slots__ = ("w", "r")

    def __init__(self):
        self.w = None
        self.r = []


class Op:
    __slots__ = ("eng", "fn", "deps", "signal", "k", "is_dma", "slot", "dval")

    def __init__(self, eng, fn, is_dma=False):
        self.eng = eng
        self.fn = fn
        self.deps = set()
        self.signal = False
        self.k = None
        self.is_dma = is_dma
        self.slot = None
        self.dval = None


class _Rec:
    def __getattr__(self, name):
        return lambda *a, **k: (name, a, k)


_REC = _Rec()


class Prog:
    def __init__(self, nc, es):
        self.nc = nc
        self.sems = {e: [es.enter_context(nc.semaphore(f"s_{e}_{i}")) for i in range(6)] for e in ENGS if e != "sp"}
        self.nsig = {e: 0 for e in ENGS}
        self.dq = ("sp", "act", "pool")
        self.dsems = {e: [es.enter_context(nc.semaphore(f"d_{e}_{i}")) for i in range(DMA_RING)] for e in self.dq}
        self.ndma = {e: 0 for e in self.dq}
        self.ring_last = {e: [None] * DMA_RING for e in self.dq}
        self.waited = {e: {} for e in ENGS}
        self.ops = []
        self.last_op = {e: None for e in ENGS}
        self.stage_dmas = []

    def _add(self, op, reads, writes):
        deps = set()
        for r in reads:
            if r.w is not None:
                deps.add(r.w)
        for w in writes:
            if w.w is not None:
                deps.add(w.w)
            for o in w.r:
                deps.add(o)
        for d in deps:
            if d is op:
                continue
            if (not d.is_dma) and d.eng == op.eng and op.eng == "pe" and not op.is_dma:
                continue
            op.deps.add(d)
            if not d.is_dma:
                d.signal = True
        for r in reads:
            r.r.append(op)
        for w in writes:
            w.w = op
            w.r = []
        self.ops.append(op)
        if not op.is_dma:
            self.last_op[op.eng] = op
        return op

    def op(self, eng, fn, reads=(), writes=()):
        return self._add(Op(eng, fn(_REC)), reads, writes)

    def dma(self, q, out, in_, reads=(), writes=()):
        op = Op(q, ("dma_start", (), dict(out=out, in_=in_)), is_dma=True)
        i = self.ndma[q]
        self.ndma[q] += 1
        op.slot = i % DMA_RING
        op.dval = 16 * (i // DMA_RING + 1)
        prev = self.ring_last[q][op.slot]
        if prev is not None:
            op.deps.add(prev)
        self.ring_last[q][op.slot] = op
        self.stage_dmas.append(op)
        return self._add(op, reads, writes)

    def raw(self, eng, fn, reads=(), writes=(), is_dma=False):
        op = Op(eng, fn, is_dma=is_dma)
        if is_dma:
            i = self.ndma[eng]
            self.ndma[eng] += 1
            op.slot = i % DMA_RING
            op.dval = 16 * (i // DMA_RING + 1)
            prev = self.ring_last[eng][op.slot]
            if prev is not None:
                op.deps.add(prev)
            self.ring_last[eng][op.slot] = op
            self.stage_dmas.append(op)
        return self._add(op, reads, writes)

    def barrier(self):
        lasts = [o for o in self.last_op.values() if o is not None]
        dmas = list(self.stage_dmas)
        for e in ENGS:
            op = Op(e, None)
            for d in lasts:
                if d.eng != e:
                    op.deps.add(d)
                    d.signal = True
            for d in dmas:
                op.deps.add(d)
            self.ops.append(op)
        self.stage_dmas = []
        self.last_op = {e: None for e in ENGS}

    def emit(self):
        nc = self.nc
        self.barrier()
        ops = self.ops
        self.ops = []
        for o in ops:
            if o.signal and not o.is_dma and o.k is None:
                o.k = self.nsig[o.eng]
                self.nsig[o.eng] += 1
        per = {e: [o for o in ops if o.eng == e] for e in ENGS}
        engobj = {"pe": "tensor", "act": "scalar", "dve": "vector", "pool": "gpsimd", "sp": "sync"}

        def run(e, eng):
            waited = self.waited[e]
            for o in per[e]:
                need = {}
                for d in o.deps:
                    if d.is_dma:
                        key = ("d", d.eng, d.slot)
                        sem = self.dsems[d.eng][d.slot]
                        val = d.dval
                    else:
                        key = ("c", d.eng, d.k // SEM_LIM)
                        sem = self.sems[d.eng][d.k // SEM_LIM]
                        val = d.k % SEM_LIM + 1
                    if waited.get(key, 0) >= val:
                        continue
                    if key not in need or need[key][1] < val:
                        need[key] = (sem, val)
                for key, (sem, val) in need.items():
                    eng.wait_ge(sem, val)
                    waited[key] = val
                if o.fn is None:
                    continue
                if callable(o.fn):
                    ins = o.fn(eng)
                else:
                    name, a, k = o.fn
                    ins = getattr(eng, name)(*a, **k)
                if o.is_dma:
                    ins.then_inc(self.dsems[o.eng][o.slot], 16)
                elif o.signal:
                    ins.then_inc(self.sems[o.eng][o.k // SEM_LIM], 1)

        with nc.allow_non_contiguous_dma(reason="small strided parameter loads"):
            with nc.Block() as block:
                for e in ENGS:
                    if not per[e]:
                        continue
                    getattr(block, engobj[e])(lambda eng, e=e: run(e, eng))


class Stage:
    count = 0

    def __init__(self, P):
        self.P = P
        self.nc = P.nc
        self.es = ExitStack()
        self.n = 0
        Stage.count += 1
        self.sid = Stage.count

    def sb(self, shape, dt):
        self.n += 1
        t = self.es.enter_context(self.nc.sbuf_tensor(f"t{self.sid}_{self.n}", list(shape), dt))
        return t, Res()

    def ps(self, shape, dt):
        self.n += 1
        t = self.es.enter_context(self.nc.psum_tensor(f"p{self.sid}_{self.n}", list(shape), dt))
        return t, Res()

    def done(self, name=None):
        if name:
            with self.nc.named_scope(name):
                self.P.emit()
        else:
            self.P.emit()
        self.es.close()


def col(ap1d):
    return ap1d.rearrange("(p o) -> p o", o=1)


def make_consts():
    p = np.arange(128)
    ident = np.eye(128, dtype=np.float32)
    blk64 = ((p[:, None] // 64) == (p[None, :] // 64)).astype(np.float32) / 64.0
    rotT = np.zeros((128, 128), np.float32)
    for k in range(128):
        if k % 64 >= 32:
            rotT[k, k - 32] = -1.0
        else:
            rotT[k, k + 32] = 1.0
    ones = np.ones((128, 128), np.float32)
    ustrict = (p[:, None] > p[None, :]).astype(np.float32)
    uincl = (p[:, None] >= p[None, :]).astype(np.float32)
    ulow = (p[:, None] < p[None, :]).astype(np.float32)
    pincl = (p[:, None] <= p[None, :]).astype(np.float32)
    iota = p.astype(np.float32)
    qq = np.arange(512)
    maskc = np.stack([((qq[None, :] - j * 128 - p[:, None]) >= 0) for j in range(4)]).astype(np.float32)
    masks = np.stack([((qq[None, :] - j * 128 - p[:, None]) > 0) for j in range(4)]).astype(np.float32)
    inv = (10000.0 ** (-np.arange(0, 64, 2, dtype=np.float32) / np.float32(64))).astype(np.float32)
    invc = inv[p % 32].astype(np.float32)
    return dict(k_ident=ident, k_blk64=blk64, k_rotT=rotT, k_ones=ones, k_ustrict=ustrict, k_uincl=uincl, k_ulow=ulow, k_pincl=pincl, k_iota=iota,
                k_maskc=maskc, k_masks=masks, k_invc=invc)


W_SHAPES = dict(
    w_mod=(D, 6 * D), b_mod=(6 * D,), norm_mix_g=(D,), norm_ffn_g=(D,), w_in=(D, 7168),
    qn_g=(64,), kn_g=(64,), lam_q1=(64,), lam_k1=(64,), lam_q2=(64,), lam_k2=(64,), subln_g=(128,),
    w_proj_a=(512, D), w_dw=(31, 512), b_dw=(512,), conv_ln_g=(512,), conv_ln_b=(512,),
    w_proj_b=(512, D), b_proj_b=(D,), w_proj_c=(512, D), w_out=(D, D), w_router=(D, NE), b_router=(NE,),
    w1=(NE, D, FF), w3=(NE, D, FF), w2=(NE, FF, D), ws1=(D, FF), ws3=(D, FF), ws2=(FF, D),
)


def build(S, L=2, dbg=(), stages=None):
    NT = S // 128
    NQ = S // 512
    nc = bass.Bass("TRN2", target_bir_lowering=False)
    I = {}
    I["x"] = nc.dram_tensor("x", [S, D], F32, kind="ExternalInput").ap()
    I["c"] = nc.dram_tensor("c", [D], F32, kind="ExternalInput").ap()
    I["pos"] = nc.dram_tensor("pos", [S], I32, kind="ExternalInput").ap()
    dense_moe = stages is not None and "moe" in stages
    for k, shp in W_SHAPES.items():
        if k in ("w1", "w3", "w2") and not dense_moe:
            continue
        I[k] = nc.dram_tensor(k, [L] + list(shp), F32, kind="ExternalInput").ap()
    for k, v in make_consts().items():
        I[k] = nc.dram_tensor(k, list(v.shape), F32, kind="ExternalInput").ap()
    for k in ("w1h", "w3h", "w2h"):
        I[k] = nc.dram_tensor(k, [L * NE * 128, 2048], F32, kind="ExternalInput").ap()
    out = nc.dram_tensor("out", [S, D], F32, kind="ExternalOutput").ap()
    NB = S * 8 // 512 + NE
    NSLOT = NB * 512

    def scratch(name, shape, dt):
        kind = "ExternalOutput" if name in dbg else "Internal"
        return nc.dram_tensor(name, list(shape), dt, kind=kind).ap()

    mod_d = scratch("mod_d", [L, 6 * D], F32)
    cos_d = scratch("cos_d", [128, S], F32)
    sin_d = scratch("sin_d", [128, S], F32)
    hT_d = scratch("hT_d", [D, S], BF16)
    qaT_d = scratch("qaT_d", [512, S], BF16)
    kaT_d = scratch("kaT_d", [512, S], BF16)
    va_d = scratch("va_d", [S, 512], BF16)
    uT_d = scratch("uT_d", [512, S], BF16)
    qcT_d = scratch("qcT_d", [512, S], BF16)
    kcT_d = scratch("kcT_d", [512, S], BF16)
    vc_d = scratch("vc_d", [S, 512], BF16)
    oaT_d = scratch("oaT_d", [512, S], BF16)
    cvT_d = scratch("cvT_d", [512, S], BF16)
    ocT_d = scratch("ocT_d", [512, S], BF16)
    x1_d = scratch("x1_d", [S, D], F32)
    x2_d = scratch("x2_d", [S, D], F32)
    h2T_d = scratch("h2T_d", [D, S], BF16)
    gT_d = scratch("gT_d", [NE, S], F32)
    gate_d = scratch("gate_d", [S, NE], F32)
    pos_d = scratch("pos_d", [S, NE], F32)
    h2_d = scratch("h2_d", [S, D], BF16)
    sbase_d = scratch("sbase_d", [NE], F32)
    eb_d = scratch("eb_d", [128], F32)
    slotk_d = scratch("slotk_d", [S, 8], I32)
    gk_d = scratch("gk_d", [S, 8], F32)
    xs_d = scratch("xs_d", [NSLOT, D], BF16)
    ys_d = scratch("ys_d", [NSLOT, D], BF16)
    ysh_d = scratch("ysh_d", [S, D], BF16)

    def kp(ap2d):
        return ap2d.rearrange("(c p) n -> p c n", p=128)

    es = ExitStack()
    P = Prog(nc, es)

    def stage_mod():
        st = Stage(P)
        cT, r_cT = st.sb([128, 8], F32)
        cA, r_cA = st.sb([128, 8], F32)
        wm = [st.sb([128, 8, 512], F32) for _ in range(2)]
        bm, r_bm = st.sb([1, L * 6 * D], F32)
        mr, r_mr = st.sb([1, L * 6 * D], F32)
        pm = [st.ps([1, 512], F32) for _ in range(2)]
        P.dma("sp", cT[:], I["c"].rearrange("(c p) -> p c", p=128), writes=[r_cT])
        P.dma("sp", bm[:], I["b_mod"].rearrange("(o l) n -> o (l n)", o=1), writes=[r_bm])
        P.op("act", lambda e: e.activation(out=cA[:], in_=cT[:], func=AF.Silu), reads=[r_cT], writes=[r_cA])
        i = 0
        for l in range(L):
            for blk in range(12):
                w_t, r_w = wm[i % 2]
                p_t, r_p = pm[i % 2]
                P.dma("sp", w_t[:], kp(I["w_mod"][l])[:, :, blk * 512:(blk + 1) * 512], writes=[r_w])
                for c in range(8):
                    P.op("pe", lambda e, c=c, w_t=w_t, p_t=p_t: e.matmul(p_t[:], lhsT=cA[:, c:c + 1], rhs=w_t[:, c, :], start=(c == 0), stop=(c == 7)),
                         reads=[r_cA, r_w], writes=[r_p])
                o = l * 6 * D + blk * 512
                P.op("dve", lambda e, o=o, p_t=p_t: e.tensor_tensor(out=mr[:, o:o + 512], in0=p_t[:], in1=bm[:, o:o + 512], op=ALU.add),
                     reads=[r_p, r_bm], writes=[r_mr])
                i += 1
        P.dma("sp", mod_d.rearrange("(o l) n -> o (l n)", o=1), mr[:], reads=[r_mr])
        st.done("mod")

    def stage_rope():
        st = Stage(P)
        pi_t, r_pi = st.sb([128, S], I32)
        pf, r_pf = st.sb([128, S], F32)
        inv, r_inv = st.sb([128, 1], F32)
        ang, r_ang = st.sb([128, S], F32)
        u, r_u = st.sb([128, S], F32)
        ki, r_ki = st.sb([128, S], I32)
        kf, r_kf = st.sb([128, S], F32)
        m, r_m = st.sb([128, S], F32)
        res_t, r_res = st.sb([128, S], F32)
        zero, r_zero = st.sb([128, 1], F32)
        TWO_PI = 2.0 * math.pi
        C1 = 6.28125
        C2 = TWO_PI - C1
        P.dma("sp", pi_t[:], I["pos"].partition_broadcast(128), writes=[r_pi])
        P.dma("sp", inv[:], col(I["k_invc"]), writes=[r_inv])
        P.op("pool", lambda e: e.memset(zero[:], 0.0), writes=[r_zero])
        P.op("dve", lambda e: e.tensor_copy(out=pf[:], in_=pi_t[:]), reads=[r_pi], writes=[r_pf])
        for which, dst in ((0, sin_d), (1, cos_d)):
            shift = 0.0 if which == 0 else math.pi / 2
            P.op("dve", lambda e, shift=shift: e.tensor_scalar(out=ang[:], in0=pf[:], scalar1=inv[:, 0:1], scalar2=shift, op0=ALU.mult, op1=ALU.add),
                 reads=[r_pf, r_inv], writes=[r_ang])
            P.op("dve", lambda e: e.tensor_scalar(out=u[:], in0=ang[:], scalar1=1.0 / TWO_PI, scalar2=None, op0=ALU.mult),
                 reads=[r_ang], writes=[r_u])
            P.op("dve", lambda e: e.tensor_copy(out=ki[:], in_=u[:]), reads=[r_u], writes=[r_ki])
            P.op("dve", lambda e: e.tensor_copy(out=kf[:], in_=ki[:]), reads=[r_ki], writes=[r_kf])
            P.op("dve", lambda e: e.scalar_tensor_tensor(out=u[:], in0=kf[:], scalar=-C1, in1=ang[:], op0=ALU.mult, op1=ALU.add),
                 reads=[r_kf, r_ang], writes=[r_u])
            P.op("dve", lambda e: e.scalar_tensor_tensor(out=u[:], in0=kf[:], scalar=-C2, in1=u[:], op0=ALU.mult, op1=ALU.add),
                 reads=[r_kf, r_u], writes=[r_u])
            P.op("dve", lambda e: e.tensor_scalar(out=m[:], in0=u[:], scalar1=math.pi, scalar2=-TWO_PI, op0=ALU.is_gt, op1=ALU.mult),
                 reads=[r_u], writes=[r_m])
            P.op("dve", lambda e: e.tensor_tensor(out=u[:], in0=u[:], in1=m[:], op=ALU.add), reads=[r_u, r_m], writes=[r_u])
            P.op("dve", lambda e: e.tensor_scalar(out=m[:], in0=u[:], scalar1=-math.pi, scalar2=TWO_PI, op0=ALU.is_lt, op1=ALU.mult),
                 reads=[r_u], writes=[r_m])
            P.op("dve", lambda e: e.tensor_tensor(out=u[:], in0=u[:], in1=m[:], op=ALU.add), reads=[r_u, r_m], writes=[r_u])
            P.op("act", lambda e: e.activation(out=res_t[:], in_=u[:], func=AF.Sin, bias=zero[:, 0:1]), reads=[r_u, r_zero], writes=[r_res])
            P.dma("sp", dst, res_t[:], reads=[r_res])
        st.done("rope")

    def stage_norm(l, x_src, g_name, sc_off, sh_off, hT_dst, router):
        st = Stage(P)
        gB, r_gB = st.sb([128, D], F32)
        scB, r_scB = st.sb([128, D], F32)
        shB, r_shB = st.sb([128, D], F32)
        A, r_A = st.sb([128, D], F32)
        epsb, r_eps = st.sb([128, 1], F32)
        identb, r_idb = st.sb([128, 128], BF16)
        xt = [st.sb([128, D], F32) for _ in range(4)]
        sq, r_sq = st.sb([128, D], F32)
        ss, r_ss = st.sb([128, 1], F32)
        rstd, r_rstd = st.sb([128, 1], F32)
        hf, r_hf = st.sb([128, D], F32)
        hb, r_hb = st.sb([128, D], BF16)
        hTs = [st.sb([128, 8, 128], BF16) for _ in range(2)]
        pt = [st.ps([128, 8, 128], BF16) for _ in range(2)]
        P.dma("sp", gB[:], I[g_name][l].partition_broadcast(128), writes=[r_gB])
        P.dma("sp", scB[:], mod_d[l, sc_off:sc_off + D].partition_broadcast(128), writes=[r_scB])
        P.dma("sp", shB[:], mod_d[l, sh_off:sh_off + D].partition_broadcast(128), writes=[r_shB])
        P.dma("pool", identb[:], I["k_ident"], writes=[r_idb])
        P.op("pool", lambda e: e.memset(epsb[:], EPS), writes=[r_eps])
        P.op("dve", lambda e: e.scalar_tensor_tensor(out=A[:], in0=scB[:], scalar=1.0, in1=gB[:], op0=ALU.add, op1=ALU.mult),
             reads=[r_scB, r_gB], writes=[r_A])
        if router:
            identf, r_idf = st.sb([128, 128], F32)
            wr, r_wr = st.sb([128, 8, NE], F32)
            brB, r_brB = st.sb([128, NE], F32)
            h32, r_h32 = st.sb([128, D], F32)
            hTf, r_hTf = st.sb([128, 8, 128], F32)
            ptf = [st.ps([128, 4, 128], F32) for _ in range(2)]
            plg, r_plg = st.ps([128, NE], F32)
            PC, r_PC = st.ps([128, NE], F32)
            pinc, r_pinc = st.sb([128, 128], F32)
            pgtm, r_pgtm = st.sb([128, 128], F32)
            iotac, r_iotac = st.sb([128, 1], F32)
            posb = [st.sb([128, NE], F32) for _ in range(4)]
            selb = [st.sb([128, NE], F32) for _ in range(4)]
            P.dma("sp", pinc[:], I["k_pincl"], writes=[r_pinc])
            P.dma("sp", pgtm[:], I["k_ustrict"], writes=[r_pgtm])
            P.dma("sp", iotac[:], col(I["k_iota"]), writes=[r_iotac])
            sc_t, r_sc = st.sb([128, NE], F32)
            bi, r_bi = st.sb([128, NE], F32)
            tmp, r_tmp = st.sb([128, NE], F32)
            m1, r_m1 = st.sb([128, 8], F32)
            m2, r_m2 = st.sb([128, 8], F32)
            gs, r_gs = st.sb([128, 8], F32)
            t8, r_t8 = st.sb([128, 8], F32)
            pen, r_pen = st.sb([128, 8], F32)
            sel, r_sel = st.sb([128, NE], F32)
            ssum, r_ssum = st.sb([128, 1], F32)
            gate, r_gate = st.sb([128, NE], F32)
            gTs, r_gTs = st.sb([NE, 128], F32)
            P.dma("sp", identf[:], I["k_ident"], writes=[r_idf])
            P.dma("sp", wr[:], kp(I["w_router"][l]), writes=[r_wr])
            P.dma("sp", brB[:], I["b_router"][l].partition_broadcast(128), writes=[r_brB])
        sq2 = [(sq, r_sq)] + [st.sb([128, D], F32) for _ in range(3)]
        ss2 = [(ss, r_ss)] + [st.sb([128, 1], F32) for _ in range(3)]
        rstd2 = [(rstd, r_rstd)] + [st.sb([128, 1], F32) for _ in range(3)]
        hf2 = [(hf, r_hf)] + [st.sb([128, D], F32) for _ in range(3)]
        hb2 = [(hb, r_hb)] + [st.sb([128, D], BF16) for _ in range(3)]
        if router:
            h322 = [(h32, r_h32)] + [st.sb([128, D], F32) for _ in range(3)]
            dup_sc = [(sc_t, r_sc), st.sb([128, NE], F32)]
            dup_bi = [(bi, r_bi), st.sb([128, NE], F32)]
            dup_tmp = [(tmp, r_tmp), st.sb([128, NE], F32)]
            dup_m1 = [(m1, r_m1), st.sb([128, 8], F32)]
            dup_m2 = [(m2, r_m2), st.sb([128, 8], F32)]
            dup_gs = [(gs, r_gs), st.sb([128, 8], F32)]
            dup_t8 = [(t8, r_t8), st.sb([128, 8], F32)]
            dup_pen = [(pen, r_pen), st.sb([128, 8], F32)]
            dup_sel = [(sel, r_sel), st.sb([128, NE], F32)]
            dup_ssum = [(ssum, r_ssum), st.sb([128, 1], F32)]
            dup_gate = [(gate, r_gate), st.sb([128, NE], F32)]
            dup_gTs = [(gTs, r_gTs), st.sb([NE, 128], F32)]
            dup_plg = [(plg, r_plg), st.ps([128, NE], F32)]
            plg2 = dup_plg
            onec, r_onec = st.sb([128, 1], F32)
            P.op("pool", lambda e: e.memset(onec[:], 1.0), writes=[r_onec])

        def ph1(t):
            x_t, r_x = xt[t % 4]
            sq_t, r_sq_ = sq2[t % 4]
            ss_t, r_ss_ = ss2[t % 4]
            rs_t, r_rs_ = rstd2[t % 4]
            hf_t, r_hf_ = hf2[t % 4]
            hb_t, r_hb_ = hb2[t % 4]
            P.dma("sp", x_t[:], x_src[t * 128:(t + 1) * 128, :], writes=[r_x])
            P.op("act", lambda e: e.activation(out=sq_t[:], in_=x_t[:], func=AF.Square, scale=float(D ** -0.5), accum_out=ss_t[:]),
                 reads=[r_x], writes=[r_sq_, r_ss_])
            P.op("act", lambda e: e.activation(out=rs_t[:], in_=ss_t[:], func=AF.Ln, bias=epsb[:, 0:1]), reads=[r_ss_, r_eps], writes=[r_rs_])
            P.op("act", lambda e: e.activation(out=rs_t[:], in_=rs_t[:], func=AF.Exp, scale=-0.5), reads=[r_rs_], writes=[r_rs_])
            P.op("dve", lambda e: e.scalar_tensor_tensor(out=hf_t[:], in0=x_t[:], scalar=rs_t[:, 0:1], in1=A[:], op0=ALU.mult, op1=ALU.mult),
                 reads=[r_x, r_rs_, r_A], writes=[r_hf_])
            P.op("pool", lambda e: e.tensor_tensor(out=hb_t[:], in0=hf_t[:], in1=shB[:], op=ALU.add), reads=[r_hf_, r_shB], writes=[r_hb_])
            if router:
                h32_t, r_h32_ = h322[t % 4]
                P.op("dve", lambda e: e.tensor_tensor(out=h32_t[:], in0=hf_t[:], in1=shB[:], op=ALU.add), reads=[r_hf_, r_shB], writes=[r_h32_])

        def ph2a(t):
            hb_t, r_hb_ = hb2[t % 4]
            p_t, r_p = pt[t % 2]
            h_t, r_h = hTs[t % 2]
            for c in range(8):
                P.op("pe", lambda e: e.transpose(out=p_t[:, c, :], in_=hb_t[:, c * 128:(c + 1) * 128], identity=identb[:]),
                     reads=[r_hb_, r_idb], writes=[r_p])
            P.op("act", lambda e: e.copy(out=h_t[:], in_=p_t[:]), reads=[r_p], writes=[r_h])
            P.dma("sp", kp(hT_dst)[:, :, t * 128:(t + 1) * 128], h_t[:], reads=[r_h])
            if router:
                P.dma("sp", h2_d[t * 128:(t + 1) * 128, :], hb_t[:], reads=[r_hb_])
                plg, r_plg = plg2[t % 2]
                h32_t, r_h32_ = h322[t % 4]
                for half in range(2):
                    pf_t, r_pf = ptf[half]
                    for c in range(4):
                        cc = half * 4 + c
                        P.op("pe", lambda e: e.transpose(out=pf_t[:, c, :], in_=h32_t[:, cc * 128:(cc + 1) * 128], identity=identf[:]),
                             reads=[r_h32_, r_idf], writes=[r_pf])
                    P.op("dve", lambda e: e.tensor_copy(out=hTf[:, half * 4:(half + 1) * 4, :], in_=pf_t[:]), reads=[r_pf], writes=[r_hTf])
                for c in range(8):
                    P.op("pe", lambda e: e.matmul(plg[:], lhsT=hTf[:, c, :], rhs=wr[:, c, :], start=(c == 0), stop=(c == 7)),
                         reads=[r_hTf, r_wr], writes=[r_plg])

        def ph2b(t):
            sc_t, r_sc = dup_sc[t % 2]
            bi, r_bi = dup_bi[t % 2]
            tmp, r_tmp = dup_tmp[t % 2]
            m1, r_m1 = dup_m1[t % 2]
            m2, r_m2 = dup_m2[t % 2]
            gs, r_gs = dup_gs[t % 2]
            t8, r_t8 = dup_t8[t % 2]
            pen, r_pen = dup_pen[t % 2]
            sel, r_sel = dup_sel[t % 2]
            ssum, r_ssum = dup_ssum[t % 2]
            gate, r_gate = dup_gate[t % 2]
            gTs, r_gTs = dup_gTs[t % 2]
            plg, r_plg = dup_plg[t % 2]
            P.op("act", lambda e: e.activation(out=sc_t[:], in_=plg[:], func=AF.Exp, scale=-1.0), reads=[r_plg], writes=[r_sc])
            yield
            P.op("dve", lambda e: e.tensor_scalar(out=sc_t[:], in0=sc_t[:], scalar1=1.0, scalar2=None, op0=ALU.add), reads=[r_sc], writes=[r_sc])
            yield
            P.op("dve", lambda e: e.reciprocal(out=sc_t[:], in_=sc_t[:]), reads=[r_sc], writes=[r_sc])
            yield
            P.op("dve", lambda e: e.tensor_tensor(out=bi[:], in0=sc_t[:], in1=brB[:], op=ALU.add), reads=[r_sc, r_brB], writes=[r_bi])
            yield
            bi3 = bi[:].rearrange("p (g e) -> p g e", g=8)
            tmp3 = tmp[:].rearrange("p (g e) -> p g e", g=8)
            P.op("dve", lambda e: e.tensor_reduce(out=m1[:], in_=bi3, axis=AX.X, op=ALU.max), reads=[r_bi], writes=[r_m1])
            yield
            P.op("dve", lambda e: e.tensor_tensor(out=tmp3, in0=bi3, in1=m1[:].unsqueeze(2).to_broadcast([128, 8, 8]), op=ALU.is_equal),
                 reads=[r_bi, r_m1], writes=[r_tmp])
            yield
            P.op("dve", lambda e: e.scalar_tensor_tensor(out=tmp[:], in0=tmp[:], scalar=-1e30, in1=bi[:], op0=ALU.mult, op1=ALU.add),
                 reads=[r_tmp, r_bi], writes=[r_tmp])
            yield
            P.op("dve", lambda e: e.tensor_reduce(out=m2[:], in_=tmp3, axis=AX.X, op=ALU.max), reads=[r_tmp], writes=[r_m2])
            yield
            P.op("dve", lambda e: e.tensor_tensor(out=gs[:], in0=m1[:], in1=m2[:], op=ALU.add), reads=[r_m1, r_m2], writes=[r_gs])
            yield
            P.op("dve", lambda e: e.max(out=t8[:], in_=gs[:]), reads=[r_gs], writes=[r_t8])
            yield
            P.op("dve", lambda e: e.tensor_scalar(out=pen[:], in0=gs[:], scalar1=t8[:, 3:4], scalar2=-1e30, op0=ALU.is_lt, op1=ALU.mult),
                 reads=[r_gs, r_t8], writes=[r_pen])
            yield
            P.op("dve", lambda e: e.tensor_tensor(out=tmp3, in0=bi3, in1=pen[:].unsqueeze(2).to_broadcast([128, 8, 8]), op=ALU.add),
                 reads=[r_bi, r_pen], writes=[r_tmp])
            yield
            P.op("dve", lambda e: e.max(out=t8[:], in_=tmp[:]), reads=[r_tmp], writes=[r_t8])
            yield
            P.op("dve", lambda e: e.tensor_scalar(out=sel[:], in0=tmp[:], scalar1=t8[:, 7:8], scalar2=None, op0=ALU.is_ge),
                 reads=[r_tmp, r_t8], writes=[r_sel])
            yield
            P.op("dve", lambda e: e.tensor_tensor(out=sel[:], in0=sel[:], in1=sc_t[:], op=ALU.mult), reads=[r_sel, r_sc], writes=[r_sel])
            yield
            P.op("dve", lambda e: e.tensor_reduce(out=ssum[:], in_=sel[:], axis=AX.X, op=ALU.add), reads=[r_sel], writes=[r_ssum])
            yield
            P.op("dve", lambda e: e.tensor_scalar(out=ssum[:], in0=ssum[:], scalar1=1e-20, scalar2=None, op0=ALU.add), reads=[r_ssum], writes=[r_ssum])
            yield
            P.op("dve", lambda e: e.reciprocal(out=ssum[:], in_=ssum[:]), reads=[r_ssum], writes=[r_ssum])
            yield
            P.op("dve", lambda e: e.tensor_scalar(out=gate[:], in0=sel[:], scalar1=ssum[:, 0:1], scalar2=2.5, op0=ALU.mult, op1=ALU.mult),
                 reads=[r_sel, r_ssum], writes=[r_gate])
            yield
            P.op("dve", lambda e: e.tensor_scalar(out=selb[t % 4][0][:], in0=gate[:], scalar1=0.0, scalar2=None, op0=ALU.is_gt),
                 reads=[r_gate], writes=[selb[t % 4][1]])
            yield
            P.dma("sp", gate_d[t * 128:(t + 1) * 128, :], gate[:], reads=[r_gate])
            yield

        def drain(*gens):
            gens = list(gens)
            while gens:
                for g in list(gens):
                    try:
                        next(g)
                    except StopIteration:
                        gens.remove(g)

        prev_tl = []

        def pc_ops(tl_):
            for tt_ in tl_:
                s_t, r_s = selb[tt_ % 4]
                p_t, r_p = posb[tt_ % 4]
                P.op("pe", lambda e: e.matmul(PC[:], lhsT=pinc[:], rhs=s_t[:], start=(tt_ == 0), stop=False, skip_group_check=True),
                     reads=[r_pinc, r_s], writes=[r_PC])
                P.op("act", lambda e: e.copy(out=p_t[:], in_=PC[:]), reads=[r_PC], writes=[r_p])
                P.op("pe", lambda e: e.matmul(PC[:], lhsT=pgtm[:], rhs=s_t[:], start=False, stop=(tt_ == NT - 1), skip_group_check=True),
                     reads=[r_pgtm, r_s, r_p], writes=[r_PC])
                P.dma("sp", pos_d[tt_ * 128:(tt_ + 1) * 128, :], p_t[:], reads=[r_p])

        for t0_ in range(min(4, NT)):
            ph1(t0_)
        for t in range(0, NT, 2):
            tl = [t] + ([t + 1] if t + 1 < NT else [])
            for tt_ in tl:
                ph2a(tt_)
            if t + 4 < NT:
                ph1(t + 4)
            if t + 5 < NT:
                ph1(t + 5)
            if router:
                pc_ops(prev_tl)
                prev_tl = tl
                drain(*[ph2b(tt_) for tt_ in tl])
        if router:
            pc_ops(prev_tl)
        if router:
            cnt, r_cnt = st.sb([128, NE], F32)
            nbt, r_nbt = st.sb([128, NE], F32)
            ca, r_ca = st.sb([128, NE], F32)
            cb, r_cb = st.sb([128, NE], F32)
            ebc, r_ebc = st.sb([128, 1], F32)
            P.op("act", lambda e: e.copy(out=cnt[:], in_=PC[:]), reads=[r_PC], writes=[r_cnt])
            P.op("dve", lambda e: e.tensor_scalar(out=nbt[:], in0=cnt[:], scalar1=0.0, scalar2=None, op0=ALU.is_gt), reads=[r_cnt], writes=[r_nbt])
            for j_ in range(1, 8):
                P.op("dve", lambda e: e.scalar_tensor_tensor(out=nbt[:], in0=cnt[:], scalar=512.0 * j_, in1=nbt[:], op0=ALU.is_gt, op1=ALU.add),
                     reads=[r_cnt, r_nbt], writes=[r_nbt])
            P.op("dve", lambda e: e.tensor_copy(out=ca[:], in_=nbt[:]), reads=[r_nbt], writes=[r_ca])
            src, r_src, dst_, r_dst = ca, r_ca, cb, r_cb
            k_ = 1
            while k_ < NE:
                P.op("dve", lambda e: e.tensor_copy(out=dst_[:, 0:k_], in_=src[:, 0:k_]), reads=[r_src], writes=[r_dst])
                P.op("dve", lambda e: e.tensor_tensor(out=dst_[:, k_:NE], in0=src[:, k_:NE], in1=src[:, 0:NE - k_], op=ALU.add), reads=[r_src], writes=[r_dst])
                src, r_src, dst_, r_dst = dst_, r_dst, src, r_src
                k_ *= 2
            bend, r_bend = src, r_src
            P.op("dve", lambda e: e.tensor_scalar(out=dst_[:], in0=bend[:], scalar1=iotac[:, 0:1], scalar2=None, op0=ALU.is_le),
                 reads=[r_bend, r_iotac], writes=[r_dst])
            P.op("dve", lambda e: e.tensor_reduce(out=ebc[:], in_=dst_[:], axis=AX.X, op=ALU.add), reads=[r_dst], writes=[r_ebc])
            P.op("dve", lambda e: e.tensor_scalar(out=ebc[:], in0=ebc[:], scalar1=float(NE - 1), scalar2=None, op0=ALU.min), reads=[r_ebc], writes=[r_ebc])
            P.dma("sp", col(eb_d), ebc[:], reads=[r_ebc])
            P.op("dve", lambda e: e.tensor_tensor(out=cnt[:], in0=bend[:], in1=nbt[:], op=ALU.subtract), reads=[r_bend, r_nbt], writes=[r_cnt])
            P.op("dve", lambda e: e.tensor_scalar(out=cnt[:], in0=cnt[:], scalar1=512.0, scalar2=None, op0=ALU.mult), reads=[r_cnt], writes=[r_cnt])
            P.dma("sp", sbase_d.rearrange("(o n) -> o n", o=1), cnt[0:1, :], reads=[r_cnt])
        st.done(f"norm{int(router)}_{l}")

    def stage_proj(l):
        st = Stage(P)
        NCOL = 4096
        w, r_w = st.sb([128, 8, NCOL], BF16)
        blk, r_blk = st.sb([128, 128], BF16)
        rot, r_rot = st.sb([128, 128], BF16)
        epsb, r_eps = st.sb([128, 1], F32)
        gq, r_gq = st.sb([128, 1], F32)
        gk, r_gk = st.sb([128, 1], F32)
        hT = [st.sb([128, 8, 512], BF16) for _ in range(2)]
        cosT = [st.sb([128, 512], F32) for _ in range(2)]
        sinT = [st.sb([128, 512], F32) for _ in range(2)]
        pp = [st.ps([128, 512], F32) for _ in range(4)]
        pms, r_pms = st.ps([128, 512], F32)
        prot, r_prot = st.ps([128, 512], F32)
        sqb, r_sqb = st.sb([128, 512], BF16)
        rs, r_rs = st.sb([128, 512], F32)
        qn, r_qn = st.sb([128, 512], BF16)
        t1, r_t1 = st.sb([128, 512], F32)
        t2, r_t2 = st.sb([128, 512], F32)
        sg, r_sg = st.sb([128, 512], F32)
        ob = [st.sb([128, 512], BF16) for _ in range(4)]
        for c in range(8):
            P.dma("pool", w[:, c, :], I["w_in"][l][c * 128:(c + 1) * 128, 0:NCOL], writes=[r_w])
        P.dma("pool", blk[:], I["k_blk64"], writes=[r_blk])
        P.dma("pool", rot[:], I["k_rotT"], writes=[r_rot])
        P.op("pool", lambda e: e.memset(epsb[:], EPS), writes=[r_eps])
        for hh in range(2):
            P.dma("sp", gq[hh * 64:(hh + 1) * 64, :], col(I["qn_g"][l]), writes=[r_gq])
            P.dma("sp", gk[hh * 64:(hh + 1) * 64, :], col(I["kn_g"][l]), writes=[r_gk])
        pp = pp + [st.ps([128, 512], F32) for _ in range(2)]
        NPP = len(pp)
        loaded = set()

        def load_tile(tt):
            if tt in loaded or tt >= NQ:
                return
            loaded.add(tt)
            ts_ = slice(tt * 512, (tt + 1) * 512)
            P.dma("sp", hT[tt % 2][0][:], kp(hT_d)[:, :, ts_], writes=[hT[tt % 2][1]])
            P.dma("sp", cosT[tt % 2][0][:], cos_d[:, ts_], writes=[cosT[tt % 2][1]])
            P.dma("sp", sinT[tt % 2][0][:], sin_d[:, ts_], writes=[sinT[tt % 2][1]])

        units = []
        for tt in range(NQ):
            for which in range(2):
                for j in range(4):
                    units.append(("qk", tt, (which, j)))
            for j in range(4):
                units.append(("glu", tt, (j,)))
            for which in range(2):
                for j in range(4):
                    units.append(("cp", tt, (which, j)))
            for which in range(2):
                for sub in range(4):
                    units.append(("tm", tt, (which, sub)))
        pidx = []
        cur = 0
        for kind, tt, args in units:
            nb_ = 2 if kind == "glu" else 1
            pidx.append([(cur + i) % NPP for i in range(nb_)])
            cur += nb_
        state = {"oi": 0}

        def fmm(tt, col0, p_t, r_p):
            h_t, r_h = hT[tt % 2]
            for c in range(8):
                P.op("pe", lambda e: e.matmul(p_t[:], lhsT=w[:, c, col0:col0 + 128], rhs=h_t[:, c, :], start=(c == 0), stop=(c == 7)),
                     reads=[r_w, r_h], writes=[r_p])

        def F(u):
            kind, tt, args = units[u]
            load_tile(tt)
            bufs = [pp[i] for i in pidx[u]]
            if kind == "qk":
                which, j = args
                fmm(tt, which * 512 + j * 128, *bufs[0])
            elif kind == "glu":
                (j,) = args
                fmm(tt, 1536 + j * 128, *bufs[0])
                fmm(tt, 2048 + j * 128, *bufs[1])
            elif kind == "cp":
                which, j = args
                fmm(tt, (2560 if which == 0 else 3072) + j * 128, *bufs[0])
            else:
                which, sub = args
                base = 1024 if which == 0 else 3584
                h_t, r_h = hT[tt % 2]
                p_t, r_p = bufs[0]
                for c in range(8):
                    P.op("pe", lambda e: e.matmul(p_t[:], lhsT=h_t[:, c, sub * 128:(sub + 1) * 128], rhs=w[:, c, base:base + 512], start=(c == 0), stop=(c == 7)),
                         reads=[r_w, r_h], writes=[r_p])

        def nxt_ob():
            o = ob[state["oi"] % 4]
            state["oi"] += 1
            return o

        def G(u):
            kind, tt, args = units[u]
            ts_ = slice(tt * 512, (tt + 1) * 512)
            bufs = [pp[i] for i in pidx[u]]
            c_t, r_c = cosT[tt % 2]
            s_t, r_s = sinT[tt % 2]
            if kind == "qk":
                which, j = args
                g_t, r_g, dst = (gq, r_gq, qaT_d) if which == 0 else (gk, r_gk, kaT_d)
                p_t, r_p = bufs[0]
                P.op("act", lambda e: e.activation(out=sqb[:], in_=p_t[:], func=AF.Square), reads=[r_p], writes=[r_sqb])
                P.op("pe", lambda e: e.matmul(pms[:], lhsT=blk[:], rhs=sqb[:], start=True, stop=True), reads=[r_blk, r_sqb], writes=[r_pms])
                P.op("act", lambda e: e.activation(out=rs[:], in_=pms[:], func=AF.Ln, bias=epsb[:, 0:1]), reads=[r_pms, r_eps], writes=[r_rs])
                P.op("act", lambda e: e.activation(out=rs[:], in_=rs[:], func=AF.Exp, scale=-0.5), reads=[r_rs], writes=[r_rs])
                P.op("dve", lambda e: e.scalar_tensor_tensor(out=qn[:], in0=p_t[:], scalar=g_t[:, 0:1], in1=rs[:], op0=ALU.mult, op1=ALU.mult),
                     reads=[r_p, r_g, r_rs], writes=[r_qn])
                P.op("pe", lambda e: e.matmul(prot[:], lhsT=rot[:], rhs=qn[:], start=True, stop=True), reads=[r_rot, r_qn], writes=[r_prot])
                P.op("pool", lambda e: e.tensor_tensor(out=t1[:], in0=qn[:], in1=c_t[:], op=ALU.mult), reads=[r_qn, r_c], writes=[r_t1])
                P.op("dve", lambda e: e.tensor_tensor(out=t2[:], in0=prot[:], in1=s_t[:], op=ALU.mult), reads=[r_prot, r_s], writes=[r_t2])
                o_t, r_o = nxt_ob()
                P.op("dve", lambda e: e.tensor_tensor(out=o_t[:], in0=t1[:], in1=t2[:], op=ALU.add), reads=[r_t1, r_t2], writes=[r_o])
                P.dma("sp", dst[j * 128:(j + 1) * 128, ts_], o_t[:], reads=[r_o])
            elif kind == "glu":
                (j,) = args
                (pa, r_pa), (pg, r_pg) = bufs
                P.op("act", lambda e: e.activation(out=sg[:], in_=pg[:], func=AF.Sigmoid), reads=[r_pg], writes=[r_sg])
                o_t, r_o = nxt_ob()
                P.op("dve", lambda e: e.tensor_tensor(out=o_t[:], in0=pa[:], in1=sg[:], op=ALU.mult), reads=[r_pa, r_sg], writes=[r_o])
                P.dma("sp", uT_d[j * 128:(j + 1) * 128, ts_], o_t[:], reads=[r_o])
            elif kind == "cp":
                which, j = args
                dst, qscale = (qcT_d, 0.125) if which == 0 else (kcT_d, 1.0)
                p_t, r_p = bufs[0]
                o_t, r_o = nxt_ob()
                P.op("act", lambda e: e.mul(out=o_t[:], in_=p_t[:], mul=qscale), reads=[r_p], writes=[r_o])
                P.dma("sp", dst[j * 128:(j + 1) * 128, ts_], o_t[:], reads=[r_o])
            else:
                which, sub = args
                dst = va_d if which == 0 else vc_d
                p_t, r_p = bufs[0]
                o_t, r_o = nxt_ob()
                P.op("dve" if sub % 2 == 0 else "act", (lambda e: e.tensor_copy(out=o_t[:], in_=p_t[:])) if sub % 2 == 0 else (lambda e: e.copy(out=o_t[:], in_=p_t[:])),
                     reads=[r_p], writes=[r_o])
                r0 = tt * 512 + sub * 128
                P.dma("sp", dst[r0:r0 + 128, :], o_t[:], reads=[r_o])

        nU = len(units)
        AHEAD = 2
        for u in range(min(AHEAD, nU)):
            F(u)
        for u in range(nU):
            if u + AHEAD < nU:
                F(u + AHEAD)
            G(u)
        st.done(f"proj{l}")

    def stage_da(l):
        lambda_init = 0.8 - 0.6 * math.exp(-0.3 * l)
        st = Stage(P)
        msk, r_msk = st.sb([128, 4, 512], BF16)
        ones, r_ones = st.sb([128, 128], BF16)
        o128, r_o128 = st.sb([128, 128], BF16)
        epsb, r_eps = st.sb([128, 1], F32)
        lv = [st.sb([128, 64], F32) for _ in range(4)]
        lp, r_lp = st.sb([128, 64], F32)
        ld, r_ld = st.sb([128, 2], F32)
        nlam, r_nlam = st.sb([128, 1], F32)
        gcol, r_gcol = st.sb([128, 1], F32)
        qT = [[st.sb([128, S], BF16) for _ in range(2)] for _ in range(2)]
        kT = [st.sb([128, S], BF16) for _ in range(2)]
        vv = [st.sb([128, NT, 128], BF16) for _ in range(2)]
        pz = [st.ps([128, 512], F32) for _ in range(2)]
        pO = [st.ps([128, 512], F32) for _ in range(2)]
        pL = [st.ps([128, 512], F32) for _ in range(2)]
        for hp_ in range(2):
            for m_ in range(2):
                zr = slice((1 - m_) * 64, (1 - m_) * 64 + 64)
                P.op("pool", lambda e: e.memset(qT[hp_][m_][0][zr, :], 0.0), writes=[qT[hp_][m_][1]])
        pms, r_pms = st.ps([128, 512], F32)
        E = [st.sb([128, 512], BF16) for _ in range(4)]
        rL = [st.sb([128, 512], F32) for _ in range(2)]
        tO = [st.sb([128, 512], F32) for _ in range(2)]
        o_t, r_o = st.sb([128, 512], F32)
        sqb, r_sqb = st.sb([128, 512], BF16)
        rs, r_rs = st.sb([128, 512], F32)
        ob = [st.sb([128, 512], BF16) for _ in range(2)]
        P.dma("pool", msk[:], I["k_maskc"].rearrange("j p q -> p j q"), writes=[r_msk])
        if l == 0:
            zt, r_zt = st.sb([128, 4096], BF16)
            P.op("pool", lambda e: e.memset(zt[:], 0.0), writes=[r_zt])
        zfill = {"next": 0}

        def zero_fill_some(n_):
            if l != 0:
                return
            for _ in range(n_):
                zi_ = zfill["next"]
                if zi_ >= NSLOT // 512:
                    return
                zfill["next"] += 1
                P.dma("pool", xs_d[zi_ * 512:(zi_ + 1) * 512, :].rearrange("(p r) d -> p (r d)", p=128), zt[:], reads=[r_zt])
        P.op("pool", lambda e: e.memset(ones[:], 1.0), writes=[r_ones])
        P.op("pool", lambda e: e.memset(o128[:], 1.0 / 128), writes=[r_o128])
        P.op("pool", lambda e: e.memset(epsb[:], EPS), writes=[r_eps])
        for i, nm in enumerate(("lam_q1", "lam_k1", "lam_q2", "lam_k2")):
            P.dma("sp", lv[i][0][:], I[nm][l].partition_broadcast(128), writes=[lv[i][1]])
        for i in range(2):
            P.op("dve", lambda e, i=i: e.tensor_tensor(out=lp[:], in0=lv[2 * i][0][:], in1=lv[2 * i + 1][0][:], op=ALU.mult),
                 reads=[lv[2 * i][1], lv[2 * i + 1][1]], writes=[r_lp])
            P.op("dve", lambda e, i=i: e.tensor_reduce(out=ld[:, i:i + 1], in_=lp[:], axis=AX.X, op=ALU.add), reads=[r_lp], writes=[r_ld])
        P.op("act", lambda e: e.activation(out=ld[:], in_=ld[:], func=AF.Exp), reads=[r_ld], writes=[r_ld])
        P.op("dve", lambda e: e.tensor_tensor(out=nlam[:], in0=ld[:, 1:2], in1=ld[:, 0:1], op=ALU.subtract), reads=[r_ld], writes=[r_nlam])
        P.op("dve", lambda e: e.tensor_scalar(out=nlam[:], in0=nlam[:], scalar1=-lambda_init, scalar2=None, op0=ALU.add), reads=[r_nlam], writes=[r_nlam])
        P.dma("sp", gcol[:], col(I["subln_g"][l]), writes=[r_gcol])
        P.op("dve", lambda e: e.tensor_scalar(out=gcol[:], in0=gcol[:], scalar1=1.0 - lambda_init, scalar2=None, op0=ALU.mult), reads=[r_gcol], writes=[r_gcol])
        oi = 0
        pz3 = pz + [st.ps([128, 512], F32)]
        units = []
        for hd in range(4):
            for Qi in range(NQ):
                for m in range(2):
                    for kt in range(4 * Qi + 4):
                        units.append((hd, Qi, m, kt))
        loaded = set()

        def load_head(hd):
            if hd in loaded or hd >= 4:
                return
            loaded.add(hd)
            k_t, r_k = kT[hd % 2]
            v_t, r_v = vv[hd % 2]
            for m_ in range(2):
                rr_ = slice(m_ * 64, m_ * 64 + 64)
                P.dma("sp", qT[hd % 2][m_][0][rr_, :], qaT_d[hd * 128 + m_ * 64:hd * 128 + m_ * 64 + 64, :], writes=[qT[hd % 2][m_][1]])
            P.dma("sp", k_t[:], kaT_d[hd * 128:(hd + 1) * 128, :], writes=[r_k])
            P.dma("sp", v_t[:], va_d[:, hd * 128:(hd + 1) * 128].rearrange("(t p) e -> p t e", p=128), writes=[r_v])

        def phA(i):
            hd, Qi, m, kt = units[i]
            load_head(hd)
            q_t, r_q = qT[hd % 2][m]
            k_t, r_k = kT[hd % 2]
            z_t, r_z = pz3[i % 3]
            c0 = max(kt - 4 * Qi, 0) * 128
            P.op("pe", lambda e: e.matmul(z_t[:, c0:512], lhsT=k_t[:, kt * 128:(kt + 1) * 128], rhs=q_t[:, Qi * 512 + c0:(Qi + 1) * 512], start=True, stop=True),
                 reads=[r_k, r_q], writes=[r_z])

        def phB(i):
            nonlocal oi
            hd, Qi, m, kt = units[i]
            v_t, r_v = vv[hd % 2]
            z_t, r_z = pz3[i % 3]
            e_t, r_e = E[i % 4]
            pO_t, r_pO = pO[m]
            pL_t, r_pL = pL[m]
            nk = 4 * Qi + 4
            j = kt - 4 * Qi
            qs = slice(Qi * 512, (Qi + 1) * 512)
            c0 = max(j, 0) * 128
            cs = slice(c0, 512)
            P.op("act", lambda e: e.activation(out=e_t[:, cs], in_=z_t[:, cs], func=AF.Exp, scale=0.125), reads=[r_z], writes=[r_e])
            if j >= 0:
                P.op("dve", lambda e: e.tensor_tensor(out=e_t[:, c0:c0 + 128], in0=e_t[:, c0:c0 + 128], in1=msk[:, 0, 0:128], op=ALU.mult),
                     reads=[r_e, r_msk], writes=[r_e])
            P.op("pe", lambda e: e.matmul(pO_t[:, cs], lhsT=v_t[:, kt, :], rhs=e_t[:, cs], start=(kt == 0), stop=(kt == nk - 1)),
                 reads=[r_v, r_e], writes=[r_pO])
            P.op("pe", lambda e: e.matmul(pL_t[:, cs], lhsT=ones[:], rhs=e_t[:, cs], start=(kt == 0), stop=(kt == nk - 1)),
                 reads=[r_ones, r_e], writes=[r_pL])
            if kt == nk - 1:
                P.op("act", lambda e: e.activation(out=rL[m][0][:], in_=pL_t[:], func=AF.Ln), reads=[r_pL], writes=[rL[m][1]])
                P.op("act", lambda e: e.activation(out=rL[m][0][:], in_=rL[m][0][:], func=AF.Exp, scale=-1.0), reads=[rL[m][1]], writes=[rL[m][1]])
                P.op("dve", lambda e: e.tensor_tensor(out=tO[m][0][:], in0=pO_t[:], in1=rL[m][0][:], op=ALU.mult), reads=[r_pO, rL[m][1]], writes=[tO[m][1]])
                if m == 1:
                    P.op("dve", lambda e: e.scalar_tensor_tensor(out=o_t[:], in0=tO[1][0][:], scalar=nlam[:, 0:1], in1=tO[0][0][:], op0=ALU.mult, op1=ALU.add),
                         reads=[tO[0][1], tO[1][1], r_nlam], writes=[r_o])
                    P.op("act", lambda e: e.activation(out=sqb[:], in_=o_t[:], func=AF.Square), reads=[r_o], writes=[r_sqb])
                    P.op("pe", lambda e: e.matmul(pms[:], lhsT=o128[:], rhs=sqb[:], start=True, stop=True), reads=[r_o128, r_sqb], writes=[r_pms])
                    P.op("act", lambda e: e.activation(out=rs[:], in_=pms[:], func=AF.Ln, bias=epsb[:, 0:1]), reads=[r_pms, r_eps], writes=[r_rs])
                    P.op("act", lambda e: e.activation(out=rs[:], in_=rs[:], func=AF.Exp, scale=-0.5), reads=[r_rs], writes=[r_rs])
                    b_t, r_b = ob[oi % 2]
                    oi += 1
                    P.op("dve", lambda e: e.scalar_tensor_tensor(out=b_t[:], in0=o_t[:], scalar=gcol[:, 0:1], in1=rs[:], op0=ALU.mult, op1=ALU.mult),
                         reads=[r_o, r_gcol, r_rs], writes=[r_b])
                    P.dma("sp", oaT_d[hd * 128:(hd + 1) * 128, qs], b_t[:], reads=[r_b])

        n = len(units)
        for i in range(min(2, n)):
            phA(i)
        for i in range(n):
            if i + 2 < n:
                phA(i + 2)
            phB(i)
            if i % 4 == 3:
                zero_fill_some(1)
        zero_fill_some(NSLOT)
        st.done(f"da{l}")

    def stage_sb(l):
        st = Stage(P)
        mskb, r_mskb = st.sb([128, 4, 512], BF16)
        uinc, r_uinc = st.sb([128, 128], BF16)
        ulow, r_ulow = st.sb([128, 128], BF16)
        onec, r_onec = st.sb([128, 1], F32)
        qT = [[st.sb([128, S], BF16) for _ in range(2)] for _ in range(2)]
        kT = [st.sb([128, S], BF16) for _ in range(2)]
        kN = [st.sb([128, S], BF16) for _ in range(2)]
        vv = [st.sb([128, NT, 128], BF16) for _ in range(2)]
        pz = [st.ps([128, 2, 512], F32) for _ in range(2)]
        PT, r_PT = st.ps([128, 2, 512], F32)
        pO, r_pO = st.ps([128, 2, 512], F32)
        ex = [st.sb([128, 2, 512], F32) for _ in range(2)]
        sp_ = [st.sb([128, 2, 512], F32) for _ in range(2)]
        lom = [st.sb([128, 2, 512], BF16) for _ in range(3)]
        ab = [st.sb([128, 2, 512], BF16) for _ in range(3)]
        ob = [st.sb([128, 512], BF16) for _ in range(2)]
        for cp_ in range(2):
            for hh_ in range(2):
                zr = slice((1 - hh_) * 64, (1 - hh_) * 64 + 64)
                P.op("pool", lambda e: e.memset(qT[cp_][hh_][0][zr, :], 0.0), writes=[qT[cp_][hh_][1]])
        P.dma("pool", mskb[:], I["k_masks"].rearrange("j p q -> p j q"), writes=[r_mskb])
        P.dma("pool", uinc[:], I["k_uincl"], writes=[r_uinc])
        P.dma("pool", ulow[:], I["k_ulow"], writes=[r_ulow])
        P.op("pool", lambda e: e.memset(onec[:], 1.0), writes=[r_onec])
        state = {"oi": 0}
        r_PTh = [Res(), Res()]
        abr = [[Res(), Res()] for _ in range(3)]
        units = [(ch, Qi, kt) for ch in range(4) for Qi in range(NQ) for kt in range(4 * Qi + 3, -1, -1)]
        n = len(units)
        loaded = set()

        def load_pair(ch):
            if ch in loaded:
                return
            loaded.add(ch)
            for hh_ in range(2):
                rr_ = slice(hh_ * 64, hh_ * 64 + 64)
                P.dma("sp", qT[ch % 2][hh_][0][rr_, :], qcT_d[ch * 128 + hh_ * 64:ch * 128 + hh_ * 64 + 64, :], writes=[qT[ch % 2][hh_][1]])
            P.dma("sp", kT[ch % 2][0][:], kcT_d[ch * 128:(ch + 1) * 128, :], writes=[kT[ch % 2][1]])
            P.dma("sp", vv[ch % 2][0][:], vc_d[:, ch * 128:(ch + 1) * 128].rearrange("(t p) e -> p t e", p=128), writes=[vv[ch % 2][1]])
            P.op("pool", lambda e: e.tensor_scalar(out=kN[ch % 2][0][:], in0=kT[ch % 2][0][:], scalar1=-1.0, scalar2=None, op0=ALU.mult),
                 reads=[kT[ch % 2][1]], writes=[kN[ch % 2][1]])

        def info(i):
            ch, Qi, kt = units[i]
            return ch, Qi, kt, kt - 4 * Qi, 4 * Qi + 4, slice(Qi * 512, (Qi + 1) * 512), slice(kt * 128, (kt + 1) * 128)

        def mask2(j):
            return mskb[:, j:j + 1, :].to_broadcast([128, 2, 512])

        def phA(i):
            ch, Qi, kt, j, nk, qs, ks = info(i)
            load_pair(ch)
            k_t, r_k = kT[ch % 2]
            z_t, r_z = pz[i % 2]
            for hh in range(2):
                q_t, r_q = qT[ch % 2][hh]
                P.op("pe", lambda e: e.matmul(z_t[:, hh, :], lhsT=k_t[:, ks], rhs=q_t[:, qs], start=True, stop=True), reads=[r_k, r_q], writes=[r_z])

        def phB(i):
            ch, Qi, kt, j, nk, qs, ks = info(i)
            z_t, r_z = pz[i % 2]
            e_t, r_e = ex[i % 2]
            p_t, r_p = sp_[i % 2]
            l_t, r_l = lom[i % 3]
            c0 = max(j, 0) * 128
            cs = slice(c0, 512)
            P.op("act", lambda e: e.activation(out=e_t[:, :, cs], in_=z_t[:, :, cs], func=AF.Exp, scale=-1.0), reads=[r_z], writes=[r_e])
            P.op("act", lambda e: e.activation(out=p_t[:, :, cs], in_=e_t[:, :, cs], func=AF.Ln, bias=onec[:, 0:1]), reads=[r_e, r_onec], writes=[r_p])
            P.op("dve", lambda e: e.scalar_tensor_tensor(out=l_t[:, :, cs], in0=z_t[:, :, cs], scalar=-1.0, in1=p_t[:, :, cs], op0=ALU.mult, op1=ALU.subtract),
                 reads=[r_z, r_p], writes=[r_l])
            if j >= 0:
                P.op("dve", lambda e: e.tensor_tensor(out=l_t[:, :, c0:c0 + 128], in0=l_t[:, :, c0:c0 + 128],
                                                     in1=mskb[:, 0:1, 0:128].to_broadcast([128, 2, 128]), op=ALU.mult), reads=[r_l, r_mskb], writes=[r_l])
                if c0 > 0:
                    P.op("dve", lambda e: e.memset(l_t[:, :, 0:c0], 0.0), reads=[r_l], writes=[r_l])

        def phC(i, hh):
            ch, Qi, kt, j, nk, qs, ks = info(i)
            k_t, r_k = kT[ch % 2]
            l_t, r_l = lom[i % 3]
            q_t, r_q = qT[ch % 2][hh]
            P.op("pe", lambda e: e.matmul(PT[:, hh, :], lhsT=uinc[:], rhs=l_t[:, hh, :], start=(kt == nk - 1), stop=False, skip_group_check=True),
                 reads=[r_uinc, r_l], writes=[r_PTh[hh]])
            P.op("pe", lambda e: e.matmul(PT[:, hh, :], lhsT=k_t[:, ks], rhs=q_t[:, qs], start=False, stop=False, skip_group_check=True),
                 reads=[r_k, r_q], writes=[r_PTh[hh]])

        def phD_act(i):
            ch, Qi, kt, j, nk, qs, ks = info(i)
            a_t, _ = ab[i % 3]
            c0 = max(j, 0) * 128
            for hh in range(2):
                r_a = abr[i % 3][hh]
                P.op("act", lambda e: e.activation(out=a_t[:, hh, c0:512], in_=PT[:, hh, c0:512], func=AF.Exp), reads=[r_PTh[hh]], writes=[r_a])
                if j >= 0:
                    P.op("dve", lambda e: e.tensor_tensor(out=a_t[:, hh, c0:c0 + 128], in0=a_t[:, hh, c0:c0 + 128], in1=mskb[:, 0, 0:128], op=ALU.mult),
                         reads=[r_a, r_mskb], writes=[r_a])
                    if c0 > 0:
                        P.op("dve", lambda e: e.memset(a_t[:, hh, 0:c0], 0.0), reads=[r_a], writes=[r_a])

        def phD_corr(i, hh):
            ch, Qi, kt, j, nk, qs, ks = info(i)
            kn_t, r_kn = kN[ch % 2]
            l_t, r_l = lom[i % 3]
            r_a = abr[i % 3][hh]
            if kt > 0:
                q_t, r_q = qT[ch % 2][hh]
                P.op("pe", lambda e: e.matmul(PT[:, hh, :], lhsT=ulow[:], rhs=l_t[:, hh, :], start=False, stop=False, skip_group_check=True),
                     reads=[r_ulow, r_l, r_a], writes=[r_PTh[hh]])
                P.op("pe", lambda e: e.matmul(PT[:, hh, :], lhsT=kn_t[:, ks], rhs=q_t[:, qs], start=False, stop=(kt == 1), skip_group_check=True),
                     reads=[r_kn, r_q], writes=[r_PTh[hh]])

        def phD_pv(i):
            ch, Qi, kt, j, nk, qs, ks = info(i)
            v_t, r_v = vv[ch % 2]
            a_t, _ = ab[i % 3]
            for hh in range(2):
                P.op("pe", lambda e: e.matmul(pO[:, hh, :], lhsT=v_t[:, kt, :], rhs=a_t[:, hh, :], start=(kt == nk - 1), stop=(kt == 0)),
                     reads=[r_v, abr[i % 3][hh]], writes=[r_pO])
            if kt == 0:
                b_t, r_b = ob[state["oi"] % 2]
                state["oi"] += 1
                for hh in range(2):
                    pr = slice(hh * 64, hh * 64 + 64)
                    P.op("act", lambda e: e.copy(out=b_t[pr, :], in_=pO[pr, hh, :]), reads=[r_pO], writes=[r_b])
                P.dma("sp", ocT_d[ch * 128:(ch + 1) * 128, qs], b_t[:], reads=[r_b])

        for it in range(-3, n):
            if 0 <= it < n:
                phD_act(it)
            if 0 <= it + 3 < n:
                phA(it + 3)
            if 0 <= it + 2 < n:
                phB(it + 2)
            for hh in range(2):
                if 0 <= it < n:
                    phD_corr(it, hh)
                if 0 <= it + 1 < n:
                    phC(it + 1, hh)
            if 0 <= it < n:
                phD_pv(it)
        st.done(f"sb{l}")

    def stage_cv(l):
        st = Stage(P)
        up, r_up = st.sb([128, 4, 30 + S], BF16)
        wcol, r_wcol = st.sb([128, 4, 31], F32)
        identf, r_idf = st.sb([128, 128], F32)
        dg = [st.sb([128, 31, 128], BF16) for _ in range(4)]
        bcol, r_bcol = st.sb([128, 4], F32)
        gcol, r_gcol = st.sb([128, 4], F32)
        lbcol, r_lbcol = st.sb([128, 4], F32)
        o512, r_o512 = st.sb([128, 128], F32)
        epsb, r_eps = st.sb([128, 1], F32)
        pc = [st.ps([128, 512], F32) for _ in range(4)]
        pmean, r_pmean = st.ps([128, 512], F32)
        pex2, r_pex2 = st.ps([128, 512], F32)
        cv32, r_cv = st.sb([128, 4, 512], F32)
        sq32, r_sq = st.sb([128, 4, 512], F32)
        mean, r_mean = st.sb([128, 512], F32)
        msq, r_msq = st.sb([128, 512], F32)
        rs, r_rs = st.sb([128, 512], F32)
        y = [st.sb([128, 512], F32) for _ in range(2)]
        ob = [st.sb([128, 512], BF16) for _ in range(2)]
        P.op("pool", lambda e: e.memset(up[:, :, 0:30], 0.0), writes=[r_up])
        for j in range(4):
            P.dma("sp", up[:, j, 30:30 + S], uT_d[j * 128:(j + 1) * 128, :], writes=[r_up])
            P.dma("sp", wcol[:, j, :], I["w_dw"][l][:, j * 128:(j + 1) * 128].rearrange("k p -> p k"), writes=[r_wcol])
        P.dma("sp", identf[:], I["k_ident"], writes=[r_idf])
        P.dma("sp", bcol[:], I["b_dw"][l].rearrange("(j p) -> p j", p=128), writes=[r_bcol])
        P.dma("sp", gcol[:], I["conv_ln_g"][l].rearrange("(j p) -> p j", p=128), writes=[r_gcol])
        P.dma("sp", lbcol[:], I["conv_ln_b"][l].rearrange("(j p) -> p j", p=128), writes=[r_lbcol])
        P.op("pool", lambda e: e.memset(o512[:], 1.0 / 512), writes=[r_o512])
        P.op("pool", lambda e: e.memset(epsb[:], EPS), writes=[r_eps])
        junk, r_junk = st.sb([128, 1], F32)
        for j in range(4):
            rr = []
            for k in range(31):
                eng = "dve" if k % 2 == 0 else "pool"
                r1 = Res()
                rr.append(r1)
                P.op(eng, lambda e: e.tensor_scalar(out=dg[j][0][:, k, :], in0=identf[:], scalar1=wcol[:, j, k:k + 1], scalar2=None, op0=ALU.mult),
                     reads=[r_idf, r_wcol], writes=[r1])
            P.op("dve", lambda e: e.memset(junk[:], 0.0), reads=rr, writes=[dg[j][1], r_junk])
        yi = 0
        for tt in range(NQ):
            for j in range(4):
                p_t, r_p = pc[j]
                for k in range(31):
                    P.op("pe", lambda e: e.matmul(p_t[:], lhsT=dg[j][0][:, k, :], rhs=up[:, j, tt * 512 + k:tt * 512 + k + 512], start=(k == 0), stop=(k == 30)),
                         reads=[dg[j][1], r_up], writes=[r_p])
                P.op("dve", lambda e: e.tensor_scalar(out=cv32[:, j, :], in0=p_t[:], scalar1=bcol[:, j:j + 1], scalar2=None, op0=ALU.add),
                     reads=[r_p, r_bcol], writes=[r_cv])
                P.op("pool", lambda e: e.tensor_tensor(out=sq32[:, j, :], in0=cv32[:, j, :], in1=cv32[:, j, :], op=ALU.mult), reads=[r_cv], writes=[r_sq])
            for j in range(4):
                P.op("pe", lambda e: e.matmul(pmean[:], lhsT=o512[:], rhs=cv32[:, j, :], start=(j == 0), stop=(j == 3)), reads=[r_o512, r_cv], writes=[r_pmean])
            for j in range(4):
                P.op("pe", lambda e: e.matmul(pex2[:], lhsT=o512[:], rhs=sq32[:, j, :], start=(j == 0), stop=(j == 3)), reads=[r_o512, r_sq], writes=[r_pex2])
            P.op("act", lambda e: e.copy(out=mean[:], in_=pmean[:]), reads=[r_pmean], writes=[r_mean])
            P.op("pool", lambda e: e.tensor_tensor(out=msq[:], in0=mean[:], in1=mean[:], op=ALU.mult), reads=[r_mean], writes=[r_msq])
            P.op("dve", lambda e: e.tensor_tensor(out=rs[:], in0=pex2[:], in1=msq[:], op=ALU.subtract), reads=[r_pex2, r_msq], writes=[r_rs])
            P.op("act", lambda e: e.activation(out=rs[:], in_=rs[:], func=AF.Ln, bias=epsb[:, 0:1]), reads=[r_rs, r_eps], writes=[r_rs])
            P.op("act", lambda e: e.activation(out=rs[:], in_=rs[:], func=AF.Exp, scale=-0.5), reads=[r_rs], writes=[r_rs])
            for j in range(4):
                y_t, r_y = y[yi % 2]
                b_t, r_b = ob[yi % 2]
                yi += 1
                P.op("pool", lambda e: e.tensor_tensor(out=y_t[:], in0=cv32[:, j, :], in1=mean[:], op=ALU.subtract), reads=[r_cv, r_mean], writes=[r_y])
                P.op("dve", lambda e: e.tensor_tensor(out=y_t[:], in0=y_t[:], in1=rs[:], op=ALU.mult), reads=[r_y, r_rs], writes=[r_y])
                P.op("act", lambda e: e.activation(out=b_t[:], in_=y_t[:], func=AF.Silu, scale=gcol[:, j:j + 1], bias=lbcol[:, j:j + 1]),
                     reads=[r_y, r_gcol, r_lbcol], writes=[r_b])
                P.dma("sp", cvT_d[j * 128:(j + 1) * 128, tt * 512:(tt + 1) * 512], b_t[:], reads=[r_b])
        st.done(f"cv{l}")

    def stage_mg(l, x_src):
        st = Stage(P)
        wg, r_wg = st.sb([128, 8, 3072], BF16)
        wp = [st.sb([128, 4, D], BF16) for _ in range(3)]
        wo, r_wo = st.sb([128, 8, D], BF16)
        bpb, r_bpb = st.sb([128, 8], F32)
        gmB, r_gmB = st.sb([128, D], F32)
        hT = [st.sb([128, 8, 512], BF16) for _ in range(2)]
        obr = [[st.sb([128, 4, 512], BF16) for _ in range(2)] for _ in range(3)]
        mT, r_mT = st.sb([128, 8, 512], BF16)
        py = [st.ps([128, 512], F32) for _ in range(2)]
        pg = [st.ps([128, 512], F32) for _ in range(2)]
        po = [st.ps([128, 512], F32) for _ in range(2)]
        sg = [st.sb([128, 512], F32) for _ in range(2)]
        mb = [st.sb([128, 512], F32) for _ in range(2)]
        acc, r_acc = st.sb([128, 512], F32)
        xt = [st.sb([128, D], F32) for _ in range(2)]
        tmp, r_tmp = st.sb([128, 512], F32)
        xn = [st.sb([128, D], F32) for _ in range(2)]
        for c in range(8):
            P.dma("pool", wg[:, c, :], I["w_in"][l][c * 128:(c + 1) * 128, 4096:7168], writes=[r_wg])
        for b, nm in enumerate(("w_proj_a", "w_proj_b", "w_proj_c")):
            P.dma("pool", wp[b][0][:], kp(I[nm][l]), writes=[wp[b][1]])
        P.dma("pool", wo[:], kp(I["w_out"][l]), writes=[r_wo])
        P.dma("sp", bpb[:], I["b_proj_b"][l].rearrange("(j p) -> p j", p=128), writes=[r_bpb])
        P.dma("sp", gmB[:], mod_d[l, 2 * D:3 * D].partition_broadcast(128), writes=[r_gmB])
        srcs = (oaT_d, cvT_d, ocT_d)
        loaded = set()

        def load_tile(tt):
            if tt in loaded or tt >= NQ:
                return
            loaded.add(tt)
            ts_ = slice(tt * 512, (tt + 1) * 512)
            P.dma("sp", hT[tt % 2][0][:], kp(hT_d)[:, :, ts_], writes=[hT[tt % 2][1]])
            for b_ in range(3):
                P.dma("sp", obr[b_][tt % 2][0][:], kp(srcs[b_])[:, :, ts_], writes=[obr[b_][tt % 2][1]])

        units = [(tt, j, b_) for tt in range(NQ) for j in range(8) for b_ in range(3)]
        state = {"xi": 0}

        def F(u):
            tt, j, b_ = units[u]
            load_tile(tt)
            y_t, r_y = py[u % 2]
            g_t, r_g = pg[u % 2]
            h_t, r_h = hT[tt % 2]
            o_b, r_ob = obr[b_][tt % 2]
            for c in range(4):
                P.op("pe", lambda e: e.matmul(y_t[:], lhsT=wp[b_][0][:, c, j * 128:(j + 1) * 128], rhs=o_b[:, c, :], start=(c == 0), stop=(c == 3)),
                     reads=[wp[b_][1], r_ob], writes=[r_y])
            for c in range(8):
                P.op("pe", lambda e: e.matmul(g_t[:], lhsT=wg[:, c, b_ * D + j * 128:b_ * D + (j + 1) * 128], rhs=h_t[:, c, :], start=(c == 0), stop=(c == 7)),
                     reads=[r_wg, r_h], writes=[r_g])

        def G(u):
            tt, j, b_ = units[u]
            y_t, r_y = py[u % 2]
            g_t, r_g = pg[u % 2]
            s_t, r_s = sg[u % 2]
            m_t, r_m = mb[u % 2]
            P.op("act", lambda e: e.activation(out=s_t[:], in_=g_t[:], func=AF.Sigmoid), reads=[r_g], writes=[r_s])
            if b_ == 0:
                P.op("dve", lambda e: e.tensor_tensor(out=acc[:], in0=y_t[:], in1=s_t[:], op=ALU.mult), reads=[r_y, r_s], writes=[r_acc])
            elif b_ == 1:
                P.op("dve", lambda e: e.scalar_tensor_tensor(out=m_t[:], in0=y_t[:], scalar=bpb[:, j:j + 1], in1=s_t[:], op0=ALU.add, op1=ALU.mult),
                     reads=[r_y, r_bpb, r_s], writes=[r_m])
                P.op("pool", lambda e: e.tensor_tensor(out=acc[:], in0=acc[:], in1=m_t[:], op=ALU.add), reads=[r_acc, r_m], writes=[r_acc])
            else:
                P.op("dve", lambda e: e.tensor_tensor(out=m_t[:], in0=y_t[:], in1=s_t[:], op=ALU.mult), reads=[r_y, r_s], writes=[r_m])
                P.op("dve", lambda e: e.tensor_tensor(out=mT[:, j, :], in0=acc[:], in1=m_t[:], op=ALU.add), reads=[r_acc, r_m], writes=[r_mT])
            if j == 7 and b_ == 2:
                OUT(tt)

        def OUT(tt):
            for sub in range(4):
                x_t, r_x = xt[state["xi"] % 2]
                n_t, r_n = xn[state["xi"] % 2]
                state["xi"] += 1
                r0 = tt * 512 + sub * 128
                P.dma("sp", x_t[:], x_src[r0:r0 + 128, :], writes=[r_x])
                for half in range(2):
                    o_t, r_o = po[half]
                    hs = slice(half * 512, (half + 1) * 512)
                    for c in range(8):
                        P.op("pe", lambda e: e.matmul(o_t[:], lhsT=mT[:, c, sub * 128:(sub + 1) * 128], rhs=wo[:, c, hs], start=(c == 0), stop=(c == 7)),
                             reads=[r_mT, r_wo], writes=[r_o])
                    P.op("dve", lambda e: e.tensor_tensor(out=tmp[:], in0=o_t[:], in1=gmB[:, hs], op=ALU.mult), reads=[r_o, r_gmB], writes=[r_tmp])
                    P.op("pool", lambda e: e.tensor_tensor(out=n_t[:, hs], in0=tmp[:], in1=x_t[:, hs], op=ALU.add), reads=[r_tmp, r_x], writes=[r_n])
                P.dma("sp", x1_d[r0:r0 + 128, :], n_t[:], reads=[r_n])

        nU = len(units)
        F(0)
        for u in range(nU):
            if u + 1 < nU:
                F(u + 1)
            G(u)
        st.done(f"mg{l}")

    def stage_moe(l, dst):
        TS = min(S, 2048)
        NTS = TS // 128
        st = Stage(P)
        hT, r_hT = st.sb([128, 8, TS], BF16)
        acc, r_acc_all = st.sb([128, NTS, D], F32)
        r_acc = [[Res() for _ in range(2)] for _ in range(NTS)]
        w1b = [st.sb([128, 8, FF], BF16) for _ in range(2)]
        w3b = [st.sb([128, 8, FF], BF16) for _ in range(2)]
        w2b = [st.sb([128, 2, D], BF16) for _ in range(2)]
        gB = [st.sb([128, TS], F32) for _ in range(2)]
        gfB, r_gfB = st.sb([128, D], F32)
        pa = [st.ps([128, 512], F32) for _ in range(2)]
        pb = [st.ps([128, 512], F32) for _ in range(2)]
        po = [st.ps([128, 512], F32) for _ in range(4)]
        sa = [st.sb([128, 512], F32) for _ in range(2)]
        tb = [st.sb([128, 512], F32) for _ in range(2)]
        hid = [st.sb([128, 2, 512], BF16) for _ in range(2)]
        xt = [st.sb([128, D], F32) for _ in range(2)]
        xn = [st.sb([128, D], F32) for _ in range(2)]
        P.dma("sp", gfB[:], mod_d[l, 5 * D:6 * D].partition_broadcast(128), writes=[r_gfB])
        oi = 0
        for sti in range(S // TS):
            t0 = sti * TS
            P.dma("sp", hT[:], kp(h2T_d)[:, :, t0:t0 + TS], writes=[r_hT])
            units = [(ex, tt) for ex in range(NE + 1) for tt in range(TS // 512)]
            loaded = set()

            def load_w(ex):
                if ex in loaded or ex > NE:
                    return
                loaded.add(ex)
                w1_t, r_w1 = w1b[ex % 2]
                w3_t, r_w3 = w3b[ex % 2]
                w2_t, r_w2 = w2b[ex % 2]
                g_t, r_g = gB[ex % 2]
                if ex < NE:
                    P.dma("pool", w1_t[:], kp(I["w1"][l, ex]), writes=[r_w1])
                    P.dma("pool", w3_t[:], kp(I["w3"][l, ex]), writes=[r_w3])
                    P.dma("pool", w2_t[:], kp(I["w2"][l, ex]), writes=[r_w2])
                    P.dma("sp", g_t[:], gT_d[ex, t0:t0 + TS].partition_broadcast(128), writes=[r_g])
                else:
                    P.dma("pool", w1_t[:], kp(I["ws1"][l]), writes=[r_w1])
                    P.dma("pool", w3_t[:], kp(I["ws3"][l]), writes=[r_w3])
                    P.dma("pool", w2_t[:], kp(I["ws2"][l]), writes=[r_w2])
                    P.op("dve", lambda e: e.memset(g_t[:], 1.0), writes=[r_g])

            def up(i):
                ex, tt = units[i]
                load_w(ex)
                w1_t, r_w1 = w1b[ex % 2]
                w3_t, r_w3 = w3b[ex % 2]
                g_t, r_g = gB[ex % 2]
                ts_ = slice(tt * 512, (tt + 1) * 512)
                h_t, r_h = hid[i % 2]
                for f in range(2):
                    a_t, r_a = pa[f]
                    b_t, r_b = pb[f]
                    s_t, r_s = sa[f]
                    t_t, r_t = tb[f]
                    for c in range(8):
                        P.op("pe", lambda e: e.matmul(a_t[:], lhsT=w1_t[:, c, f * 128:(f + 1) * 128], rhs=hT[:, c, ts_], start=(c == 0), stop=(c == 7)),
                             reads=[r_w1, r_hT], writes=[r_a])
                    for c in range(8):
                        P.op("pe", lambda e: e.matmul(b_t[:], lhsT=w3_t[:, c, f * 128:(f + 1) * 128], rhs=hT[:, c, ts_], start=(c == 0), stop=(c == 7)),
                             reads=[r_w3, r_hT], writes=[r_b])
                    P.op("act", lambda e: e.activation(out=s_t[:], in_=a_t[:], func=AF.Silu), reads=[r_a], writes=[r_s])
                    P.op("dve", lambda e: e.tensor_tensor(out=t_t[:], in0=b_t[:], in1=g_t[:, ts_], op=ALU.mult), reads=[r_b, r_g], writes=[r_t])
                    P.op("dve", lambda e: e.tensor_tensor(out=h_t[:, f, :], in0=s_t[:], in1=t_t[:], op=ALU.mult), reads=[r_s, r_t], writes=[r_h])

            def down(i):
                nonlocal oi
                ex, tt = units[i]
                w2_t, r_w2 = w2b[ex % 2]
                h_t, r_h = hid[i % 2]
                for sub in range(4):
                    ti = tt * 4 + sub
                    for half in range(2):
                        o_t, r_o = po[oi % 4]
                        oi += 1
                        hs = slice(half * 512, (half + 1) * 512)
                        for f in range(2):
                            P.op("pe", lambda e: e.matmul(o_t[:], lhsT=h_t[:, f, sub * 128:(sub + 1) * 128], rhs=w2_t[:, f, hs], start=(f == 0), stop=(f == 1)),
                                 reads=[r_h, r_w2], writes=[r_o])
                        if ex == 0:
                            P.op("dve", lambda e: e.tensor_copy(out=acc[:, ti, hs], in_=o_t[:]), reads=[r_o], writes=[r_acc[ti][half]])
                        else:
                            P.op("dve", lambda e: e.tensor_tensor(out=acc[:, ti, hs], in0=o_t[:], in1=acc[:, ti, hs], op=ALU.add),
                                 reads=[r_o, r_acc[ti][half]], writes=[r_acc[ti][half]])

            n = len(units)
            load_w(0)
            load_w(1)
            up(0)
            for i in range(n):
                if i + 1 < n:
                    up(i + 1)
                down(i)
                if i + 1 < n and units[i + 1][0] != units[i][0]:
                    load_w(units[i][0] + 2)
            for ti in range(NTS):
                x_t, r_x = xt[ti % 2]
                n_t, r_n = xn[ti % 2]
                r0 = t0 + ti * 128
                P.dma("sp", x_t[:], x1_d[r0:r0 + 128, :], writes=[r_x])
                P.op("dve", lambda e: e.tensor_tensor(out=n_t[:], in0=acc[:, ti, :], in1=gfB[:], op=ALU.mult), reads=[r_acc[ti][0], r_acc[ti][1], r_gfB], writes=[r_n])
                P.op("pool", lambda e: e.tensor_tensor(out=n_t[:], in0=n_t[:], in1=x_t[:], op=ALU.add), reads=[r_n, r_x], writes=[r_n])
                P.dma("sp", dst[r0:r0 + 128, :], n_t[:], reads=[r_n])
        st.done(f"moe{l}")

    def stage_route(l):
        st = Stage(P)
        sbB, r_sbB = st.sb([128, NE], F32)
        P.dma("sp", sbB[:], sbase_d.partition_broadcast(128), writes=[r_sbB])
        pos = [st.sb([128, NE], F32) for _ in range(2)]
        gat = [st.sb([128, NE], F32) for _ in range(2)]
        hrow = [st.sb([128, D], BF16) for _ in range(2)]
        a_ = [st.sb([128, NE], F32) for _ in range(2)]
        sel_ = [st.sb([128, NE], F32) for _ in range(2)]
        t8 = [st.sb([128, 8], F32) for _ in range(2)]
        si = [st.sb([128, 8], I32) for _ in range(2)]
        gk = [st.sb([128, 8], F32) for _ in range(2)]
        junk = [st.sb([128, NE], F32) for _ in range(2)]

        def chain(t):
            b = t % 2
            rows = slice(t * 128, (t + 1) * 128)
            P.dma("sp", pos[b][0][:], pos_d[rows, :], writes=[pos[b][1]])
            P.dma("sp", gat[b][0][:], gate_d[rows, :], writes=[gat[b][1]])
            P.dma("sp", hrow[b][0][:], h2_d[rows, :], writes=[hrow[b][1]])
            yield
            P.op("dve", lambda e: e.tensor_scalar(out=sel_[b][0][:], in0=gat[b][0][:], scalar1=0.0, scalar2=None, op0=ALU.is_gt),
                 reads=[gat[b][1]], writes=[sel_[b][1]])
            yield
            P.op("dve", lambda e: e.tensor_tensor(out=a_[b][0][:], in0=pos[b][0][:], in1=sbB[:], op=ALU.add), reads=[pos[b][1], r_sbB], writes=[a_[b][1]])
            yield
            P.op("dve", lambda e: e.tensor_tensor(out=a_[b][0][:], in0=a_[b][0][:], in1=sel_[b][0][:], op=ALU.mult), reads=[a_[b][1], sel_[b][1]], writes=[a_[b][1]])
            yield
            P.op("dve", lambda e: e.tensor_scalar(out=a_[b][0][:], in0=a_[b][0][:], scalar1=-1.0, scalar2=None, op0=ALU.add), reads=[a_[b][1]], writes=[a_[b][1]])
            yield
            P.op("dve", lambda e: e.max(out=t8[b][0][:], in_=a_[b][0][:]), reads=[a_[b][1]], writes=[t8[b][1]])
            yield
            P.op("dve", lambda e: e.tensor_copy(out=si[b][0][:], in_=t8[b][0][:]), reads=[t8[b][1]], writes=[si[b][1]])
            yield
            for k in range(8):
                P.op("dve", lambda e: e.scalar_tensor_tensor(out=junk[b][0][:], in0=a_[b][0][:], scalar=t8[b][0][:, k:k + 1], in1=gat[b][0][:], op0=ALU.is_equal, op1=ALU.mult),
                     reads=[a_[b][1], t8[b][1], gat[b][1]], writes=[junk[b][1]])
                yield
                P.op("dve", lambda e: e.tensor_reduce(out=gk[b][0][:, k:k + 1], in_=junk[b][0][:], axis=AX.X, op=ALU.add), reads=[junk[b][1]], writes=[gk[b][1]])
                yield
            P.dma("sp", slotk_d[rows, :], si[b][0][:], reads=[si[b][1]])
            P.dma("sp", gk_d[rows, :], gk[b][0][:], reads=[gk[b][1]])
            for k in range(8):
                def fs(eng, b=b, k=k):
                    return eng.indirect_dma_start(out=xs_d[:, :], out_offset=bass.IndirectOffsetOnAxis(ap=si[b][0][:, k:k + 1], axis=0),
                                                  in_=hrow[b][0][:, :], in_offset=None)
                P.raw("pool", fs, reads=[si[b][1], hrow[b][1]], is_dma=True)
            yield

        def drain(*gens):
            gens = list(gens)
            while gens:
                for g in list(gens):
                    try:
                        next(g)
                    except StopIteration:
                        gens.remove(g)

        for t in range(0, NT, 2):
            drain(*[chain(tt_) for tt_ in range(t, min(t + 2, NT))])
        st.done(f"route{l}")

    def stage_moe2(l):
        st = Stage(P)
        identb, r_idb = st.sb([128, 128], BF16)
        ebB, r_ebB = st.sb([128, 128], F32)
        iotac, r_iotac = st.sb([128, 1], F32)
        idxw, r_idxw = st.sb([128, 128], I32)
        w1b = [st.sb([128, 8, FF], BF16) for _ in range(3)]
        w3b = [st.sb([128, 8, FF], BF16) for _ in range(3)]
        w2b = [st.sb([128, 2, D], BF16) for _ in range(3)]
        xtok = [st.sb([128, 4, D], BF16) for _ in range(3)]
        XT = [st.sb([128, 8, 512], BF16) for _ in range(3)]
        hid = [st.sb([128, 2, 512], BF16) for _ in range(2)]
        sa = [st.sb([128, 512], F32) for _ in range(2)]
        ysb = [st.sb([128, 4, D], BF16) for _ in range(2)]
        pt = [st.ps([128, 8, 128], BF16) for _ in range(2)]
        pa = [st.ps([128, 512], F32) for _ in range(2)]
        pb = [st.ps([128, 512], F32) for _ in range(2)]
        po = [st.ps([128, 512], F32) for _ in range(2)]
        P.dma("pool", identb[:], I["k_ident"], writes=[r_idb])
        P.dma("sp", ebB[:], eb_d.partition_broadcast(128), writes=[r_ebB])
        P.dma("sp", iotac[:], col(I["k_iota"]), writes=[r_iotac])
        P.op("dve", lambda e: e.tensor_scalar(out=ebB[:], in0=ebB[:], scalar1=128.0, scalar2=float(l * NE * 128), op0=ALU.mult, op1=ALU.add),
             reads=[r_ebB], writes=[r_ebB])
        P.op("dve", lambda e: e.tensor_scalar(out=ebB[:], in0=ebB[:], scalar1=iotac[:, 0:1], scalar2=None, op0=ALU.add), reads=[r_ebB, r_iotac], writes=[r_ebB])
        P.op("dve", lambda e: e.tensor_copy(out=idxw[:], in_=ebB[:]), reads=[r_ebB], writes=[r_idxw])
        NU = NB + NQ
        state = {"oi": 0}

        def load(u):
            if u >= NU:
                return
            bf = u % 3
            if u < NB:
                for nm, (w_t, r_w) in (("w1h", w1b[bf]), ("w3h", w3b[bf]), ("w2h", w2b[bf])):
                    def fg(eng, nm=nm, w_t=w_t, u=u):
                        return eng.indirect_dma_start(out=w_t[:].rearrange("p c f -> p (c f)"), out_offset=None, in_=I[nm][:, :],
                                                      in_offset=bass.IndirectOffsetOnAxis(ap=idxw[:, u:u + 1], axis=0))
                    P.raw("pool", fg, reads=[r_idxw], writes=[r_w], is_dma=True)
                P.dma("sp", xtok[bf][0][:], xs_d[u * 512:(u + 1) * 512, :].rearrange("(s p) d -> p s d", p=128), writes=[xtok[bf][1]])
            else:
                if u in (NB, NB + 1, NB + 2):
                    P.dma("pool", w1b[bf][0][:], kp(I["ws1"][l]), writes=[w1b[bf][1]])
                    P.dma("pool", w3b[bf][0][:], kp(I["ws3"][l]), writes=[w3b[bf][1]])
                    P.dma("pool", w2b[bf][0][:], kp(I["ws2"][l]), writes=[w2b[bf][1]])
                tt = u - NB
                P.dma("sp", XT[bf][0][:], kp(h2T_d)[:, :, tt * 512:(tt + 1) * 512], writes=[XT[bf][1]])

        def tr(u):
            if u >= NB:
                return
            bf = u % 3
            x_t, r_x = xtok[bf]
            X_t, r_X = XT[bf]
            for sub in range(4):
                p_t, r_p = pt[sub % 2]
                for c in range(8):
                    P.op("pe", lambda e: e.transpose(out=p_t[:, c, :], in_=x_t[:, sub, c * 128:(c + 1) * 128], identity=identb[:]),
                         reads=[r_x, r_idb], writes=[r_p])
                if sub % 2 == 0:
                    P.op("act", lambda e: e.copy(out=X_t[:, :, sub * 128:(sub + 1) * 128], in_=p_t[:]), reads=[r_p], writes=[r_X])
                else:
                    P.op("dve", lambda e: e.tensor_copy(out=X_t[:, :, sub * 128:(sub + 1) * 128], in_=p_t[:]), reads=[r_p], writes=[r_X])

        def up(u):
            bf = u % 3
            w1_t, r_w1 = w1b[bf]
            w3_t, r_w3 = w3b[bf]
            X_t, r_X = XT[bf]
            h_t, r_h = hid[u % 2]
            for f in range(2):
                a_t, r_a = pa[f]
                b_t, r_b = pb[f]
                s_t, r_s = sa[f]
                for c in range(8):
                    P.op("pe", lambda e: e.matmul(a_t[:], lhsT=w1_t[:, c, f * 128:(f + 1) * 128], rhs=X_t[:, c, :], start=(c == 0), stop=(c == 7)),
                         reads=[r_w1, r_X], writes=[r_a])
                for c in range(8):
                    P.op("pe", lambda e: e.matmul(b_t[:], lhsT=w3_t[:, c, f * 128:(f + 1) * 128], rhs=X_t[:, c, :], start=(c == 0), stop=(c == 7)),
                         reads=[r_w3, r_X], writes=[r_b])
                P.op("act", lambda e: e.activation(out=s_t[:], in_=a_t[:], func=AF.Silu), reads=[r_a], writes=[r_s])
                P.op("dve", lambda e: e.tensor_tensor(out=h_t[:, f, :], in0=b_t[:], in1=s_t[:], op=ALU.mult), reads=[r_b, r_s], writes=[r_h])

        def down(u):
            w2_t, r_w2 = w2b[u % 3]
            h_t, r_h = hid[u % 2]
            y_t, r_y = ysb[u % 2]
            for sub in range(4):
                for half in range(2):
                    o_t, r_o = po[state["oi"] % 2]
                    state["oi"] += 1
                    hs = slice(half * 512, (half + 1) * 512)
                    for f in range(2):
                        P.op("pe", lambda e: e.matmul(o_t[:], lhsT=h_t[:, f, sub * 128:(sub + 1) * 128], rhs=w2_t[:, f, hs], start=(f == 0), stop=(f == 1)),
                             reads=[r_h, r_w2], writes=[r_o])
                    if half == 0:
                        P.op("act", lambda e: e.copy(out=y_t[:, sub, hs], in_=o_t[:]), reads=[r_o], writes=[r_y])
                    else:
                        P.op("dve", lambda e: e.tensor_copy(out=y_t[:, sub, hs], in_=o_t[:]), reads=[r_o], writes=[r_y])
            if u < NB:
                P.dma("sp", ys_d[u * 512:(u + 1) * 512, :].rearrange("(s p) d -> p s d", p=128), y_t[:], reads=[r_y])
            else:
                tt = u - NB
                P.dma("sp", ysh_d[tt * 512:(tt + 1) * 512, :].rearrange("(s p) d -> p s d", p=128), y_t[:], reads=[r_y])

        load(0)
        load(1)
        tr(0)
        up(0)
        for u in range(NU):
            load(u + 2)
            if u + 1 < NU:
                tr(u + 1)
                up(u + 1)
            down(u)
        st.done(f"moe{l}")

    def stage_comb(l, dst):
        st = Stage(P)
        gfB, r_gfB = st.sb([128, D], F32)
        P.dma("sp", gfB[:], mod_d[l, 5 * D:6 * D].partition_broadcast(128), writes=[r_gfB])
        si = [st.sb([128, 8], I32) for _ in range(2)]
        gk = [st.sb([128, 8], F32) for _ in range(2)]
        xt = [st.sb([128, D], F32) for _ in range(2)]
        ysh = [st.sb([128, D], BF16) for _ in range(2)]
        yg = [[st.sb([128, D], BF16) for _ in range(8)] for _ in range(2)]
        acc = [st.sb([128, D], F32) for _ in range(2)]
        def fetch(t):
            if t >= NT:
                return
            b = t % 2
            rows = slice(t * 128, (t + 1) * 128)
            P.dma("sp", si[b][0][:], slotk_d[rows, :], writes=[si[b][1]])
            P.dma("sp", gk[b][0][:], gk_d[rows, :], writes=[gk[b][1]])
            P.dma("sp", xt[b][0][:], x1_d[rows, :], writes=[xt[b][1]])
            P.dma("sp", ysh[b][0][:], ysh_d[rows, :], writes=[ysh[b][1]])
            for k in range(8):
                def fg(eng, b=b, k=k):
                    return eng.indirect_dma_start(out=yg[b][k][0][:, :], out_offset=None, in_=ys_d[:, :],
                                                  in_offset=bass.IndirectOffsetOnAxis(ap=si[b][0][:, k:k + 1], axis=0))
                P.raw("pool", fg, reads=[si[b][1]], writes=[yg[b][k][1]], is_dma=True)

        def comp(t):
            b = t % 2
            rows = slice(t * 128, (t + 1) * 128)
            a_t, r_a = acc[b]
            P.op("dve", lambda e: e.scalar_tensor_tensor(out=a_t[:], in0=yg[b][0][0][:], scalar=gk[b][0][:, 0:1], in1=ysh[b][0][:], op0=ALU.mult, op1=ALU.add),
                 reads=[yg[b][0][1], gk[b][1], ysh[b][1]], writes=[r_a])
            for k in range(1, 8):
                P.op("dve", lambda e: e.scalar_tensor_tensor(out=a_t[:], in0=yg[b][k][0][:], scalar=gk[b][0][:, k:k + 1], in1=a_t[:], op0=ALU.mult, op1=ALU.add),
                     reads=[yg[b][k][1], gk[b][1], r_a], writes=[r_a])
            P.op("dve", lambda e: e.tensor_tensor(out=a_t[:], in0=a_t[:], in1=gfB[:], op=ALU.mult), reads=[r_a, r_gfB], writes=[r_a])
            P.op("dve", lambda e: e.tensor_tensor(out=a_t[:], in0=a_t[:], in1=xt[b][0][:], op=ALU.add), reads=[r_a, xt[b][1]], writes=[r_a])
            P.dma("sp", dst[rows, :], a_t[:], reads=[r_a])

        fetch(0)
        for t in range(NT):
            comp_deferred = t
            fetch(t + 1)
            comp(t)
        st.done(f"comb{l}")

    todo = stages if stages is not None else ("mod", "rope", "norm1", "proj")
    if "mod" in todo:
        stage_mod()
    if "rope" in todo:
        stage_rope()
    for l in range(L):
        x_in = I["x"] if l == 0 else x2_d
        if "norm1" in todo:
            stage_norm(l, x_in, "norm_mix_g", 1 * D, 0 * D, hT_d, router=False)
        if "proj" in todo:
            stage_proj(l)
        if "da" in todo:
            stage_da(l)
        if "sb" in todo:
            stage_sb(l)
        if "cv" in todo:
            stage_cv(l)
        if "mg" in todo:
            stage_mg(l, x_in)
        if "norm2" in todo:
            stage_norm(l, x1_d, "norm_ffn_g", 4 * D, 3 * D, h2T_d, router=True)
        if "moe" in todo:
            stage_moe(l, out if l == L - 1 else x2_d)
        if "smoe" in todo:
            stage_route(l)
            stage_moe2(l)
            stage_comb(l, out if l == L - 1 else x2_d)
    es.close()
    return nc


ALL_STAGES = ("mod", "rope", "norm1", "proj", "da", "sb", "cv", "mg", "norm2", "smoe")
_CACHE = {}


def kernel(**inputs):
    x = np.ascontiguousarray(np.asarray(inputs["x"], dtype=np.float32))
    B, S, _ = x.shape
    L = int(np.asarray(inputs["w_mod"]).shape[0])
    key = (S, L)
    if key not in _CACHE:
        _CACHE[key] = build(S, L=L, stages=ALL_STAGES)
    nc = _CACHE[key]
    consts = make_consts()
    shared = {k: np.ascontiguousarray(np.asarray(inputs[k], dtype=np.float32)) for k in W_SHAPES if k not in ("w1", "w3", "w2")}
    shared.update(relayout_experts(inputs, L))
    c = np.asarray(inputs["c"], dtype=np.float32)
    pos = np.asarray(inputs["positions"]).astype(np.int32)
    in_maps = []
    for b in range(B):
        m = {"x": x[b], "c": np.ascontiguousarray(c[b]), "pos": np.ascontiguousarray(pos[b])}
        m.update(shared)
        m.update(consts)
        in_maps.append(m)
    res = run_bass_kernel_spmd(nc, in_maps, core_ids=list(range(B)))
    return np.stack([np.asarray(r["out"], dtype=np.float32) for r in res.results], axis=0)


def relayout_experts(W, L):
    o = {}
    w1 = np.asarray(W["w1"], dtype=np.float32)[:L]
    w3 = np.asarray(W["w3"], dtype=np.float32)[:L]
    w2 = np.asarray(W["w2"], dtype=np.float32)[:L]
    o["w1h"] = np.ascontiguousarray(w1.reshape(L, NE, 8, 128, FF).transpose(0, 1, 3, 2, 4)).reshape(L * NE * 128, 8 * FF)
    o["w3h"] = np.ascontiguousarray(w3.reshape(L, NE, 8, 128, FF).transpose(0, 1, 3, 2, 4)).reshape(L * NE * 128, 8 * FF)
    o["w2h"] = np.ascontiguousarray(w2.reshape(L, NE, 2, 128, D).transpose(0, 1, 3, 2, 4)).reshape(L * NE * 128, 2 * D)
    return o
```
